# Optimizing a Trainium2 kernel written in Bass

```python
import math
import jax, jax.numpy as jnp
from jax import lax
import numpy as np

D_MODEL = 1024
BATCH = 4
SEQ = 4096
DEPTH = 4

GRID_W = 64
CTX_LEN = 256
HEAD_DIM = 64
LRU_WIDTH = 256
LRU_BLOCKS = 4
LRU_BLOCK = LRU_WIDTH // LRU_BLOCKS
CONV_W = 4
LRU_C = 8.0
SWA_Q_HEADS = 8
SWA_KV_HEADS = 2
SWA_GROUP = SWA_Q_HEADS // SWA_KV_HEADS
SWA_WIDTH = SWA_Q_HEADS * HEAD_DIM
WINDOW = 128
BLOCK = 128
DIFF_HEADS = 4
DIFF_QK = HEAD_DIM // 2
DIFF_WIDTH = DIFF_HEADS * HEAD_DIM
MIX_WIDTH = LRU_WIDTH + SWA_WIDTH + DIFF_WIDTH
IN_SPLIT = (LRU_WIDTH, LRU_WIDTH, SWA_Q_HEADS * HEAD_DIM, SWA_KV_HEADS * HEAD_DIM, SWA_KV_HEADS * HEAD_DIM,
            DIFF_HEADS * 2 * DIFF_QK, DIFF_HEADS * 2 * DIFF_QK, DIFF_HEADS * HEAD_DIM)
IN_COLS = sum(IN_SPLIT)
D_FF = 2816
N_EXPERTS = 8
TOP_K = 2
EXPERT_FF = 2816
N_DENSE = (DEPTH + 1) // 2
N_MOE = DEPTH // 2
ROPE_BASE = 10000.0
LN_EPS = 1e-5
NEG_INF = -1e30
DEEPNORM_ALPHA = (2.0 * DEPTH) ** 0.25
DEEPNORM_BETA = (8.0 * DEPTH) ** -0.25

kernel_name = "hymba_style_hybrid_diffusion_trunk"

F32 = jnp.float32


def layer_norm(x, g, b):
    xf = x.astype(F32)
    mu = jnp.mean(xf, -1, keepdims=True)
    var = jnp.mean(jnp.square(xf - mu), -1, keepdims=True)
    return ((xf - mu) * lax.rsqrt(var + LN_EPS)).astype(x.dtype) * g + b


def axial_rope(rows, rot_dim):
    n_freq = rot_dim // 4
    inv = ROPE_BASE ** (-jnp.arange(n_freq, dtype=F32) / n_freq)
    row = jnp.repeat(jnp.arange(rows, dtype=F32), GRID_W)
    col = jnp.tile(jnp.arange(GRID_W, dtype=F32), rows)
    ang = jnp.concatenate([row[:, None] * inv, col[:, None] * inv], -1)
    return jnp.cos(ang), jnp.sin(ang)


def apply_rope(t, cos, sin):
    shp = (cos.shape[0],) + (1,) * (t.ndim - 3) + (cos.shape[1],)
    cos = cos.reshape(shp).astype(t.dtype)
    sin = sin.reshape(shp).astype(t.dtype)
    half = t.shape[-1] // 2
    t1, t2 = t[..., :half], t[..., half:]
    return jnp.concatenate([t1 * cos - t2 * sin, t2 * cos + t1 * sin], -1)


def split_cols(p):
    outs, o = [], 0
    for n in IN_SPLIT:
        outs.append(p[..., o:o + n])
        o += n
    return outs


def centred_dwconv(u, w, b):
    pad_l = (CONV_W - 1) // 2
    pad_r = CONV_W - 1 - pad_l
    out = lax.conv_general_dilated(u, w[:, None, :], window_strides=(1,), padding=[(pad_l, pad_r)],
                                   dimension_numbers=('NWC', 'WIO', 'NWC'), feature_group_count=u.shape[-1])
    return out + b


def block_diag_linear(u, w, b):
    ub = u.reshape(u.shape[:-1] + (LRU_BLOCKS, LRU_BLOCK))
    return jnp.einsum('bnki,kio->bnko', ub, w.astype(F32)).reshape(u.shape) + b.astype(F32)


def rglru_coeffs(u, w_a, b_a, w_x, b_x, lam):
    r = jax.nn.sigmoid(block_diag_linear(u, w_a, b_a))
    i = jax.nn.sigmoid(block_diag_linear(u, w_x, b_x))
    log_a = -LRU_C * r * jax.nn.softplus(-lam.astype(F32))
    a = jnp.exp(log_a)
    mult = jnp.sqrt(-jnp.expm1(2.0 * log_a))
    return a, mult * (i * u)


def linear_scan(a, bx, h0, reverse):
    def comb(l, r):
        return r[0] * l[0], r[0] * l[1] + r[1]
    a_cum, b_cum = lax.associative_scan(comb, (a, bx), axis=1, reverse=reverse)
    return a_cum * h0[:, None, :] + b_cum


def rglru_group(x_ctx, x_lat, conv_w, conv_b, gate_w, gate_b, lam):
    uc = centred_dwconv(x_ctx, conv_w, conv_b).astype(F32)
    ul = centred_dwconv(x_lat, conv_w, conv_b).astype(F32)
    ys_c, ys_l = [], []
    for d, rev in enumerate((False, True)):
        a_c, b_c = rglru_coeffs(uc, gate_w[d, 0], gate_b[d, 0], gate_w[d, 1], gate_b[d, 1], lam[d])
        h_c = linear_scan(a_c, b_c, jnp.zeros_like(uc[:, 0]), rev)
        h_fin = h_c[:, 0] if rev else h_c[:, -1]
        a_l, b_l = rglru_coeffs(ul, gate_w[d, 0], gate_b[d, 0], gate_w[d, 1], gate_b[d, 1], lam[d])
        h_l = linear_scan(a_l, b_l, h_fin, rev)
        ys_c.append(h_c)
        ys_l.append(h_l)
    return (ys_c[0] + ys_c[1]).astype(x_ctx.dtype), (ys_l[0] + ys_l[1]).astype(x_lat.dtype)


def windowed_attention(q, k, v, k_ctx, v_ctx, sink_hg):
    B, S, Hk, G, d = q.shape
    nb = S // BLOCK
    L = k_ctx.shape[1]
    scale = d ** -0.5
    qb = q.reshape(B, nb, BLOCK, Hk, G, d)

    def band(t):
        tb = t.reshape(B, nb, BLOCK, Hk, d)
        tp = jnp.pad(tb, ((0, 0), (1, 1), (0, 0), (0, 0), (0, 0)))
        return jnp.concatenate([tp[:, :-2], tp[:, 1:-1], tp[:, 2:]], axis=2)

    kb, vb = band(k), band(v)
    s_band = jnp.einsum('bnqhgd,bnkhd->bnhgqk', qb, kb, preferred_element_type=F32) * scale
    q_abs = jnp.arange(nb)[:, None, None] * BLOCK + jnp.arange(BLOCK)[None, :, None]
    k_abs = jnp.arange(nb)[:, None, None] * BLOCK - BLOCK + jnp.arange(3 * BLOCK)[None, None, :]
    valid = (jnp.abs(k_abs - q_abs) <= WINDOW) & (k_abs >= 0) & (k_abs < S)
    s_band = jnp.where(valid[None, :, None, None], s_band, NEG_INF)
    s_ctx = jnp.einsum('bnqhgd,bkhd->bnhgqk', qb, k_ctx, preferred_element_type=F32) * scale
    s_sink = jnp.broadcast_to(sink_hg.astype(F32)[None, None, :, :, None, None], s_ctx.shape[:-1] + (1,))
    p = jax.nn.softmax(jnp.concatenate([s_ctx, s_band, s_sink], -1), axis=-1)
    o = (jnp.einsum('bnhgqk,bkhd->bnqhgd', p[..., :L], v_ctx.astype(F32))
         + jnp.einsum('bnhgqk,bnkhd->bnqhgd', p[..., L:-1], vb.astype(F32)))
    return o.reshape(B, S, Hk * G * d).astype(v.dtype)


def context_sink_attention(q, k, v, sink_hg):
    B, L, Hk, G, d = q.shape
    s = jnp.einsum('bqhgd,bkhd->bhgqk', q, k, preferred_element_type=F32) * (d ** -0.5)
    s_sink = jnp.broadcast_to(sink_hg.astype(F32)[None, :, :, None, None], s.shape[:-1] + (1,))
    p = jax.nn.softmax(jnp.concatenate([s, s_sink], -1), axis=-1)[..., :-1]
    o = jnp.einsum('bhgqk,bkhd->bqhgd', p, v.astype(F32))
    return o.reshape(B, L, Hk * G * d).astype(v.dtype)


def diff_attention(q, k, v, lam):
    s = jnp.einsum('bqhmd,bkhmd->bhmqk', q, k, preferred_element_type=F32) * (DIFF_QK ** -0.5)
    p = jax.nn.softmax(s, axis=-1)
    w = p[:, :, 0] - lam * p[:, :, 1]
    return jnp.einsum('bhqk,bkhd->bqhd', w, v.astype(F32)).astype(v.dtype)


def diff_latent(q, k_all, v_all, lam):
    B, S = q.shape[:2]
    nb = S // BLOCK
    qb = jnp.moveaxis(q.reshape((B, nb, BLOCK) + q.shape[2:]), 1, 0)
    ob = lax.map(lambda qblk: diff_attention(qblk, k_all, v_all, lam), qb)
    return jnp.moveaxis(ob, 0, 1).reshape((B, S) + ob.shape[3:])


def diff_head_norm(o, g, lam_init):
    of = o.astype(F32)
    of = of * lax.rsqrt(jnp.mean(jnp.square(of), -1, keepdims=True) + LN_EPS)
    return (of * (1.0 - lam_init)).astype(o.dtype) * g


def token_mix(u_ctx, u_lat, w_in, w_out, conv_w, conv_b, gate_w, gate_b, lru_lam, sink, lam_vec, norm_g,
              lam_init, rope64, rope32, need_ctx):
    B, S, _ = u_lat.shape
    L = u_ctx.shape[1]
    pc = split_cols(u_ctx @ w_in)
    pl = split_cols(u_lat @ w_in)

    lru_c, lru_l = rglru_group(pc[0], pl[0], conv_w, conv_b, gate_w, gate_b, lru_lam)
    lru_c = lru_c * jax.nn.gelu(pc[1])
    lru_l = lru_l * jax.nn.gelu(pl[1])

    sink_hg = sink.reshape(SWA_KV_HEADS, SWA_GROUP)
    qs_l = apply_rope(pl[2].reshape(B, S, SWA_Q_HEADS, HEAD_DIM), *rope64).reshape(B, S, SWA_KV_HEADS, SWA_GROUP, HEAD_DIM)
    ks_l = apply_rope(pl[3].reshape(B, S, SWA_KV_HEADS, HEAD_DIM), *rope64)
    vs_l = pl[4].reshape(B, S, SWA_KV_HEADS, HEAD_DIM)
    ks_c = pc[3].reshape(B, L, SWA_KV_HEADS, HEAD_DIM)
    vs_c = pc[4].reshape(B, L, SWA_KV_HEADS, HEAD_DIM)
    swa_l = windowed_attention(qs_l, ks_l, vs_l, ks_c, vs_c, sink_hg)

    lam_f = lam_vec.astype(F32)
    lam = jnp.exp(jnp.sum(lam_f[0] * lam_f[1])) - jnp.exp(jnp.sum(lam_f[2] * lam_f[3])) + lam_init
    qd_l = apply_rope(pl[5].reshape(B, S, DIFF_HEADS, 2, DIFF_QK), *rope32)
    kd_l = apply_rope(pl[6].reshape(B, S, DIFF_HEADS, 2, DIFF_QK), *rope32)
    vd_l = pl[7].reshape(B, S, DIFF_HEADS, HEAD_DIM)
    kd_c = pc[6].reshape(B, L, DIFF_HEADS, 2, DIFF_QK)
    vd_c = pc[7].reshape(B, L, DIFF_HEADS, HEAD_DIM)
    k_all = jnp.concatenate([kd_c, kd_l], axis=1)
    v_all = jnp.concatenate([vd_c, vd_l], axis=1)
    diff_l = diff_head_norm(diff_latent(qd_l, k_all, v_all, lam), norm_g, lam_init).reshape(B, S, DIFF_WIDTH)

    out_l = jnp.concatenate([lru_l, swa_l, diff_l], axis=-1) @ w_out
    if not need_ctx:
        return None, out_l
    qs_c = pc[2].reshape(B, L, SWA_KV_HEADS, SWA_GROUP, HEAD_DIM)
    swa_c = context_sink_attention(qs_c, ks_c, vs_c, sink_hg)
    qd_c = pc[5].reshape(B, L, DIFF_HEADS, 2, DIFF_QK)
    diff_c = diff_head_norm(diff_attention(qd_c, kd_c, vd_c, lam), norm_g, lam_init).reshape(B, L, DIFF_WIDTH)
    out_c = jnp.concatenate([lru_c, swa_c, diff_c], axis=-1) @ w_out
    return out_c, out_l


def swiglu(u, w1, w3, w2):
    return (jax.nn.silu(u @ w1) * (u @ w3)) @ w2


def moe_swiglu(u, router_w, router_b, w1, w3, w2):
    logits = (u @ router_w + router_b).astype(F32)
    top_v, top_i = lax.top_k(logits, TOP_K)
    gates = jax.nn.softmax(top_v, axis=-1)
    combine = jnp.sum(jax.nn.one_hot(top_i, N_EXPERTS, dtype=F32) * gates[..., None], axis=-2)
    out = jnp.zeros_like(u)
    for e in range(N_EXPERTS):
        out = out + combine[..., e:e + 1].astype(u.dtype) * swiglu(u, w1[e], w3[e], w2[e])
    return out


def setup_inputs(seed: int = 0) -> dict:
    key = jax.random.key(seed)
    ks = jax.random.split(key, 32)
    nrm = jax.random.normal
    D = D_MODEL
    a0 = jax.random.uniform(ks[10], (DEPTH, 2, LRU_WIDTH), minval=0.9, maxval=0.999)
    root = a0 ** (1.0 / LRU_C)
    return {
        "x": nrm(ks[0], (BATCH, SEQ, D), F32),
        "c": nrm(ks[1], (BATCH, D), F32),
        "ctx": nrm(ks[2], (BATCH, CTX_LEN, D), F32),
        "c_ctx": nrm(ks[3], (D,), F32),
        "ada_w": nrm(ks[4], (DEPTH, D, 6 * D), F32) * (0.5 * D ** -0.5),
        "ada_b": nrm(ks[5], (DEPTH, 6 * D), F32) * 0.02,
        "w_in": nrm(ks[6], (DEPTH, D, IN_COLS), F32) * D ** -0.5,
        "w_out": nrm(ks[7], (DEPTH, MIX_WIDTH, D), F32) * (MIX_WIDTH ** -0.5 * DEEPNORM_BETA),
        "lru_conv_w": nrm(ks[8], (DEPTH, CONV_W, LRU_WIDTH), F32) * CONV_W ** -0.5,
        "lru_conv_b": nrm(ks[9], (DEPTH, LRU_WIDTH), F32) * 0.02,
        "lru_gate_w": nrm(ks[11], (DEPTH, 2, 2, LRU_BLOCKS, LRU_BLOCK, LRU_BLOCK), F32) * LRU_BLOCK ** -0.5,
        "lru_gate_b": nrm(ks[12], (DEPTH, 2, 2, LRU_WIDTH), F32) * 0.02,
        "lru_lam": jnp.log(root) - jnp.log1p(-root),
        "swa_sink": nrm(ks[13], (DEPTH, SWA_Q_HEADS), F32) * 0.5,
        "diff_lam": nrm(ks[14], (DEPTH, 4, DIFF_QK), F32) * 0.1,
        "diff_norm_g": 1.0 + 0.02 * nrm(ks[15], (DEPTH, HEAD_DIM), F32),
        "ln_g": 1.0 + 0.02 * nrm(ks[16], (DEPTH, 2, D), F32),
        "ln_b": 0.02 * nrm(ks[17], (DEPTH, 2, D), F32),
        "ffn_w1": nrm(ks[18], (N_DENSE, D, D_FF), F32) * D ** -0.5,
        "ffn_w3": nrm(ks[19], (N_DENSE, D, D_FF), F32) * D ** -0.5,
        "ffn_w2": nrm(ks[20], (N_DENSE, D_FF, D), F32) * (D_FF ** -0.5 * DEEPNORM_BETA),
        "moe_router_w": nrm(ks[21], (N_MOE, D, N_EXPERTS), F32) * D ** -0.5,
        "moe_router_b": nrm(ks[22], (N_MOE, N_EXPERTS), F32) * 0.01,
        "moe_w1": nrm(ks[23], (N_MOE, N_EXPERTS, D, EXPERT_FF), F32) * D ** -0.5,
        "moe_w3": nrm(ks[24], (N_MOE, N_EXPERTS, D, EXPERT_FF), F32) * D ** -0.5,
        "moe_w2": nrm(ks[25], (N_MOE, N_EXPERTS, EXPERT_FF, D), F32) * (EXPERT_FF ** -0.5 * DEEPNORM_BETA),
    }


def reference(x, c, ctx, c_ctx, ada_w, ada_b, w_in, w_out, lru_conv_w, lru_conv_b, lru_gate_w, lru_gate_b,
              lru_lam, swa_sink, diff_lam, diff_norm_g, ln_g, ln_b, ffn_w1, ffn_w3, ffn_w2,
              moe_router_w, moe_router_b, moe_w1, moe_w3, moe_w2):
    S = x.shape[1]
    rows = S // GRID_W
    rope64 = axial_rope(rows, HEAD_DIM)
    rope32 = axial_rope(rows, DIFF_QK)
    silu_c = jax.nn.silu(c)
    silu_cc = jax.nn.silu(c_ctx)
    h_lat, h_ctx = x, ctx
    for layer in range(DEPTH):
        need_ctx = layer < DEPTH - 1
        lam_init = 0.8 - 0.6 * math.exp(-0.3 * layer)
        mod_l = jnp.split((silu_c @ ada_w[layer] + ada_b[layer])[:, None, :], 6, axis=-1)
        mod_c = jnp.split(silu_cc @ ada_w[layer] + ada_b[layer], 6, axis=-1)

        u_lat = h_lat * (1.0 + mod_l[1]) + mod_l[0]
        u_ctx = h_ctx * (1.0 + mod_c[1]) + mod_c[0]
        m_ctx, m_lat = token_mix(u_ctx, u_lat, w_in[layer], w_out[layer], lru_conv_w[layer], lru_conv_b[layer],
                                 lru_gate_w[layer], lru_gate_b[layer], lru_lam[layer], swa_sink[layer],
                                 diff_lam[layer], diff_norm_g[layer], lam_init, rope64, rope32, need_ctx)
        h_lat = layer_norm(DEEPNORM_ALPHA * h_lat + mod_l[2] * m_lat, ln_g[layer, 0], ln_b[layer, 0])
        if need_ctx:
            h_ctx = layer_norm(DEEPNORM_ALPHA * h_ctx + mod_c[2] * m_ctx, ln_g[layer, 0], ln_b[layer, 0])

        u_lat = h_lat * (1.0 + mod_l[4]) + mod_l[3]
        u_ctx = h_ctx * (1.0 + mod_c[4]) + mod_c[3]
        j = layer // 2
        if layer % 2 == 0:
            f_lat = swiglu(u_lat, ffn_w1[j], ffn_w3[j], ffn_w2[j])
            f_ctx = swiglu(u_ctx, ffn_w1[j], ffn_w3[j], ffn_w2[j]) if need_ctx else None
        else:
            f_lat = moe_swiglu(u_lat, moe_router_w[j], moe_router_b[j], moe_w1[j], moe_w3[j], moe_w2[j])
            f_ctx = moe_swiglu(u_ctx, moe_router_w[j], moe_router_b[j], moe_w1[j], moe_w3[j], moe_w2[j]) if need_ctx else None
        h_lat = layer_norm(DEEPNORM_ALPHA * h_lat + mod_l[5] * f_lat, ln_g[layer, 1], ln_b[layer, 1])
        if need_ctx:
            h_ctx = layer_norm(DEEPNORM_ALPHA * h_ctx + mod_c[5] * f_ctx, ln_g[layer, 1], ln_b[layer, 1])
    return h_lat
```

```python
import math
from contextlib import ExitStack

import numpy as np
import ml_dtypes

import concourse.bass as bass
import concourse.mybir as mybir
from concourse.bass_utils import run_bass_kernel_spmd

F32 = mybir.dt.float32
BF16 = mybir.dt.bfloat16
AF = mybir.ActivationFunctionType
ALU = mybir.AluOpType
AX = mybir.AxisListType
NPBF = ml_dtypes.bfloat16

D_MODEL = 1024
BATCH = 4
SEQ = 4096
DEPTH = 4
CTX = 256
HALF = SEQ // 2
NT = CTX + HALF
D_FF = 2816
NJ = D_FF // 128
NEXP = 8
LN_EPS = 1e-5
ALPHA = (2.0 * DEPTH) ** 0.25
NW = 24 * 128 + 384
NCORES = 8


class Buf:
    __slots__ = ("w", "r", "excl")

    def __init__(self, excl=False):
        self.w = None
        self.r = {}
        self.excl = excl


class Tile:
    def __init__(self, t, excl=False):
        self.t = t
        self.b = Buf(excl)

    def __getitem__(self, idx):
        return self.t[idx]


class Sched:
    EPOCH = 16000
    NDMA = 28

    def __init__(self, nc, es):
        self.nc, self.es = nc, es
        self.eng = dict(pe=nc.tensor, act=nc.scalar, dve=nc.vector, pool=nc.gpsimd, sp=nc.sync)
        self.cnt = {}
        self.sem = {}
        self.own = {e: set() for e in self.eng}
        self.nsem = 0
        self.waited = {e: {} for e in self.eng}
        self.dsem = []
        self.dcnt = []
        self.dn = 0
        self.allsems = []
        self.csem = None
        self.ccnt = 0

    def _newsem(self, name):
        self.nsem += 1
        s = self.es.enter_context(self.nc.semaphore(f"{name}_{self.nsem}"))
        self.allsems.append(s)
        return s

    def _wait(self, e, deps):
        w = self.waited[e]
        for sem, val in deps:
            if w.get(sem, 0) < val:
                self.eng[e].wait_ge(sem, val)
                w[sem] = val

    def _deps(self, e, reads, writes):
        deps = []
        own = self.own[e]
        for b in reads:
            if b.w is not None:
                deps.append(b.w)
            if b.excl:
                deps.extend(b.r.items())
        for b in writes:
            if b.w is not None:
                deps.append(b.w)
            deps.extend(x for x in b.r.items() if x[0] not in own)
        if e == "pe":
            deps = [d for d in deps if d[0] not in own]
        return deps

    def _mark(self, tok, reads, writes):
        for b in reads:
            if b.excl:
                b.w = tok
                b.r = {}
            else:
                b.r[tok[0]] = tok[1]
        for b in writes:
            b.w = tok
            b.r = {}

    def op(self, e, fn, reads=(), writes=()):
        reads = [x.b if isinstance(x, Tile) else x for x in reads]
        writes = [x.b if isinstance(x, Tile) else x for x in writes]
        self._wait(e, self._deps(e, reads, writes))
        ins = fn(self.eng[e])
        if self.cnt.get(e, self.EPOCH) >= self.EPOCH:
            self.sem[e] = self._newsem(e)
            self.cnt[e] = 0
            self.own[e].add(self.sem[e])
        self.cnt[e] += 1
        ins.then_inc(self.sem[e], 1)
        tok = (self.sem[e], self.cnt[e])
        self._mark(tok, reads, writes)
        return tok

    def dma(self, q, out, in_, reads=(), writes=()):
        reads = [x.b if isinstance(x, Tile) else x for x in reads]
        writes = [x.b if isinstance(x, Tile) else x for x in writes]
        self._wait(q, self._deps(q, reads, writes))
        i = self.dn % self.NDMA
        self.dn += 1
        if i >= len(self.dsem):
            self.dsem.append(self._newsem("d"))
            self.dcnt.append(0)
        sem = self.dsem[i]
        if self.dcnt[i] > 0:
            self._wait(q, [(sem, self.dcnt[i])])
        self.dcnt[i] += 16
        self.eng[q].dma_start(out=out, in_=in_).then_inc(sem, 16)
        tok = (sem, self.dcnt[i])
        self._mark(tok, reads, writes)
        return tok

    def coll(self, kind, groups, src, dst):
        q = "pool"
        reads, writes = [src.b], [dst.b]
        self._wait(q, self._deps(q, reads, writes))
        if self.csem is None:
            self.csem = self._newsem("cc")
            self.ccnt = 0
        self.ccnt += 1
        self.nc.gpsimd.collective_compute(kind, ALU.bypass, replica_groups=groups, ins=[src.t.opt()],
                                          outs=[dst.t.opt()]).then_inc(self.csem, 1)
        tok = (self.csem, self.ccnt)
        self._mark(tok, reads, writes)
        return tok

    def barrier(self):
        deps = [(s, c) for s, c in zip(self.dsem, self.dcnt) if c > 0]
        deps += [(self.sem[e], self.cnt[e]) for e in self.sem]
        if self.csem is not None:
            deps.append((self.csem, self.ccnt))
        for e in self.eng:
            self._wait(e, deps)

    def finish(self):
        deps = [(s, c) for s, c in zip(self.dsem, self.dcnt) if c > 0]
        deps += [(self.sem[e], self.cnt[e]) for e in self.sem]
        if self.csem is not None:
            deps.append((self.csem, self.ccnt))
        self._wait("sp", deps)


class K:
    def __init__(self):
        self.nc = bass.Bass("TRN2", target_bir_lowering=False)
        self.es = ExitStack()
        self.S = Sched(self.nc, self.es)
        self.n = 0
        self.psum = None

    def dram(self, name, shape, dt, kind):
        t = self.nc.dram_tensor(name, list(shape), dt, kind=kind).ap()
        return Tile(t)

    def sb(self, shape, dt, es=None, name=None):
        self.n += 1
        t = (es or self.es).enter_context(self.nc.sbuf_tensor(f"{name or 't'}_{self.n}", list(shape), dt))
        return Tile(t)

    def scope(self):
        k = self

        class _Scope(ExitStack):
            def __exit__(self, *a):
                k.S.barrier()
                return super().__exit__(*a)
        return _Scope()

    def banks(self):
        if self.psum is None:
            self.psum = []
            for i in range(8):
                t = self.es.enter_context(self.nc.psum_tensor(f"ps{i}", [128, 512], F32))
                self.psum.append(Tile(t, excl=True))
        return self.psum


class Rot:
    def __init__(self, items):
        self.items = items
        self.i = 0

    def next(self):
        x = self.items[self.i % len(self.items)]
        self.i += 1
        return x


def segs(c0, n):
    out = []
    if c0 < CTX:
        hi = min(CTX, c0 + n)
        out.append((c0, hi, 1))
        if c0 + n > CTX:
            out.append((CTX, c0 + n, 0))
    else:
        out.append((c0, c0 + n, 0))
    return out


def emit_consts(k, cst_d):
    S = k.S
    c = {}
    c["ident"] = k.sb([128, 128], F32, name="ident")
    S.dma("sp", c["ident"][:], cst_d[:, :], reads=[cst_d], writes=[c["ident"]])
    c["onesf"] = k.sb([128, 128], F32, name="onesf")
    S.op("dve", lambda e: e.memset(c["onesf"][:], 1.0), writes=[c["onesf"]])
    c["onesb"] = k.sb([128, 128], BF16, name="onesb")
    S.op("dve", lambda e: e.memset(c["onesb"][:], 1.0), writes=[c["onesb"]])
    c["eps"] = k.sb([128, 1], F32, name="eps")
    S.op("dve", lambda e: e.memset(c["eps"][:], LN_EPS), writes=[c["eps"]])
    return c


def emit_mods(k, cc_d, adaw_d, adab_d, MODX):
    S = k.S
    P = k.banks()
    with k.scope() as es:
        cc = k.sb([128, 8, 2], F32, es)
        sl = k.sb([128, 8, 2], F32, es)
        adab = k.sb([128, 48], F32, es)
        wts = Rot([k.sb([128, 8, 512], F32, es) for _ in range(2)])
        S.dma("sp", cc[:], cc_d[:, :, :], reads=[cc_d], writes=[cc])
        S.dma("sp", adab[:], adab_d[:, :], reads=[adab_d], writes=[adab])
        S.op("act", lambda e: e.activation(out=sl[:], in_=cc[:], func=AF.Silu), reads=[cc], writes=[sl])
        ps = P[0]
        wsrc = adaw_d.t.rearrange("(k p) n -> p k n", p=128)
        for piece in range(12):
            wt = wts.next()
            S.dma("sp", wt[:], wsrc[:, :, piece * 512:(piece + 1) * 512], reads=[adaw_d], writes=[wt])
            for m in range(4):
                ma = piece * 4 + m
                for kk in range(8):
                    S.op("pe", lambda e, wt=wt, m=m, kk=kk, ma=ma: e.matmul(
                        ps[:, ma * 2:ma * 2 + 2], lhsT=wt[:, kk, m * 128:(m + 1) * 128], rhs=sl[:, kk, :],
                        start=(kk == 0), stop=(kk == 7)), reads=[wt, sl], writes=[ps])
        ps3 = ps[:, 0:96].rearrange("p (m j) -> p m j", j=2)
        mx = MODX[:, 0:6, :, :].rearrange("p w c j -> p (w c) j")
        for j in range(2):
            S.op("dve", lambda e, j=j: e.tensor_tensor(out=mx[:, :, j], in0=ps3[:, :, j], in1=adab[:, :], op=ALU.add),
                 reads=[ps, adab], writes=[MODX])
        S.op("dve", lambda e: e.tensor_scalar_add(out=MODX[:, 6, :, :], in0=MODX[:, 1, :, :], scalar1=1.0),
             reads=[MODX], writes=[MODX])
        S.op("dve", lambda e: e.tensor_scalar_add(out=MODX[:, 7, :, :], in0=MODX[:, 4, :, :], scalar1=1.0),
             reads=[MODX], writes=[MODX])


def emit_phaseA(k, hT, MODX, win_d, rope_d, o):
    S = k.S
    P = k.banks()
    with k.scope() as es:
        WIN = k.sb([128, 8, NW], BF16, es, "win")
        wsrc = win_d.t.rearrange("(k p) n -> p k n", p=128)
        for pc in range(4):
            lo, hi = pc * (NW // 4), (pc + 1) * (NW // 4)
            S.dma("pool", WIN[:, :, lo:hi], wsrc[:, :, lo:hi], reads=[win_d], writes=[WIN])
        ub = Rot([k.sb([128, 8, 512], BF16, es, "u") for _ in range(2)])
        rt = Rot([k.sb([128, 4, 512], F32, es, "rope") for _ in range(2)])
        stf = Rot([k.sb([128, 512], F32, es, "stf") for _ in range(3)])
        stb = Rot([k.sb([128, 512], BF16, es, "stb") for _ in range(3)])
        tm1 = Rot([k.sb([128, 512], F32, es, "tm1") for _ in range(2)])
        tm2 = Rot([k.sb([128, 512], F32, es, "tm2") for _ in range(2)])
        vss = Rot([k.sb([128, 2, 128], BF16, es, "vss") for _ in range(2)])
        vds = Rot([k.sb([128, 4, 128], BF16, es, "vds") for _ in range(2)])
        for v in vss.items + vds.items:
            S.op("dve", lambda e, v=v: e.memset(v[:], 1.0), writes=[v])
        pb = Rot(P)

        tiles = [(0, 256, 1)] + [(CTX + i * 512, 512, 0) for i in range(4)]
        for (c0, N, j) in tiles:
            lat = j == 0
            u = ub.next()
            for kk in range(8):
                S.op("act", lambda e, kk=kk: e.activation(
                    out=u[:, kk, 0:N], in_=hT[:, kk, c0:c0 + N], func=AF.Identity,
                    scale=MODX[:, 6, kk, j:j + 1], bias=MODX[:, 0, kk, j:j + 1]), reads=[hT, MODX], writes=[u])
            if lat:
                R = rt.next()
                S.dma("sp", R[:], rope_d[:, :, c0 - CTX:c0 - CTX + 512], reads=[rope_d], writes=[R])

            def proj(ch, ps):
                for kk in range(8):
                    S.op("pe", lambda e, kk=kk: e.matmul(
                        ps[:, 0:N], lhsT=WIN[:, kk, ch * 128:(ch + 1) * 128], rhs=u[:, kk, 0:N],
                        start=(kk == 0), stop=(kk == 7)), reads=[WIN, u], writes=[ps])

            for c in range(2):
                ps = pb.next()
                proj(c, ps)
                st = stf.next()
                S.op("act", lambda e: e.activation(out=st[:, 0:N], in_=ps[:, 0:N], func=AF.Identity),
                     reads=[ps], writes=[st])
                S.dma("sp", o["XL%d" % c][:, c0:c0 + N], st[:, 0:N], reads=[st], writes=[o["XL%d" % c]])
            for c in range(2):
                ps = pb.next()
                proj(2 + c, ps)
                st = stf.next()
                S.op("act", lambda e: e.activation(out=st[:, 0:N], in_=ps[:, 0:N], func=AF.Gelu_apprx_tanh),
                     reads=[ps], writes=[st])
                S.dma("sp", o["GT"][c * 128:(c + 1) * 128, c0:c0 + N], st[:, 0:N], reads=[st], writes=[o["GT"]])
            for (nb, sb_, n, tc, dst) in [(4, 8, 4, 0, "QS"), (12, 14, 2, 0, "KS"), (16, 18, 2, 2, "QD"),
                                          (20, 22, 2, 2, "KD")]:
                for i in range(n):
                    psA = pb.next()
                    proj(nb + i, psA)
                    st = stb.next()
                    if lat:
                        psB = pb.next()
                        proj(sb_ + i, psB)
                        t1, t2 = tm1.next(), tm2.next()
                        S.op("dve", lambda e: e.tensor_tensor(out=t1[:], in0=psA[:, :], in1=R[:, tc, :], op=ALU.mult),
                             reads=[psA, R], writes=[t1])
                        S.op("dve", lambda e: e.tensor_tensor(out=t2[:], in0=psB[:, :], in1=R[:, tc + 1, :],
                                                              op=ALU.mult), reads=[psB, R], writes=[t2])
                        S.op("pool", lambda e: e.tensor_tensor(out=st[:], in0=t1[:], in1=t2[:], op=ALU.add),
                             reads=[t1, t2], writes=[st])
                    else:
                        S.op("act", lambda e: e.activation(out=st[:, 0:N], in_=psA[:, 0:N], func=AF.Identity),
                             reads=[psA], writes=[st])
                    S.dma("sp", o[dst][i * 128:(i + 1) * 128, c0:c0 + N], st[:, 0:N], reads=[st], writes=[o[dst]])
            for blk in range(N // 128):
                ps = pb.next()
                for kk in range(8):
                    S.op("pe", lambda e, kk=kk: e.matmul(
                        ps[:, 0:384], lhsT=u[:, kk, blk * 128:(blk + 1) * 128], rhs=WIN[:, kk, 3072:3456],
                        start=(kk == 0), stop=(kk == 7)), reads=[WIN, u], writes=[ps])
                vs, vd = vss.next(), vds.next()
                S.op("act", lambda e: e.activation(out=vs[:, :, 0:64],
                                                   in_=ps[:, 0:128].rearrange("p (h d) -> p h d", d=64),
                                                   func=AF.Identity), reads=[ps], writes=[vs])
                S.op("dve", lambda e: e.tensor_copy(out=vd[:, :, 0:64],
                                                    in_=ps[:, 128:384].rearrange("p (h d) -> p h d", d=64)),
                     reads=[ps], writes=[vd])
                r0 = c0 + blk * 128
                S.dma("sp", o["VS"][r0:r0 + 128, :], vs[:].rearrange("p h d -> p (h d)"), reads=[vs],
                      writes=[o["VS"]])
                vch, vr = r0 // VDR, r0 % VDR
                S.dma("sp", o["VD%d" % vch][vr:vr + 128, :], vd[:].rearrange("p h d -> p (h d)"), reads=[vd],
                      writes=[o["VD%d" % vch]])


def emit_lru(k, mixT, C, g, prm):
    S = k.S
    P = k.banks()
    with k.scope() as es:
        xs = k.sb([128, 1 + SEQ + 2], F32, es, "xs")
        xc = k.sb([128, 1 + CTX + 2], F32, es, "xc")
        U = k.sb([128, CTX + SEQ], F32, es, "U")
        OUT = k.sb([128, NT], F32, es, "lout")
        GTs = k.sb([128, NT], F32, es, "gts")
        tR = Rot([k.sb([128, 512], F32, es, "tR") for _ in range(2)])
        tI = Rot([k.sb([128, 512], F32, es, "tI") for _ in range(2)])
        tA = Rot([k.sb([128, 512], F32, es, "tA") for _ in range(2)])
        tH = Rot([k.sb([128, 512], F32, es, "tH") for _ in range(3)])
        sp = k.sb([128, 4], F32, es, "sp")
        nsp8 = k.sb([128, 2, 2], F32, es, "nsp8")
        nsp16 = k.sb([128, 2, 2], F32, es, "nsp16")
        lam = prm["LAM"]
        spv = sp[:, 0:4]
        lamv = lam[:].rearrange("p c d -> p (c d)")
        S.op("act", lambda e: e.activation(out=spv, in_=lamv, func=AF.Exp, scale=-1.0), reads=[lam], writes=[sp])
        S.op("dve", lambda e: e.tensor_scalar_add(out=spv, in0=spv, scalar1=1.0), reads=[sp], writes=[sp])
        S.op("act", lambda e: e.activation(out=spv, in_=spv, func=AF.Ln), reads=[sp], writes=[sp])
        S.op("dve", lambda e: e.tensor_scalar_mul(out=nsp8[:].rearrange("p c d -> p (c d)"), in0=spv, scalar1=-8.0),
             reads=[sp], writes=[nsp8])
        S.op("dve", lambda e: e.tensor_scalar_mul(out=nsp16[:].rearrange("p c d -> p (c d)"), in0=spv, scalar1=-16.0),
             reads=[sp], writes=[nsp16])
        pg = Rot(P[0:4])
        CW, CB, GB, GW, SEL = prm["CW"], prm["CB"], prm["GB"], prm["GW"], prm["SEL"]
        for c in range(2):
            rows = slice(c * 128, (c + 1) * 128)
            S.op("dve", lambda e: e.memset(xs[:, 0:1], 0.0), writes=[xs])
            S.op("dve", lambda e: e.memset(xs[:, 1 + SEQ:3 + SEQ], 0.0), writes=[xs])
            S.op("dve", lambda e: e.memset(xc[:, 0:1], 0.0), writes=[xc])
            S.op("dve", lambda e: e.memset(xc[:, 1 + CTX:3 + CTX], 0.0), writes=[xc])
            XGc = g["XG"][c]
            S.dma("sp", xs[:, 1:1 + HALF], XGc[0, :, CTX:NT], reads=[XGc], writes=[xs])
            S.dma("sp", xs[:, 1 + HALF:1 + SEQ], XGc[1, :, CTX:NT], reads=[XGc], writes=[xs])
            S.dma("sp", xc[:, 1:1 + CTX], XGc[0, :, 0:CTX], reads=[XGc], writes=[xc])
            S.dma("sp", GTs[:], g["GT"][rows, :], reads=[g["GT"]], writes=[GTs])
            for (src, L, d0) in [(xc, CTX, 0), (xs, SEQ, CTX)]:
                S.op("act", lambda e: e.activation(out=U[:, d0:d0 + L], in_=src[:, 1:1 + L], func=AF.Identity,
                                                   scale=CW[:, c, 1:2], bias=CB[:, c:c + 1]),
                     reads=[src, CW, CB], writes=[U])
                for (off, wi) in [(0, 0), (2, 2), (3, 3)]:
                    S.op("dve", lambda e, off=off, wi=wi: e.scalar_tensor_tensor(
                        out=U[:, d0:d0 + L], in0=src[:, off:off + L], scalar=CW[:, c, wi:wi + 1], in1=U[:, d0:d0 + L],
                        op0=ALU.mult, op1=ALU.add), reads=[src, CW, U], writes=[U])
            S.op("dve", lambda e: e.memset(OUT[:], 0.0), writes=[OUT])
            for d in range(2):
                lat = [(CTX + i * 512, 512, i) for i in range(8)]
                if d == 1:
                    lat = lat[::-1]
                state = None
                hprev = None
                for (u0, L, kind) in [(0, CTX, -1)] + lat:
                    pss = []
                    for gi in range(2):
                        ps = pg.next()
                        S.op("pe", lambda e, gi=gi, ps=ps: e.matmul(ps[:, 0:L], lhsT=GW[:, c, d, gi, :],
                                                                    rhs=U[:, u0:u0 + L], start=True, stop=True),
                             reads=[GW, U], writes=[ps])
                        pss.append(ps)
                    r, ii, a, h = tR.next(), tI.next(), tA.next(), tH.next()
                    S.op("act", lambda e: e.activation(out=r[:, 0:L], in_=pss[0][:, 0:L], func=AF.Sigmoid,
                                                       bias=GB[:, c, d, 0:1]), reads=[pss[0], GB], writes=[r])
                    S.op("act", lambda e: e.activation(out=ii[:, 0:L], in_=pss[1][:, 0:L], func=AF.Sigmoid,
                                                       bias=GB[:, c, d, 1:2]), reads=[pss[1], GB], writes=[ii])
                    S.op("act", lambda e: e.activation(out=a[:, 0:L], in_=r[:, 0:L], func=AF.Exp,
                                                       scale=nsp8[:, c, d:d + 1]), reads=[r, nsp8], writes=[a])
                    S.op("act", lambda e: e.activation(out=r[:, 0:L], in_=r[:, 0:L], func=AF.Exp,
                                                       scale=nsp16[:, c, d:d + 1]), reads=[r, nsp16], writes=[r])
                    S.op("act", lambda e: e.activation(out=r[:, 0:L], in_=r[:, 0:L], func=AF.Sqrt, scale=-1.0,
                                                       bias=1.0), reads=[r], writes=[r])
                    S.op("dve", lambda e: e.tensor_tensor(out=ii[:, 0:L], in0=ii[:, 0:L], in1=U[:, u0:u0 + L],
                                                          op=ALU.mult), reads=[ii, U], writes=[ii])
                    S.op("dve", lambda e: e.tensor_tensor(out=ii[:, 0:L], in0=ii[:, 0:L], in1=r[:, 0:L],
                                                          op=ALU.mult), reads=[ii, r], writes=[ii])
                    if d == 0:
                        vo, va, vb = h[:, 0:L], a[:, 0:L], ii[:, 0:L]
                    else:
                        vo, va, vb = h[:, L - 1::-1], a[:, L - 1::-1], ii[:, L - 1::-1]
                    init = 0.0 if state is None else state
                    rd = [a, ii] + ([hprev] if hprev is not None else [])
                    S.op("dve", lambda e: e.tensor_tensor_scan(out=vo, data0=va, data1=vb, initial=init,
                                                               op0=ALU.mult, op1=ALU.add), reads=rd, writes=[h])
                    state = h[:, L - 1:L] if d == 0 else h[:, 0:1]
                    hprev = h
                    if kind < 0:
                        S.op("dve", lambda e: e.tensor_tensor(out=OUT[:, 0:CTX], in0=h[:, 0:L], in1=OUT[:, 0:CTX],
                                                              op=ALU.add), reads=[h, OUT], writes=[OUT])
                    else:
                        hf = kind // 4
                        lo = CTX + (kind % 4) * 512
                        S.op("dve", lambda e: e.scalar_tensor_tensor(
                            out=OUT[:, lo:lo + L], in0=h[:, 0:L], scalar=SEL[:, hf:hf + 1], in1=OUT[:, lo:lo + L],
                            op0=ALU.mult, op1=ALU.add), reads=[h, OUT, SEL], writes=[OUT])
            S.op("pool", lambda e: e.tensor_tensor(out=mixT[:, c, :], in0=OUT[:], in1=GTs[:], op=ALU.mult),
                 reads=[OUT, GTs], writes=[mixT])


def emit_swa(k, mixT, C, g, prm):
    S = k.S
    P = k.banks()
    with k.scope() as es:
        KS = [k.sb([128, CTX + 128 + HALF + 128], BF16, es, "ks") for _ in range(2)]
        VSa = k.sb([128, 20, 256], BF16, es, "vsa")
        MSK = k.sb([128, 8, 512], BF16, es, "msk")
        ESK = k.sb([128, 8], F32, es, "esk")
        qb = Rot([k.sb([128, 4, 512], BF16, es, "qs") for _ in range(2)])
        Eb = Rot([k.sb([128, 512], BF16, es, "E") for _ in range(4)])
        den = Rot([k.sb([128, 512], F32, es, "den") for _ in range(2)])
        S.dma("sp", MSK[:], g["MSK"][:, :, :], reads=[g["MSK"]], writes=[MSK])
        S.op("act", lambda e: e.activation(out=ESK[:], in_=prm["SINK"][:], func=AF.Exp), reads=[prm["SINK"]],
             writes=[ESK])
        for hk in range(2):
            rows = slice(hk * 128, (hk + 1) * 128)
            S.dma("sp", KS[hk][:, 0:CTX], g["KSO"][rows, 0:CTX], reads=[g["KSO"]], writes=[KS[hk]])
            S.dma("sp", KS[hk][:, CTX + 128:CTX + 128 + HALF], g["KSO"][rows, CTX:NT], reads=[g["KSO"]],
                  writes=[KS[hk]])
            S.dma("sp", KS[hk][:, CTX:CTX + 128], g["KSG"][0, rows, NT - 128:NT], reads=[g["KSG"]], writes=[KS[hk]])
            S.dma("sp", KS[hk][:, CTX + 128 + HALF:], g["KSG"][1, rows, CTX:CTX + 128], reads=[g["KSG"]],
                  writes=[KS[hk]])
        vo = g["VSO"].t.rearrange("(b p) n -> p b n", p=128)
        S.dma("sp", VSa[:, 0:2, :], vo[:, 0:2, :], reads=[g["VSO"]], writes=[VSa])
        S.dma("sp", VSa[:, 3:19, :], vo[:, 2:18, :], reads=[g["VSO"]], writes=[VSa])
        S.dma("sp", VSa[:, 2, :], g["VSG"][0, NT - 128:NT, :], reads=[g["VSG"]], writes=[VSa])
        S.dma("sp", VSa[:, 19, :], g["VSG"][1, CTX:CTX + 128, :], reads=[g["VSG"]], writes=[VSa])
        accs = Rot(P[0:2])
        scs = Rot(P[2:8])
        qsrc = g["QS"].t.rearrange("(c p) n -> p c n", p=128)
        for t in [-1, 0, 1, 2, 3]:
            N = CTX if t < 0 else 512
            c0 = 0 if t < 0 else CTX + t * 512
            Q = qb.next()
            S.dma("sp", Q[:, :, 0:N], qsrc[:, :, c0:c0 + N], reads=[g["QS"]], writes=[Q])
            blocks = [(0, 0, None), (1, 128, None)]
            if t >= 0:
                for r in range(6):
                    mk = r
                    if t == 0 and r == 0:
                        mk = 6
                    if t == 3 and r == 5:
                        mk = 7
                    blocks.append((2 + 4 * t + r, CTX + (4 * t + r) * 128, mk))
            for h in range(8):
                qc, pb, hk = h // 2, (h % 2) * 64, h // 4
                acc = accs.next()
                for bi, (vb, kcol, mk) in enumerate(blocks):
                    sps = scs.next()
                    S.op("pe", lambda e: e.matmul(sps[:, 0:N], lhsT=KS[hk][pb:pb + 64, kcol:kcol + 128],
                                                  rhs=Q[pb:pb + 64, qc, 0:N], start=True, stop=True),
                         reads=[KS[hk], Q], writes=[sps])
                    E = Eb.next()
                    S.op("act", lambda e: e.activation(out=E[:, 0:N], in_=sps[:, 0:N], func=AF.Exp, scale=0.125),
                         reads=[sps], writes=[E])
                    if mk is not None:
                        S.op("pool", lambda e: e.tensor_tensor(out=E[:, 0:N], in0=E[:, 0:N], in1=MSK[:, mk, 0:N],
                                                               op=ALU.mult), reads=[E, MSK], writes=[E])
                    S.op("pe", lambda e: e.matmul(acc[:, 0:N], lhsT=VSa[:, vb, hk * 128:(hk + 1) * 128],
                                                  rhs=E[:, 0:N], start=(bi == 0), stop=(bi == len(blocks) - 1)),
                         reads=[VSa, E], writes=[acc])
                dn = den.next()
                S.op("dve", lambda e: e.tensor_scalar(out=dn[0:64, 0:N], in0=acc[64:128, 0:N],
                                                      scalar1=ESK[64:128, h:h + 1], scalar2=None, op0=ALU.add),
                     reads=[acc, ESK], writes=[dn])
                S.op("dve", lambda e: e.reciprocal(out=dn[0:64, 0:N], in_=dn[0:64, 0:N]), reads=[dn], writes=[dn])
                S.op("dve", lambda e: e.tensor_tensor(out=mixT[pb:pb + 64, 2 + qc, c0:c0 + N], in0=acc[0:64, 0:N],
                                                      in1=dn[0:64, 0:N], op=ALU.mult), reads=[acc, dn],
                     writes=[mixT])


def emit_diff(k, mixT, C, g, prm, lam_init):
    S = k.S
    P = k.banks()
    with k.scope() as es:
        KD = k.sb([128, 2, CTX + SEQ], BF16, es, "kd")
        VDa = k.sb([128, 34, 512], BF16, es, "vda")
        qb = Rot([k.sb([128, 2, 512], BF16, es, "qd") for _ in range(2)])
        qm = [Rot([k.sb([128, 2, 512], BF16, es, "qm") for _ in range(2)]) for _ in range(2)]
        Eb = Rot([k.sb([128, 512], BF16, es, "E") for _ in range(5)])
        tr = Rot([k.sb([128, 512], F32, es, "tr") for _ in range(4)])
        tt = Rot([k.sb([128, 512], F32, es, "tt") for _ in range(4)])
        tO = Rot([k.sb([128, 512], F32, es, "tO") for _ in range(2)])
        tq = Rot([k.sb([128, 512], F32, es, "tq") for _ in range(2)])
        lt = k.sb([128, 2, 32], F32, es, "lt")
        ls = k.sb([128, 2], F32, es, "ls")
        NLAM = k.sb([128, 1], F32, es, "nlam")
        GN = k.sb([128, 1], F32, es, "gn")
        DL = prm["DL"]
        for i in range(2):
            S.op("dve", lambda e, i=i: e.tensor_tensor(out=lt[:, i, :], in0=DL[:, 2 * i, :], in1=DL[:, 2 * i + 1, :],
                                                       op=ALU.mult), reads=[DL], writes=[lt])
        S.op("dve", lambda e: e.reduce_sum(out=ls[:], in_=lt[:], axis=AX.X), reads=[lt], writes=[ls])
        S.op("act", lambda e: e.activation(out=ls[:], in_=ls[:], func=AF.Exp), reads=[ls], writes=[ls])
        S.op("dve", lambda e: e.tensor_tensor(out=NLAM[:], in0=ls[:, 1:2], in1=ls[:, 0:1], op=ALU.subtract),
             reads=[ls], writes=[NLAM])
        S.op("dve", lambda e: e.tensor_scalar_add(out=NLAM[:], in0=NLAM[:], scalar1=-lam_init), reads=[NLAM],
             writes=[NLAM])
        S.op("dve", lambda e: e.tensor_scalar_mul(out=GN[:], in0=prm["DG"][:], scalar1=1.0 - lam_init),
             reads=[prm["DG"]], writes=[GN])
        ko = g["KDO"].t.rearrange("(c p) n -> p c n", p=128)
        S.dma("sp", KD[:, :, 0:CTX], ko[:, :, 0:CTX], reads=[g["KDO"]], writes=[KD])
        for hf in range(2):
            kg = g["KDG"][hf].rearrange("(c p) n -> p c n", p=128)
            S.dma("sp", KD[:, :, CTX + hf * HALF:CTX + (hf + 1) * HALF], kg[:, :, CTX:NT], reads=[g["KDG"]],
                  writes=[KD])
            vg0 = g["VDG"][0][hf].rearrange("(b p) n -> p b n", p=128)
            vg1 = g["VDG"][1][hf].rearrange("(b p) n -> p b n", p=128)
            S.dma("sp", VDa[:, 2 + hf * 16:2 + hf * 16 + 7, :], vg0[:, 2:9, :], reads=[g["VDG"][0]], writes=[VDa])
            S.dma("sp", VDa[:, 2 + hf * 16 + 7:2 + (hf + 1) * 16, :], vg1[:, 0:9, :], reads=[g["VDG"][1]],
                  writes=[VDa])
        vo = g["VDO"].t.rearrange("(b p) n -> p b n", p=128)
        S.dma("sp", VDa[:, 0:2, :], vo[:, 0:2, :], reads=[g["VDO"]], writes=[VDa])
        accs = Rot(P[0:2])
        scs = Rot(P[2:7])
        pn = P[7]
        qsrc = g["QD"].t.rearrange("(c p) n -> p c n", p=128)
        SC = 32 ** -0.5
        for t in [-1, 0, 1, 2, 3]:
            N = CTX if t < 0 else 512
            c0 = 0 if t < 0 else CTX + t * 512
            nkb = 2 if t < 0 else 34
            Q = qb.next()
            S.dma("sp", Q[:, :, 0:N], qsrc[:, :, c0:c0 + N], reads=[g["QD"]], writes=[Q])
            Qm = [qm[0].next(), qm[1].next()]
            for m in range(2):
                S.op("dve", lambda e: e.tensor_scalar(out=Qm[m][:, :, 0:N], in0=Q[:, :, 0:N],
                                                      scalar1=prm["PM"][:, m:m + 1], scalar2=None, op0=ALU.mult),
                     reads=[Q, prm["PM"]], writes=[Qm[m]])
            for h in range(4):
                c = h // 2
                ac = [accs.next(), accs.next()]
                for m in range(2):
                    pb = (h % 2) * 64
                    for kb in range(nkb):
                        sps = scs.next()
                        S.op("pe", lambda e: e.matmul(sps[:, 0:N], lhsT=KD[pb:pb + 64, c, kb * 128:(kb + 1) * 128],
                                                      rhs=Qm[m][pb:pb + 64, c, 0:N], start=True, stop=True),
                             reads=[KD, Qm[m]], writes=[sps])
                        E = Eb.next()
                        S.op("act", lambda e: e.activation(out=E[:, 0:N], in_=sps[:, 0:N], func=AF.Exp, scale=SC),
                             reads=[sps], writes=[E])
                        S.op("pe", lambda e: e.matmul(ac[m][:, 0:N], lhsT=VDa[:, kb, h * 128:(h + 1) * 128],
                                                      rhs=E[:, 0:N], start=(kb == 0), stop=(kb == nkb - 1)),
                             reads=[VDa, E], writes=[ac[m]])
                ts = []
                for m in range(2):
                    r_, t_ = tr.next(), tt.next()
                    S.op("dve", lambda e: e.reciprocal(out=r_[0:64, 0:N], in_=ac[m][64:128, 0:N]), reads=[ac[m]],
                         writes=[r_])
                    S.op("dve", lambda e: e.tensor_tensor(out=t_[0:64, 0:N], in0=ac[m][0:64, 0:N], in1=r_[0:64, 0:N],
                                                          op=ALU.mult), reads=[ac[m], r_], writes=[t_])
                    ts.append(t_)
                O, sq = tO.next(), tq.next()
                S.op("dve", lambda e: e.scalar_tensor_tensor(out=O[0:64, 0:N], in0=ts[1][0:64, 0:N],
                                                             scalar=NLAM[0:64, 0:1], in1=ts[0][0:64, 0:N],
                                                             op0=ALU.mult, op1=ALU.add), reads=ts + [NLAM],
                     writes=[O])
                S.op("act", lambda e: e.activation(out=sq[0:64, 0:N], in_=O[0:64, 0:N], func=AF.Square), reads=[O],
                     writes=[sq])
                S.op("pe", lambda e: e.matmul(pn[0:64, 0:N], lhsT=C["onesf"][0:64, 0:64], rhs=sq[0:64, 0:N],
                                              start=True, stop=True), reads=[C["onesf"], sq], writes=[pn])
                S.op("act", lambda e: e.activation(out=sq[0:64, 0:N], in_=pn[0:64, 0:N], func=AF.Sqrt,
                                                   scale=1.0 / 64.0, bias=C["eps"][0:64, :]),
                     reads=[pn, C["eps"]], writes=[sq])
                S.op("dve", lambda e: e.reciprocal(out=sq[0:64, 0:N], in_=sq[0:64, 0:N]), reads=[sq], writes=[sq])
                S.op("dve", lambda e: e.tensor_tensor(out=O[0:64, 0:N], in0=O[0:64, 0:N], in1=sq[0:64, 0:N],
                                                      op=ALU.mult), reads=[O, sq], writes=[O])
                ob = (h % 2) * 64
                S.op("dve", lambda e: e.tensor_scalar(out=mixT[ob:ob + 64, 6 + c, c0:c0 + N], in0=O[0:64, 0:N],
                                                      scalar1=GN[0:64, 0:1], scalar2=None, op0=ALU.mult),
                     reads=[O, GN], writes=[mixT])


def emit_ln(k, hT, C, c0, N, LNG, LNB, which, es, pool):
    S = k.S
    ybf, ysq = pool["ybf"].next(), pool["ysq"].next()
    S1, S2 = pool["ps"].next(), pool["ps"].next()
    for c in range(8):
        S.op("act", lambda e, c=c: e.activation(out=ybf[:, c, 0:N], in_=hT[:, c, c0:c0 + N], func=AF.Identity),
             reads=[hT], writes=[ybf])
        S.op("act", lambda e, c=c: e.activation(out=ysq[:, c, 0:N], in_=hT[:, c, c0:c0 + N], func=AF.Square),
             reads=[hT], writes=[ysq])
    for c in range(8):
        S.op("pe", lambda e, c=c: e.matmul(S1[:, 0:N], lhsT=C["onesb"][:, :], rhs=ybf[:, c, 0:N], start=(c == 0),
                                           stop=(c == 7)), reads=[C["onesb"], ybf], writes=[S1])
    for c in range(8):
        S.op("pe", lambda e, c=c: e.matmul(S2[:, 0:N], lhsT=C["onesb"][:, :], rhs=ysq[:, c, 0:N], start=(c == 0),
                                           stop=(c == 7)), reads=[C["onesb"], ysq], writes=[S2])
    mean, rstd = pool["f"].next(), pool["f"].next()
    S.op("act", lambda e: e.activation(out=mean[:, 0:N], in_=S1[:, 0:N], func=AF.Identity, scale=1.0 / D_MODEL),
         reads=[S1], writes=[mean])
    S.op("dve", lambda e: e.tensor_tensor(out=rstd[:, 0:N], in0=mean[:, 0:N], in1=mean[:, 0:N], op=ALU.mult),
         reads=[mean], writes=[rstd])
    S.op("dve", lambda e: e.scalar_tensor_tensor(out=rstd[:, 0:N], in0=S2[:, 0:N], scalar=1.0 / D_MODEL,
                                                 in1=rstd[:, 0:N], op0=ALU.mult, op1=ALU.subtract),
         reads=[S2, rstd], writes=[rstd])
    S.op("act", lambda e: e.activation(out=rstd[:, 0:N], in_=rstd[:, 0:N], func=AF.Sqrt, bias=C["eps"][:, :]),
         reads=[rstd, C["eps"]], writes=[rstd])
    S.op("dve", lambda e: e.reciprocal(out=rstd[:, 0:N], in_=rstd[:, 0:N]), reads=[rstd], writes=[rstd])
    for c in range(8):
        t1 = pool["t"].next()
        S.op("dve", lambda e, c=c: e.tensor_tensor(out=t1[:, 0:N], in0=hT[:, c, c0:c0 + N], in1=mean[:, 0:N],
                                                   op=ALU.subtract), reads=[hT, mean], writes=[t1])
        S.op("pool", lambda e: e.tensor_tensor(out=t1[:, 0:N], in0=t1[:, 0:N], in1=rstd[:, 0:N], op=ALU.mult),
             reads=[t1, rstd], writes=[t1])
        S.op("act", lambda e, c=c: e.activation(out=hT[:, c, c0:c0 + N], in_=t1[:, 0:N], func=AF.Identity,
                                                scale=LNG[:, which, c:c + 1], bias=LNB[:, which, c:c + 1]),
             reads=[t1, LNG, LNB], writes=[hT])


def ln_pool(k, es, P, W=512):
    return dict(ybf=Rot([k.sb([128, 8, W], BF16, es, "ybf")]), ysq=Rot([k.sb([128, 8, W], BF16, es, "ysq")]),
                f=Rot([k.sb([128, W], F32, es, "lnf") for _ in range(2)]),
                t=Rot([k.sb([128, W], F32, es, "lnt") for _ in range(3)]), ps=Rot(P))


def emit_phaseC(k, hT, mixT, MODX, C, wout_d, prm):
    S = k.S
    P = k.banks()
    with k.scope() as es:
        WO = k.sb([128, 8, D_MODEL], BF16, es, "wout")
        S.dma("pool", WO[:], wout_d.t.rearrange("(k p) n -> p k n", p=128), reads=[wout_d], writes=[WO])
        lp = ln_pool(k, es, P[6:8])
        tm = Rot([k.sb([128, 512], F32, es, "ctm") for _ in range(3)])
        pb = Rot(P[0:6])
        tiles = [(0, 256)] + [(CTX + i * 512, 512) for i in range(4)]
        for (c0, N) in tiles:
            j = 1 if c0 < CTX else 0
            for c in range(8):
                ps = pb.next()
                for kk in range(8):
                    S.op("pe", lambda e, kk=kk: e.matmul(ps[:, 0:N], lhsT=WO[:, kk, c * 128:(c + 1) * 128],
                                                         rhs=mixT[:, kk, c0:c0 + N], start=(kk == 0), stop=(kk == 7)),
                         reads=[WO, mixT], writes=[ps])
                t = tm.next()
                S.op("dve", lambda e: e.tensor_scalar(out=t[:, 0:N], in0=ps[:, 0:N], scalar1=MODX[:, 2, c, j:j + 1],
                                                      scalar2=None, op0=ALU.mult), reads=[ps, MODX], writes=[t])
                S.op("dve", lambda e: e.scalar_tensor_tensor(out=hT[:, c, c0:c0 + N], in0=hT[:, c, c0:c0 + N],
                                                             scalar=ALPHA, in1=t[:, 0:N], op0=ALU.mult, op1=ALU.add),
                     reads=[hT, t], writes=[hT])
            emit_ln(k, hT, C, c0, N, prm["LNG"], prm["LNB"], 0, es, lp)


def emit_ffn(k, hT, MODX, C, prm, w13_d, w2_d, nexp, rw_d=None):
    S = k.S
    P = k.banks()
    G = NT // 2
    TN = 384
    moe = nexp > 1
    with k.scope() as es:
        u2 = k.sb([128, 8, G], BF16, es, "u2")
        gT = k.sb([128, NJ, G], BF16, es, "gT")
        W13 = Rot([k.sb([128, 8, 256], BF16, es, "w13") for _ in range(3)])
        W2 = Rot([k.sb([128, NJ, 128], BF16, es, "w2") for _ in range(2)])
        sil = Rot([k.sb([128, TN], F32, es, "sil") for _ in range(2)])
        tmp = Rot([k.sb([128, TN], F32, es, "ftmp") for _ in range(2)])
        lp = ln_pool(k, es, P[6:8], TN)
        p13 = Rot(P[0:4])
        po = Rot(P[4:6])
        pm = Rot(P[6:8])
        if moe:
            RW = k.sb([128, 8, NEXP], BF16, es, "rw")
            S.dma("pool", RW[:], rw_d.t.rearrange("(k p) e -> p k e", p=128), reads=[rw_d], writes=[RW])
            CWt = k.sb([128, G // 128, NEXP], F32, es, "cw")
            cwT = Rot([k.sb([128, G], F32, es, "cwT") for _ in range(1)])
            rt = {n: k.sb([128, NEXP], F32, es, "r" + n) for n in ["lg", "eq", "l2", "sel", "ex"]}
            rs = {n: k.sb([128, 1], F32, es, "s" + n) for n in ["m1", "m2", "nm1", "sum"]}
            dg = Rot([k.sb([128, 128], F32, es, "dg") for _ in range(2)])

        jobs = []
        for grp in range(2):
            for e_ in range(nexp):
                for jj in range(NJ):
                    jobs.append(("w13", e_, jj))
                for c in range(8):
                    jobs.append(("w2", e_, c))
        loaded = {}
        state = {"next": 0}

        def prefetch(upto):
            while state["next"] < min(upto, len(jobs)):
                i = state["next"]
                kind, e_, x = jobs[i]
                if kind == "w13":
                    w = W13.next()
                    S.dma("pool", w[:], w13_d[e_, x], reads=[w13_d], writes=[w])
                else:
                    w = W2.next()
                    S.dma("pool", w[:], w2_d[e_, x], reads=[w2_d], writes=[w])
                loaded[i] = w
                state["next"] += 1

        ji = 0
        for grp in range(2):
            g0 = grp * G
            for (lo, hi, j) in segs(g0, G):
                for kk in range(8):
                    S.op("act", lambda e, kk=kk: e.activation(
                        out=u2[:, kk, lo - g0:hi - g0], in_=hT[:, kk, lo:hi], func=AF.Identity,
                        scale=MODX[:, 7, kk, j:j + 1], bias=MODX[:, 3, kk, j:j + 1]), reads=[hT, MODX], writes=[u2])
            S.op("pool", lambda e: e.tensor_scalar(out=hT[:, :, g0:g0 + G], in0=hT[:, :, g0:g0 + G], scalar1=ALPHA,
                                                   scalar2=None, op0=ALU.mult), reads=[hT], writes=[hT])
            if moe:
                for blk in range(G // 128):
                    ps = pm.next()
                    for kk in range(8):
                        S.op("pe", lambda e, kk=kk: e.matmul(ps[:, 0:NEXP], lhsT=u2[:, kk, blk * 128:(blk + 1) * 128],
                                                             rhs=RW[:, kk, :], start=(kk == 0), stop=(kk == 7)),
                             reads=[u2, RW], writes=[ps])
                    lg, eq, l2, sel, ex = rt["lg"], rt["eq"], rt["l2"], rt["sel"], rt["ex"]
                    m1, m2, nm1, sm = rs["m1"], rs["m2"], rs["nm1"], rs["sum"]
                    S.op("dve", lambda e: e.tensor_tensor(out=lg[:], in0=ps[:, 0:NEXP], in1=prm["RB"][:], op=ALU.add),
                         reads=[ps, prm["RB"]], writes=[lg])
                    S.op("dve", lambda e: e.reduce_max(out=m1[:], in_=lg[:], axis=AX.X), reads=[lg], writes=[m1])
                    S.op("dve", lambda e: e.tensor_scalar(out=eq[:], in0=lg[:], scalar1=m1[:, 0:1], scalar2=None,
                                                          op0=ALU.is_equal), reads=[lg, m1], writes=[eq])
                    S.op("dve", lambda e: e.scalar_tensor_tensor(out=l2[:], in0=eq[:], scalar=-1e30, in1=lg[:],
                                                                 op0=ALU.mult, op1=ALU.add), reads=[eq, lg],
                         writes=[l2])
                    S.op("dve", lambda e: e.reduce_max(out=m2[:], in_=l2[:], axis=AX.X), reads=[l2], writes=[m2])
                    S.op("dve", lambda e: e.tensor_scalar(out=sel[:], in0=lg[:], scalar1=m2[:, 0:1], scalar2=None,
                                                          op0=ALU.is_ge), reads=[lg, m2], writes=[sel])
                    S.op("dve", lambda e: e.tensor_scalar_mul(out=nm1[:], in0=m1[:], scalar1=-1.0), reads=[m1],
                         writes=[nm1])
                    S.op("act", lambda e: e.activation(out=ex[:], in_=lg[:], func=AF.Exp, bias=nm1[:, 0:1]),
                         reads=[lg, nm1], writes=[ex])
                    S.op("dve", lambda e: e.tensor_tensor(out=ex[:], in0=ex[:], in1=sel[:], op=ALU.mult),
                         reads=[ex, sel], writes=[ex])
                    S.op("dve", lambda e: e.reduce_sum(out=sm[:], in_=ex[:], axis=AX.X), reads=[ex], writes=[sm])
                    S.op("dve", lambda e: e.reciprocal(out=sm[:], in_=sm[:]), reads=[sm], writes=[sm])
                    S.op("dve", lambda e: e.tensor_scalar(out=CWt[:, blk, :], in0=ex[:], scalar1=sm[:, 0:1],
                                                          scalar2=None, op0=ALU.mult), reads=[ex, sm], writes=[CWt])
            for e_ in range(nexp):
                if moe:
                    cw = cwT.next()
                    for b0 in range(0, G // 128, 4):
                        nb = min(4, G // 128 - b0)
                        ps = pm.next()
                        for bb in range(nb):
                            blk = b0 + bb
                            d_ = dg.next()
                            S.op("dve", lambda e: e.tensor_scalar(out=d_[:], in0=C["ident"][:],
                                                                  scalar1=CWt[:, blk, e_:e_ + 1], scalar2=None,
                                                                  op0=ALU.mult), reads=[C["ident"], CWt], writes=[d_])
                            S.op("pe", lambda e: e.matmul(ps[:, bb * 128:(bb + 1) * 128], lhsT=C["onesf"][:, :],
                                                          rhs=d_[:], start=True, stop=True),
                                 reads=[C["onesf"], d_], writes=[ps])
                        S.op("act", lambda e: e.activation(out=cw[:, b0 * 128:(b0 + nb) * 128], in_=ps[:, 0:nb * 128],
                                                           func=AF.Identity), reads=[ps], writes=[cw])
                for jj in range(NJ):
                    prefetch(ji + 3)
                    w = loaded.pop(ji)
                    ji += 1
                    for t in range(G // TN):
                        p1, p3 = p13.next(), p13.next()
                        for (pp, wo) in [(p1, 0), (p3, 128)]:
                            for kk in range(8):
                                S.op("pe", lambda e, kk=kk: e.matmul(pp[:, 0:TN], lhsT=w[:, kk, wo:wo + 128],
                                                                     rhs=u2[:, kk, t * TN:(t + 1) * TN],
                                                                     start=(kk == 0), stop=(kk == 7)),
                                     reads=[w, u2], writes=[pp])
                        s_ = sil.next()
                        S.op("act", lambda e: e.activation(out=s_[:], in_=p1[:, 0:TN], func=AF.Silu), reads=[p1],
                             writes=[s_])
                        S.op("dve", lambda e: e.tensor_tensor(out=gT[:, jj, t * TN:(t + 1) * TN], in0=s_[:],
                                                              in1=p3[:, 0:TN], op=ALU.mult), reads=[s_, p3],
                             writes=[gT])
                for c in range(8):
                    prefetch(ji + 2)
                    w = loaded.pop(ji)
                    ji += 1
                    for t in range(G // TN):
                        pso = po.next()
                        for jj in range(NJ):
                            S.op("pe", lambda e, jj=jj: e.matmul(pso[:, 0:TN], lhsT=w[:, jj, :],
                                                                 rhs=gT[:, jj, t * TN:(t + 1) * TN], start=(jj == 0),
                                                                 stop=(jj == NJ - 1)), reads=[w, gT], writes=[pso])
                        for (lo, hi, j) in segs(g0 + t * TN, TN):
                            a, b = lo - g0 - t * TN, hi - g0 - t * TN
                            if moe:
                                tp = tmp.next()
                                S.op("dve", lambda e: e.tensor_tensor(out=tp[:, a:b], in0=pso[:, a:b],
                                                                      in1=cw[:, lo - g0:hi - g0], op=ALU.mult),
                                     reads=[pso, cw], writes=[tp])
                                S.op("dve", lambda e: e.scalar_tensor_tensor(
                                    out=hT[:, c, lo:hi], in0=tp[:, a:b], scalar=MODX[:, 5, c, j:j + 1],
                                    in1=hT[:, c, lo:hi], op0=ALU.mult, op1=ALU.add), reads=[tp, MODX, hT],
                                    writes=[hT])
                            else:
                                S.op("dve", lambda e: e.scalar_tensor_tensor(
                                    out=hT[:, c, lo:hi], in0=pso[:, a:b], scalar=MODX[:, 5, c, j:j + 1],
                                    in1=hT[:, c, lo:hi], op0=ALU.mult, op1=ALU.add), reads=[pso, MODX, hT],
                                    writes=[hT])
            for t in range(G // TN):
                emit_ln(k, hT, C, g0 + t * TN, TN, prm["LNG"], prm["LNB"], 1, es, lp)


SMALL = dict(CW=[128, 2, 4], CB=[128, 2], GB=[128, 2, 2, 2], LAM=[128, 2, 2], GW=[128, 2, 2, 2, 128], SEL=[128, 2],
             SINK=[128, 8], DL=[128, 4, 32], DG=[128, 1], PM=[128, 2], LNG=[128, 2, 8], LNB=[128, 2, 8], RB=[128, 8])
VDR = NT // 2
A_OUT = dict(XL0=([128, NT], F32), XL1=([128, NT], F32), GT=([256, NT], F32), QS=([512, NT], BF16),
             KS=([256, NT], BF16), VS=([NT, 256], BF16), QD=([256, NT], BF16), KD=([256, NT], BF16),
             VD0=([VDR, 512], BF16), VD1=([VDR, 512], BF16))


def to_fm(x2d):
    return np.ascontiguousarray(x2d.T.reshape(8, 128, -1).transpose(1, 0, 2))


def from_fm(h):
    return np.ascontiguousarray(h.transpose(1, 0, 2).reshape(D_MODEL, -1).T)


def win_perm():
    lx, lg, sq, sk, sv, dq, dk, dv = 0, 256, 512, 1024, 1152, 1280, 1536, 1792
    cols = []
    cols += list(range(lx, lx + 256))
    cols += list(range(lg, lg + 256))
    cols += list(range(sq, sq + 512))
    sw64 = lambda base, h: [base + h * 64 + ((d + 32) % 64) for d in range(64)]
    for h in range(8):
        cols += sw64(sq, h)
    for hk in range(2):
        cols += list(range(sk + hk * 64, sk + hk * 64 + 64)) * 2
    for hk in range(2):
        cols += sw64(sk, hk) * 2
    sw32 = lambda base: [base + b * 32 + ((d + 16) % 32) for b in range(8) for d in range(32)]
    cols += list(range(dq, dq + 256))
    cols += sw32(dq)
    cols += list(range(dk, dk + 256))
    cols += sw32(dk)
    cols += list(range(sv, sv + 128))
    cols += list(range(dv, dv + 256))
    assert len(cols) == NW
    return np.array(cols)


def rope_tables(half):
    t = np.arange(HALF, dtype=np.float32) + np.float32(half * HALF)
    row = np.floor(t / 64).astype(np.float32)
    col = (t - row * 64).astype(np.float32)
    out = np.zeros((128, 4, HALF), np.float32)
    for (ti, hd) in [(0, 64), (2, 32)]:
        nf = hd // 4
        inv = (np.float32(10000.0) ** (-np.arange(nf, dtype=np.float32) / np.float32(nf))).astype(np.float32)
        ang = np.concatenate([row[:, None] * inv, col[:, None] * inv], -1).astype(np.float32)
        cs, sn = np.cos(ang).astype(np.float32), np.sin(ang).astype(np.float32)
        for p in range(128):
            d = p % hd
            jx = d % (hd // 2)
            out[p, ti] = cs[:, jx]
            out[p, ti + 1] = -sn[:, jx] if d < hd // 2 else sn[:, jx]
    return out


def swa_masks(half):
    m = np.zeros((128, 8, 512), np.float32)
    kk = np.arange(128)[:, None]
    q = np.arange(512)[None, :]
    for r in range(6):
        m[:, r] = (np.abs((r - 1) * 128 + kk - q) <= 128)
    m[:, 6] = m[:, 0] if half == 1 else 0.0
    m[:, 7] = m[:, 5] if half == 0 else 0.0
    return m.astype(NPBF)


def rep(v):
    return np.ascontiguousarray(np.broadcast_to(np.asarray(v, np.float32).reshape(1, -1), (128, np.size(v))))


def small_params(inp, layer, half):
    f = lambda a: np.ascontiguousarray(np.asarray(a, np.float32))
    p = {}
    p["CW"] = f(inp["lru_conv_w"][layer].reshape(4, 2, 128).transpose(2, 1, 0))
    p["CB"] = f(inp["lru_conv_b"][layer].reshape(2, 128).T)
    p["GB"] = f(inp["lru_gate_b"][layer].reshape(2, 2, 2, 128).transpose(3, 2, 0, 1))
    p["LAM"] = f(inp["lru_lam"][layer].reshape(2, 2, 128).transpose(2, 1, 0))
    gw = np.zeros((128, 2, 2, 2, 128), np.float32)
    w = inp["lru_gate_w"][layer]
    for c in range(2):
        for bb in range(2):
            blk = c * 2 + bb
            gw[bb * 64:(bb + 1) * 64, c, :, :, bb * 64:(bb + 1) * 64] = w[:, :, blk].transpose(2, 0, 1, 3)
    p["GW"] = gw
    sel = np.zeros((128, 2), np.float32)
    sel[:, half] = 1.0
    p["SEL"] = sel
    p["SINK"] = rep(inp["swa_sink"][layer])
    p["DL"] = rep(inp["diff_lam"][layer].reshape(-1)).reshape(128, 4, 32)
    p["DG"] = f(np.tile(inp["diff_norm_g"][layer], 2).reshape(128, 1))
    pm = np.zeros((128, 2), np.float32)
    pm[:, 0] = (np.arange(128) % 64) < 32
    pm[:, 1] = (np.arange(128) % 64) >= 32
    p["PM"] = pm
    p["LNG"] = f(inp["ln_g"][layer].reshape(2, 8, 128).transpose(2, 0, 1))
    p["LNB"] = f(inp["ln_b"][layer].reshape(2, 8, 128).transpose(2, 0, 1))
    if layer % 2 == 1:
        p["RB"] = rep(inp["moe_router_b"][layer // 2])
    else:
        p["RB"] = np.zeros((128, 8), np.float32)
    return p


def ffn_layout(w1, w3, w2):
    E = w1.shape[0]
    a = w1.reshape(E, 8, 128, NJ, 128).transpose(0, 3, 2, 1, 4)
    b = w3.reshape(E, 8, 128, NJ, 128).transpose(0, 3, 2, 1, 4)
    w13 = np.ascontiguousarray(np.concatenate([a, b], axis=-1))
    w2r = np.ascontiguousarray(w2.reshape(E, NJ, 128, 8, 128).transpose(0, 3, 2, 1, 4))
    return w13, w2r


_PROGS = {}


def _prog(key, fn):
    if key not in _PROGS:
        _PROGS[key] = fn()
    return _PROGS[key]


PAIRS = [[0, 1], [2, 3], [4, 5], [6, 7]]
PUB = ["XL0", "XL1", "KS", "VS", "KD", "VD0", "VD1"]


def build_fused(depth=DEPTH):
    k = K()
    S = k.S
    nc = k.nc
    I = lambda n, s, dt: k.dram(n, s, dt, "ExternalInput")
    hT_d = I("hT", [128, 8, NT], F32)
    cc_d = I("cc", [128, 8, 2], F32)
    cst_d = I("ident", [128, 128], F32)
    rope_d = I("rope", [128, 4, HALF], F32)
    msk_d = I("MSK", [128, 8, 512], BF16)
    L = []
    for l in range(depth):
        moe = l % 2 == 1
        nexp = NEXP if moe else 1
        d = dict(adaw=I(f"adaw{l}", [D_MODEL, 6 * D_MODEL], F32), adab=I(f"adab{l}", [128, 48], F32),
                 win=I(f"win{l}", [D_MODEL, NW], F32), wout=I(f"wout{l}", [D_MODEL, D_MODEL], F32),
                 w13=I(f"w13_{l}", [nexp, NJ, 128, 8, 256], F32), w2=I(f"w2_{l}", [nexp, 8, 128, NJ, 128], F32),
                 rw=I(f"rw{l}", [D_MODEL, NEXP], F32) if moe else None,
                 sm={n: I(f"{n}{l}", s_, F32) for n, s_ in SMALL.items()})
        L.append(d)
    out_d = k.dram("hout", [128, 8, NT], F32, "ExternalOutput")
    scr = []
    for par in range(2):
        o = {n: Tile(nc.dram_tensor(f"{n}_{par}", list(sh), dt).ap()) for n, (sh, dt) in A_OUT.items()}
        gth = {n: Tile(nc.dram_tensor(f"{n}G_{par}", [2 * A_OUT[n][0][0], A_OUT[n][0][1]], A_OUT[n][1]).ap())
               for n in PUB}
        scr.append((o, gth))
    with k.es:
        hT = k.sb([128, 8, NT], F32, name="hT")
        MODX = k.sb([128, 8, 8, 2], F32, name="modx")
        S.dma("sp", hT[:], hT_d[:, :, :], reads=[hT_d], writes=[hT])
        C = emit_consts(k, cst_d)
        prm = {n: k.sb(s_, F32, name=n) for n, s_ in SMALL.items()}
        for l in range(depth):
            moe = l % 2 == 1
            lam_init = 0.8 - 0.6 * math.exp(-0.3 * l)
            o, gth = scr[l % 2]
            emit_mods(k, cc_d, L[l]["adaw"], L[l]["adab"], MODX)
            emit_phaseA(k, hT, MODX, L[l]["win"], rope_d, o)
            for n in PUB:
                S.coll("AllGather", PAIRS, o[n], gth[n])
            for n in SMALL:
                S.dma("sp", prm[n][:], L[l]["sm"][n].t, reads=[L[l]["sm"][n]], writes=[prm[n]])

            def G(n):
                t = Tile(gth[n].t.rearrange("(h r) n -> h r n", h=2))
                t.b = gth[n].b
                return t
            g = dict(XG=[G("XL0"), G("XL1")], GT=o["GT"], QS=o["QS"], KSO=o["KS"], KSG=G("KS"), VSO=o["VS"],
                     VSG=G("VS"), QD=o["QD"], KDO=o["KD"], KDG=G("KD"), VDO=o["VD0"], VDG=[G("VD0"), G("VD1")],
                     MSK=msk_d)
            with k.scope() as es:
                mixT = k.sb([128, 8, NT], BF16, es, "mixT")
                emit_lru(k, mixT, C, g, prm)
                emit_swa(k, mixT, C, g, prm)
                emit_diff(k, mixT, C, g, prm, lam_init)
                emit_phaseC(k, hT, mixT, MODX, C, L[l]["wout"], prm)
            emit_ffn(k, hT, MODX, C, prm, L[l]["w13"], L[l]["w2"], NEXP if moe else 1, L[l]["rw"])
        S.dma("sp", out_d[:, :, :], hT[:], reads=[hT], writes=[out_d])
        S.finish()
    return k.nc


def fused_inputs(inp, depth=DEPTH):
    x, c, ctx, c_ctx = inp["x"], inp["c"], inp["ctx"], inp["c_ctx"]
    cores = [(b, hf) for b in range(BATCH) for hf in range(2)]
    perm = win_perm()
    ropes = [rope_tables(hf) for hf in range(2)]
    masks = [swa_masks(hf) for hf in range(2)]
    shared = dict(ident=np.eye(128, dtype=np.float32))
    for l in range(depth):
        j = l // 2
        shared[f"adaw{l}"] = np.ascontiguousarray(inp["ada_w"][l])
        shared[f"adab{l}"] = np.ascontiguousarray(inp["ada_b"][l].reshape(48, 128).T)
        shared[f"win{l}"] = np.ascontiguousarray(inp["w_in"][l][:, perm])
        shared[f"wout{l}"] = np.ascontiguousarray(inp["w_out"][l])
        if l % 2 == 0:
            w13, w2r = ffn_layout(inp["ffn_w1"][j][None], inp["ffn_w3"][j][None], inp["ffn_w2"][j][None])
        else:
            w13, w2r = ffn_layout(inp["moe_w1"][j], inp["moe_w3"][j], inp["moe_w2"][j])
            shared[f"rw{l}"] = np.ascontiguousarray(inp["moe_router_w"][j])
        shared[f"w13_{l}"] = w13
        shared[f"w2_{l}"] = w2r
    in_maps = []
    for (b, hf) in cores:
        m = dict(shared)
        toks = np.concatenate([ctx[b], x[b, hf * HALF:(hf + 1) * HALF]], 0)
        m["hT"] = to_fm(toks)
        m["cc"] = np.ascontiguousarray(np.stack([c[b].reshape(8, 128).T, c_ctx.reshape(8, 128).T], -1))
        m["rope"] = ropes[hf]
        m["MSK"] = masks[hf]
        for l in range(depth):
            for n, v in small_params(inp, l, hf).items():
                m[f"{n}{l}"] = v
        in_maps.append(m)
    return cores, in_maps


def kernel(**inp):
    inp = {k_: np.asarray(v) for k_, v in inp.items()}
    cores, in_maps = fused_inputs(inp)
    res = run_bass_kernel_spmd(_prog("fused", build_fused), in_maps, core_ids=list(range(NCORES))).results
    out = np.zeros((BATCH, SEQ, D_MODEL), np.float32)
    for ci, (b, hf) in enumerate(cores):
        out[b, hf * HALF:(hf + 1) * HALF] = from_fm(np.asarray(res[ci]["hout"]))[CTX:]
    return out
```

```python
import math
from contextlib import ExitStack

import numpy as np
import ml_dtypes

import concourse.bass as bass
import concourse.mybir as mybir
from concourse.bass_utils import run_bass_kernel_spmd

F32 = mybir.dt.float32
BF16 = mybir.dt.bfloat16
AF = mybir.ActivationFunctionType
ALU = mybir.AluOpType
AX = mybir.AxisListType
NPBF = ml_dtypes.bfloat16

D_MODEL = 1024
BATCH = 4
SEQ = 4096
DEPTH = 4
CTX = 256
HALF = SEQ // 2
NT = CTX + HALF
D_FF = 2816
NJ = D_FF // 128
NEXP = 8
LN_EPS = 1e-5
ALPHA = (2.0 * DEPTH) ** 0.25
NW = 24 * 128 + 384
NCORES = 8


class Buf:
    __slots__ = ("w", "r", "excl")

    def __init__(self, excl=False):
        self.w = None
        self.r = {}
        self.excl = excl


class Tile:
    def __init__(self, t, excl=False):
        self.t = t
        self.b = Buf(excl)

    def __getitem__(self, idx):
        return self.t[idx]


class Sched:
    EPOCH = 16000
    NDMA = 28

    def __init__(self, nc, es):
        self.nc, self.es = nc, es
        self.eng = dict(pe=nc.tensor, act=nc.scalar, dve=nc.vector, pool=nc.gpsimd, sp=nc.sync)
        self.cnt = {}
        self.sem = {}
        self.own = {e: set() for e in self.eng}
        self.nsem = 0
        self.waited = {e: {} for e in self.eng}
        self.dsem = []
        self.dcnt = []
        self.dn = 0
        self.allsems = []
        self.csem = None
        self.ccnt = 0

    def _newsem(self, name):
        self.nsem += 1
        s = self.es.enter_context(self.nc.semaphore(f"{name}_{self.nsem}"))
        self.allsems.append(s)
        return s

    def _wait(self, e, deps):
        w = self.waited[e]
        for sem, val in deps:
            if w.get(sem, 0) < val:
                self.eng[e].wait_ge(sem, val)
                w[sem] = val

    def _deps(self, e, reads, writes):
        deps = []
        own = self.own[e]
        for b in reads:
            if b.w is not None:
                deps.append(b.w)
            if b.excl:
                deps.extend(b.r.items())
        for b in writes:
            if b.w is not None:
                deps.append(b.w)
            deps.extend(x for x in b.r.items() if x[0] not in own)
        if e == "pe":
            deps = [d for d in deps if d[0] not in own]
        return deps

    def _mark(self, tok, reads, writes):
        for b in reads:
            if b.excl:
                b.w = tok
                b.r = {}
            else:
                b.r[tok[0]] = tok[1]
        for b in writes:
            b.w = tok
            b.r = {}

    def op(self, e, fn, reads=(), writes=()):
        reads = [x.b if isinstance(x, Tile) else x for x in reads]
        writes = [x.b if isinstance(x, Tile) else x for x in writes]
        self._wait(e, self._deps(e, reads, writes))
        ins = fn(self.eng[e])
        if self.cnt.get(e, self.EPOCH) >= self.EPOCH:
            self.sem[e] = self._newsem(e)
            self.cnt[e] = 0
            self.own[e].add(self.sem[e])
        self.cnt[e] += 1
        ins.then_inc(self.sem[e], 1)
        tok = (self.sem[e], self.cnt[e])
        self._mark(tok, reads, writes)
        return tok

    def dma(self, q, out, in_, reads=(), writes=()):
        reads = [x.b if isinstance(x, Tile) else x for x in reads]
        writes = [x.b if isinstance(x, Tile) else x for x in writes]
        self._wait(q, self._deps(q, reads, writes))
        i = self.dn % self.NDMA
        self.dn += 1
        if i >= len(self.dsem):
            self.dsem.append(self._newsem("d"))
            self.dcnt.append(0)
        sem = self.dsem[i]
        if self.dcnt[i] > 0:
            self._wait(q, [(sem, self.dcnt[i])])
        self.dcnt[i] += 16
        self.eng[q].dma_start(out=out, in_=in_).then_inc(sem, 16)
        tok = (sem, self.dcnt[i])
        self._mark(tok, reads, writes)
        return tok

    def coll(self, kind, groups, src, dst):
        q = "pool"
        reads, writes = [src.b], [dst.b]
        self._wait(q, self._deps(q, reads, writes))
        if self.csem is None:
            self.csem = self._newsem("cc")
            self.ccnt = 0
        self.ccnt += 1
        self.nc.gpsimd.collective_compute(kind, ALU.bypass, replica_groups=groups, ins=[src.t.opt()],
                                          outs=[dst.t.opt()]).then_inc(self.csem, 1)
        tok = (self.csem, self.ccnt)
        self._mark(tok, reads, writes)
        return tok

    def barrier(self):
        deps = [(s, c) for s, c in zip(self.dsem, self.dcnt) if c > 0]
        deps += [(self.sem[e], self.cnt[e]) for e in self.sem]
        if self.csem is not None:
            deps.append((self.csem, self.ccnt))
        for e in self.eng:
            self._wait(e, deps)

    def finish(self):
        deps = [(s, c) for s, c in zip(self.dsem, self.dcnt) if c > 0]
        deps += [(self.sem[e], self.cnt[e]) for e in self.sem]
        if self.csem is not None:
            deps.append((self.csem, self.ccnt))
        self._wait("sp", deps)


class K:
    def __init__(self):
        self.nc = bass.Bass("TRN2", target_bir_lowering=False)
        self.es = ExitStack()
        self.S = Sched(self.nc, self.es)
        self.n = 0
        self.psum = None

    def dram(self, name, shape, dt, kind):
        t = self.nc.dram_tensor(name, list(shape), dt, kind=kind).ap()
        return Tile(t)

    def sb(self, shape, dt, es=None, name=None):
        self.n += 1
        t = (es or self.es).enter_context(self.nc.sbuf_tensor(f"{name or 't'}_{self.n}", list(shape), dt))
        return Tile(t)

    def scope(self):
        k = self

        class _Scope(ExitStack):
            def __exit__(self, *a):
                k.S.barrier()
                return super().__exit__(*a)
        return _Scope()

    def banks(self):
        if self.psum is None:
            self.psum = []
            for i in range(8):
                t = self.es.enter_context(self.nc.psum_tensor(f"ps{i}", [128, 512], F32))
                self.psum.append(Tile(t, excl=True))
        return self.psum


class Rot:
    def __init__(self, items):
        self.items = items
        self.i = 0

    def next(self):
        x = self.items[self.i % len(self.items)]
        self.i += 1
        return x


def segs(c0, n):
    out = []
    if c0 < CTX:
        hi = min(CTX, c0 + n)
        out.append((c0, hi, 1))
        if c0 + n > CTX:
            out.append((CTX, c0 + n, 0))
    else:
        out.append((c0, c0 + n, 0))
    return out


def emit_consts(k, cst_d):
    S = k.S
    c = {}
    c["ident"] = k.sb([128, 128], F32, name="ident")
    S.dma("sp", c["ident"][:], cst_d[:, :], reads=[cst_d], writes=[c["ident"]])
    c["onesf"] = k.sb([128, 128], F32, name="onesf")
    S.op("dve", lambda e: e.memset(c["onesf"][:], 1.0), writes=[c["onesf"]])
    c["onesb"] = k.sb([128, 128], BF16, name="onesb")
    S.op("dve", lambda e: e.memset(c["onesb"][:], 1.0), writes=[c["onesb"]])
    c["eps"] = k.sb([128, 1], F32, name="eps")
    S.op("dve", lambda e: e.memset(c["eps"][:], LN_EPS), writes=[c["eps"]])
    return c


def emit_mods(k, cc_d, adaw_d, adab_d, MODX):
    S = k.S
    P = k.banks()
    with k.scope() as es:
        cc = k.sb([128, 8, 2], F32, es)
        sl = k.sb([128, 8, 2], F32, es)
        adab = k.sb([128, 48], F32, es)
        wts = Rot([k.sb([128, 8, 512], F32, es) for _ in range(2)])
        S.dma("sp", cc[:], cc_d[:, :, :], reads=[cc_d], writes=[cc])
        S.dma("sp", adab[:], adab_d[:, :], reads=[adab_d], writes=[adab])
        S.op("act", lambda e: e.activation(out=sl[:], in_=cc[:], func=AF.Silu), reads=[cc], writes=[sl])
        ps = P[0]
        wsrc = adaw_d.t.rearrange("(k p) n -> p k n", p=128)
        for piece in range(12):
            wt = wts.next()
            S.dma("sp", wt[:], wsrc[:, :, piece * 512:(piece + 1) * 512], reads=[adaw_d], writes=[wt])
            for m in range(4):
                ma = piece * 4 + m
                for kk in range(8):
                    S.op("pe", lambda e, wt=wt, m=m, kk=kk, ma=ma: e.matmul(
                        ps[:, ma * 2:ma * 2 + 2], lhsT=wt[:, kk, m * 128:(m + 1) * 128], rhs=sl[:, kk, :],
                        start=(kk == 0), stop=(kk == 7)), reads=[wt, sl], writes=[ps])
        ps3 = ps[:, 0:96].rearrange("p (m j) -> p m j", j=2)
        mx = MODX[:, 0:6, :, :].rearrange("p w c j -> p (w c) j")
        for j in range(2):
            S.op("dve", lambda e, j=j: e.tensor_tensor(out=mx[:, :, j], in0=ps3[:, :, j], in1=adab[:, :], op=ALU.add),
                 reads=[ps, adab], writes=[MODX])
        S.op("dve", lambda e: e.tensor_scalar_add(out=MODX[:, 6, :, :], in0=MODX[:, 1, :, :], scalar1=1.0),
             reads=[MODX], writes=[MODX])
        S.op("dve", lambda e: e.tensor_scalar_add(out=MODX[:, 7, :, :], in0=MODX[:, 4, :, :], scalar1=1.0),
             reads=[MODX], writes=[MODX])


def emit_phaseA(k, hT, MODX, win_d, rope_d, o):
    S = k.S
    P = k.banks()
    with k.scope() as es:
        WIN = k.sb([128, 8, NW], BF16, es, "win")
        wsrc = win_d.t.rearrange("(k p) n -> p k n", p=128)
        for pc in range(4):
            lo, hi = pc * (NW // 4), (pc + 1) * (NW // 4)
            S.dma("pool", WIN[:, :, lo:hi], wsrc[:, :, lo:hi], reads=[win_d], writes=[WIN])
        ub = Rot([k.sb([128, 8, 512], BF16, es, "u") for _ in range(2)])
        rt = Rot([k.sb([128, 4, 512], F32, es, "rope") for _ in range(2)])
        stf = Rot([k.sb([128, 512], F32, es, "stf") for _ in range(3)])
        stb = Rot([k.sb([128, 512], BF16, es, "stb") for _ in range(3)])
        tm1 = Rot([k.sb([128, 512], F32, es, "tm1") for _ in range(2)])
        tm2 = Rot([k.sb([128, 512], F32, es, "tm2") for _ in range(2)])
        vss = Rot([k.sb([128, 2, 128], BF16, es, "vss") for _ in range(2)])
        vds = Rot([k.sb([128, 4, 128], BF16, es, "vds") for _ in range(2)])
        for v in vss.items + vds.items:
            S.op("dve", lambda e, v=v: e.memset(v[:], 1.0), writes=[v])
        pb = Rot(P)

        tiles = [(0, 256, 1)] + [(CTX + i * 512, 512, 0) for i in range(4)]
        for (c0, N, j) in tiles:
            lat = j == 0
            u = ub.next()
            for kk in range(8):
                S.op("act", lambda e, kk=kk: e.activation(
                    out=u[:, kk, 0:N], in_=hT[:, kk, c0:c0 + N], func=AF.Identity,
                    scale=MODX[:, 6, kk, j:j + 1], bias=MODX[:, 0, kk, j:j + 1]), reads=[hT, MODX], writes=[u])
            if lat:
                R = rt.next()
                S.dma("sp", R[:], rope_d[:, :, c0 - CTX:c0 - CTX + 512], reads=[rope_d], writes=[R])

            def proj(ch, ps):
                for kk in range(8):
                    S.op("pe", lambda e, kk=kk: e.matmul(
                        ps[:, 0:N], lhsT=WIN[:, kk, ch * 128:(ch + 1) * 128], rhs=u[:, kk, 0:N],
                        start=(kk == 0), stop=(kk == 7)), reads=[WIN, u], writes=[ps])

            for c in range(2):
                ps = pb.next()
                proj(c, ps)
                st = stf.next()
                S.op("act", lambda e: e.activation(out=st[:, 0:N], in_=ps[:, 0:N], func=AF.Identity),
                     reads=[ps], writes=[st])
                S.dma("sp", o["XL%d" % c][:, c0:c0 + N], st[:, 0:N], reads=[st], writes=[o["XL%d" % c]])
            for c in range(2):
                ps = pb.next()
                proj(2 + c, ps)
                st = stf.next()
                S.op("act", lambda e: e.activation(out=st[:, 0:N], in_=ps[:, 0:N], func=AF.Gelu_apprx_tanh),
                     reads=[ps], writes=[st])
                S.dma("sp", o["GT"][c * 128:(c + 1) * 128, c0:c0 + N], st[:, 0:N], reads=[st], writes=[o["GT"]])
            for (nb, sb_, n, tc, dst) in [(4, 8, 4, 0, "QS"), (12, 14, 2, 0, "KS"), (16, 18, 2, 2, "QD"),
                                          (20, 22, 2, 2, "KD")]:
                for i in range(n):
                    psA = pb.next()
                    proj(nb + i, psA)
                    st = stb.next()
                    if lat:
                        psB = pb.next()
                        proj(sb_ + i, psB)
                        t1, t2 = tm1.next(), tm2.next()
                        S.op("dve", lambda e: e.tensor_tensor(out=t1[:], in0=psA[:, :], in1=R[:, tc, :], op=ALU.mult),
                             reads=[psA, R], writes=[t1])
                        S.op("dve", lambda e: e.tensor_tensor(out=t2[:], in0=psB[:, :], in1=R[:, tc + 1, :],
                                                              op=ALU.mult), reads=[psB, R], writes=[t2])
                        S.op("pool", lambda e: e.tensor_tensor(out=st[:], in0=t1[:], in1=t2[:], op=ALU.add),
                             reads=[t1, t2], writes=[st])
                    else:
                        S.op("act", lambda e: e.activation(out=st[:, 0:N], in_=psA[:, 0:N], func=AF.Identity),
                             reads=[psA], writes=[st])
                    S.dma("sp", o[dst][i * 128:(i + 1) * 128, c0:c0 + N], st[:, 0:N], reads=[st], writes=[o[dst]])
            for blk in range(N // 128):
                ps = pb.next()
                for kk in range(8):
                    S.op("pe", lambda e, kk=kk: e.matmul(
                        ps[:, 0:384], lhsT=u[:, kk, blk * 128:(blk + 1) * 128], rhs=WIN[:, kk, 3072:3456],
                        start=(kk == 0), stop=(kk == 7)), reads=[WIN, u], writes=[ps])
                vs, vd = vss.next(), vds.next()
                S.op("act", lambda e: e.activation(out=vs[:, :, 0:64],
                                                   in_=ps[:, 0:128].rearrange("p (h d) -> p h d", d=64),
                                                   func=AF.Identity), reads=[ps], writes=[vs])
                S.op("dve", lambda e: e.tensor_copy(out=vd[:, :, 0:64],
                                                    in_=ps[:, 128:384].rearrange("p (h d) -> p h d", d=64)),
                     reads=[ps], writes=[vd])
                r0 = c0 + blk * 128
                S.dma("sp", o["VS"][r0:r0 + 128, :], vs[:].rearrange("p h d -> p (h d)"), reads=[vs],
                      writes=[o["VS"]])
                vch, vr = r0 // VDR, r0 % VDR
                S.dma("sp", o["VD%d" % vch][vr:vr + 128, :], vd[:].rearrange("p h d -> p (h d)"), reads=[vd],
                      writes=[o["VD%d" % vch]])


def emit_lru(k, mixT, C, g, prm):
    S = k.S
    P = k.banks()
    with k.scope() as es:
        xs = k.sb([128, 1 + SEQ + 2], F32, es, "xs")
        xc = k.sb([128, 1 + CTX + 2], F32, es, "xc")
        U = k.sb([128, CTX + SEQ], F32, es, "U")
        OUT = k.sb([128, NT], F32, es, "lout")
        GTs = k.sb([128, NT], F32, es, "gts")
        tR = Rot([k.sb([128, 512], F32, es, "tR") for _ in range(2)])
        tI = Rot([k.sb([128, 512], F32, es, "tI") for _ in range(2)])
        tA = Rot([k.sb([128, 512], F32, es, "tA") for _ in range(2)])
        tH = Rot([k.sb([128, 512], F32, es, "tH") for _ in range(3)])
        sp = k.sb([128, 4], F32, es, "sp")
        nsp8 = k.sb([128, 2, 2], F32, es, "nsp8")
        nsp16 = k.sb([128, 2, 2], F32, es, "nsp16")
        lam = prm["LAM"]
        spv = sp[:, 0:4]
        lamv = lam[:].rearrange("p c d -> p (c d)")
        S.op("act", lambda e: e.activation(out=spv, in_=lamv, func=AF.Exp, scale=-1.0), reads=[lam], writes=[sp])
        S.op("dve", lambda e: e.tensor_scalar_add(out=spv, in0=spv, scalar1=1.0), reads=[sp], writes=[sp])
        S.op("act", lambda e: e.activation(out=spv, in_=spv, func=AF.Ln), reads=[sp], writes=[sp])
        S.op("dve", lambda e: e.tensor_scalar_mul(out=nsp8[:].rearrange("p c d -> p (c d)"), in0=spv, scalar1=-8.0),
             reads=[sp], writes=[nsp8])
        S.op("dve", lambda e: e.tensor_scalar_mul(out=nsp16[:].rearrange("p c d -> p (c d)"), in0=spv, scalar1=-16.0),
             reads=[sp], writes=[nsp16])
        pg = Rot(P[0:4])
        CW, CB, GB, GW, SEL = prm["CW"], prm["CB"], prm["GB"], prm["GW"], prm["SEL"]
        for c in range(2):
            rows = slice(c * 128, (c + 1) * 128)
            S.op("dve", lambda e: e.memset(xs[:, 0:1], 0.0), writes=[xs])
            S.op("dve", lambda e: e.memset(xs[:, 1 + SEQ:3 + SEQ], 0.0), writes=[xs])
            S.op("dve", lambda e: e.memset(xc[:, 0:1], 0.0), writes=[xc])
            S.op("dve", lambda e: e.memset(xc[:, 1 + CTX:3 + CTX], 0.0), writes=[xc])
            XGc = g["XG"][c]
            S.dma("sp", xs[:, 1:1 + HALF], XGc[0, :, CTX:NT], reads=[XGc], writes=[xs])
            S.dma("sp", xs[:, 1 + HALF:1 + SEQ], XGc[1, :, CTX:NT], reads=[XGc], writes=[xs])
            S.dma("sp", xc[:, 1:1 + CTX], XGc[0, :, 0:CTX], reads=[XGc], writes=[xc])
            S.dma("sp", GTs[:], g["GT"][rows, :], reads=[g["GT"]], writes=[GTs])
            for (src, L, d0) in [(xc, CTX, 0), (xs, SEQ, CTX)]:
                S.op("act", lambda e: e.activation(out=U[:, d0:d0 + L], in_=src[:, 1:1 + L], func=AF.Identity,
                                                   scale=CW[:, c, 1:2], bias=CB[:, c:c + 1]),
                     reads=[src, CW, CB], writes=[U])
                for (off, wi) in [(0, 0), (2, 2), (3, 3)]:
                    S.op("dve", lambda e, off=off, wi=wi: e.scalar_tensor_tensor(
                        out=U[:, d0:d0 + L], in0=src[:, off:off + L], scalar=CW[:, c, wi:wi + 1], in1=U[:, d0:d0 + L],
                        op0=ALU.mult, op1=ALU.add), reads=[src, CW, U], writes=[U])
            S.op("dve", lambda e: e.memset(OUT[:], 0.0), writes=[OUT])
            for d in range(2):
                lat = [(CTX + i * 512, 512, i) for i in range(8)]
                if d == 1:
                    lat = lat[::-1]
                state = None
                hprev = None
                for (u0, L, kind) in [(0, CTX, -1)] + lat:
                    pss = []
                    for gi in range(2):
                        ps = pg.next()
                        S.op("pe", lambda e, gi=gi, ps=ps: e.matmul(ps[:, 0:L], lhsT=GW[:, c, d, gi, :],
                                                                    rhs=U[:, u0:u0 + L], start=True, stop=True),
                             reads=[GW, U], writes=[ps])
                        pss.append(ps)
                    r, ii, a, h = tR.next(), tI.next(), tA.next(), tH.next()
                    S.op("act", lambda e: e.activation(out=r[:, 0:L], in_=pss[0][:, 0:L], func=AF.Sigmoid,
                                                       bias=GB[:, c, d, 0:1]), reads=[pss[0], GB], writes=[r])
                    S.op("act", lambda e: e.activation(out=ii[:, 0:L], in_=pss[1][:, 0:L], func=AF.Sigmoid,
                                                       bias=GB[:, c, d, 1:2]), reads=[pss[1], GB], writes=[ii])
                    S.op("act", lambda e: e.activation(out=a[:, 0:L], in_=r[:, 0:L], func=AF.Exp,
                                                       scale=nsp8[:, c, d:d + 1]), reads=[r, nsp8], writes=[a])
                    S.op("act", lambda e: e.activation(out=r[:, 0:L], in_=r[:, 0:L], func=AF.Exp,
                                                       scale=nsp16[:, c, d:d + 1]), reads=[r, nsp16], writes=[r])
                    S.op("act", lambda e: e.activation(out=r[:, 0:L], in_=r[:, 0:L], func=AF.Sqrt, scale=-1.0,
                                                       bias=1.0), reads=[r], writes=[r])
                    S.op("dve", lambda e: e.tensor_tensor(out=ii[:, 0:L], in0=ii[:, 0:L], in1=U[:, u0:u0 + L],
                                                          op=ALU.mult), reads=[ii, U], writes=[ii])
                    S.op("dve", lambda e: e.tensor_tensor(out=ii[:, 0:L], in0=ii[:, 0:L], in1=r[:, 0:L],
                                                          op=ALU.mult), reads=[ii, r], writes=[ii])
                    if d == 0:
                        vo, va, vb = h[:, 0:L], a[:, 0:L], ii[:, 0:L]
                    else:
                        vo, va, vb = h[:, L - 1::-1], a[:, L - 1::-1], ii[:, L - 1::-1]
                    init = 0.0 if state is None else state
                    rd = [a, ii] + ([hprev] if hprev is not None else [])
                    S.op("dve", lambda e: e.tensor_tensor_scan(out=vo, data0=va, data1=vb, initial=init,
                                                               op0=ALU.mult, op1=ALU.add), reads=rd, writes=[h])
                    state = h[:, L - 1:L] if d == 0 else h[:, 0:1]
                    hprev = h
                    if kind < 0:
                        S.op("dve", lambda e: e.tensor_tensor(out=OUT[:, 0:CTX], in0=h[:, 0:L], in1=OUT[:, 0:CTX],
                                                              op=ALU.add), reads=[h, OUT], writes=[OUT])
                    else:
                        hf = kind // 4
                        lo = CTX + (kind % 4) * 512
                        S.op("dve", lambda e: e.scalar_tensor_tensor(
                            out=OUT[:, lo:lo + L], in0=h[:, 0:L], scalar=SEL[:, hf:hf + 1], in1=OUT[:, lo:lo + L],
                            op0=ALU.mult, op1=ALU.add), reads=[h, OUT, SEL], writes=[OUT])
            S.op("pool", lambda e: e.tensor_tensor(out=mixT[:, c, :], in0=OUT[:], in1=GTs[:], op=ALU.mult),
                 reads=[OUT, GTs], writes=[mixT])


def pipeline(n, front, back, la, deferred):
    for i in range(n + la):
        if i < n:
            front(i)
        if i >= la:
            back(i - la)
        while deferred and deferred[0][0] <= i:
            deferred.pop(0)[1]()
    while deferred:
        deferred.pop(0)[1]()


def emit_swa(k, mixT, C, g, prm):
    S = k.S
    P = k.banks()
    LA = 3
    with k.scope() as es:
        KS = [k.sb([128, CTX + 128 + HALF + 128], BF16, es, "ks") for _ in range(2)]
        VSa = k.sb([128, 20, 256], BF16, es, "vsa")
        MSK = k.sb([128, 8, 512], BF16, es, "msk")
        ESK = k.sb([128, 8], F32, es, "esk")
        qb = Rot([k.sb([128, 4, 512], BF16, es, "qs") for _ in range(2)])
        Eb = Rot([k.sb([128, 512], BF16, es, "E") for _ in range(6)])
        den = Rot([k.sb([128, 512], F32, es, "den") for _ in range(2)])
        S.dma("sp", MSK[:], g["MSK"][:, :, :], reads=[g["MSK"]], writes=[MSK])
        S.op("act", lambda e: e.activation(out=ESK[:], in_=prm["SINK"][:], func=AF.Exp), reads=[prm["SINK"]],
             writes=[ESK])
        for hk in range(2):
            rows = slice(hk * 128, (hk + 1) * 128)
            S.dma("sp", KS[hk][:, 0:CTX], g["KSO"][rows, 0:CTX], reads=[g["KSO"]], writes=[KS[hk]])
            S.dma("sp", KS[hk][:, CTX + 128:CTX + 128 + HALF], g["KSO"][rows, CTX:NT], reads=[g["KSO"]],
                  writes=[KS[hk]])
            S.dma("sp", KS[hk][:, CTX:CTX + 128], g["KSG"][0, rows, NT - 128:NT], reads=[g["KSG"]], writes=[KS[hk]])
            S.dma("sp", KS[hk][:, CTX + 128 + HALF:], g["KSG"][1, rows, CTX:CTX + 128], reads=[g["KSG"]],
                  writes=[KS[hk]])
        vo = g["VSO"].t.rearrange("(b p) n -> p b n", p=128)
        S.dma("sp", VSa[:, 0:2, :], vo[:, 0:2, :], reads=[g["VSO"]], writes=[VSa])
        S.dma("sp", VSa[:, 3:19, :], vo[:, 2:18, :], reads=[g["VSO"]], writes=[VSa])
        S.dma("sp", VSa[:, 2, :], g["VSG"][0, NT - 128:NT, :], reads=[g["VSG"]], writes=[VSa])
        S.dma("sp", VSa[:, 19, :], g["VSG"][1, CTX:CTX + 128, :], reads=[g["VSG"]], writes=[VSa])
        accs = Rot(P[0:2])
        scs = Rot(P[2:8])
        qsrc = g["QS"].t.rearrange("(c p) n -> p c n", p=128)
        tiles = [-1, 0, 1, 2, 3]
        Qt = {}

        def load_q(t):
            N = CTX if t < 0 else 512
            c0 = 0 if t < 0 else CTX + t * 512
            Q = qb.next()
            S.dma("sp", Q[:, :, 0:N], qsrc[:, :, c0:c0 + N], reads=[g["QS"]], writes=[Q])
            Qt[t] = Q

        units = []
        for ti, t in enumerate(tiles):
            N = CTX if t < 0 else 512
            c0 = 0 if t < 0 else CTX + t * 512
            blocks = [(0, 0, None), (1, 128, None)]
            if t >= 0:
                for r in range(6):
                    mk = r
                    if t == 0 and r == 0:
                        mk = 6
                    if t == 3 and r == 5:
                        mk = 7
                    blocks.append((2 + 4 * t + r, CTX + (4 * t + r) * 128, mk))
            for h in range(8):
                for bi, blk in enumerate(blocks):
                    units.append((ti, t, N, c0, h, bi, len(blocks), blk))
        Ef = {}
        cur = {}
        load_q(tiles[0])

        def front(i):
            ti, t, N, c0, h, bi, nb, (vb, kcol, mk) = units[i]
            if h == 0 and bi == 0 and ti + 1 < len(tiles):
                load_q(tiles[ti + 1])
            Q = Qt[t]
            qc, pb, hk = h // 2, (h % 2) * 64, h // 4
            sps, E = scs.next(), Eb.next()
            S.op("pe", lambda e: e.matmul(sps[:, 0:N], lhsT=KS[hk][pb:pb + 64, kcol:kcol + 128],
                                          rhs=Q[pb:pb + 64, qc, 0:N], start=True, stop=True),
                 reads=[KS[hk], Q], writes=[sps])
            S.op("act", lambda e: e.activation(out=E[:, 0:N], in_=sps[:, 0:N], func=AF.Exp, scale=0.125),
                 reads=[sps], writes=[E])
            if mk is not None:
                S.op("pool", lambda e: e.tensor_tensor(out=E[:, 0:N], in0=E[:, 0:N], in1=MSK[:, mk, 0:N],
                                                       op=ALU.mult), reads=[E, MSK], writes=[E])
            Ef[i] = E

        def back(i):
            ti, t, N, c0, h, bi, nb, (vb, kcol, mk) = units[i]
            qc, pb, hk = h // 2, (h % 2) * 64, h // 4
            E = Ef.pop(i)
            if bi == 0:
                cur["acc"] = accs.next()
            acc = cur["acc"]
            S.op("pe", lambda e: e.matmul(acc[:, 0:N], lhsT=VSa[:, vb, hk * 128:(hk + 1) * 128],
                                          rhs=E[:, 0:N], start=(bi == 0), stop=(bi == nb - 1)),
                 reads=[VSa, E], writes=[acc])
            if bi == nb - 1:
                dn = den.next()
                S.op("dve", lambda e: e.tensor_scalar(out=dn[0:64, 0:N], in0=acc[64:128, 0:N],
                                                      scalar1=ESK[64:128, h:h + 1], scalar2=None, op0=ALU.add),
                     reads=[acc, ESK], writes=[dn])
                S.op("dve", lambda e: e.reciprocal(out=dn[0:64, 0:N], in_=dn[0:64, 0:N]), reads=[dn], writes=[dn])
                S.op("dve", lambda e: e.tensor_tensor(out=mixT[pb:pb + 64, 2 + qc, c0:c0 + N], in0=acc[0:64, 0:N],
                                                      in1=dn[0:64, 0:N], op=ALU.mult), reads=[acc, dn],
                     writes=[mixT])

        pipeline(len(units), front, back, LA, [])


def emit_diff(k, mixT, C, g, prm, lam_init):
    S = k.S
    P = k.banks()
    LA = 3
    with k.scope() as es:
        KD = k.sb([128, 2, CTX + SEQ], BF16, es, "kd")
        VDa = k.sb([128, 34, 512], BF16, es, "vda")
        qb = Rot([k.sb([128, 2, 512], BF16, es, "qd") for _ in range(2)])
        qm = [Rot([k.sb([128, 2, 512], BF16, es, "qm") for _ in range(2)]) for _ in range(2)]
        Eb = Rot([k.sb([128, 512], BF16, es, "E") for _ in range(6)])
        tr = Rot([k.sb([128, 512], F32, es, "tr") for _ in range(2)])
        tt = Rot([k.sb([128, 512], F32, es, "tt") for _ in range(2)])
        tO = Rot([k.sb([128, 512], F32, es, "tO") for _ in range(2)])
        tq = Rot([k.sb([128, 512], F32, es, "tq") for _ in range(2)])
        lt = k.sb([128, 2, 32], F32, es, "lt")
        ls = k.sb([128, 2], F32, es, "ls")
        NLAM = k.sb([128, 1], F32, es, "nlam")
        GN = k.sb([128, 1], F32, es, "gn")
        DL = prm["DL"]
        for i in range(2):
            S.op("dve", lambda e, i=i: e.tensor_tensor(out=lt[:, i, :], in0=DL[:, 2 * i, :], in1=DL[:, 2 * i + 1, :],
                                                       op=ALU.mult), reads=[DL], writes=[lt])
        S.op("dve", lambda e: e.reduce_sum(out=ls[:], in_=lt[:], axis=AX.X), reads=[lt], writes=[ls])
        S.op("act", lambda e: e.activation(out=ls[:], in_=ls[:], func=AF.Exp), reads=[ls], writes=[ls])
        S.op("dve", lambda e: e.tensor_tensor(out=NLAM[:], in0=ls[:, 1:2], in1=ls[:, 0:1], op=ALU.subtract),
             reads=[ls], writes=[NLAM])
        S.op("dve", lambda e: e.tensor_scalar_add(out=NLAM[:], in0=NLAM[:], scalar1=-lam_init), reads=[NLAM],
             writes=[NLAM])
        S.op("dve", lambda e: e.tensor_scalar_mul(out=GN[:], in0=prm["DG"][:], scalar1=1.0 - lam_init),
             reads=[prm["DG"]], writes=[GN])
        ko = g["KDO"].t.rearrange("(c p) n -> p c n", p=128)
        S.dma("sp", KD[:, :, 0:CTX], ko[:, :, 0:CTX], reads=[g["KDO"]], writes=[KD])
        for hf in range(2):
            kg = g["KDG"][hf].rearrange("(c p) n -> p c n", p=128)
            S.dma("sp", KD[:, :, CTX + hf * HALF:CTX + (hf + 1) * HALF], kg[:, :, CTX:NT], reads=[g["KDG"]],
                  writes=[KD])
            vg0 = g["VDG"][0][hf].rearrange("(b p) n -> p b n", p=128)
            vg1 = g["VDG"][1][hf].rearrange("(b p) n -> p b n", p=128)
            S.dma("sp", VDa[:, 2 + hf * 16:2 + hf * 16 + 7, :], vg0[:, 2:9, :], reads=[g["VDG"][0]], writes=[VDa])
            S.dma("sp", VDa[:, 2 + hf * 16 + 7:2 + (hf + 1) * 16, :], vg1[:, 0:9, :], reads=[g["VDG"][1]],
                  writes=[VDa])
        vo = g["VDO"].t.rearrange("(b p) n -> p b n", p=128)
        S.dma("sp", VDa[:, 0:2, :], vo[:, 0:2, :], reads=[g["VDO"]], writes=[VDa])
        accs = Rot(P[0:2])
        scs = Rot(P[2:7])
        pn = P[7]
        qsrc = g["QD"].t.rearrange("(c p) n -> p c n", p=128)
        SC = 32 ** -0.5
        tiles = [-1, 0, 1, 2, 3]
        Qt = {}

        def load_q(t):
            N = CTX if t < 0 else 512
            c0 = 0 if t < 0 else CTX + t * 512
            Q = qb.next()
            S.dma("sp", Q[:, :, 0:N], qsrc[:, :, c0:c0 + N], reads=[g["QD"]], writes=[Q])
            Qm = [qm[0].next(), qm[1].next()]
            for m in range(2):
                S.op("dve", lambda e: e.tensor_scalar(out=Qm[m][:, :, 0:N], in0=Q[:, :, 0:N],
                                                      scalar1=prm["PM"][:, m:m + 1], scalar2=None, op0=ALU.mult),
                     reads=[Q, prm["PM"]], writes=[Qm[m]])
            Qt[t] = Qm

        units = []
        for ti, t in enumerate(tiles):
            N = CTX if t < 0 else 512
            c0 = 0 if t < 0 else CTX + t * 512
            nkb = 2 if t < 0 else 34
            for h in range(4):
                for m in range(2):
                    for kb in range(nkb):
                        units.append((ti, t, N, c0, h, m, kb, nkb))
        Ef = {}
        cur = {}
        deferred = []
        load_q(tiles[0])

        def front(i):
            ti, t, N, c0, h, m, kb, nkb = units[i]
            if h == 0 and m == 0 and kb == 0 and ti + 1 < len(tiles):
                load_q(tiles[ti + 1])
            Qm = Qt[t]
            c, pb = h // 2, (h % 2) * 64
            sps, E = scs.next(), Eb.next()
            S.op("pe", lambda e: e.matmul(sps[:, 0:N], lhsT=KD[pb:pb + 64, c, kb * 128:(kb + 1) * 128],
                                          rhs=Qm[m][pb:pb + 64, c, 0:N], start=True, stop=True),
                 reads=[KD, Qm[m]], writes=[sps])
            S.op("act", lambda e: e.activation(out=E[:, 0:N], in_=sps[:, 0:N], func=AF.Exp, scale=SC),
                 reads=[sps], writes=[E])
            Ef[i] = E

        def back(i):
            ti, t, N, c0, h, m, kb, nkb = units[i]
            c = h // 2
            E = Ef.pop(i)
            if m == 0 and kb == 0:
                cur["ac"] = [accs.next(), accs.next()]
            ac = cur["ac"]
            S.op("pe", lambda e: e.matmul(ac[m][:, 0:N], lhsT=VDa[:, kb, h * 128:(h + 1) * 128],
                                          rhs=E[:, 0:N], start=(kb == 0), stop=(kb == nkb - 1)),
                 reads=[VDa, E], writes=[ac[m]])
            if not (m == 1 and kb == nkb - 1):
                return
            ts = []
            for mm in range(2):
                r_, t_ = tr.next(), tt.next()
                S.op("dve", lambda e: e.reciprocal(out=r_[0:64, 0:N], in_=ac[mm][64:128, 0:N]), reads=[ac[mm]],
                     writes=[r_])
                S.op("dve", lambda e: e.tensor_tensor(out=t_[0:64, 0:N], in0=ac[mm][0:64, 0:N], in1=r_[0:64, 0:N],
                                                      op=ALU.mult), reads=[ac[mm], r_], writes=[t_])
                ts.append(t_)
            O, sq = tO.next(), tq.next()
            S.op("dve", lambda e: e.scalar_tensor_tensor(out=O[0:64, 0:N], in0=ts[1][0:64, 0:N],
                                                         scalar=NLAM[0:64, 0:1], in1=ts[0][0:64, 0:N],
                                                         op0=ALU.mult, op1=ALU.add), reads=ts + [NLAM],
                 writes=[O])
            S.op("act", lambda e: e.activation(out=sq[0:64, 0:N], in_=O[0:64, 0:N], func=AF.Square), reads=[O],
                 writes=[sq])

            def tail():
                S.op("pe", lambda e: e.matmul(pn[0:64, 0:N], lhsT=C["onesf"][0:64, 0:64], rhs=sq[0:64, 0:N],
                                              start=True, stop=True), reads=[C["onesf"], sq], writes=[pn])
                S.op("act", lambda e: e.activation(out=sq[0:64, 0:N], in_=pn[0:64, 0:N], func=AF.Sqrt,
                                                   scale=1.0 / 64.0, bias=C["eps"][0:64, :]),
                     reads=[pn, C["eps"]], writes=[sq])
                S.op("dve", lambda e: e.reciprocal(out=sq[0:64, 0:N], in_=sq[0:64, 0:N]), reads=[sq], writes=[sq])
                S.op("dve", lambda e: e.tensor_tensor(out=O[0:64, 0:N], in0=O[0:64, 0:N], in1=sq[0:64, 0:N],
                                                      op=ALU.mult), reads=[O, sq], writes=[O])
                ob = (h % 2) * 64
                S.op("dve", lambda e: e.tensor_scalar(out=mixT[ob:ob + 64, 6 + c, c0:c0 + N], in0=O[0:64, 0:N],
                                                      scalar1=GN[0:64, 0:1], scalar2=None, op0=ALU.mult),
                     reads=[O, GN], writes=[mixT])
            deferred.append((i + LA + 6, tail))

        pipeline(len(units), front, back, LA, deferred)


def emit_ln(k, hT, C, c0, N, LNG, LNB, which, es, pool):
    S = k.S
    ybf, ysq = pool["ybf"].next(), pool["ysq"].next()
    S1, S2 = pool["ps"].next(), pool["ps"].next()
    for c in range(8):
        S.op("act", lambda e, c=c: e.activation(out=ybf[:, c, 0:N], in_=hT[:, c, c0:c0 + N], func=AF.Identity),
             reads=[hT], writes=[ybf])
        S.op("act", lambda e, c=c: e.activation(out=ysq[:, c, 0:N], in_=hT[:, c, c0:c0 + N], func=AF.Square),
             reads=[hT], writes=[ysq])
    for c in range(8):
        S.op("pe", lambda e, c=c: e.matmul(S1[:, 0:N], lhsT=C["onesb"][:, :], rhs=ybf[:, c, 0:N], start=(c == 0),
                                           stop=(c == 7)), reads=[C["onesb"], ybf], writes=[S1])
    for c in range(8):
        S.op("pe", lambda e, c=c: e.matmul(S2[:, 0:N], lhsT=C["onesb"][:, :], rhs=ysq[:, c, 0:N], start=(c == 0),
                                           stop=(c == 7)), reads=[C["onesb"], ysq], writes=[S2])
    mean, rstd = pool["f"].next(), pool["f"].next()
    S.op("act", lambda e: e.activation(out=mean[:, 0:N], in_=S1[:, 0:N], func=AF.Identity, scale=1.0 / D_MODEL),
         reads=[S1], writes=[mean])
    S.op("dve", lambda e: e.tensor_tensor(out=rstd[:, 0:N], in0=mean[:, 0:N], in1=mean[:, 0:N], op=ALU.mult),
         reads=[mean], writes=[rstd])
    S.op("dve", lambda e: e.scalar_tensor_tensor(out=rstd[:, 0:N], in0=S2[:, 0:N], scalar=1.0 / D_MODEL,
                                                 in1=rstd[:, 0:N], op0=ALU.mult, op1=ALU.subtract),
         reads=[S2, rstd], writes=[rstd])
    S.op("act", lambda e: e.activation(out=rstd[:, 0:N], in_=rstd[:, 0:N], func=AF.Sqrt, bias=C["eps"][:, :]),
         reads=[rstd, C["eps"]], writes=[rstd])
    S.op("dve", lambda e: e.reciprocal(out=rstd[:, 0:N], in_=rstd[:, 0:N]), reads=[rstd], writes=[rstd])
    for c in range(8):
        t1 = pool["t"].next()
        S.op("dve", lambda e, c=c: e.tensor_tensor(out=t1[:, 0:N], in0=hT[:, c, c0:c0 + N], in1=mean[:, 0:N],
                                                   op=ALU.subtract), reads=[hT, mean], writes=[t1])
        S.op("pool", lambda e: e.tensor_tensor(out=t1[:, 0:N], in0=t1[:, 0:N], in1=rstd[:, 0:N], op=ALU.mult),
             reads=[t1, rstd], writes=[t1])
        S.op("act", lambda e, c=c: e.activation(out=hT[:, c, c0:c0 + N], in_=t1[:, 0:N], func=AF.Identity,
                                                scale=LNG[:, which, c:c + 1], bias=LNB[:, which, c:c + 1]),
             reads=[t1, LNG, LNB], writes=[hT])


def ln_pool(k, es, P, W=512):
    return dict(ybf=Rot([k.sb([128, 8, W], BF16, es, "ybf")]), ysq=Rot([k.sb([128, 8, W], BF16, es, "ysq")]),
                f=Rot([k.sb([128, W], F32, es, "lnf") for _ in range(2)]),
                t=Rot([k.sb([128, W], F32, es, "lnt") for _ in range(3)]), ps=Rot(P))


def emit_phaseC(k, hT, mixT, MODX, C, wout_d, prm):
    S = k.S
    P = k.banks()
    with k.scope() as es:
        WO = k.sb([128, 8, D_MODEL], BF16, es, "wout")
        S.dma("pool", WO[:], wout_d.t.rearrange("(k p) n -> p k n", p=128), reads=[wout_d], writes=[WO])
        lp = ln_pool(k, es, P[6:8])
        tm = Rot([k.sb([128, 512], F32, es, "ctm") for _ in range(3)])
        pb = Rot(P[0:6])
        tiles = [(0, 256)] + [(CTX + i * 512, 512) for i in range(4)]
        for (c0, N) in tiles:
            j = 1 if c0 < CTX else 0
            for c in range(8):
                ps = pb.next()
                for kk in range(8):
                    S.op("pe", lambda e, kk=kk: e.matmul(ps[:, 0:N], lhsT=WO[:, kk, c * 128:(c + 1) * 128],
                                                         rhs=mixT[:, kk, c0:c0 + N], start=(kk == 0), stop=(kk == 7)),
                         reads=[WO, mixT], writes=[ps])
                t = tm.next()
                S.op("dve", lambda e: e.tensor_scalar(out=t[:, 0:N], in0=ps[:, 0:N], scalar1=MODX[:, 2, c, j:j + 1],
                                                      scalar2=None, op0=ALU.mult), reads=[ps, MODX], writes=[t])
                S.op("dve", lambda e: e.scalar_tensor_tensor(out=hT[:, c, c0:c0 + N], in0=hT[:, c, c0:c0 + N],
                                                             scalar=ALPHA, in1=t[:, 0:N], op0=ALU.mult, op1=ALU.add),
                     reads=[hT, t], writes=[hT])
            emit_ln(k, hT, C, c0, N, prm["LNG"], prm["LNB"], 0, es, lp)


def emit_ffn(k, hT, MODX, C, prm, w13_d, w2_d, nexp, rw_d=None):
    S = k.S
    P = k.banks()
    G = NT // 2
    TN = 384
    moe = nexp > 1
    with k.scope() as es:
        u2 = k.sb([128, 8, G], BF16, es, "u2")
        gT = k.sb([128, NJ, G], BF16, es, "gT")
        W13 = Rot([k.sb([128, 8, 256], BF16, es, "w13") for _ in range(3)])
        W2 = Rot([k.sb([128, NJ, 128], BF16, es, "w2") for _ in range(2)])
        sil = Rot([k.sb([128, TN], F32, es, "sil") for _ in range(2)])
        tmp = Rot([k.sb([128, TN], F32, es, "ftmp") for _ in range(2)])
        lp = ln_pool(k, es, P[6:8], TN)
        p13 = Rot(P[0:4])
        po = Rot(P[4:6])
        pm = Rot(P[6:8])
        if moe:
            RW = k.sb([128, 8, NEXP], BF16, es, "rw")
            S.dma("pool", RW[:], rw_d.t.rearrange("(k p) e -> p k e", p=128), reads=[rw_d], writes=[RW])
            CWt = k.sb([128, G // 128, NEXP], F32, es, "cw")
            cwT = Rot([k.sb([128, G], F32, es, "cwT") for _ in range(1)])
            rt = {n: k.sb([128, NEXP], F32, es, "r" + n) for n in ["lg", "eq", "l2", "sel", "ex"]}
            rs = {n: k.sb([128, 1], F32, es, "s" + n) for n in ["m1", "m2", "nm1", "sum"]}
            dg = Rot([k.sb([128, 128], F32, es, "dg") for _ in range(2)])

        jobs = []
        for grp in range(2):
            for e_ in range(nexp):
                for jj in range(NJ):
                    jobs.append(("w13", e_, jj))
                for c in range(8):
                    jobs.append(("w2", e_, c))
        loaded = {}
        state = {"next": 0}

        def prefetch(upto):
            while state["next"] < min(upto, len(jobs)):
                i = state["next"]
                kind, e_, x = jobs[i]
                if kind == "w13":
                    w = W13.next()
                    S.dma("pool", w[:], w13_d[e_, x], reads=[w13_d], writes=[w])
                else:
                    w = W2.next()
                    S.dma("pool", w[:], w2_d[e_, x], reads=[w2_d], writes=[w])
                loaded[i] = w
                state["next"] += 1

        ji = 0
        for grp in range(2):
            g0 = grp * G
            for (lo, hi, j) in segs(g0, G):
                for kk in range(8):
                    S.op("act", lambda e, kk=kk: e.activation(
                        out=u2[:, kk, lo - g0:hi - g0], in_=hT[:, kk, lo:hi], func=AF.Identity,
                        scale=MODX[:, 7, kk, j:j + 1], bias=MODX[:, 3, kk, j:j + 1]), reads=[hT, MODX], writes=[u2])
            S.op("dve", lambda e: e.tensor_scalar(out=hT[:, :, g0:g0 + G], in0=hT[:, :, g0:g0 + G], scalar1=ALPHA,
                                                  scalar2=None, op0=ALU.mult), reads=[hT], writes=[hT])
            if moe:
                for blk in range(G // 128):
                    ps = pm.next()
                    for kk in range(8):
                        S.op("pe", lambda e, kk=kk: e.matmul(ps[:, 0:NEXP], lhsT=u2[:, kk, blk * 128:(blk + 1) * 128],
                                                             rhs=RW[:, kk, :], start=(kk == 0), stop=(kk == 7)),
                             reads=[u2, RW], writes=[ps])
                    lg, eq, l2, sel, ex = rt["lg"], rt["eq"], rt["l2"], rt["sel"], rt["ex"]
                    m1, m2, nm1, sm = rs["m1"], rs["m2"], rs["nm1"], rs["sum"]
                    S.op("dve", lambda e: e.tensor_tensor(out=lg[:], in0=ps[:, 0:NEXP], in1=prm["RB"][:], op=ALU.add),
                         reads=[ps, prm["RB"]], writes=[lg])
                    S.op("dve", lambda e: e.reduce_max(out=m1[:], in_=lg[:], axis=AX.X), reads=[lg], writes=[m1])
                    S.op("dve", lambda e: e.tensor_scalar(out=eq[:], in0=lg[:], scalar1=m1[:, 0:1], scalar2=None,
                                                          op0=ALU.is_equal), reads=[lg, m1], writes=[eq])
                    S.op("dve", lambda e: e.scalar_tensor_tensor(out=l2[:], in0=eq[:], scalar=-1e30, in1=lg[:],
                                                                 op0=ALU.mult, op1=ALU.add), reads=[eq, lg],
                         writes=[l2])
                    S.op("dve", lambda e: e.reduce_max(out=m2[:], in_=l2[:], axis=AX.X), reads=[l2], writes=[m2])
                    S.op("dve", lambda e: e.tensor_scalar(out=sel[:], in0=lg[:], scalar1=m2[:, 0:1], scalar2=None,
                                                          op0=ALU.is_ge), reads=[lg, m2], writes=[sel])
                    S.op("dve", lambda e: e.tensor_scalar_mul(out=nm1[:], in0=m1[:], scalar1=-1.0), reads=[m1],
                         writes=[nm1])
                    S.op("act", lambda e: e.activation(out=ex[:], in_=lg[:], func=AF.Exp, bias=nm1[:, 0:1]),
                         reads=[lg, nm1], writes=[ex])
                    S.op("dve", lambda e: e.tensor_tensor(out=ex[:], in0=ex[:], in1=sel[:], op=ALU.mult),
                         reads=[ex, sel], writes=[ex])
                    S.op("dve", lambda e: e.reduce_sum(out=sm[:], in_=ex[:], axis=AX.X), reads=[ex], writes=[sm])
                    S.op("dve", lambda e: e.reciprocal(out=sm[:], in_=sm[:]), reads=[sm], writes=[sm])
                    S.op("dve", lambda e: e.tensor_scalar(out=CWt[:, blk, :], in0=ex[:], scalar1=sm[:, 0:1],
                                                          scalar2=None, op0=ALU.mult), reads=[ex, sm], writes=[CWt])
            for e_ in range(nexp):
                if moe:
                    cw = cwT.next()
                    for b0 in range(0, G // 128, 4):
                        nb = min(4, G // 128 - b0)
                        ps = pm.next()
                        for bb in range(nb):
                            blk = b0 + bb
                            d_ = dg.next()
                            S.op("dve", lambda e: e.tensor_scalar(out=d_[:], in0=C["ident"][:],
                                                                  scalar1=CWt[:, blk, e_:e_ + 1], scalar2=None,
                                                                  op0=ALU.mult), reads=[C["ident"], CWt], writes=[d_])
                            S.op("pe", lambda e: e.matmul(ps[:, bb * 128:(bb + 1) * 128], lhsT=C["onesf"][:, :],
                                                          rhs=d_[:], start=True, stop=True),
                                 reads=[C["onesf"], d_], writes=[ps])
                        S.op("act", lambda e: e.activation(out=cw[:, b0 * 128:(b0 + nb) * 128], in_=ps[:, 0:nb * 128],
                                                           func=AF.Identity), reads=[ps], writes=[cw])
                for jj in range(NJ):
                    prefetch(ji + 3)
                    w = loaded.pop(ji)
                    ji += 1
                    for t in range(G // TN):
                        p1, p3 = p13.next(), p13.next()
                        for (pp, wo) in [(p1, 0), (p3, 128)]:
                            for kk in range(8):
                                S.op("pe", lambda e, kk=kk: e.matmul(pp[:, 0:TN], lhsT=w[:, kk, wo:wo + 128],
                                                                     rhs=u2[:, kk, t * TN:(t + 1) * TN],
                                                                     start=(kk == 0), stop=(kk == 7)),
                                     reads=[w, u2], writes=[pp])
                        s_ = sil.next()
                        S.op("act", lambda e: e.activation(out=s_[:], in_=p1[:, 0:TN], func=AF.Silu), reads=[p1],
                             writes=[s_])
                        S.op("dve", lambda e: e.tensor_tensor(out=gT[:, jj, t * TN:(t + 1) * TN], in0=s_[:],
                                                              in1=p3[:, 0:TN], op=ALU.mult), reads=[s_, p3],
                             writes=[gT])
                for c in range(8):
                    prefetch(ji + 2)
                    w = loaded.pop(ji)
                    ji += 1
                    for t in range(G // TN):
                        pso = po.next()
                        for jj in range(NJ):
                            S.op("pe", lambda e, jj=jj: e.matmul(pso[:, 0:TN], lhsT=w[:, jj, :],
                                                                 rhs=gT[:, jj, t * TN:(t + 1) * TN], start=(jj == 0),
                                                                 stop=(jj == NJ - 1)), reads=[w, gT], writes=[pso])
                        for (lo, hi, j) in segs(g0 + t * TN, TN):
                            a, b = lo - g0 - t * TN, hi - g0 - t * TN
                            if moe:
                                tp = tmp.next()
                                S.op("dve", lambda e: e.tensor_tensor(out=tp[:, a:b], in0=pso[:, a:b],
                                                                      in1=cw[:, lo - g0:hi - g0], op=ALU.mult),
                                     reads=[pso, cw], writes=[tp])
                                S.op("dve", lambda e: e.scalar_tensor_tensor(
                                    out=hT[:, c, lo:hi], in0=tp[:, a:b], scalar=MODX[:, 5, c, j:j + 1],
                                    in1=hT[:, c, lo:hi], op0=ALU.mult, op1=ALU.add), reads=[tp, MODX, hT],
                                    writes=[hT])
                            else:
                                S.op("dve", lambda e: e.scalar_tensor_tensor(
                                    out=hT[:, c, lo:hi], in0=pso[:, a:b], scalar=MODX[:, 5, c, j:j + 1],
                                    in1=hT[:, c, lo:hi], op0=ALU.mult, op1=ALU.add), reads=[pso, MODX, hT],
                                    writes=[hT])
            for t in range(G // TN):
                emit_ln(k, hT, C, g0 + t * TN, TN, prm["LNG"], prm["LNB"], 1, es, lp)


SMALL = dict(CW=[128, 2, 4], CB=[128, 2], GB=[128, 2, 2, 2], LAM=[128, 2, 2], GW=[128, 2, 2, 2, 128], SEL=[128, 2],
             SINK=[128, 8], DL=[128, 4, 32], DG=[128, 1], PM=[128, 2], LNG=[128, 2, 8], LNB=[128, 2, 8], RB=[128, 8])
VDR = NT // 2
A_OUT = dict(XL0=([128, NT], F32), XL1=([128, NT], F32), GT=([256, NT], F32), QS=([512, NT], BF16),
             KS=([256, NT], BF16), VS=([NT, 256], BF16), QD=([256, NT], BF16), KD=([256, NT], BF16),
             VD0=([VDR, 512], BF16), VD1=([VDR, 512], BF16))


def to_fm(x2d):
    return np.ascontiguousarray(x2d.T.reshape(8, 128, -1).transpose(1, 0, 2))


def from_fm(h):
    return np.ascontiguousarray(h.transpose(1, 0, 2).reshape(D_MODEL, -1).T)


def win_perm():
    lx, lg, sq, sk, sv, dq, dk, dv = 0, 256, 512, 1024, 1152, 1280, 1536, 1792
    cols = []
    cols += list(range(lx, lx + 256))
    cols += list(range(lg, lg + 256))
    cols += list(range(sq, sq + 512))
    sw64 = lambda base, h: [base + h * 64 + ((d + 32) % 64) for d in range(64)]
    for h in range(8):
        cols += sw64(sq, h)
    for hk in range(2):
        cols += list(range(sk + hk * 64, sk + hk * 64 + 64)) * 2
    for hk in range(2):
        cols += sw64(sk, hk) * 2
    sw32 = lambda base: [base + b * 32 + ((d + 16) % 32) for b in range(8) for d in range(32)]
    cols += list(range(dq, dq + 256))
    cols += sw32(dq)
    cols += list(range(dk, dk + 256))
    cols += sw32(dk)
    cols += list(range(sv, sv + 128))
    cols += list(range(dv, dv + 256))
    assert len(cols) == NW
    return np.array(cols)


def rope_tables(half):
    t = np.arange(HALF, dtype=np.float32) + np.float32(half * HALF)
    row = np.floor(t / 64).astype(np.float32)
    col = (t - row * 64).astype(np.float32)
    out = np.zeros((128, 4, HALF), np.float32)
    for (ti, hd) in [(0, 64), (2, 32)]:
        nf = hd // 4
        inv = (np.float32(10000.0) ** (-np.arange(nf, dtype=np.float32) / np.float32(nf))).astype(np.float32)
        ang = np.concatenate([row[:, None] * inv, col[:, None] * inv], -1).astype(np.float32)
        cs, sn = np.cos(ang).astype(np.float32), np.sin(ang).astype(np.float32)
        for p in range(128):
            d = p % hd
            jx = d % (hd // 2)
            out[p, ti] = cs[:, jx]
            out[p, ti + 1] = -sn[:, jx] if d < hd // 2 else sn[:, jx]
    return out


def swa_masks(half):
    m = np.zeros((128, 8, 512), np.float32)
    kk = np.arange(128)[:, None]
    q = np.arange(512)[None, :]
    for r in range(6):
        m[:, r] = (np.abs((r - 1) * 128 + kk - q) <= 128)
    m[:, 6] = m[:, 0] if half == 1 else 0.0
    m[:, 7] = m[:, 5] if half == 0 else 0.0
    return m.astype(NPBF)


def rep(v):
    return np.ascontiguousarray(np.broadcast_to(np.asarray(v, np.float32).reshape(1, -1), (128, np.size(v))))


def small_params(inp, layer, half):
    f = lambda a: np.ascontiguousarray(np.asarray(a, np.float32))
    p = {}
    p["CW"] = f(inp["lru_conv_w"][layer].reshape(4, 2, 128).transpose(2, 1, 0))
    p["CB"] = f(inp["lru_conv_b"][layer].reshape(2, 128).T)
    p["GB"] = f(inp["lru_gate_b"][layer].reshape(2, 2, 2, 128).transpose(3, 2, 0, 1))
    p["LAM"] = f(inp["lru_lam"][layer].reshape(2, 2, 128).transpose(2, 1, 0))
    gw = np.zeros((128, 2, 2, 2, 128), np.float32)
    w = inp["lru_gate_w"][layer]
    for c in range(2):
        for bb in range(2):
            blk = c * 2 + bb
            gw[bb * 64:(bb + 1) * 64, c, :, :, bb * 64:(bb + 1) * 64] = w[:, :, blk].transpose(2, 0, 1, 3)
    p["GW"] = gw
    sel = np.zeros((128, 2), np.float32)
    sel[:, half] = 1.0
    p["SEL"] = sel
    p["SINK"] = rep(inp["swa_sink"][layer])
    p["DL"] = rep(inp["diff_lam"][layer].reshape(-1)).reshape(128, 4, 32)
    p["DG"] = f(np.tile(inp["diff_norm_g"][layer], 2).reshape(128, 1))
    pm = np.zeros((128, 2), np.float32)
    pm[:, 0] = (np.arange(128) % 64) < 32
    pm[:, 1] = (np.arange(128) % 64) >= 32
    p["PM"] = pm
    p["LNG"] = f(inp["ln_g"][layer].reshape(2, 8, 128).transpose(2, 0, 1))
    p["LNB"] = f(inp["ln_b"][layer].reshape(2, 8, 128).transpose(2, 0, 1))
    if layer % 2 == 1:
        p["RB"] = rep(inp["moe_router_b"][layer // 2])
    else:
        p["RB"] = np.zeros((128, 8), np.float32)
    return p


def ffn_layout(w1, w3, w2):
    E = w1.shape[0]
    a = w1.reshape(E, 8, 128, NJ, 128).transpose(0, 3, 2, 1, 4)
    b = w3.reshape(E, 8, 128, NJ, 128).transpose(0, 3, 2, 1, 4)
    w13 = np.ascontiguousarray(np.concatenate([a, b], axis=-1))
    w2r = np.ascontiguousarray(w2.reshape(E, NJ, 128, 8, 128).transpose(0, 3, 2, 1, 4))
    return w13, w2r


_PROGS = {}


def _prog(key, fn):
    if key not in _PROGS:
        _PROGS[key] = fn()
    return _PROGS[key]


PAIRS = [[0, 1], [2, 3], [4, 5], [6, 7]]
PUB = ["XL0", "XL1", "KS", "VS", "KD", "VD0", "VD1"]


def build_fused(depth=DEPTH):
    k = K()
    S = k.S
    nc = k.nc
    I = lambda n, s, dt: k.dram(n, s, dt, "ExternalInput")
    hT_d = I("hT", [128, 8, NT], F32)
    cc_d = I("cc", [128, 8, 2], F32)
    cst_d = I("ident", [128, 128], F32)
    rope_d = I("rope", [128, 4, HALF], F32)
    msk_d = I("MSK", [128, 8, 512], BF16)
    L = []
    for l in range(depth):
        moe = l % 2 == 1
        nexp = NEXP if moe else 1
        d = dict(adaw=I(f"adaw{l}", [D_MODEL, 6 * D_MODEL], F32), adab=I(f"adab{l}", [128, 48], F32),
                 win=I(f"win{l}", [D_MODEL, NW], F32), wout=I(f"wout{l}", [D_MODEL, D_MODEL], F32),
                 w13=I(f"w13_{l}", [nexp, NJ, 128, 8, 256], F32), w2=I(f"w2_{l}", [nexp, 8, 128, NJ, 128], F32),
                 rw=I(f"rw{l}", [D_MODEL, NEXP], F32) if moe else None,
                 sm={n: I(f"{n}{l}", s_, F32) for n, s_ in SMALL.items()})
        L.append(d)
    out_d = k.dram("hout", [128, 8, NT], F32, "ExternalOutput")
    scr = []
    for par in range(2):
        o = {n: Tile(nc.dram_tensor(f"{n}_{par}", list(sh), dt).ap()) for n, (sh, dt) in A_OUT.items()}
        gth = {n: Tile(nc.dram_tensor(f"{n}G_{par}", [2 * A_OUT[n][0][0], A_OUT[n][0][1]], A_OUT[n][1]).ap())
               for n in PUB}
        scr.append((o, gth))
    with k.es:
        hT = k.sb([128, 8, NT], F32, name="hT")
        MODX = k.sb([128, 8, 8, 2], F32, name="modx")
        S.dma("sp", hT[:], hT_d[:, :, :], reads=[hT_d], writes=[hT])
        C = emit_consts(k, cst_d)
        prm = {n: k.sb(s_, F32, name=n) for n, s_ in SMALL.items()}
        for l in range(depth):
            moe = l % 2 == 1
            lam_init = 0.8 - 0.6 * math.exp(-0.3 * l)
            o, gth = scr[l % 2]
            emit_mods(k, cc_d, L[l]["adaw"], L[l]["adab"], MODX)
            emit_phaseA(k, hT, MODX, L[l]["win"], rope_d, o)
            for n in PUB:
                S.coll("AllGather", PAIRS, o[n], gth[n])
            for n in SMALL:
                S.dma("sp", prm[n][:], L[l]["sm"][n].t, reads=[L[l]["sm"][n]], writes=[prm[n]])

            def G(n):
                t = Tile(gth[n].t.rearrange("(h r) n -> h r n", h=2))
                t.b = gth[n].b
                return t
            g = dict(XG=[G("XL0"), G("XL1")], GT=o["GT"], QS=o["QS"], KSO=o["KS"], KSG=G("KS"), VSO=o["VS"],
                     VSG=G("VS"), QD=o["QD"], KDO=o["KD"], KDG=G("KD"), VDO=o["VD0"], VDG=[G("VD0"), G("VD1")],
                     MSK=msk_d)
            with k.scope() as es:
                mixT = k.sb([128, 8, NT], BF16, es, "mixT")
                emit_lru(k, mixT, C, g, prm)
                emit_swa(k, mixT, C, g, prm)
                emit_diff(k, mixT, C, g, prm, lam_init)
                emit_phaseC(k, hT, mixT, MODX, C, L[l]["wout"], prm)
            emit_ffn(k, hT, MODX, C, prm, L[l]["w13"], L[l]["w2"], NEXP if moe else 1, L[l]["rw"])
        S.dma("sp", out_d[:, :, :], hT[:], reads=[hT], writes=[out_d])
        S.finish()
    return k.nc


def fused_inputs(inp, depth=DEPTH):
    x, c, ctx, c_ctx = inp["x"], inp["c"], inp["ctx"], inp["c_ctx"]
    cores = [(b, hf) for b in range(BATCH) for hf in range(2)]
    perm = win_perm()
    ropes = [rope_tables(hf) for hf in range(2)]
    masks = [swa_masks(hf) for hf in range(2)]
    shared = dict(ident=np.eye(128, dtype=np.float32))
    for l in range(depth):
        j = l // 2
        shared[f"adaw{l}"] = np.ascontiguousarray(inp["ada_w"][l])
        shared[f"adab{l}"] = np.ascontiguousarray(inp["ada_b"][l].reshape(48, 128).T)
        shared[f"win{l}"] = np.ascontiguousarray(inp["w_in"][l][:, perm])
        shared[f"wout{l}"] = np.ascontiguousarray(inp["w_out"][l])
        if l % 2 == 0:
            w13, w2r = ffn_layout(inp["ffn_w1"][j][None], inp["ffn_w3"][j][None], inp["ffn_w2"][j][None])
        else:
            w13, w2r = ffn_layout(inp["moe_w1"][j], inp["moe_w3"][j], inp["moe_w2"][j])
            shared[f"rw{l}"] = np.ascontiguousarray(inp["moe_router_w"][j])
        shared[f"w13_{l}"] = w13
        shared[f"w2_{l}"] = w2r
    in_maps = []
    for (b, hf) in cores:
        m = dict(shared)
        toks = np.concatenate([ctx[b], x[b, hf * HALF:(hf + 1) * HALF]], 0)
        m["hT"] = to_fm(toks)
        m["cc"] = np.ascontiguousarray(np.stack([c[b].reshape(8, 128).T, c_ctx.reshape(8, 128).T], -1))
        m["rope"] = ropes[hf]
        m["MSK"] = masks[hf]
        for l in range(depth):
            for n, v in small_params(inp, l, hf).items():
                m[f"{n}{l}"] = v
        in_maps.append(m)
    return cores, in_maps


def kernel(**inp):
    inp = {k_: np.asarray(v) for k_, v in inp.items()}
    cores, in_maps = fused_inputs(inp)
    res = run_bass_kernel_spmd(_prog("fused", build_fused), in_maps, core_ids=list(range(NCORES))).results
    out = np.zeros((BATCH, SEQ, D_MODEL), np.float32)
    for ci, (b, hf) in enumerate(cores):
        out[b, hf * HALF:(hf + 1) * HALF] = from_fm(np.asarray(res[ci]["hout"]))[CTX:]
    return out
```

```python
import math
from contextlib import ExitStack

import numpy as np
import ml_dtypes

import concourse.bass as bass
import concourse.mybir as mybir
from concourse.bass_utils import run_bass_kernel_spmd

F32 = mybir.dt.float32
BF16 = mybir.dt.bfloat16
AF = mybir.ActivationFunctionType
ALU = mybir.AluOpType
AX = mybir.AxisListType
NPBF = ml_dtypes.bfloat16

D_MODEL = 1024
BATCH = 4
SEQ = 4096
DEPTH = 4
CTX = 256
HALF = SEQ // 2
NT = CTX + HALF
D_FF = 2816
NJ = D_FF // 128
NEXP = 8
LN_EPS = 1e-5
ALPHA = (2.0 * DEPTH) ** 0.25
NW = 24 * 128 + 384
NCORES = 8


class Buf:
    __slots__ = ("w", "r", "excl")

    def __init__(self, excl=False):
        self.w = None
        self.r = {}
        self.excl = excl


class Tile:
    def __init__(self, t, excl=False):
        self.t = t
        self.b = Buf(excl)

    def __getitem__(self, idx):
        return self.t[idx]


class Sched:
    EPOCH = 16000
    NDMA = 28

    def __init__(self, nc, es):
        self.nc, self.es = nc, es
        self.eng = dict(pe=nc.tensor, act=nc.scalar, dve=nc.vector, pool=nc.gpsimd, sp=nc.sync)
        self.cnt = {}
        self.sem = {}
        self.own = {e: set() for e in self.eng}
        self.nsem = 0
        self.waited = {e: {} for e in self.eng}
        self.dsem = []
        self.dcnt = []
        self.dn = 0
        self.allsems = []
        self.csem = None
        self.ccnt = 0

    def _newsem(self, name):
        self.nsem += 1
        s = self.es.enter_context(self.nc.semaphore(f"{name}_{self.nsem}"))
        self.allsems.append(s)
        return s

    def _wait(self, e, deps):
        w = self.waited[e]
        for sem, val in deps:
            if w.get(sem, 0) < val:
                self.eng[e].wait_ge(sem, val)
                w[sem] = val

    def _deps(self, e, reads, writes):
        deps = []
        own = self.own[e]
        for b in reads:
            if b.w is not None:
                deps.append(b.w)
            if b.excl:
                deps.extend(b.r.items())
        for b in writes:
            if b.w is not None:
                deps.append(b.w)
            deps.extend(x for x in b.r.items() if x[0] not in own)
        if e == "pe":
            deps = [d for d in deps if d[0] not in own]
        return deps

    def _mark(self, tok, reads, writes):
        for b in reads:
            if b.excl:
                b.w = tok
                b.r = {}
            else:
                b.r[tok[0]] = tok[1]
        for b in writes:
            b.w = tok
            b.r = {}

    def op(self, e, fn, reads=(), writes=()):
        reads = [x.b if isinstance(x, Tile) else x for x in reads]
        writes = [x.b if isinstance(x, Tile) else x for x in writes]
        self._wait(e, self._deps(e, reads, writes))
        ins = fn(self.eng[e])
        if self.cnt.get(e, self.EPOCH) >= self.EPOCH:
            self.sem[e] = self._newsem(e)
            self.cnt[e] = 0
            self.own[e].add(self.sem[e])
        self.cnt[e] += 1
        ins.then_inc(self.sem[e], 1)
        tok = (self.sem[e], self.cnt[e])
        self._mark(tok, reads, writes)
        return tok

    def dma(self, q, out, in_, reads=(), writes=()):
        reads = [x.b if isinstance(x, Tile) else x for x in reads]
        writes = [x.b if isinstance(x, Tile) else x for x in writes]
        self._wait(q, self._deps(q, reads, writes))
        i = self.dn % self.NDMA
        self.dn += 1
        if i >= len(self.dsem):
            self.dsem.append(self._newsem("d"))
            self.dcnt.append(0)
        sem = self.dsem[i]
        if self.dcnt[i] > 0:
            self._wait(q, [(sem, self.dcnt[i])])
        self.dcnt[i] += 16
        self.eng[q].dma_start(out=out, in_=in_).then_inc(sem, 16)
        tok = (sem, self.dcnt[i])
        self._mark(tok, reads, writes)
        return tok

    def coll(self, kind, groups, src, dst):
        q = "pool"
        reads, writes = [src.b], [dst.b]
        self._wait(q, self._deps(q, reads, writes))
        if self.csem is None:
            self.csem = self._newsem("cc")
            self.ccnt = 0
        self.ccnt += 1
        self.nc.gpsimd.collective_compute(kind, ALU.bypass, replica_groups=groups, ins=[src.t.opt()],
                                          outs=[dst.t.opt()]).then_inc(self.csem, 1)
        tok = (self.csem, self.ccnt)
        self._mark(tok, reads, writes)
        return tok

    def barrier(self):
        deps = [(s, c) for s, c in zip(self.dsem, self.dcnt) if c > 0]
        deps += [(self.sem[e], self.cnt[e]) for e in self.sem]
        if self.csem is not None:
            deps.append((self.csem, self.ccnt))
        for e in self.eng:
            self._wait(e, deps)

    def finish(self):
        deps = [(s, c) for s, c in zip(self.dsem, self.dcnt) if c > 0]
        deps += [(self.sem[e], self.cnt[e]) for e in self.sem]
        if self.csem is not None:
            deps.append((self.csem, self.ccnt))
        self._wait("sp", deps)


class K:
    def __init__(self):
        self.nc = bass.Bass("TRN2", target_bir_lowering=False)
        self.es = ExitStack()
        self.S = Sched(self.nc, self.es)
        self.n = 0
        self.psum = None

    def dram(self, name, shape, dt, kind):
        t = self.nc.dram_tensor(name, list(shape), dt, kind=kind).ap()
        return Tile(t)

    def sb(self, shape, dt, es=None, name=None):
        self.n += 1
        t = (es or self.es).enter_context(self.nc.sbuf_tensor(f"{name or 't'}_{self.n}", list(shape), dt))
        return Tile(t)

    def scope(self):
        k = self

        class _Scope(ExitStack):
            def __exit__(self, *a):
                k.S.barrier()
                return super().__exit__(*a)
        return _Scope()

    def banks(self):
        if self.psum is None:
            self.psum = []
            for i in range(8):
                t = self.es.enter_context(self.nc.psum_tensor(f"ps{i}", [128, 512], F32))
                self.psum.append(Tile(t, excl=True))
        return self.psum


class Rot:
    def __init__(self, items):
        self.items = items
        self.i = 0

    def next(self):
        x = self.items[self.i % len(self.items)]
        self.i += 1
        return x


def segs(c0, n):
    out = []
    if c0 < CTX:
        hi = min(CTX, c0 + n)
        out.append((c0, hi, 1))
        if c0 + n > CTX:
            out.append((CTX, c0 + n, 0))
    else:
        out.append((c0, c0 + n, 0))
    return out


def emit_consts(k, cst_d):
    S = k.S
    c = {}
    c["ident"] = k.sb([128, 128], F32, name="ident")
    S.dma("sp", c["ident"][:], cst_d[:, :], reads=[cst_d], writes=[c["ident"]])
    c["onesf"] = k.sb([128, 128], F32, name="onesf")
    S.op("dve", lambda e: e.memset(c["onesf"][:], 1.0), writes=[c["onesf"]])
    c["onesb"] = k.sb([128, 128], BF16, name="onesb")
    S.op("dve", lambda e: e.memset(c["onesb"][:], 1.0), writes=[c["onesb"]])
    c["eps"] = k.sb([128, 1], F32, name="eps")
    S.op("dve", lambda e: e.memset(c["eps"][:], LN_EPS), writes=[c["eps"]])
    return c


def emit_mods(k, cc_d, adaw_d, adab_d, MODX):
    S = k.S
    P = k.banks()
    with k.scope() as es:
        cc = k.sb([128, 8, 2], F32, es)
        sl = k.sb([128, 8, 2], F32, es)
        adab = k.sb([128, 48], F32, es)
        wts = Rot([k.sb([128, 8, 512], F32, es) for _ in range(2)])
        S.dma("sp", cc[:], cc_d[:, :, :], reads=[cc_d], writes=[cc])
        S.dma("sp", adab[:], adab_d[:, :], reads=[adab_d], writes=[adab])
        S.op("act", lambda e: e.activation(out=sl[:], in_=cc[:], func=AF.Silu), reads=[cc], writes=[sl])
        ps = P[0]
        wsrc = adaw_d.t.rearrange("(k p) n -> p k n", p=128)
        for piece in range(12):
            wt = wts.next()
            S.dma("sp", wt[:], wsrc[:, :, piece * 512:(piece + 1) * 512], reads=[adaw_d], writes=[wt])
            for m in range(4):
                ma = piece * 4 + m
                for kk in range(8):
                    S.op("pe", lambda e, wt=wt, m=m, kk=kk, ma=ma: e.matmul(
                        ps[:, ma * 2:ma * 2 + 2], lhsT=wt[:, kk, m * 128:(m + 1) * 128], rhs=sl[:, kk, :],
                        start=(kk == 0), stop=(kk == 7)), reads=[wt, sl], writes=[ps])
        ps3 = ps[:, 0:96].rearrange("p (m j) -> p m j", j=2)
        mx = MODX[:, 0:6, :, :].rearrange("p w c j -> p (w c) j")
        for j in range(2):
            S.op("dve", lambda e, j=j: e.tensor_tensor(out=mx[:, :, j], in0=ps3[:, :, j], in1=adab[:, :], op=ALU.add),
                 reads=[ps, adab], writes=[MODX])
        S.op("dve", lambda e: e.tensor_scalar_add(out=MODX[:, 6, :, :], in0=MODX[:, 1, :, :], scalar1=1.0),
             reads=[MODX], writes=[MODX])
        S.op("dve", lambda e: e.tensor_scalar_add(out=MODX[:, 7, :, :], in0=MODX[:, 4, :, :], scalar1=1.0),
             reads=[MODX], writes=[MODX])


def emit_phaseA(k, hT, MODX, win_d, rope_d, o):
    S = k.S
    P = k.banks()
    with k.scope() as es:
        WIN = k.sb([128, 8, NW], BF16, es, "win")
        wsrc = win_d.t.rearrange("(k p) n -> p k n", p=128)
        for pc in range(4):
            lo, hi = pc * (NW // 4), (pc + 1) * (NW // 4)
            S.dma("pool", WIN[:, :, lo:hi], wsrc[:, :, lo:hi], reads=[win_d], writes=[WIN])
        ub = Rot([k.sb([128, 8, 512], BF16, es, "u") for _ in range(2)])
        rt = Rot([k.sb([128, 4, 512], F32, es, "rope") for _ in range(2)])
        stf = Rot([k.sb([128, 512], F32, es, "stf") for _ in range(3)])
        stb = Rot([k.sb([128, 512], BF16, es, "stb") for _ in range(3)])
        tm1 = Rot([k.sb([128, 512], F32, es, "tm1") for _ in range(2)])
        tm2 = Rot([k.sb([128, 512], F32, es, "tm2") for _ in range(2)])
        vss = Rot([k.sb([128, 2, 128], BF16, es, "vss") for _ in range(2)])
        vds = Rot([k.sb([128, 4, 128], BF16, es, "vds") for _ in range(2)])
        for v in vss.items + vds.items:
            S.op("dve", lambda e, v=v: e.memset(v[:], 1.0), writes=[v])
        pb = Rot(P)

        tiles = [(0, 256, 1)] + [(CTX + i * 512, 512, 0) for i in range(4)]
        for (c0, N, j) in tiles:
            lat = j == 0
            u = ub.next()
            for kk in range(8):
                S.op("act", lambda e, kk=kk: e.activation(
                    out=u[:, kk, 0:N], in_=hT[:, kk, c0:c0 + N], func=AF.Identity,
                    scale=MODX[:, 6, kk, j:j + 1], bias=MODX[:, 0, kk, j:j + 1]), reads=[hT, MODX], writes=[u])
            if lat:
                R = rt.next()
                S.dma("sp", R[:], rope_d[:, :, c0 - CTX:c0 - CTX + 512], reads=[rope_d], writes=[R])

            def proj(ch, ps):
                for kk in range(8):
                    S.op("pe", lambda e, kk=kk: e.matmul(
                        ps[:, 0:N], lhsT=WIN[:, kk, ch * 128:(ch + 1) * 128], rhs=u[:, kk, 0:N],
                        start=(kk == 0), stop=(kk == 7)), reads=[WIN, u], writes=[ps])

            for c in range(2):
                ps = pb.next()
                proj(c, ps)
                st = stf.next()
                S.op("act", lambda e: e.activation(out=st[:, 0:N], in_=ps[:, 0:N], func=AF.Identity),
                     reads=[ps], writes=[st])
                S.dma("sp", o["XL%d" % c][:, c0:c0 + N], st[:, 0:N], reads=[st], writes=[o["XL%d" % c]])
            for c in range(2):
                ps = pb.next()
                proj(2 + c, ps)
                st = stf.next()
                S.op("act", lambda e: e.activation(out=st[:, 0:N], in_=ps[:, 0:N], func=AF.Gelu_apprx_tanh),
                     reads=[ps], writes=[st])
                S.dma("sp", o["GT"][c * 128:(c + 1) * 128, c0:c0 + N], st[:, 0:N], reads=[st], writes=[o["GT"]])
            for (nb, sb_, n, tc, dst) in [(4, 8, 4, 0, "QS"), (12, 14, 2, 0, "KS"), (16, 18, 2, 2, "QD"),
                                          (20, 22, 2, 2, "KD")]:
                for i in range(n):
                    psA = pb.next()
                    proj(nb + i, psA)
                    st = stb.next()
                    if lat:
                        psB = pb.next()
                        proj(sb_ + i, psB)
                        t1, t2 = tm1.next(), tm2.next()
                        S.op("dve", lambda e: e.tensor_tensor(out=t1[:], in0=psA[:, :], in1=R[:, tc, :], op=ALU.mult),
                             reads=[psA, R], writes=[t1])
                        S.op("dve", lambda e: e.tensor_tensor(out=t2[:], in0=psB[:, :], in1=R[:, tc + 1, :],
                                                              op=ALU.mult), reads=[psB, R], writes=[t2])
                        S.op("pool", lambda e: e.tensor_tensor(out=st[:], in0=t1[:], in1=t2[:], op=ALU.add),
                             reads=[t1, t2], writes=[st])
                    else:
                        S.op("act", lambda e: e.activation(out=st[:, 0:N], in_=psA[:, 0:N], func=AF.Identity),
                             reads=[psA], writes=[st])
                    S.dma("sp", o[dst][i * 128:(i + 1) * 128, c0:c0 + N], st[:, 0:N], reads=[st], writes=[o[dst]])
            for blk in range(N // 128):
                ps = pb.next()
                for kk in range(8):
                    S.op("pe", lambda e, kk=kk: e.matmul(
                        ps[:, 0:384], lhsT=u[:, kk, blk * 128:(blk + 1) * 128], rhs=WIN[:, kk, 3072:3456],
                        start=(kk == 0), stop=(kk == 7)), reads=[WIN, u], writes=[ps])
                vs, vd = vss.next(), vds.next()
                S.op("act", lambda e: e.activation(out=vs[:, :, 0:64],
                                                   in_=ps[:, 0:128].rearrange("p (h d) -> p h d", d=64),
                                                   func=AF.Identity), reads=[ps], writes=[vs])
                S.op("dve", lambda e: e.tensor_copy(out=vd[:, :, 0:64],
                                                    in_=ps[:, 128:384].rearrange("p (h d) -> p h d", d=64)),
                     reads=[ps], writes=[vd])
                r0 = c0 + blk * 128
                S.dma("sp", o["VS"][r0:r0 + 128, :], vs[:].rearrange("p h d -> p (h d)"), reads=[vs],
                      writes=[o["VS"]])
                vch, vr = r0 // VDR, r0 % VDR
                S.dma("sp", o["VD%d" % vch][vr:vr + 128, :], vd[:].rearrange("p h d -> p (h d)"), reads=[vd],
                      writes=[o["VD%d" % vch]])


def emit_lru(k, mixT, C, g, prm):
    S = k.S
    P = k.banks()
    with k.scope() as es:
        xs = k.sb([128, 1 + SEQ + 2], F32, es, "xs")
        xc = k.sb([128, 1 + CTX + 2], F32, es, "xc")
        U = k.sb([128, CTX + SEQ], F32, es, "U")
        OUT = k.sb([128, NT], F32, es, "lout")
        GTs = k.sb([128, NT], F32, es, "gts")
        tR = Rot([k.sb([128, 512], F32, es, "tR") for _ in range(4)])
        tI = Rot([k.sb([128, 512], F32, es, "tI") for _ in range(4)])
        tA = Rot([k.sb([128, 512], F32, es, "tA") for _ in range(4)])
        tH = Rot([k.sb([128, 512], F32, es, "tH") for _ in range(3)])
        sp = k.sb([128, 4], F32, es, "sp")
        nsp8 = k.sb([128, 2, 2], F32, es, "nsp8")
        nsp16 = k.sb([128, 2, 2], F32, es, "nsp16")
        lam = prm["LAM"]
        spv = sp[:, 0:4]
        lamv = lam[:].rearrange("p c d -> p (c d)")
        S.op("act", lambda e: e.activation(out=spv, in_=lamv, func=AF.Exp, scale=-1.0), reads=[lam], writes=[sp])
        S.op("dve", lambda e: e.tensor_scalar_add(out=spv, in0=spv, scalar1=1.0), reads=[sp], writes=[sp])
        S.op("act", lambda e: e.activation(out=spv, in_=spv, func=AF.Ln), reads=[sp], writes=[sp])
        S.op("dve", lambda e: e.tensor_scalar_mul(out=nsp8[:].rearrange("p c d -> p (c d)"), in0=spv, scalar1=-8.0),
             reads=[sp], writes=[nsp8])
        S.op("dve", lambda e: e.tensor_scalar_mul(out=nsp16[:].rearrange("p c d -> p (c d)"), in0=spv, scalar1=-16.0),
             reads=[sp], writes=[nsp16])
        pg = Rot(P[0:8])
        CW, CB, GB, GW, SEL = prm["CW"], prm["CB"], prm["GB"], prm["GW"], prm["SEL"]
        for c in range(2):
            rows = slice(c * 128, (c + 1) * 128)
            S.op("dve", lambda e: e.memset(xs[:, 0:1], 0.0), writes=[xs])
            S.op("dve", lambda e: e.memset(xs[:, 1 + SEQ:3 + SEQ], 0.0), writes=[xs])
            S.op("dve", lambda e: e.memset(xc[:, 0:1], 0.0), writes=[xc])
            S.op("dve", lambda e: e.memset(xc[:, 1 + CTX:3 + CTX], 0.0), writes=[xc])
            XGc = g["XG"][c]
            S.dma("sp", xs[:, 1:1 + HALF], XGc[0, :, CTX:NT], reads=[XGc], writes=[xs])
            S.dma("sp", xs[:, 1 + HALF:1 + SEQ], XGc[1, :, CTX:NT], reads=[XGc], writes=[xs])
            S.dma("sp", xc[:, 1:1 + CTX], XGc[0, :, 0:CTX], reads=[XGc], writes=[xc])
            S.dma("sp", GTs[:], g["GT"][rows, :], reads=[g["GT"]], writes=[GTs])
            for (src, L, d0) in [(xc, CTX, 0), (xs, SEQ, CTX)]:
                S.op("act", lambda e: e.activation(out=U[:, d0:d0 + L], in_=src[:, 1:1 + L], func=AF.Identity,
                                                   scale=CW[:, c, 1:2], bias=CB[:, c:c + 1]),
                     reads=[src, CW, CB], writes=[U])
                for (off, wi) in [(0, 0), (2, 2), (3, 3)]:
                    S.op("dve", lambda e, off=off, wi=wi: e.scalar_tensor_tensor(
                        out=U[:, d0:d0 + L], in0=src[:, off:off + L], scalar=CW[:, c, wi:wi + 1], in1=U[:, d0:d0 + L],
                        op0=ALU.mult, op1=ALU.add), reads=[src, CW, U], writes=[U])
            S.op("dve", lambda e: e.memset(OUT[:], 0.0), writes=[OUT])
            for d in range(2):
                lat = [(CTX + i * 512, 512, i) for i in range(8)]
                if d == 1:
                    lat = lat[::-1]
                state = None
                hprev = None
                seq = [(0, CTX, -1)] + lat
                for grp_ in [seq[0:1]] + [seq[i_:i_ + 2] for i_ in range(1, 9, 2)]:
                    items = []
                    for (u0, L, kind) in grp_:
                        pss = []
                        for gi in range(2):
                            ps = pg.next()
                            S.op("pe", lambda e: e.matmul(ps[:, 0:L], lhsT=GW[:, c, d, gi, :], rhs=U[:, u0:u0 + L],
                                                          start=True, stop=True), reads=[GW, U], writes=[ps])
                            pss.append(ps)
                        r, ii, a, h = tR.next(), tI.next(), tA.next(), tH.next()
                        S.op("act", lambda e: e.activation(out=r[:, 0:L], in_=pss[0][:, 0:L], func=AF.Sigmoid,
                                                           bias=GB[:, c, d, 0:1]), reads=[pss[0], GB], writes=[r])
                        S.op("act", lambda e: e.activation(out=ii[:, 0:L], in_=pss[1][:, 0:L], func=AF.Sigmoid,
                                                           bias=GB[:, c, d, 1:2]), reads=[pss[1], GB], writes=[ii])
                        items.append((u0, L, kind, r, ii, a, h))
                    for (u0, L, kind, r, ii, a, h) in items:
                        S.op("act", lambda e: e.activation(out=a[:, 0:L], in_=r[:, 0:L], func=AF.Exp,
                                                           scale=nsp8[:, c, d:d + 1]), reads=[r, nsp8], writes=[a])
                        S.op("pool", lambda e: e.tensor_tensor(out=r[:, 0:L], in0=a[:, 0:L], in1=a[:, 0:L],
                                                               op=ALU.mult), reads=[a], writes=[r])
                    for (u0, L, kind, r, ii, a, h) in items:
                        S.op("act", lambda e: e.activation(out=r[:, 0:L], in_=r[:, 0:L], func=AF.Sqrt, scale=-1.0,
                                                           bias=1.0), reads=[r], writes=[r])
                    for (u0, L, kind, r, ii, a, h) in items:
                        S.op("dve", lambda e: e.tensor_tensor(out=ii[:, 0:L], in0=ii[:, 0:L], in1=U[:, u0:u0 + L],
                                                              op=ALU.mult), reads=[ii, U], writes=[ii])
                        S.op("dve", lambda e: e.tensor_tensor(out=ii[:, 0:L], in0=ii[:, 0:L], in1=r[:, 0:L],
                                                              op=ALU.mult), reads=[ii, r], writes=[ii])
                        if d == 0:
                            vo, va, vb = h[:, 0:L], a[:, 0:L], ii[:, 0:L]
                        else:
                            vo, va, vb = h[:, L - 1::-1], a[:, L - 1::-1], ii[:, L - 1::-1]
                        init = 0.0 if state is None else state
                        rd = [a, ii] + ([hprev] if hprev is not None else [])
                        S.op("dve", lambda e: e.tensor_tensor_scan(out=vo, data0=va, data1=vb, initial=init,
                                                                   op0=ALU.mult, op1=ALU.add), reads=rd, writes=[h])
                        state = h[:, L - 1:L] if d == 0 else h[:, 0:1]
                        hprev = h
                        if kind < 0:
                            S.op("dve", lambda e: e.tensor_tensor(out=OUT[:, 0:CTX], in0=h[:, 0:L], in1=OUT[:, 0:CTX],
                                                                  op=ALU.add), reads=[h, OUT], writes=[OUT])
                        else:
                            hf = kind // 4
                            lo = CTX + (kind % 4) * 512
                            S.op("dve", lambda e: e.scalar_tensor_tensor(
                                out=OUT[:, lo:lo + L], in0=h[:, 0:L], scalar=SEL[:, hf:hf + 1], in1=OUT[:, lo:lo + L],
                                op0=ALU.mult, op1=ALU.add), reads=[h, OUT, SEL], writes=[OUT])
            S.op("pool", lambda e: e.tensor_tensor(out=mixT[:, c, :], in0=OUT[:], in1=GTs[:], op=ALU.mult),
                 reads=[OUT, GTs], writes=[mixT])


def pipeline(n, front, back, la, deferred):
    for i in range(n + la):
        if i < n:
            front(i)
        if i >= la:
            back(i - la)
        while deferred and deferred[0][0] <= i:
            deferred.pop(0)[1]()
    while deferred:
        deferred.pop(0)[1]()


def emit_swa(k, mixT, C, g, prm):
    S = k.S
    P = k.banks()
    LA = 3
    with k.scope() as es:
        KS = [[k.sb([128, CTX + 128 + HALF + 128], BF16, es, "ks") for _ in range(2)] for _ in range(2)]
        VSa = k.sb([128, 20, 256], BF16, es, "vsa")
        MSK = k.sb([128, 8, 512], BF16, es, "msk")
        ESK = k.sb([128, 8], F32, es, "esk")
        qb = Rot([k.sb([128, 4, 512], BF16, es, "qs") for _ in range(2)])
        Eb = Rot([k.sb([128, 512], BF16, es, "E") for _ in range(6)])
        den = Rot([k.sb([128, 512], F32, es, "den") for _ in range(2)])
        S.dma("sp", MSK[:], g["MSK"][:, :, :], reads=[g["MSK"]], writes=[MSK])
        S.op("act", lambda e: e.activation(out=ESK[:], in_=prm["SINK"][:], func=AF.Exp), reads=[prm["SINK"]],
             writes=[ESK])
        for hk in range(2):
            for hp in range(2):
                T_ = KS[hk][hp]
                ps_ = slice(hp * 64, hp * 64 + 64)
                zs_ = slice((1 - hp) * 64, (1 - hp) * 64 + 64)
                rows = slice(hk * 128 + hp * 64, hk * 128 + hp * 64 + 64)
                S.op("dve", lambda e: e.memset(T_[zs_, :], 0.0), writes=[T_])
                S.dma("sp", T_[ps_, 0:CTX], g["KSO"][rows, 0:CTX], reads=[g["KSO"]], writes=[T_])
                S.dma("sp", T_[ps_, CTX + 128:CTX + 128 + HALF], g["KSO"][rows, CTX:NT], reads=[g["KSO"]],
                      writes=[T_])
                S.dma("sp", T_[ps_, CTX:CTX + 128], g["KSG"][0, rows, NT - 128:NT], reads=[g["KSG"]], writes=[T_])
                S.dma("sp", T_[ps_, CTX + 128 + HALF:], g["KSG"][1, rows, CTX:CTX + 128], reads=[g["KSG"]],
                      writes=[T_])
        vo = g["VSO"].t.rearrange("(b p) n -> p b n", p=128)
        S.dma("sp", VSa[:, 0:2, :], vo[:, 0:2, :], reads=[g["VSO"]], writes=[VSa])
        S.dma("sp", VSa[:, 3:19, :], vo[:, 2:18, :], reads=[g["VSO"]], writes=[VSa])
        S.dma("sp", VSa[:, 2, :], g["VSG"][0, NT - 128:NT, :], reads=[g["VSG"]], writes=[VSa])
        S.dma("sp", VSa[:, 19, :], g["VSG"][1, CTX:CTX + 128, :], reads=[g["VSG"]], writes=[VSa])
        accs = Rot(P[0:2])
        scs = Rot(P[2:8])
        qsrc = g["QS"].t.rearrange("(c p) n -> p c n", p=128)
        tiles = [-1, 0, 1, 2, 3]
        Qt = {}

        def load_q(t):
            N = CTX if t < 0 else 512
            c0 = 0 if t < 0 else CTX + t * 512
            Q = qb.next()
            S.dma("sp", Q[:, :, 0:N], qsrc[:, :, c0:c0 + N], reads=[g["QS"]], writes=[Q])
            Qt[t] = Q

        units = []
        for ti, t in enumerate(tiles):
            N = CTX if t < 0 else 512
            c0 = 0 if t < 0 else CTX + t * 512
            blocks = [(0, 0, None), (1, 128, None)]
            if t >= 0:
                for r in range(6):
                    mk = r
                    if t == 0 and r == 0:
                        mk = 6
                    if t == 3 and r == 5:
                        mk = 7
                    blocks.append((2 + 4 * t + r, CTX + (4 * t + r) * 128, mk))
            for h in range(8):
                for bi, blk in enumerate(blocks):
                    units.append((ti, t, N, c0, h, bi, len(blocks), blk))
        Ef = {}
        cur = {}
        load_q(tiles[0])

        def front(i):
            ti, t, N, c0, h, bi, nb, (vb, kcol, mk) = units[i]
            if h == 0 and bi == 0 and ti + 1 < len(tiles):
                load_q(tiles[ti + 1])
            Q = Qt[t]
            qc, pb, hk = h // 2, (h % 2) * 64, h // 4
            sps, E = scs.next(), Eb.next()
            S.op("pe", lambda e: e.matmul(sps[:, 0:N], lhsT=KS[hk][h % 2][:, kcol:kcol + 128],
                                          rhs=Q[:, qc, 0:N], start=True, stop=True),
                 reads=[KS[hk][h % 2], Q], writes=[sps])
            S.op("act", lambda e: e.activation(out=E[:, 0:N], in_=sps[:, 0:N], func=AF.Exp, scale=0.125),
                 reads=[sps], writes=[E])
            if mk is not None:
                S.op("pool" if i % 2 else "dve", lambda e: e.tensor_tensor(out=E[:, 0:N], in0=E[:, 0:N],
                                                                          in1=MSK[:, mk, 0:N], op=ALU.mult),
                     reads=[E, MSK], writes=[E])
            Ef[i] = E

        def back(i):
            ti, t, N, c0, h, bi, nb, (vb, kcol, mk) = units[i]
            qc, pb, hk = h // 2, (h % 2) * 64, h // 4
            E = Ef.pop(i)
            if bi == 0:
                cur["acc"] = accs.next()
            acc = cur["acc"]
            S.op("pe", lambda e: e.matmul(acc[:, 0:N], lhsT=VSa[:, vb, hk * 128:(hk + 1) * 128],
                                          rhs=E[:, 0:N], start=(bi == 0), stop=(bi == nb - 1)),
                 reads=[VSa, E], writes=[acc])
            if bi == nb - 1:
                dn = den.next()
                S.op("dve", lambda e: e.tensor_scalar(out=dn[0:64, 0:N], in0=acc[64:128, 0:N],
                                                      scalar1=ESK[64:128, h:h + 1], scalar2=None, op0=ALU.add),
                     reads=[acc, ESK], writes=[dn])
                S.op("dve", lambda e: e.reciprocal(out=dn[0:64, 0:N], in_=dn[0:64, 0:N]), reads=[dn], writes=[dn])
                S.op("dve", lambda e: e.tensor_tensor(out=mixT[pb:pb + 64, 2 + qc, c0:c0 + N], in0=acc[0:64, 0:N],
                                                      in1=dn[0:64, 0:N], op=ALU.mult), reads=[acc, dn],
                     writes=[mixT])

        pipeline(len(units), front, back, LA, [])


def emit_diff(k, mixT, C, g, prm, lam_init):
    S = k.S
    P = k.banks()
    LA = 3
    with k.scope() as es:
        KD = k.sb([128, 2, CTX + SEQ], BF16, es, "kd")
        VDa = k.sb([128, 34, 512], BF16, es, "vda")
        qb = Rot([k.sb([128, 2, 512], BF16, es, "qd") for _ in range(2)])
        qm = [Rot([k.sb([128, 2, 512], BF16, es, "qm") for _ in range(2)]) for _ in range(4)]
        Eb = Rot([k.sb([128, 512], BF16, es, "E") for _ in range(5)])
        tr = Rot([k.sb([128, 512], F32, es, "tr") for _ in range(2)])
        tt = Rot([k.sb([128, 512], F32, es, "tt") for _ in range(2)])
        tO = Rot([k.sb([128, 512], F32, es, "tO") for _ in range(1)])
        tq = Rot([k.sb([128, 512], F32, es, "tq") for _ in range(1)])
        lt = k.sb([128, 2, 32], F32, es, "lt")
        ls = k.sb([128, 2], F32, es, "ls")
        NLAM = k.sb([128, 1], F32, es, "nlam")
        GN = k.sb([128, 1], F32, es, "gn")
        DL = prm["DL"]
        for i in range(2):
            S.op("dve", lambda e, i=i: e.tensor_tensor(out=lt[:, i, :], in0=DL[:, 2 * i, :], in1=DL[:, 2 * i + 1, :],
                                                       op=ALU.mult), reads=[DL], writes=[lt])
        S.op("dve", lambda e: e.reduce_sum(out=ls[:], in_=lt[:], axis=AX.X), reads=[lt], writes=[ls])
        S.op("act", lambda e: e.activation(out=ls[:], in_=ls[:], func=AF.Exp), reads=[ls], writes=[ls])
        S.op("dve", lambda e: e.tensor_tensor(out=NLAM[:], in0=ls[:, 1:2], in1=ls[:, 0:1], op=ALU.subtract),
             reads=[ls], writes=[NLAM])
        S.op("dve", lambda e: e.tensor_scalar_add(out=NLAM[:], in0=NLAM[:], scalar1=-lam_init), reads=[NLAM],
             writes=[NLAM])
        S.op("dve", lambda e: e.tensor_scalar_mul(out=GN[:], in0=prm["DG"][:], scalar1=1.0 - lam_init),
             reads=[prm["DG"]], writes=[GN])
        ko = g["KDO"].t.rearrange("(c p) n -> p c n", p=128)
        S.dma("sp", KD[:, :, 0:CTX], ko[:, :, 0:CTX], reads=[g["KDO"]], writes=[KD])
        for hf in range(2):
            kg = g["KDG"][hf].rearrange("(c p) n -> p c n", p=128)
            S.dma("sp", KD[:, :, CTX + hf * HALF:CTX + (hf + 1) * HALF], kg[:, :, CTX:NT], reads=[g["KDG"]],
                  writes=[KD])
            vg0 = g["VDG"][0][hf].rearrange("(b p) n -> p b n", p=128)
            vg1 = g["VDG"][1][hf].rearrange("(b p) n -> p b n", p=128)
            S.dma("sp", VDa[:, 2 + hf * 16:2 + hf * 16 + 7, :], vg0[:, 2:9, :], reads=[g["VDG"][0]], writes=[VDa])
            S.dma("sp", VDa[:, 2 + hf * 16 + 7:2 + (hf + 1) * 16, :], vg1[:, 0:9, :], reads=[g["VDG"][1]],
                  writes=[VDa])
        vo = g["VDO"].t.rearrange("(b p) n -> p b n", p=128)
        S.dma("sp", VDa[:, 0:2, :], vo[:, 0:2, :], reads=[g["VDO"]], writes=[VDa])
        accs = Rot(P[0:2])
        scs = Rot(P[2:7])
        pn = P[7]
        qsrc = g["QD"].t.rearrange("(c p) n -> p c n", p=128)
        SC = 32 ** -0.5
        tiles = [-1, 0, 1, 2, 3]
        Qt = {}

        def load_q(t):
            N = CTX if t < 0 else 512
            c0 = 0 if t < 0 else CTX + t * 512
            Q = qb.next()
            S.dma("sp", Q[:, :, 0:N], qsrc[:, :, c0:c0 + N], reads=[g["QD"]], writes=[Q])
            Qm = [qm[v].next() for v in range(4)]
            for v in range(4):
                S.op("dve", lambda e: e.tensor_scalar(out=Qm[v][:, :, 0:N], in0=Q[:, :, 0:N],
                                                      scalar1=prm["PM"][:, v:v + 1], scalar2=None, op0=ALU.mult),
                     reads=[Q, prm["PM"]], writes=[Qm[v]])
            Qt[t] = Qm

        units = []
        for ti, t in enumerate(tiles):
            N = CTX if t < 0 else 512
            c0 = 0 if t < 0 else CTX + t * 512
            nkb = 2 if t < 0 else 34
            for h in range(4):
                for m in range(2):
                    for kb in range(nkb):
                        units.append((ti, t, N, c0, h, m, kb, nkb))
        Ef = {}
        cur = {}
        deferred = []
        load_q(tiles[0])

        def front(i):
            ti, t, N, c0, h, m, kb, nkb = units[i]
            if h == 0 and m == 0 and kb == 0 and ti + 1 < len(tiles):
                load_q(tiles[ti + 1])
            Qm = Qt[t]
            c, pb = h // 2, (h % 2) * 64
            sps, E = scs.next(), Eb.next()
            qv = Qm[(h % 2) * 2 + m]
            S.op("pe", lambda e: e.matmul(sps[:, 0:N], lhsT=KD[:, c, kb * 128:(kb + 1) * 128],
                                          rhs=qv[:, c, 0:N], start=True, stop=True),
                 reads=[KD, qv], writes=[sps])
            S.op("act", lambda e: e.activation(out=E[:, 0:N], in_=sps[:, 0:N], func=AF.Exp, scale=SC),
                 reads=[sps], writes=[E])
            Ef[i] = E

        def back(i):
            ti, t, N, c0, h, m, kb, nkb = units[i]
            c = h // 2
            E = Ef.pop(i)
            if m == 0 and kb == 0:
                cur["ac"] = [accs.next(), accs.next()]
            ac = cur["ac"]
            S.op("pe", lambda e: e.matmul(ac[m][:, 0:N], lhsT=VDa[:, kb, h * 128:(h + 1) * 128],
                                          rhs=E[:, 0:N], start=(kb == 0), stop=(kb == nkb - 1)),
                 reads=[VDa, E], writes=[ac[m]])
            if not (m == 1 and kb == nkb - 1):
                return
            ts = []
            for mm in range(2):
                r_, t_ = tr.next(), tt.next()
                S.op("dve", lambda e: e.reciprocal(out=r_[0:64, 0:N], in_=ac[mm][64:128, 0:N]), reads=[ac[mm]],
                     writes=[r_])
                S.op("dve", lambda e: e.tensor_tensor(out=t_[0:64, 0:N], in0=ac[mm][0:64, 0:N], in1=r_[0:64, 0:N],
                                                      op=ALU.mult), reads=[ac[mm], r_], writes=[t_])
                ts.append(t_)
            O, sq = tO.next(), tq.next()
            S.op("dve", lambda e: e.scalar_tensor_tensor(out=O[0:64, 0:N], in0=ts[1][0:64, 0:N],
                                                         scalar=NLAM[0:64, 0:1], in1=ts[0][0:64, 0:N],
                                                         op0=ALU.mult, op1=ALU.add), reads=ts + [NLAM],
                 writes=[O])
            S.op("act", lambda e: e.activation(out=sq[0:64, 0:N], in_=O[0:64, 0:N], func=AF.Square), reads=[O],
                 writes=[sq])

            def tail():
                S.op("pe", lambda e: e.matmul(pn[0:64, 0:N], lhsT=C["onesf"][0:64, 0:64], rhs=sq[0:64, 0:N],
                                              start=True, stop=True), reads=[C["onesf"], sq], writes=[pn])
                S.op("act", lambda e: e.activation(out=sq[0:64, 0:N], in_=pn[0:64, 0:N], func=AF.Sqrt,
                                                   scale=1.0 / 64.0, bias=C["eps"][0:64, :]),
                     reads=[pn, C["eps"]], writes=[sq])
                S.op("dve", lambda e: e.reciprocal(out=sq[0:64, 0:N], in_=sq[0:64, 0:N]), reads=[sq], writes=[sq])
                S.op("dve", lambda e: e.tensor_tensor(out=O[0:64, 0:N], in0=O[0:64, 0:N], in1=sq[0:64, 0:N],
                                                      op=ALU.mult), reads=[O, sq], writes=[O])
                ob = (h % 2) * 64
                S.op("dve", lambda e: e.tensor_scalar(out=mixT[ob:ob + 64, 6 + c, c0:c0 + N], in0=O[0:64, 0:N],
                                                      scalar1=GN[0:64, 0:1], scalar2=None, op0=ALU.mult),
                     reads=[O, GN], writes=[mixT])
            deferred.append((i + LA + min(6, 2 * nkb - 1), tail))

        pipeline(len(units), front, back, LA, deferred)


def emit_ln(k, hT, C, c0, N, LNG, LNB, which, es, pool):
    S = k.S
    ysq = pool["ysq"].next()
    S1, S2 = pool["ps"].next(), pool["ps"].next()
    for c in range(8):
        S.op("act", lambda e, c=c: e.activation(out=ysq[:, c, 0:N], in_=hT[:, c, c0:c0 + N], func=AF.Square),
             reads=[hT], writes=[ysq])
    for c in range(8):
        S.op("pe", lambda e, c=c: e.matmul(S1[:, 0:N], lhsT=C["onesf"][:, :], rhs=hT[:, c, c0:c0 + N], start=(c == 0),
                                           stop=(c == 7)), reads=[C["onesf"], hT], writes=[S1])
    for c in range(8):
        S.op("pe", lambda e, c=c: e.matmul(S2[:, 0:N], lhsT=C["onesb"][:, :], rhs=ysq[:, c, 0:N], start=(c == 0),
                                           stop=(c == 7)), reads=[C["onesb"], ysq], writes=[S2])
    mean, rstd, nmr = pool["f"].next(), pool["f"].next(), pool["f"].next()
    S.op("act", lambda e: e.activation(out=mean[:, 0:N], in_=S1[:, 0:N], func=AF.Identity, scale=1.0 / D_MODEL),
         reads=[S1], writes=[mean])
    S.op("dve", lambda e: e.tensor_tensor(out=rstd[:, 0:N], in0=mean[:, 0:N], in1=mean[:, 0:N], op=ALU.mult),
         reads=[mean], writes=[rstd])
    S.op("dve", lambda e: e.scalar_tensor_tensor(out=rstd[:, 0:N], in0=S2[:, 0:N], scalar=1.0 / D_MODEL,
                                                 in1=rstd[:, 0:N], op0=ALU.mult, op1=ALU.subtract),
         reads=[S2, rstd], writes=[rstd])
    S.op("act", lambda e: e.activation(out=rstd[:, 0:N], in_=rstd[:, 0:N], func=AF.Sqrt, bias=C["eps"][:, :]),
         reads=[rstd, C["eps"]], writes=[rstd])
    S.op("dve", lambda e: e.reciprocal(out=rstd[:, 0:N], in_=rstd[:, 0:N]), reads=[rstd], writes=[rstd])
    S.op("dve", lambda e: e.scalar_tensor_tensor(out=nmr[:, 0:N], in0=mean[:, 0:N], scalar=-1.0, in1=rstd[:, 0:N],
                                                 op0=ALU.mult, op1=ALU.mult), reads=[mean, rstd], writes=[nmr])
    for c in range(8):
        t1 = pool["t"].next()
        S.op("dve", lambda e, c=c: e.scalar_tensor_tensor(out=t1[:, 0:N], in0=hT[:, c, c0:c0 + N],
                                                          scalar=LNG[:, which, c:c + 1], in1=rstd[:, 0:N],
                                                          op0=ALU.mult, op1=ALU.mult), reads=[hT, LNG, rstd],
             writes=[t1])
        S.op("dve", lambda e, c=c: e.scalar_tensor_tensor(out=t1[:, 0:N], in0=nmr[:, 0:N],
                                                          scalar=LNG[:, which, c:c + 1], in1=t1[:, 0:N],
                                                          op0=ALU.mult, op1=ALU.add), reads=[nmr, LNG, t1],
             writes=[t1])
        S.op("act", lambda e, c=c: e.activation(out=hT[:, c, c0:c0 + N], in_=t1[:, 0:N], func=AF.Identity,
                                                bias=LNB[:, which, c:c + 1]), reads=[t1, LNB], writes=[hT])


def ln_pool(k, es, P, W=512):
    return dict(ysq=Rot([k.sb([128, 8, W], BF16, es, "ysq") for _ in range(2)]),
                f=Rot([k.sb([128, W], F32, es, "lnf") for _ in range(6)]),
                t=Rot([k.sb([128, W], F32, es, "lnt") for _ in range(3)]), ps=Rot(P))


def emit_phaseC(k, hT, mixT, MODX, C, wout_d, prm):
    S = k.S
    P = k.banks()
    with k.scope() as es:
        WO = k.sb([128, 8, D_MODEL], BF16, es, "wout")
        S.dma("pool", WO[:], wout_d.t.rearrange("(k p) n -> p k n", p=128), reads=[wout_d], writes=[WO])
        lp = ln_pool(k, es, P[6:8])
        tm = Rot([k.sb([128, 512], F32, es, "ctm") for _ in range(3)])
        pb = Rot(P[0:6])
        tiles = [(0, 256)] + [(CTX + i * 512, 512) for i in range(4)]
        prev = None
        for (c0, N) in tiles:
            j = 1 if c0 < CTX else 0
            for c in range(8):
                ps = pb.next()
                for kk in range(8):
                    S.op("pe", lambda e, kk=kk: e.matmul(ps[:, 0:N], lhsT=WO[:, kk, c * 128:(c + 1) * 128],
                                                         rhs=mixT[:, kk, c0:c0 + N], start=(kk == 0), stop=(kk == 7)),
                         reads=[WO, mixT], writes=[ps])
                t = tm.next()
                S.op("dve", lambda e: e.tensor_scalar(out=t[:, 0:N], in0=ps[:, 0:N], scalar1=MODX[:, 2, c, j:j + 1],
                                                      scalar2=None, op0=ALU.mult), reads=[ps, MODX], writes=[t])
                S.op("dve", lambda e: e.scalar_tensor_tensor(out=hT[:, c, c0:c0 + N], in0=hT[:, c, c0:c0 + N],
                                                             scalar=ALPHA, in1=t[:, 0:N], op0=ALU.mult, op1=ALU.add),
                     reads=[hT, t], writes=[hT])
            if prev is not None:
                emit_ln(k, hT, C, prev[0], prev[1], prm["LNG"], prm["LNB"], 0, es, lp)
            prev = (c0, N)
        emit_ln(k, hT, C, prev[0], prev[1], prm["LNG"], prm["LNB"], 0, es, lp)


def emit_ffn(k, hT, MODX, C, prm, w13_d, w2_d, nexp, rw_d=None):
    S = k.S
    P = k.banks()
    G = NT // 2
    TN = 384
    moe = nexp > 1
    with k.scope() as es:
        u2 = k.sb([128, 8, G], BF16, es, "u2")
        gT = k.sb([128, NJ, G], BF16, es, "gT")
        W13 = Rot([k.sb([128, 8, 256], BF16, es, "w13") for _ in range(3)])
        W2 = Rot([k.sb([128, NJ, 128], BF16, es, "w2") for _ in range(2)])
        sil = Rot([k.sb([128, TN], F32, es, "sil") for _ in range(2)])
        tmp = Rot([k.sb([128, TN], F32, es, "ftmp") for _ in range(2)])
        lp = ln_pool(k, es, P[6:8], TN)
        p13 = Rot(P[0:4])
        po = Rot(P[4:6])
        pm = Rot(P[6:8])
        if moe:
            RW = k.sb([128, 8, NEXP], BF16, es, "rw")
            S.dma("pool", RW[:], rw_d.t.rearrange("(k p) e -> p k e", p=128), reads=[rw_d], writes=[RW])
            CWt = k.sb([128, G // 128, NEXP], F32, es, "cw")
            cwT = Rot([k.sb([128, G], F32, es, "cwT") for _ in range(1)])
            rt = {n: k.sb([128, NEXP], F32, es, "r" + n) for n in ["lg", "eq", "l2", "sel", "ex"]}
            rs = {n: k.sb([128, 1], F32, es, "s" + n) for n in ["m1", "m2", "nm1", "sum"]}
            dg = Rot([k.sb([128, 128], F32, es, "dg") for _ in range(2)])

        jobs = []
        for grp in range(2):
            for e_ in range(nexp):
                for jj in range(NJ):
                    jobs.append(("w13", e_, jj))
                for c in range(8):
                    jobs.append(("w2", e_, c))
        loaded = {}
        state = {"next": 0}

        def prefetch(upto):
            while state["next"] < min(upto, len(jobs)):
                i = state["next"]
                kind, e_, x = jobs[i]
                if kind == "w13":
                    w = W13.next()
                    S.dma("pool", w[:], w13_d[e_, x], reads=[w13_d], writes=[w])
                else:
                    w = W2.next()
                    S.dma("pool", w[:], w2_d[e_, x], reads=[w2_d], writes=[w])
                loaded[i] = w
                state["next"] += 1

        pending = []

        def flush_ln():
            while pending:
                gg = pending.pop(0)
                for t in range(G // TN):
                    emit_ln(k, hT, C, gg + t * TN, TN, prm["LNG"], prm["LNB"], 1, es, lp)

        ji = 0
        for grp in range(2):
            g0 = grp * G
            for (lo, hi, j) in segs(g0, G):
                for kk in range(8):
                    S.op("act", lambda e, kk=kk: e.activation(
                        out=u2[:, kk, lo - g0:hi - g0], in_=hT[:, kk, lo:hi], func=AF.Identity,
                        scale=MODX[:, 7, kk, j:j + 1], bias=MODX[:, 3, kk, j:j + 1]), reads=[hT, MODX], writes=[u2])
            S.op("dve", lambda e: e.tensor_scalar(out=hT[:, :, g0:g0 + G], in0=hT[:, :, g0:g0 + G], scalar1=ALPHA,
                                                  scalar2=None, op0=ALU.mult), reads=[hT], writes=[hT])
            if moe:
                for blk in range(G // 128):
                    ps = pm.next()
                    for kk in range(8):
                        S.op("pe", lambda e, kk=kk: e.matmul(ps[:, 0:NEXP], lhsT=u2[:, kk, blk * 128:(blk + 1) * 128],
                                                             rhs=RW[:, kk, :], start=(kk == 0), stop=(kk == 7)),
                             reads=[u2, RW], writes=[ps])
                    lg, eq, l2, sel, ex = rt["lg"], rt["eq"], rt["l2"], rt["sel"], rt["ex"]
                    m1, m2, nm1, sm = rs["m1"], rs["m2"], rs["nm1"], rs["sum"]
                    S.op("dve", lambda e: e.tensor_tensor(out=lg[:], in0=ps[:, 0:NEXP], in1=prm["RB"][:], op=ALU.add),
                         reads=[ps, prm["RB"]], writes=[lg])
                    S.op("dve", lambda e: e.reduce_max(out=m1[:], in_=lg[:], axis=AX.X), reads=[lg], writes=[m1])
                    S.op("dve", lambda e: e.tensor_scalar(out=eq[:], in0=lg[:], scalar1=m1[:, 0:1], scalar2=None,
                                                          op0=ALU.is_equal), reads=[lg, m1], writes=[eq])
                    S.op("dve", lambda e: e.scalar_tensor_tensor(out=l2[:], in0=eq[:], scalar=-1e30, in1=lg[:],
                                                                 op0=ALU.mult, op1=ALU.add), reads=[eq, lg],
                         writes=[l2])
                    S.op("dve", lambda e: e.reduce_max(out=m2[:], in_=l2[:], axis=AX.X), reads=[l2], writes=[m2])
                    S.op("dve", lambda e: e.tensor_scalar(out=sel[:], in0=lg[:], scalar1=m2[:, 0:1], scalar2=None,
                                                          op0=ALU.is_ge), reads=[lg, m2], writes=[sel])
                    S.op("dve", lambda e: e.tensor_scalar_mul(out=nm1[:], in0=m1[:], scalar1=-1.0), reads=[m1],
                         writes=[nm1])
                    S.op("act", lambda e: e.activation(out=ex[:], in_=lg[:], func=AF.Exp, bias=nm1[:, 0:1]),
                         reads=[lg, nm1], writes=[ex])
                    S.op("dve", lambda e: e.tensor_tensor(out=ex[:], in0=ex[:], in1=sel[:], op=ALU.mult),
                         reads=[ex, sel], writes=[ex])
                    S.op("dve", lambda e: e.reduce_sum(out=sm[:], in_=ex[:], axis=AX.X), reads=[ex], writes=[sm])
                    S.op("dve", lambda e: e.reciprocal(out=sm[:], in_=sm[:]), reads=[sm], writes=[sm])
                    S.op("dve", lambda e: e.tensor_scalar(out=CWt[:, blk, :], in0=ex[:], scalar1=sm[:, 0:1],
                                                          scalar2=None, op0=ALU.mult), reads=[ex, sm], writes=[CWt])
            for e_ in range(nexp):
                if moe:
                    cw = cwT.next()
                    for b0 in range(0, G // 128, 4):
                        nb = min(4, G // 128 - b0)
                        ps = pm.next()
                        for bb in range(nb):
                            blk = b0 + bb
                            d_ = dg.next()
                            S.op("dve", lambda e: e.tensor_scalar(out=d_[:], in0=C["ident"][:],
                                                                  scalar1=CWt[:, blk, e_:e_ + 1], scalar2=None,
                                                                  op0=ALU.mult), reads=[C["ident"], CWt], writes=[d_])
                            S.op("pe", lambda e: e.matmul(ps[:, bb * 128:(bb + 1) * 128], lhsT=C["onesf"][:, :],
                                                          rhs=d_[:], start=True, stop=True),
                                 reads=[C["onesf"], d_], writes=[ps])
                        S.op("act", lambda e: e.activation(out=cw[:, b0 * 128:(b0 + nb) * 128], in_=ps[:, 0:nb * 128],
                                                           func=AF.Identity), reads=[ps], writes=[cw])
                for jj in range(NJ):
                    if e_ == 0 and jj == 6:
                        flush_ln()
                    prefetch(ji + 3)
                    w = loaded.pop(ji)
                    ji += 1
                    for t in range(G // TN):
                        p1, p3 = p13.next(), p13.next()
                        for (pp, wo) in [(p1, 0), (p3, 128)]:
                            for kk in range(8):
                                S.op("pe", lambda e, kk=kk: e.matmul(pp[:, 0:TN], lhsT=w[:, kk, wo:wo + 128],
                                                                     rhs=u2[:, kk, t * TN:(t + 1) * TN],
                                                                     start=(kk == 0), stop=(kk == 7)),
                                     reads=[w, u2], writes=[pp])
                        s_ = sil.next()
                        S.op("act", lambda e: e.activation(out=s_[:], in_=p1[:, 0:TN], func=AF.Silu), reads=[p1],
                             writes=[s_])
                        S.op("dve", lambda e: e.tensor_tensor(out=gT[:, jj, t * TN:(t + 1) * TN], in0=s_[:],
                                                              in1=p3[:, 0:TN], op=ALU.mult), reads=[s_, p3],
                             writes=[gT])
                for c in range(8):
                    prefetch(ji + 2)
                    w = loaded.pop(ji)
                    ji += 1
                    for t in range(G // TN):
                        pso = po.next()
                        for jj in range(NJ):
                            S.op("pe", lambda e, jj=jj: e.matmul(pso[:, 0:TN], lhsT=w[:, jj, :],
                                                                 rhs=gT[:, jj, t * TN:(t + 1) * TN], start=(jj == 0),
                                                                 stop=(jj == NJ - 1)), reads=[w, gT], writes=[pso])
                        for (lo, hi, j) in segs(g0 + t * TN, TN):
                            a, b = lo - g0 - t * TN, hi - g0 - t * TN
                            if moe:
                                tp = tmp.next()
                                S.op("dve", lambda e: e.tensor_tensor(out=tp[:, a:b], in0=pso[:, a:b],
                                                                      in1=cw[:, lo - g0:hi - g0], op=ALU.mult),
                                     reads=[pso, cw], writes=[tp])
                                S.op("dve", lambda e: e.scalar_tensor_tensor(
                                    out=hT[:, c, lo:hi], in0=tp[:, a:b], scalar=MODX[:, 5, c, j:j + 1],
                                    in1=hT[:, c, lo:hi], op0=ALU.mult, op1=ALU.add), reads=[tp, MODX, hT],
                                    writes=[hT])
                            else:
                                S.op("dve", lambda e: e.scalar_tensor_tensor(
                                    out=hT[:, c, lo:hi], in0=pso[:, a:b], scalar=MODX[:, 5, c, j:j + 1],
                                    in1=hT[:, c, lo:hi], op0=ALU.mult, op1=ALU.add), reads=[pso, MODX, hT],
                                    writes=[hT])
            pending.append(g0)
        flush_ln()


SMALL = dict(CW=[128, 2, 4], CB=[128, 2], GB=[128, 2, 2, 2], LAM=[128, 2, 2], GW=[128, 2, 2, 2, 128], SEL=[128, 2],
             SINK=[128, 8], DL=[128, 4, 32], DG=[128, 1], PM=[128, 4], LNG=[128, 2, 8], LNB=[128, 2, 8], RB=[128, 8])
VDR = NT // 2
A_OUT = dict(XL0=([128, NT], F32), XL1=([128, NT], F32), GT=([256, NT], F32), QS=([512, NT], BF16),
             KS=([256, NT], BF16), VS=([NT, 256], BF16), QD=([256, NT], BF16), KD=([256, NT], BF16),
             VD0=([VDR, 512], BF16), VD1=([VDR, 512], BF16))


def to_fm(x2d):
    return np.ascontiguousarray(x2d.T.reshape(8, 128, -1).transpose(1, 0, 2))


def from_fm(h):
    return np.ascontiguousarray(h.transpose(1, 0, 2).reshape(D_MODEL, -1).T)


def win_perm():
    lx, lg, sq, sk, sv, dq, dk, dv = 0, 256, 512, 1024, 1152, 1280, 1536, 1792
    cols = []
    cols += list(range(lx, lx + 256))
    cols += list(range(lg, lg + 256))
    cols += list(range(sq, sq + 512))
    sw64 = lambda base, h: [base + h * 64 + ((d + 32) % 64) for d in range(64)]
    for h in range(8):
        cols += sw64(sq, h)
    for hk in range(2):
        cols += list(range(sk + hk * 64, sk + hk * 64 + 64)) * 2
    for hk in range(2):
        cols += sw64(sk, hk) * 2
    sw32 = lambda base: [base + b * 32 + ((d + 16) % 32) for b in range(8) for d in range(32)]
    cols += list(range(dq, dq + 256))
    cols += sw32(dq)
    cols += list(range(dk, dk + 256))
    cols += sw32(dk)
    cols += list(range(sv, sv + 128))
    cols += list(range(dv, dv + 256))
    assert len(cols) == NW
    return np.array(cols)


def rope_tables(half):
    t = np.arange(HALF, dtype=np.float32) + np.float32(half * HALF)
    row = np.floor(t / 64).astype(np.float32)
    col = (t - row * 64).astype(np.float32)
    out = np.zeros((128, 4, HALF), np.float32)
    for (ti, hd) in [(0, 64), (2, 32)]:
        nf = hd // 4
        inv = (np.float32(10000.0) ** (-np.arange(nf, dtype=np.float32) / np.float32(nf))).astype(np.float32)
        ang = np.concatenate([row[:, None] * inv, col[:, None] * inv], -1).astype(np.float32)
        cs, sn = np.cos(ang).astype(np.float32), np.sin(ang).astype(np.float32)
        for p in range(128):
            d = p % hd
            jx = d % (hd // 2)
            out[p, ti] = cs[:, jx]
            out[p, ti + 1] = -sn[:, jx] if d < hd // 2 else sn[:, jx]
    return out


def swa_masks(half):
    m = np.zeros((128, 8, 512), np.float32)
    kk = np.arange(128)[:, None]
    q = np.arange(512)[None, :]
    for r in range(6):
        m[:, r] = (np.abs((r - 1) * 128 + kk - q) <= 128)
    m[:, 6] = m[:, 0] if half == 1 else 0.0
    m[:, 7] = m[:, 5] if half == 0 else 0.0
    return m.astype(NPBF)


def rep(v):
    return np.ascontiguousarray(np.broadcast_to(np.asarray(v, np.float32).reshape(1, -1), (128, np.size(v))))


def small_params(inp, layer, half):
    f = lambda a: np.ascontiguousarray(np.asarray(a, np.float32))
    p = {}
    p["CW"] = f(inp["lru_conv_w"][layer].reshape(4, 2, 128).transpose(2, 1, 0))
    p["CB"] = f(inp["lru_conv_b"][layer].reshape(2, 128).T)
    p["GB"] = f(inp["lru_gate_b"][layer].reshape(2, 2, 2, 128).transpose(3, 2, 0, 1))
    p["LAM"] = f(inp["lru_lam"][layer].reshape(2, 2, 128).transpose(2, 1, 0))
    gw = np.zeros((128, 2, 2, 2, 128), np.float32)
    w = inp["lru_gate_w"][layer]
    for c in range(2):
        for bb in range(2):
            blk = c * 2 + bb
            gw[bb * 64:(bb + 1) * 64, c, :, :, bb * 64:(bb + 1) * 64] = w[:, :, blk].transpose(2, 0, 1, 3)
    p["GW"] = gw
    sel = np.zeros((128, 2), np.float32)
    sel[:, half] = 1.0
    p["SEL"] = sel
    p["SINK"] = rep(inp["swa_sink"][layer])
    p["DL"] = rep(inp["diff_lam"][layer].reshape(-1)).reshape(128, 4, 32)
    p["DG"] = f(np.tile(inp["diff_norm_g"][layer], 2).reshape(128, 1))
    pm = np.zeros((128, 4), np.float32)
    for hp in range(2):
        for m in range(2):
            pm[:, hp * 2 + m] = ((np.arange(128) // 64) == hp) & (((np.arange(128) % 64) // 32) == m)
    p["PM"] = pm
    p["LNG"] = f(inp["ln_g"][layer].reshape(2, 8, 128).transpose(2, 0, 1))
    p["LNB"] = f(inp["ln_b"][layer].reshape(2, 8, 128).transpose(2, 0, 1))
    if layer % 2 == 1:
        p["RB"] = rep(inp["moe_router_b"][layer // 2])
    else:
        p["RB"] = np.zeros((128, 8), np.float32)
    return p


def ffn_layout(w1, w3, w2):
    E = w1.shape[0]
    a = w1.reshape(E, 8, 128, NJ, 128).transpose(0, 3, 2, 1, 4)
    b = w3.reshape(E, 8, 128, NJ, 128).transpose(0, 3, 2, 1, 4)
    w13 = np.ascontiguousarray(np.concatenate([a, b], axis=-1))
    w2r = np.ascontiguousarray(w2.reshape(E, NJ, 128, 8, 128).transpose(0, 3, 2, 1, 4))
    return w13, w2r


_PROGS = {}


def _prog(key, fn):
    if key not in _PROGS:
        _PROGS[key] = fn()
    return _PROGS[key]


PAIRS = [[0, 1], [2, 3], [4, 5], [6, 7]]
PUB = ["XL0", "XL1", "KS", "VS", "KD", "VD0", "VD1"]


def build_fused(depth=DEPTH):
    k = K()
    S = k.S
    nc = k.nc
    I = lambda n, s, dt: k.dram(n, s, dt, "ExternalInput")
    hT_d = I("hT", [128, 8, NT], F32)
    cc_d = I("cc", [128, 8, 2], F32)
    cst_d = I("ident", [128, 128], F32)
    rope_d = I("rope", [128, 4, HALF], F32)
    msk_d = I("MSK", [128, 8, 512], BF16)
    L = []
    for l in range(depth):
        moe = l % 2 == 1
        nexp = NEXP if moe else 1
        d = dict(adaw=I(f"adaw{l}", [D_MODEL, 6 * D_MODEL], F32), adab=I(f"adab{l}", [128, 48], F32),
                 win=I(f"win{l}", [D_MODEL, NW], F32), wout=I(f"wout{l}", [D_MODEL, D_MODEL], F32),
                 w13=I(f"w13_{l}", [nexp, NJ, 128, 8, 256], F32), w2=I(f"w2_{l}", [nexp, 8, 128, NJ, 128], F32),
                 rw=I(f"rw{l}", [D_MODEL, NEXP], F32) if moe else None,
                 sm={n: I(f"{n}{l}", s_, F32) for n, s_ in SMALL.items()})
        L.append(d)
    out_d = k.dram("hout", [128, 8, NT], F32, "ExternalOutput")
    scr = []
    for par in range(2):
        o = {n: Tile(nc.dram_tensor(f"{n}_{par}", list(sh), dt).ap()) for n, (sh, dt) in A_OUT.items()}
        gth = {n: Tile(nc.dram_tensor(f"{n}G_{par}", [2 * A_OUT[n][0][0], A_OUT[n][0][1]], A_OUT[n][1]).ap())
               for n in PUB}
        scr.append((o, gth))
    with k.es:
        hT = k.sb([128, 8, NT], F32, name="hT")
        MODX = k.sb([128, 8, 8, 2], F32, name="modx")
        S.dma("sp", hT[:], hT_d[:, :, :], reads=[hT_d], writes=[hT])
        C = emit_consts(k, cst_d)
        prm = {n: k.sb(s_, F32, name=n) for n, s_ in SMALL.items()}
        for l in range(depth):
            moe = l % 2 == 1
            lam_init = 0.8 - 0.6 * math.exp(-0.3 * l)
            o, gth = scr[l % 2]
            emit_mods(k, cc_d, L[l]["adaw"], L[l]["adab"], MODX)
            emit_phaseA(k, hT, MODX, L[l]["win"], rope_d, o)
            for n in PUB:
                S.coll("AllGather", PAIRS, o[n], gth[n])
            for n in SMALL:
                S.dma("sp", prm[n][:], L[l]["sm"][n].t, reads=[L[l]["sm"][n]], writes=[prm[n]])

            def G(n):
                t = Tile(gth[n].t.rearrange("(h r) n -> h r n", h=2))
                t.b = gth[n].b
                return t
            g = dict(XG=[G("XL0"), G("XL1")], GT=o["GT"], QS=o["QS"], KSO=o["KS"], KSG=G("KS"), VSO=o["VS"],
                     VSG=G("VS"), QD=o["QD"], KDO=o["KD"], KDG=G("KD"), VDO=o["VD0"], VDG=[G("VD0"), G("VD1")],
                     MSK=msk_d)
            with k.scope() as es:
                mixT = k.sb([128, 8, NT], BF16, es, "mixT")
                emit_lru(k, mixT, C, g, prm)
                emit_swa(k, mixT, C, g, prm)
                emit_diff(k, mixT, C, g, prm, lam_init)
                emit_phaseC(k, hT, mixT, MODX, C, L[l]["wout"], prm)
            emit_ffn(k, hT, MODX, C, prm, L[l]["w13"], L[l]["w2"], NEXP if moe else 1, L[l]["rw"])
        S.dma("sp", out_d[:, :, :], hT[:], reads=[hT], writes=[out_d])
        S.finish()
    return k.nc


def fused_inputs(inp, depth=DEPTH):
    x, c, ctx, c_ctx = inp["x"], inp["c"], inp["ctx"], inp["c_ctx"]
    cores = [(b, hf) for b in range(BATCH) for hf in range(2)]
    perm = win_perm()
    ropes = [rope_tables(hf) for hf in range(2)]
    masks = [swa_masks(hf) for hf in range(2)]
    shared = dict(ident=np.eye(128, dtype=np.float32))
    for l in range(depth):
        j = l // 2
        shared[f"adaw{l}"] = np.ascontiguousarray(inp["ada_w"][l])
        shared[f"adab{l}"] = np.ascontiguousarray(inp["ada_b"][l].reshape(48, 128).T)
        shared[f"win{l}"] = np.ascontiguousarray(inp["w_in"][l][:, perm])
        shared[f"wout{l}"] = np.ascontiguousarray(inp["w_out"][l])
        if l % 2 == 0:
            w13, w2r = ffn_layout(inp["ffn_w1"][j][None], inp["ffn_w3"][j][None], inp["ffn_w2"][j][None])
        else:
            w13, w2r = ffn_layout(inp["moe_w1"][j], inp["moe_w3"][j], inp["moe_w2"][j])
            shared[f"rw{l}"] = np.ascontiguousarray(inp["moe_router_w"][j])
        shared[f"w13_{l}"] = w13
        shared[f"w2_{l}"] = w2r
    in_maps = []
    for (b, hf) in cores:
        m = dict(shared)
        toks = np.concatenate([ctx[b], x[b, hf * HALF:(hf + 1) * HALF]], 0)
        m["hT"] = to_fm(toks)
        m["cc"] = np.ascontiguousarray(np.stack([c[b].reshape(8, 128).T, c_ctx.reshape(8, 128).T], -1))
        m["rope"] = ropes[hf]
        m["MSK"] = masks[hf]
        for l in range(depth):
            for n, v in small_params(inp, l, hf).items():
                m[f"{n}{l}"] = v
        in_maps.append(m)
    return cores, in_maps


def kernel(**inp):
    inp = {k_: np.asarray(v) for k_, v in inp.items()}
    cores, in_maps = fused_inputs(inp)
    res = run_bass_kernel_spmd(_prog("fused", build_fused), in_maps, core_ids=list(range(NCORES))).results
    out = np.zeros((BATCH, SEQ, D_MODEL), np.float32)
    for ci, (b, hf) in enumerate(cores):
        out[b, hf * HALF:(hf + 1) * HALF] = from_fm(np.asarray(res[ci]["hout"]))[CTX:]
    return out
```

```python
import math
from contextlib import ExitStack

import numpy as np
import ml_dtypes

import concourse.bass as bass
import concourse.mybir as mybir
from concourse.bass_utils import run_bass_kernel_spmd

F32 = mybir.dt.float32
BF16 = mybir.dt.bfloat16
AF = mybir.ActivationFunctionType
ALU = mybir.AluOpType
AX = mybir.AxisListType
NPBF = ml_dtypes.bfloat16

D_MODEL = 1024
BATCH = 4
SEQ = 4096
DEPTH = 4
CTX = 256
HALF = SEQ // 2
NT = CTX + HALF
D_FF = 2816
NJ = D_FF // 128
NEXP = 8
LN_EPS = 1e-5
ALPHA = (2.0 * DEPTH) ** 0.25
NW = 24 * 128 + 384
NCORES = 8


class Buf:
    __slots__ = ("w", "r", "excl")

    def __init__(self, excl=False):
        self.w = None
        self.r = {}
        self.excl = excl


class Tile:
    def __init__(self, t, excl=False):
        self.t = t
        self.b = Buf(excl)

    def __getitem__(self, idx):
        return self.t[idx]


class Sched:
    EPOCH = 16000
    NDMA = 28

    def __init__(self, nc, es):
        self.nc, self.es = nc, es
        self.eng = dict(pe=nc.tensor, act=nc.scalar, dve=nc.vector, pool=nc.gpsimd, sp=nc.sync)
        self.cnt = {}
        self.sem = {}
        self.own = {e: set() for e in self.eng}
        self.nsem = 0
        self.waited = {e: {} for e in self.eng}
        self.dsem = []
        self.dcnt = []
        self.dn = 0
        self.allsems = []
        self.csem = None
        self.ccnt = 0

    def _newsem(self, name):
        self.nsem += 1
        s = self.es.enter_context(self.nc.semaphore(f"{name}_{self.nsem}"))
        self.allsems.append(s)
        return s

    def _wait(self, e, deps):
        w = self.waited[e]
        for sem, val in deps:
            if w.get(sem, 0) < val:
                self.eng[e].wait_ge(sem, val)
                w[sem] = val

    def _deps(self, e, reads, writes):
        deps = []
        own = self.own[e]
        for b in reads:
            if b.w is not None:
                deps.append(b.w)
            if b.excl:
                deps.extend(b.r.items())
        for b in writes:
            if b.w is not None:
                deps.append(b.w)
            deps.extend(x for x in b.r.items() if x[0] not in own)
        if e == "pe":
            deps = [d for d in deps if d[0] not in own]
        return deps

    def _mark(self, tok, reads, writes):
        for b in reads:
            if b.excl:
                b.w = tok
                b.r = {}
            else:
                b.r[tok[0]] = tok[1]
        for b in writes:
            b.w = tok
            b.r = {}

    def op(self, e, fn, reads=(), writes=()):
        reads = [x.b if isinstance(x, Tile) else x for x in reads]
        writes = [x.b if isinstance(x, Tile) else x for x in writes]
        self._wait(e, self._deps(e, reads, writes))
        ins = fn(self.eng[e])
        if self.cnt.get(e, self.EPOCH) >= self.EPOCH:
            self.sem[e] = self._newsem(e)
            self.cnt[e] = 0
            self.own[e].add(self.sem[e])
        self.cnt[e] += 1
        ins.then_inc(self.sem[e], 1)
        tok = (self.sem[e], self.cnt[e])
        self._mark(tok, reads, writes)
        return tok

    def dma(self, q, out, in_, reads=(), writes=()):
        reads = [x.b if isinstance(x, Tile) else x for x in reads]
        writes = [x.b if isinstance(x, Tile) else x for x in writes]
        self._wait(q, self._deps(q, reads, writes))
        i = self.dn % self.NDMA
        self.dn += 1
        if i >= len(self.dsem):
            self.dsem.append(self._newsem("d"))
            self.dcnt.append(0)
        sem = self.dsem[i]
        if self.dcnt[i] > 0:
            self._wait(q, [(sem, self.dcnt[i])])
        self.dcnt[i] += 16
        self.eng[q].dma_start(out=out, in_=in_).then_inc(sem, 16)
        tok = (sem, self.dcnt[i])
        self._mark(tok, reads, writes)
        return tok

    def coll(self, kind, groups, src, dst):
        q = "pool"
        reads, writes = [src.b], [dst.b]
        self._wait(q, self._deps(q, reads, writes))
        if self.csem is None:
            self.csem = self._newsem("cc")
            self.ccnt = 0
        self.ccnt += 1
        self.nc.gpsimd.collective_compute(kind, ALU.bypass, replica_groups=groups, ins=[src.t.opt()],
                                          outs=[dst.t.opt()]).then_inc(self.csem, 1)
        tok = (self.csem, self.ccnt)
        self._mark(tok, reads, writes)
        return tok

    def barrier(self):
        deps = [(s, c) for s, c in zip(self.dsem, self.dcnt) if c > 0]
        deps += [(self.sem[e], self.cnt[e]) for e in self.sem]
        if self.csem is not None:
            deps.append((self.csem, self.ccnt))
        for e in self.eng:
            self._wait(e, deps)

    def finish(self):
        deps = [(s, c) for s, c in zip(self.dsem, self.dcnt) if c > 0]
        deps += [(self.sem[e], self.cnt[e]) for e in self.sem]
        if self.csem is not None:
            deps.append((self.csem, self.ccnt))
        self._wait("sp", deps)


class K:
    def __init__(self):
        self.nc = bass.Bass("TRN2", target_bir_lowering=False)
        self.es = ExitStack()
        self.S = Sched(self.nc, self.es)
        self.n = 0
        self.psum = None

    def dram(self, name, shape, dt, kind):
        t = self.nc.dram_tensor(name, list(shape), dt, kind=kind).ap()
        return Tile(t)

    def sb(self, shape, dt, es=None, name=None):
        self.n += 1
        t = (es or self.es).enter_context(self.nc.sbuf_tensor(f"{name or 't'}_{self.n}", list(shape), dt))
        return Tile(t)

    def scope(self):
        k = self

        class _Scope(ExitStack):
            def __exit__(self, *a):
                k.S.barrier()
                return super().__exit__(*a)
        return _Scope()

    def banks(self):
        if self.psum is None:
            self.psum, self.pairs = [], []
            for i in range(4):
                t = self.es.enter_context(self.nc.psum_tensor(f"pp{i}", [128, 1024], F32))
                self.pairs.append(Tile(t, excl=True))
                self.psum.append(Tile(t[:, 0:512], excl=True))
                self.psum.append(Tile(t[:, 512:1024], excl=True))
        return self.psum


class Rot:
    def __init__(self, items):
        self.items = items
        self.i = 0

    def next(self):
        x = self.items[self.i % len(self.items)]
        self.i += 1
        return x


def segs(c0, n):
    out = []
    if c0 < CTX:
        hi = min(CTX, c0 + n)
        out.append((c0, hi, 1))
        if c0 + n > CTX:
            out.append((CTX, c0 + n, 0))
    else:
        out.append((c0, c0 + n, 0))
    return out


def emit_consts(k, cst_d):
    S = k.S
    c = {}
    c["ident"] = k.sb([128, 128], F32, name="ident")
    S.dma("sp", c["ident"][:], cst_d[:, :], reads=[cst_d], writes=[c["ident"]])
    c["onesf"] = k.sb([128, 128], F32, name="onesf")
    S.op("dve", lambda e: e.memset(c["onesf"][:], 1.0), writes=[c["onesf"]])
    c["onesb"] = k.sb([128, 128], BF16, name="onesb")
    S.op("dve", lambda e: e.memset(c["onesb"][:], 1.0), writes=[c["onesb"]])
    c["eps"] = k.sb([128, 1], F32, name="eps")
    S.op("dve", lambda e: e.memset(c["eps"][:], LN_EPS), writes=[c["eps"]])
    return c


def emit_mods(k, cc_d, adaw_d, adab_d, MODX):
    S = k.S
    P = k.banks()
    with k.scope() as es:
        cc = k.sb([128, 8, 2], F32, es)
        sl = k.sb([128, 8, 2], BF16, es)
        adab = k.sb([128, 48], F32, es)
        wts = Rot([k.sb([128, 8, 512], BF16, es) for _ in range(3)])
        S.dma("sp", cc[:], cc_d[:, :, :], reads=[cc_d], writes=[cc])
        S.dma("sp", adab[:], adab_d[:, :], reads=[adab_d], writes=[adab])
        S.op("act", lambda e: e.activation(out=sl[:], in_=cc[:], func=AF.Silu), reads=[cc], writes=[sl])
        ps = P[0]
        wsrc = adaw_d.t.rearrange("(k p) n -> p k n", p=128)
        for piece in range(12):
            wt = wts.next()
            S.dma("pool", wt[:], wsrc[:, :, piece * 512:(piece + 1) * 512], reads=[adaw_d], writes=[wt])
            for m in range(4):
                ma = piece * 4 + m
                for kk in range(8):
                    S.op("pe", lambda e, wt=wt, m=m, kk=kk, ma=ma: e.matmul(
                        ps[:, ma * 2:ma * 2 + 2], lhsT=wt[:, kk, m * 128:(m + 1) * 128], rhs=sl[:, kk, :],
                        start=(kk == 0), stop=(kk == 7)), reads=[wt, sl], writes=[ps])
        ps3 = ps[:, 0:96].rearrange("p (m j) -> p m j", j=2)
        mx = MODX[:, 0:6, :, :].rearrange("p w c j -> p (w c) j")
        for j in range(2):
            S.op("dve", lambda e, j=j: e.tensor_tensor(out=mx[:, :, j], in0=ps3[:, :, j], in1=adab[:, :], op=ALU.add),
                 reads=[ps, adab], writes=[MODX])
        S.op("dve", lambda e: e.tensor_scalar_add(out=MODX[:, 6, :, :], in0=MODX[:, 1, :, :], scalar1=1.0),
             reads=[MODX], writes=[MODX])
        S.op("dve", lambda e: e.tensor_scalar_add(out=MODX[:, 7, :, :], in0=MODX[:, 4, :, :], scalar1=1.0),
             reads=[MODX], writes=[MODX])


def emit_phaseA(k, hT, MODX, win_d, rope_d, o):
    S = k.S
    P = k.banks()
    with k.scope() as es:
        WIN = k.sb([128, 8, NW], BF16, es, "win")
        wsrc = win_d.t.rearrange("(k p) n -> p k n", p=128)
        for pc in range(4):
            lo, hi = pc * (NW // 4), (pc + 1) * (NW // 4)
            S.dma("pool", WIN[:, :, lo:hi], wsrc[:, :, lo:hi], reads=[win_d], writes=[WIN])
        ub = Rot([k.sb([128, 8, 512], BF16, es, "u") for _ in range(2)])
        rt = Rot([k.sb([128, 4, 512], F32, es, "rope") for _ in range(2)])
        stf = Rot([k.sb([128, 512], F32, es, "stf") for _ in range(3)])
        stb = Rot([k.sb([128, 512], BF16, es, "stb") for _ in range(3)])
        tm1 = Rot([k.sb([128, 512], F32, es, "tm1") for _ in range(2)])
        tm2 = Rot([k.sb([128, 512], F32, es, "tm2") for _ in range(2)])
        vss = Rot([k.sb([128, 2, 128], BF16, es, "vss") for _ in range(2)])
        vds = Rot([k.sb([128, 4, 128], BF16, es, "vds") for _ in range(2)])
        for v in vss.items + vds.items:
            S.op("dve", lambda e, v=v: e.memset(v[:], 1.0), writes=[v])
        pb = Rot(P)

        tiles = [(0, 256, 1)] + [(CTX + i * 512, 512, 0) for i in range(4)]
        for (c0, N, j) in tiles:
            lat = j == 0
            u = ub.next()
            for kk in range(8):
                S.op("act", lambda e, kk=kk: e.activation(
                    out=u[:, kk, 0:N], in_=hT[:, kk, c0:c0 + N], func=AF.Identity,
                    scale=MODX[:, 6, kk, j:j + 1], bias=MODX[:, 0, kk, j:j + 1]), reads=[hT, MODX], writes=[u])
            if lat:
                R = rt.next()
                S.dma("sp", R[:], rope_d[:, :, c0 - CTX:c0 - CTX + 512], reads=[rope_d], writes=[R])

            def proj(ch, ps):
                for kk in range(8):
                    S.op("pe", lambda e, kk=kk: e.matmul(
                        ps[:, 0:N], lhsT=WIN[:, kk, ch * 128:(ch + 1) * 128], rhs=u[:, kk, 0:N],
                        start=(kk == 0), stop=(kk == 7)), reads=[WIN, u], writes=[ps])

            for c in range(2):
                ps = pb.next()
                proj(c, ps)
                st = stf.next()
                S.op("act", lambda e: e.activation(out=st[:, 0:N], in_=ps[:, 0:N], func=AF.Identity),
                     reads=[ps], writes=[st])
                S.dma("sp", o["XL%d" % c][:, c0:c0 + N], st[:, 0:N], reads=[st], writes=[o["XL%d" % c]])
            for c in range(2):
                ps = pb.next()
                proj(2 + c, ps)
                st = stf.next()
                S.op("act", lambda e: e.activation(out=st[:, 0:N], in_=ps[:, 0:N], func=AF.Gelu_apprx_tanh),
                     reads=[ps], writes=[st])
                S.dma("sp", o["GT"][c * 128:(c + 1) * 128, c0:c0 + N], st[:, 0:N], reads=[st], writes=[o["GT"]])
            for (nb, sb_, n, tc, dst) in [(4, 8, 4, 0, "QS"), (12, 14, 2, 0, "KS"), (16, 18, 2, 2, "QD"),
                                          (20, 22, 2, 2, "KD")]:
                for i in range(n):
                    psA = pb.next()
                    proj(nb + i, psA)
                    st = stb.next()
                    if lat:
                        psB = pb.next()
                        proj(sb_ + i, psB)
                        t1, t2 = tm1.next(), tm2.next()
                        S.op("dve", lambda e: e.tensor_tensor(out=t1[:], in0=psA[:, :], in1=R[:, tc, :], op=ALU.mult),
                             reads=[psA, R], writes=[t1])
                        S.op("dve", lambda e: e.tensor_tensor(out=t2[:], in0=psB[:, :], in1=R[:, tc + 1, :],
                                                              op=ALU.mult), reads=[psB, R], writes=[t2])
                        S.op("pool", lambda e: e.tensor_tensor(out=st[:], in0=t1[:], in1=t2[:], op=ALU.add),
                             reads=[t1, t2], writes=[st])
                    else:
                        S.op("act", lambda e: e.activation(out=st[:, 0:N], in_=psA[:, 0:N], func=AF.Identity),
                             reads=[psA], writes=[st])
                    S.dma("sp", o[dst][i * 128:(i + 1) * 128, c0:c0 + N], st[:, 0:N], reads=[st], writes=[o[dst]])
            for blk in range(N // 128):
                ps = pb.next()
                for kk in range(8):
                    S.op("pe", lambda e, kk=kk: e.matmul(
                        ps[:, 0:384], lhsT=u[:, kk, blk * 128:(blk + 1) * 128], rhs=WIN[:, kk, 3072:3456],
                        start=(kk == 0), stop=(kk == 7)), reads=[WIN, u], writes=[ps])
                vs, vd = vss.next(), vds.next()
                S.op("act", lambda e: e.activation(out=vs[:, :, 0:64],
                                                   in_=ps[:, 0:128].rearrange("p (h d) -> p h d", d=64),
                                                   func=AF.Identity), reads=[ps], writes=[vs])
                S.op("dve", lambda e: e.tensor_copy(out=vd[:, :, 0:64],
                                                    in_=ps[:, 128:384].rearrange("p (h d) -> p h d", d=64)),
                     reads=[ps], writes=[vd])
                r0 = c0 + blk * 128
                S.dma("sp", o["VS"][r0:r0 + 128, :], vs[:].rearrange("p h d -> p (h d)"), reads=[vs],
                      writes=[o["VS"]])
                vch, vr = r0 // VDR, r0 % VDR
                S.dma("sp", o["VD%d" % vch][vr:vr + 128, :], vd[:].rearrange("p h d -> p (h d)"), reads=[vd],
                      writes=[o["VD%d" % vch]])


def emit_lru(k, mixT, C, g, prm):
    S = k.S
    P = k.banks()
    with k.scope() as es:
        xs = k.sb([128, 1 + SEQ + 2], F32, es, "xs")
        xc = k.sb([128, 1 + CTX + 2], F32, es, "xc")
        U = k.sb([128, CTX + SEQ], F32, es, "U")
        OUT = k.sb([128, NT], F32, es, "lout")
        GTs = k.sb([128, NT], F32, es, "gts")
        tR = Rot([k.sb([128, 512], F32, es, "tR") for _ in range(4)])
        tI = Rot([k.sb([128, 512], F32, es, "tI") for _ in range(4)])
        tA = Rot([k.sb([128, 512], F32, es, "tA") for _ in range(4)])
        tH = Rot([k.sb([128, 512], F32, es, "tH") for _ in range(3)])
        sp = k.sb([128, 4], F32, es, "sp")
        nsp8 = k.sb([128, 2, 2], F32, es, "nsp8")
        nsp16 = k.sb([128, 2, 2], F32, es, "nsp16")
        lam = prm["LAM"]
        spv = sp[:, 0:4]
        lamv = lam[:].rearrange("p c d -> p (c d)")
        S.op("act", lambda e: e.activation(out=spv, in_=lamv, func=AF.Exp, scale=-1.0), reads=[lam], writes=[sp])
        S.op("dve", lambda e: e.tensor_scalar_add(out=spv, in0=spv, scalar1=1.0), reads=[sp], writes=[sp])
        S.op("act", lambda e: e.activation(out=spv, in_=spv, func=AF.Ln), reads=[sp], writes=[sp])
        S.op("dve", lambda e: e.tensor_scalar_mul(out=nsp8[:].rearrange("p c d -> p (c d)"), in0=spv, scalar1=-8.0),
             reads=[sp], writes=[nsp8])
        S.op("dve", lambda e: e.tensor_scalar_mul(out=nsp16[:].rearrange("p c d -> p (c d)"), in0=spv, scalar1=-16.0),
             reads=[sp], writes=[nsp16])
        pg = Rot(P[0:8])
        CW, CB, GB, GW, SEL = prm["CW"], prm["CB"], prm["GB"], prm["GW"], prm["SEL"]
        for c in range(2):
            rows = slice(c * 128, (c + 1) * 128)
            S.op("dve", lambda e: e.memset(xs[:, 0:1], 0.0), writes=[xs])
            S.op("dve", lambda e: e.memset(xs[:, 1 + SEQ:3 + SEQ], 0.0), writes=[xs])
            S.op("dve", lambda e: e.memset(xc[:, 0:1], 0.0), writes=[xc])
            S.op("dve", lambda e: e.memset(xc[:, 1 + CTX:3 + CTX], 0.0), writes=[xc])
            XGc = g["XG"][c]
            S.dma("sp", xs[:, 1:1 + HALF], XGc[0, :, CTX:NT], reads=[XGc], writes=[xs])
            S.dma("sp", xs[:, 1 + HALF:1 + SEQ], XGc[1, :, CTX:NT], reads=[XGc], writes=[xs])
            S.dma("sp", xc[:, 1:1 + CTX], XGc[0, :, 0:CTX], reads=[XGc], writes=[xc])
            S.dma("sp", GTs[:], g["GT"][rows, :], reads=[g["GT"]], writes=[GTs])
            for (src, L, d0) in [(xc, CTX, 0), (xs, SEQ, CTX)]:
                S.op("act", lambda e: e.activation(out=U[:, d0:d0 + L], in_=src[:, 1:1 + L], func=AF.Identity,
                                                   scale=CW[:, c, 1:2], bias=CB[:, c:c + 1]),
                     reads=[src, CW, CB], writes=[U])
                for (off, wi) in [(0, 0), (2, 2), (3, 3)]:
                    S.op("dve", lambda e, off=off, wi=wi: e.scalar_tensor_tensor(
                        out=U[:, d0:d0 + L], in0=src[:, off:off + L], scalar=CW[:, c, wi:wi + 1], in1=U[:, d0:d0 + L],
                        op0=ALU.mult, op1=ALU.add), reads=[src, CW, U], writes=[U])
            S.op("dve", lambda e: e.memset(OUT[:], 0.0), writes=[OUT])
            for d in range(2):
                lat = [(CTX + i * 512, 512, i) for i in range(8)]
                if d == 1:
                    lat = lat[::-1]
                state = None
                hprev = None
                seq = [(0, CTX, -1)] + lat
                for grp_ in [seq[0:1]] + [seq[i_:i_ + 2] for i_ in range(1, 9, 2)]:
                    items = []
                    for (u0, L, kind) in grp_:
                        pss = []
                        for gi in range(2):
                            ps = pg.next()
                            S.op("pe", lambda e: e.matmul(ps[:, 0:L], lhsT=GW[:, c, d, gi, :], rhs=U[:, u0:u0 + L],
                                                          start=True, stop=True), reads=[GW, U], writes=[ps])
                            pss.append(ps)
                        r, ii, a, h = tR.next(), tI.next(), tA.next(), tH.next()
                        S.op("act", lambda e: e.activation(out=r[:, 0:L], in_=pss[0][:, 0:L], func=AF.Sigmoid,
                                                           bias=GB[:, c, d, 0:1]), reads=[pss[0], GB], writes=[r])
                        S.op("act", lambda e: e.activation(out=ii[:, 0:L], in_=pss[1][:, 0:L], func=AF.Sigmoid,
                                                           bias=GB[:, c, d, 1:2]), reads=[pss[1], GB], writes=[ii])
                        items.append((u0, L, kind, r, ii, a, h))
                    for (u0, L, kind, r, ii, a, h) in items:
                        S.op("act", lambda e: e.activation(out=a[:, 0:L], in_=r[:, 0:L], func=AF.Exp,
                                                           scale=nsp8[:, c, d:d + 1]), reads=[r, nsp8], writes=[a])
                        S.op("pool", lambda e: e.tensor_tensor(out=r[:, 0:L], in0=a[:, 0:L], in1=a[:, 0:L],
                                                               op=ALU.mult), reads=[a], writes=[r])
                    for (u0, L, kind, r, ii, a, h) in items:
                        S.op("act", lambda e: e.activation(out=r[:, 0:L], in_=r[:, 0:L], func=AF.Sqrt, scale=-1.0,
                                                           bias=1.0), reads=[r], writes=[r])
                    for (u0, L, kind, r, ii, a, h) in items:
                        S.op("dve", lambda e: e.tensor_tensor(out=ii[:, 0:L], in0=ii[:, 0:L], in1=U[:, u0:u0 + L],
                                                              op=ALU.mult), reads=[ii, U], writes=[ii])
                        S.op("dve", lambda e: e.tensor_tensor(out=ii[:, 0:L], in0=ii[:, 0:L], in1=r[:, 0:L],
                                                              op=ALU.mult), reads=[ii, r], writes=[ii])
                        if d == 0:
                            vo, va, vb = h[:, 0:L], a[:, 0:L], ii[:, 0:L]
                        else:
                            vo, va, vb = h[:, L - 1::-1], a[:, L - 1::-1], ii[:, L - 1::-1]
                        init = 0.0 if state is None else state
                        rd = [a, ii] + ([hprev] if hprev is not None else [])
                        S.op("dve", lambda e: e.tensor_tensor_scan(out=vo, data0=va, data1=vb, initial=init,
                                                                   op0=ALU.mult, op1=ALU.add), reads=rd, writes=[h])
                        state = h[:, L - 1:L] if d == 0 else h[:, 0:1]
                        hprev = h
                        if kind < 0:
                            S.op("dve", lambda e: e.tensor_tensor(out=OUT[:, 0:CTX], in0=h[:, 0:L], in1=OUT[:, 0:CTX],
                                                                  op=ALU.add), reads=[h, OUT], writes=[OUT])
                        else:
                            hf = kind // 4
                            lo = CTX + (kind % 4) * 512
                            S.op("dve", lambda e: e.scalar_tensor_tensor(
                                out=OUT[:, lo:lo + L], in0=h[:, 0:L], scalar=SEL[:, hf:hf + 1], in1=OUT[:, lo:lo + L],
                                op0=ALU.mult, op1=ALU.add), reads=[h, OUT, SEL], writes=[OUT])
            S.op("pool", lambda e: e.tensor_tensor(out=mixT[:, c, :], in0=OUT[:], in1=GTs[:], op=ALU.mult),
                 reads=[OUT, GTs], writes=[mixT])


def pipeline(n, front, back, la, deferred):
    for i in range(n + la):
        if i < n:
            front(i)
        if i >= la:
            back(i - la)
        while deferred and deferred[0][0] <= i:
            deferred.pop(0)[1]()
    while deferred:
        deferred.pop(0)[1]()


def emit_swa(k, mixT, C, g, prm):
    S = k.S
    P = k.banks()
    LA = 5
    with k.scope() as es:
        KS = [[k.sb([128, CTX + 128 + HALF + 128], BF16, es, "ks") for _ in range(2)] for _ in range(2)]
        VSa = k.sb([128, 20, 256], BF16, es, "vsa")
        MSK = k.sb([128, 8, 512], BF16, es, "msk")
        ESK = k.sb([128, 8], F32, es, "esk")
        qb = Rot([k.sb([128, 4, 512], BF16, es, "qs") for _ in range(2)])
        Eb = Rot([k.sb([128, 512], BF16, es, "E") for _ in range(6)])
        den = Rot([k.sb([128, 512], F32, es, "den") for _ in range(2)])
        S.dma("sp", MSK[:], g["MSK"][:, :, :], reads=[g["MSK"]], writes=[MSK])
        S.op("act", lambda e: e.activation(out=ESK[:], in_=prm["SINK"][:], func=AF.Exp), reads=[prm["SINK"]],
             writes=[ESK])
        for hk in range(2):
            for hp in range(2):
                T_ = KS[hk][hp]
                ps_ = slice(hp * 64, hp * 64 + 64)
                zs_ = slice((1 - hp) * 64, (1 - hp) * 64 + 64)
                rows = slice(hk * 128 + hp * 64, hk * 128 + hp * 64 + 64)
                S.op("dve", lambda e: e.memset(T_[zs_, :], 0.0), writes=[T_])
                S.dma("sp", T_[ps_, 0:CTX], g["KSO"][rows, 0:CTX], reads=[g["KSO"]], writes=[T_])
                S.dma("sp", T_[ps_, CTX + 128:CTX + 128 + HALF], g["KSO"][rows, CTX:NT], reads=[g["KSO"]],
                      writes=[T_])
                S.dma("sp", T_[ps_, CTX:CTX + 128], g["KSG"][0, rows, NT - 128:NT], reads=[g["KSG"]], writes=[T_])
                S.dma("sp", T_[ps_, CTX + 128 + HALF:], g["KSG"][1, rows, CTX:CTX + 128], reads=[g["KSG"]],
                      writes=[T_])
        vo = g["VSO"].t.rearrange("(b p) n -> p b n", p=128)
        S.dma("sp", VSa[:, 0:2, :], vo[:, 0:2, :], reads=[g["VSO"]], writes=[VSa])
        S.dma("sp", VSa[:, 3:19, :], vo[:, 2:18, :], reads=[g["VSO"]], writes=[VSa])
        S.dma("sp", VSa[:, 2, :], g["VSG"][0, NT - 128:NT, :], reads=[g["VSG"]], writes=[VSa])
        S.dma("sp", VSa[:, 19, :], g["VSG"][1, CTX:CTX + 128, :], reads=[g["VSG"]], writes=[VSa])
        accs = Rot(P[0:2])
        scs = Rot(P[2:8])
        qsrc = g["QS"].t.rearrange("(c p) n -> p c n", p=128)
        tiles = [-1, 0, 1, 2, 3]
        Qt = {}

        def load_q(t):
            N = CTX if t < 0 else 512
            c0 = 0 if t < 0 else CTX + t * 512
            Q = qb.next()
            S.dma("sp", Q[:, :, 0:N], qsrc[:, :, c0:c0 + N], reads=[g["QS"]], writes=[Q])
            Qt[t] = Q

        units = []
        for ti, t in enumerate(tiles):
            N = CTX if t < 0 else 512
            c0 = 0 if t < 0 else CTX + t * 512
            blocks = [(0, 0, None), (1, 128, None)]
            if t >= 0:
                for r in range(6):
                    mk = r
                    if t == 0 and r == 0:
                        mk = 6
                    if t == 3 and r == 5:
                        mk = 7
                    blocks.append((2 + 4 * t + r, CTX + (4 * t + r) * 128, mk))
            for h in range(8):
                for bi, blk in enumerate(blocks):
                    units.append((ti, t, N, c0, h, bi, len(blocks), blk))
        Ef = {}
        cur = {}
        load_q(tiles[0])

        def front(i):
            ti, t, N, c0, h, bi, nb, (vb, kcol, mk) = units[i]
            if h == 0 and bi == 0 and ti + 1 < len(tiles):
                load_q(tiles[ti + 1])
            Q = Qt[t]
            qc, pb, hk = h // 2, (h % 2) * 64, h // 4
            sps, E = scs.next(), Eb.next()
            S.op("pe", lambda e: e.matmul(sps[:, 0:N], lhsT=KS[hk][h % 2][:, kcol:kcol + 128],
                                          rhs=Q[:, qc, 0:N], start=True, stop=True),
                 reads=[KS[hk][h % 2], Q], writes=[sps])
            S.op("act", lambda e: e.activation(out=E[:, 0:N], in_=sps[:, 0:N], func=AF.Exp, scale=0.125),
                 reads=[sps], writes=[E])
            if mk is not None:
                S.op("pool" if i % 2 else "dve", lambda e: e.tensor_tensor(out=E[:, 0:N], in0=E[:, 0:N],
                                                                          in1=MSK[:, mk, 0:N], op=ALU.mult),
                     reads=[E, MSK], writes=[E])
            Ef[i] = E

        def back(i):
            ti, t, N, c0, h, bi, nb, (vb, kcol, mk) = units[i]
            qc, pb, hk = h // 2, (h % 2) * 64, h // 4
            E = Ef.pop(i)
            if bi == 0:
                cur["acc"] = accs.next()
            acc = cur["acc"]
            S.op("pe", lambda e: e.matmul(acc[:, 0:N], lhsT=VSa[:, vb, hk * 128:(hk + 1) * 128],
                                          rhs=E[:, 0:N], start=(bi == 0), stop=(bi == nb - 1)),
                 reads=[VSa, E], writes=[acc])
            if bi == nb - 1:
                dn = den.next()
                S.op("dve", lambda e: e.tensor_scalar(out=dn[0:64, 0:N], in0=acc[64:128, 0:N],
                                                      scalar1=ESK[64:128, h:h + 1], scalar2=None, op0=ALU.add),
                     reads=[acc, ESK], writes=[dn])
                S.op("dve", lambda e: e.reciprocal(out=dn[0:64, 0:N], in_=dn[0:64, 0:N]), reads=[dn], writes=[dn])
                S.op("dve", lambda e: e.tensor_tensor(out=mixT[pb:pb + 64, 2 + qc, c0:c0 + N], in0=acc[0:64, 0:N],
                                                      in1=dn[0:64, 0:N], op=ALU.mult), reads=[acc, dn],
                     writes=[mixT])

        pipeline(len(units), front, back, LA, [])


def emit_diff(k, mixT, C, g, prm, lam_init):
    S = k.S
    P = k.banks()
    LA = 2
    with k.scope() as es:
        KD = k.sb([128, 2, CTX + SEQ], BF16, es, "kd")
        VDa = k.sb([128, 34, 512], BF16, es, "vda")
        qb = Rot([k.sb([128, 2, 512], BF16, es, "qd") for _ in range(2)])
        qm = [Rot([k.sb([128, 2, 512], BF16, es, "qm") for _ in range(2)]) for _ in range(4)]
        Eb = Rot([k.sb([128, 1024], BF16, es, "E") for _ in range(3)])
        tr = Rot([k.sb([128, 512], F32, es, "tr") for _ in range(2)])
        tt = Rot([k.sb([128, 512], F32, es, "tt") for _ in range(2)])
        tO = Rot([k.sb([128, 512], F32, es, "tO") for _ in range(1)])
        tq = Rot([k.sb([128, 512], F32, es, "tq") for _ in range(1)])
        lt = k.sb([128, 2, 32], F32, es, "lt")
        ls = k.sb([128, 2], F32, es, "ls")
        NLAM = k.sb([128, 1], F32, es, "nlam")
        GN = k.sb([128, 1], F32, es, "gn")
        DL = prm["DL"]
        for i in range(2):
            S.op("dve", lambda e, i=i: e.tensor_tensor(out=lt[:, i, :], in0=DL[:, 2 * i, :], in1=DL[:, 2 * i + 1, :],
                                                       op=ALU.mult), reads=[DL], writes=[lt])
        S.op("dve", lambda e: e.reduce_sum(out=ls[:], in_=lt[:], axis=AX.X), reads=[lt], writes=[ls])
        S.op("act", lambda e: e.activation(out=ls[:], in_=ls[:], func=AF.Exp), reads=[ls], writes=[ls])
        S.op("dve", lambda e: e.tensor_tensor(out=NLAM[:], in0=ls[:, 1:2], in1=ls[:, 0:1], op=ALU.subtract),
             reads=[ls], writes=[NLAM])
        S.op("dve", lambda e: e.tensor_scalar_add(out=NLAM[:], in0=NLAM[:], scalar1=-lam_init), reads=[NLAM],
             writes=[NLAM])
        S.op("dve", lambda e: e.tensor_scalar_mul(out=GN[:], in0=prm["DG"][:], scalar1=1.0 - lam_init),
             reads=[prm["DG"]], writes=[GN])
        ko = g["KDO"].t.rearrange("(c p) n -> p c n", p=128)
        S.dma("sp", KD[:, :, 0:CTX], ko[:, :, 0:CTX], reads=[g["KDO"]], writes=[KD])
        for hf in range(2):
            kg = g["KDG"][hf].rearrange("(c p) n -> p c n", p=128)
            S.dma("sp", KD[:, :, CTX + hf * HALF:CTX + (hf + 1) * HALF], kg[:, :, CTX:NT], reads=[g["KDG"]],
                  writes=[KD])
            vg0 = g["VDG"][0][hf].rearrange("(b p) n -> p b n", p=128)
            vg1 = g["VDG"][1][hf].rearrange("(b p) n -> p b n", p=128)
            S.dma("sp", VDa[:, 2 + hf * 16:2 + hf * 16 + 7, :], vg0[:, 2:9, :], reads=[g["VDG"][0]], writes=[VDa])
            S.dma("sp", VDa[:, 2 + hf * 16 + 7:2 + (hf + 1) * 16, :], vg1[:, 0:9, :], reads=[g["VDG"][1]],
                  writes=[VDa])
        vo = g["VDO"].t.rearrange("(b p) n -> p b n", p=128)
        S.dma("sp", VDa[:, 0:2, :], vo[:, 0:2, :], reads=[g["VDO"]], writes=[VDa])
        accs = Rot(P[0:2])
        pn = P[2]
        scs = Rot(k.pairs[2:4])
        qsrc = g["QD"].t.rearrange("(c p) n -> p c n", p=128)
        SC = 32 ** -0.5
        tiles = [-1, 0, 1, 2, 3]
        Qt = {}

        def load_q(t):
            N = CTX if t < 0 else 512
            c0 = 0 if t < 0 else CTX + t * 512
            Q = qb.next()
            S.dma("sp", Q[:, :, 0:N], qsrc[:, :, c0:c0 + N], reads=[g["QD"]], writes=[Q])
            Qm = [qm[v].next() for v in range(4)]
            for v in range(4):
                S.op("dve", lambda e: e.tensor_scalar(out=Qm[v][:, :, 0:N], in0=Q[:, :, 0:N],
                                                      scalar1=prm["PM"][:, v:v + 1], scalar2=None, op0=ALU.mult),
                     reads=[Q, prm["PM"]], writes=[Qm[v]])
            Qt[t] = Qm

        units = []
        for ti, t in enumerate(tiles):
            N = CTX if t < 0 else 512
            c0 = 0 if t < 0 else CTX + t * 512
            nkp = 1 if t < 0 else 17
            for h in range(4):
                for m in range(2):
                    for kp in range(nkp):
                        units.append((ti, t, N, c0, h, m, kp, nkp))
        Ef = {}
        cur = {}
        deferred = []
        load_q(tiles[0])

        def front(i):
            ti, t, N, c0, h, m, kp, nkp = units[i]
            if h == 0 and m == 0 and kp == 0 and ti + 1 < len(tiles):
                load_q(tiles[ti + 1])
            Qm = Qt[t]
            c = h // 2
            sps, E = scs.next(), Eb.next()
            qv = Qm[(h % 2) * 2 + m]
            for j in range(2):
                kb = 2 * kp + j
                S.op("pe", lambda e: e.matmul(sps[:, j * 512:j * 512 + N], lhsT=KD[:, c, kb * 128:(kb + 1) * 128],
                                              rhs=qv[:, c, 0:N], start=True, stop=True),
                     reads=[KD, qv], writes=[sps])
            S.op("act", lambda e: e.activation(out=E[:].rearrange("p (j n) -> p j n", j=2)[:, :, 0:N],
                                               in_=sps[:].rearrange("p (j n) -> p j n", j=2)[:, :, 0:N],
                                               func=AF.Exp, scale=SC), reads=[sps], writes=[E])
            Ef[i] = E

        def back(i):
            ti, t, N, c0, h, m, kp, nkp = units[i]
            c = h // 2
            E = Ef.pop(i)
            if m == 0 and kp == 0:
                cur["ac"] = [accs.next(), accs.next()]
            ac = cur["ac"]
            for j in range(2):
                kb = 2 * kp + j
                S.op("pe", lambda e: e.matmul(ac[m][:, 0:N], lhsT=VDa[:, kb, h * 128:(h + 1) * 128],
                                              rhs=E[:, j * 512:j * 512 + N], start=(kb == 0), stop=(kb == 2 * nkp - 1)),
                     reads=[VDa, E], writes=[ac[m]])
            if not (m == 1 and kp == nkp - 1):
                return
            ts = []
            for mm in range(2):
                r_, t_ = tr.next(), tt.next()
                S.op("dve", lambda e: e.reciprocal(out=r_[0:64, 0:N], in_=ac[mm][64:128, 0:N]), reads=[ac[mm]],
                     writes=[r_])
                S.op("dve", lambda e: e.tensor_tensor(out=t_[0:64, 0:N], in0=ac[mm][0:64, 0:N], in1=r_[0:64, 0:N],
                                                      op=ALU.mult), reads=[ac[mm], r_], writes=[t_])
                ts.append(t_)
            O, sq = tO.next(), tq.next()
            S.op("dve", lambda e: e.scalar_tensor_tensor(out=O[0:64, 0:N], in0=ts[1][0:64, 0:N],
                                                         scalar=NLAM[0:64, 0:1], in1=ts[0][0:64, 0:N],
                                                         op0=ALU.mult, op1=ALU.add), reads=ts + [NLAM],
                 writes=[O])
            S.op("act", lambda e: e.activation(out=sq[0:64, 0:N], in_=O[0:64, 0:N], func=AF.Square), reads=[O],
                 writes=[sq])

            def tail():
                S.op("pe", lambda e: e.matmul(pn[0:64, 0:N], lhsT=C["onesf"][0:64, 0:64], rhs=sq[0:64, 0:N],
                                              start=True, stop=True), reads=[C["onesf"], sq], writes=[pn])
                S.op("act", lambda e: e.activation(out=sq[0:64, 0:N], in_=pn[0:64, 0:N], func=AF.Sqrt,
                                                   scale=1.0 / 64.0, bias=C["eps"][0:64, :]),
                     reads=[pn, C["eps"]], writes=[sq])
                S.op("dve", lambda e: e.reciprocal(out=sq[0:64, 0:N], in_=sq[0:64, 0:N]), reads=[sq], writes=[sq])
                S.op("dve", lambda e: e.tensor_tensor(out=O[0:64, 0:N], in0=O[0:64, 0:N], in1=sq[0:64, 0:N],
                                                      op=ALU.mult), reads=[O, sq], writes=[O])
                ob = (h % 2) * 64
                S.op("dve", lambda e: e.tensor_scalar(out=mixT[ob:ob + 64, 6 + c, c0:c0 + N], in0=O[0:64, 0:N],
                                                      scalar1=GN[0:64, 0:1], scalar2=None, op0=ALU.mult),
                     reads=[O, GN], writes=[mixT])
            deferred.append((i + LA + min(3, 2 * nkp - 1), tail))

        pipeline(len(units), front, back, LA, deferred)


def emit_ln(k, hT, C, c0, N, LNG, LNB, which, es, pool):
    S = k.S
    ysq = pool["ysq"].next()
    S1, S2 = pool["ps"].next(), pool["ps"].next()
    for c in range(8):
        S.op("act", lambda e, c=c: e.activation(out=ysq[:, c, 0:N], in_=hT[:, c, c0:c0 + N], func=AF.Square),
             reads=[hT], writes=[ysq])
    for c in range(8):
        S.op("pe", lambda e, c=c: e.matmul(S1[:, 0:N], lhsT=C["onesf"][:, :], rhs=hT[:, c, c0:c0 + N], start=(c == 0),
                                           stop=(c == 7)), reads=[C["onesf"], hT], writes=[S1])
    for c in range(8):
        S.op("pe", lambda e, c=c: e.matmul(S2[:, 0:N], lhsT=C["onesb"][:, :], rhs=ysq[:, c, 0:N], start=(c == 0),
                                           stop=(c == 7)), reads=[C["onesb"], ysq], writes=[S2])
    mean, rstd, nmr = pool["f"].next(), pool["f"].next(), pool["f"].next()
    S.op("act", lambda e: e.activation(out=mean[:, 0:N], in_=S1[:, 0:N], func=AF.Identity, scale=1.0 / D_MODEL),
         reads=[S1], writes=[mean])
    S.op("dve", lambda e: e.tensor_tensor(out=rstd[:, 0:N], in0=mean[:, 0:N], in1=mean[:, 0:N], op=ALU.mult),
         reads=[mean], writes=[rstd])
    S.op("dve", lambda e: e.scalar_tensor_tensor(out=rstd[:, 0:N], in0=S2[:, 0:N], scalar=1.0 / D_MODEL,
                                                 in1=rstd[:, 0:N], op0=ALU.mult, op1=ALU.subtract),
         reads=[S2, rstd], writes=[rstd])
    S.op("act", lambda e: e.activation(out=rstd[:, 0:N], in_=rstd[:, 0:N], func=AF.Sqrt, bias=C["eps"][:, :]),
         reads=[rstd, C["eps"]], writes=[rstd])
    S.op("dve", lambda e: e.reciprocal(out=rstd[:, 0:N], in_=rstd[:, 0:N]), reads=[rstd], writes=[rstd])
    S.op("dve", lambda e: e.scalar_tensor_tensor(out=nmr[:, 0:N], in0=mean[:, 0:N], scalar=-1.0, in1=rstd[:, 0:N],
                                                 op0=ALU.mult, op1=ALU.mult), reads=[mean, rstd], writes=[nmr])
    for c in range(8):
        t1 = pool["t"].next()
        S.op("dve", lambda e, c=c: e.scalar_tensor_tensor(out=t1[:, 0:N], in0=hT[:, c, c0:c0 + N],
                                                          scalar=LNG[:, which, c:c + 1], in1=rstd[:, 0:N],
                                                          op0=ALU.mult, op1=ALU.mult), reads=[hT, LNG, rstd],
             writes=[t1])
        S.op("dve", lambda e, c=c: e.scalar_tensor_tensor(out=t1[:, 0:N], in0=nmr[:, 0:N],
                                                          scalar=LNG[:, which, c:c + 1], in1=t1[:, 0:N],
                                                          op0=ALU.mult, op1=ALU.add), reads=[nmr, LNG, t1],
             writes=[t1])
        S.op("act", lambda e, c=c: e.activation(out=hT[:, c, c0:c0 + N], in_=t1[:, 0:N], func=AF.Identity,
                                                bias=LNB[:, which, c:c + 1]), reads=[t1, LNB], writes=[hT])


def ln_pool(k, es, P, W=512):
    return dict(ysq=Rot([k.sb([128, 8, W], BF16, es, "ysq") for _ in range(2)]),
                f=Rot([k.sb([128, W], F32, es, "lnf") for _ in range(6)]),
                t=Rot([k.sb([128, W], F32, es, "lnt") for _ in range(3)]), ps=Rot(P))


def emit_phaseC(k, hT, mixT, MODX, C, wout_d, prm):
    S = k.S
    P = k.banks()
    with k.scope() as es:
        WO = k.sb([128, 8, D_MODEL], BF16, es, "wout")
        S.dma("pool", WO[:], wout_d.t.rearrange("(k p) n -> p k n", p=128), reads=[wout_d], writes=[WO])
        lp = ln_pool(k, es, P[6:8])
        tm = Rot([k.sb([128, 512], F32, es, "ctm") for _ in range(3)])
        pb = Rot(P[0:6])
        tiles = [(0, 256)] + [(CTX + i * 512, 512) for i in range(4)]
        prev = None
        for (c0, N) in tiles:
            j = 1 if c0 < CTX else 0
            for c in range(8):
                ps = pb.next()
                for kk in range(8):
                    S.op("pe", lambda e, kk=kk: e.matmul(ps[:, 0:N], lhsT=WO[:, kk, c * 128:(c + 1) * 128],
                                                         rhs=mixT[:, kk, c0:c0 + N], start=(kk == 0), stop=(kk == 7)),
                         reads=[WO, mixT], writes=[ps])
                t = tm.next()
                S.op("dve", lambda e: e.tensor_scalar(out=t[:, 0:N], in0=ps[:, 0:N], scalar1=MODX[:, 2, c, j:j + 1],
                                                      scalar2=None, op0=ALU.mult), reads=[ps, MODX], writes=[t])
                S.op("dve", lambda e: e.scalar_tensor_tensor(out=hT[:, c, c0:c0 + N], in0=hT[:, c, c0:c0 + N],
                                                             scalar=ALPHA, in1=t[:, 0:N], op0=ALU.mult, op1=ALU.add),
                     reads=[hT, t], writes=[hT])
            if prev is not None:
                emit_ln(k, hT, C, prev[0], prev[1], prm["LNG"], prm["LNB"], 0, es, lp)
            prev = (c0, N)
        emit_ln(k, hT, C, prev[0], prev[1], prm["LNG"], prm["LNB"], 0, es, lp)


def emit_ffn(k, hT, MODX, C, prm, w13_d, w2_d, nexp, rw_d=None):
    S = k.S
    P = k.banks()
    G = NT // 2
    TN = 384
    moe = nexp > 1
    with k.scope() as es:
        u2 = k.sb([128, 8, G], BF16, es, "u2")
        gT = k.sb([128, NJ, G], BF16, es, "gT")
        W13 = Rot([k.sb([128, 8, 256], BF16, es, "w13") for _ in range(3)])
        W2 = Rot([k.sb([128, NJ, 128], BF16, es, "w2") for _ in range(2)])
        sil = Rot([k.sb([128, TN], F32, es, "sil") for _ in range(2)])
        tmp = Rot([k.sb([128, TN], F32, es, "ftmp") for _ in range(2)])
        lp = ln_pool(k, es, P[6:8], TN)
        p13 = Rot(P[0:4])
        po = Rot(P[4:6])
        pm = Rot(P[6:8])
        if moe:
            RW = k.sb([128, 8, NEXP], BF16, es, "rw")
            S.dma("pool", RW[:], rw_d.t.rearrange("(k p) e -> p k e", p=128), reads=[rw_d], writes=[RW])
            CWt = k.sb([128, G // 128, NEXP], F32, es, "cw")
            cwT = Rot([k.sb([128, G], F32, es, "cwT") for _ in range(1)])
            rt = {n: k.sb([128, NEXP], F32, es, "r" + n) for n in ["lg", "eq", "l2", "sel", "ex"]}
            rs = {n: k.sb([128, 1], F32, es, "s" + n) for n in ["m1", "m2", "nm1", "sum"]}
            dg = Rot([k.sb([128, 128], F32, es, "dg") for _ in range(2)])

        jobs = []
        for grp in range(2):
            for e_ in range(nexp):
                for jj in range(NJ):
                    jobs.append(("w13", e_, jj))
                for c in range(8):
                    jobs.append(("w2", e_, c))
        loaded = {}
        state = {"next": 0}

        def prefetch(upto):
            while state["next"] < min(upto, len(jobs)):
                i = state["next"]
                kind, e_, x = jobs[i]
                if kind == "w13":
                    w = W13.next()
                    S.dma("pool", w[:], w13_d[e_, x], reads=[w13_d], writes=[w])
                else:
                    w = W2.next()
                    S.dma("pool", w[:], w2_d[e_, x], reads=[w2_d], writes=[w])
                loaded[i] = w
                state["next"] += 1

        pending = []

        def flush_ln():
            while pending:
                gg = pending.pop(0)
                for t in range(G // TN):
                    emit_ln(k, hT, C, gg + t * TN, TN, prm["LNG"], prm["LNB"], 1, es, lp)

        ji = 0
        for grp in range(2):
            g0 = grp * G
            for (lo, hi, j) in segs(g0, G):
                for kk in range(8):
                    S.op("act", lambda e, kk=kk: e.activation(
                        out=u2[:, kk, lo - g0:hi - g0], in_=hT[:, kk, lo:hi], func=AF.Identity,
                        scale=MODX[:, 7, kk, j:j + 1], bias=MODX[:, 3, kk, j:j + 1]), reads=[hT, MODX], writes=[u2])
            S.op("dve", lambda e: e.tensor_scalar(out=hT[:, :, g0:g0 + G], in0=hT[:, :, g0:g0 + G], scalar1=ALPHA,
                                                  scalar2=None, op0=ALU.mult), reads=[hT], writes=[hT])
            if moe:
                for blk in range(G // 128):
                    ps = pm.next()
                    for kk in range(8):
                        S.op("pe", lambda e, kk=kk: e.matmul(ps[:, 0:NEXP], lhsT=u2[:, kk, blk * 128:(blk + 1) * 128],
                                                             rhs=RW[:, kk, :], start=(kk == 0), stop=(kk == 7)),
                             reads=[u2, RW], writes=[ps])
                    lg, eq, l2, sel, ex = rt["lg"], rt["eq"], rt["l2"], rt["sel"], rt["ex"]
                    m1, m2, nm1, sm = rs["m1"], rs["m2"], rs["nm1"], rs["sum"]
                    S.op("dve", lambda e: e.tensor_tensor(out=lg[:], in0=ps[:, 0:NEXP], in1=prm["RB"][:], op=ALU.add),
                         reads=[ps, prm["RB"]], writes=[lg])
                    S.op("dve", lambda e: e.reduce_max(out=m1[:], in_=lg[:], axis=AX.X), reads=[lg], writes=[m1])
                    S.op("dve", lambda e: e.tensor_scalar(out=eq[:], in0=lg[:], scalar1=m1[:, 0:1], scalar2=None,
                                                          op0=ALU.is_equal), reads=[lg, m1], writes=[eq])
                    S.op("dve", lambda e: e.scalar_tensor_tensor(out=l2[:], in0=eq[:], scalar=-1e30, in1=lg[:],
                                                                 op0=ALU.mult, op1=ALU.add), reads=[eq, lg],
                         writes=[l2])
                    S.op("dve", lambda e: e.reduce_max(out=m2[:], in_=l2[:], axis=AX.X), reads=[l2], writes=[m2])
                    S.op("dve", lambda e: e.tensor_scalar(out=sel[:], in0=lg[:], scalar1=m2[:, 0:1], scalar2=None,
                                                          op0=ALU.is_ge), reads=[lg, m2], writes=[sel])
                    S.op("dve", lambda e: e.tensor_scalar_mul(out=nm1[:], in0=m1[:], scalar1=-1.0), reads=[m1],
                         writes=[nm1])
                    S.op("act", lambda e: e.activation(out=ex[:], in_=lg[:], func=AF.Exp, bias=nm1[:, 0:1]),
                         reads=[lg, nm1], writes=[ex])
                    S.op("dve", lambda e: e.tensor_tensor(out=ex[:], in0=ex[:], in1=sel[:], op=ALU.mult),
                         reads=[ex, sel], writes=[ex])
                    S.op("dve", lambda e: e.reduce_sum(out=sm[:], in_=ex[:], axis=AX.X), reads=[ex], writes=[sm])
                    S.op("dve", lambda e: e.reciprocal(out=sm[:], in_=sm[:]), reads=[sm], writes=[sm])
                    S.op("dve", lambda e: e.tensor_scalar(out=CWt[:, blk, :], in0=ex[:], scalar1=sm[:, 0:1],
                                                          scalar2=None, op0=ALU.mult), reads=[ex, sm], writes=[CWt])
            for e_ in range(nexp):
                if moe:
                    cw = cwT.next()
                    for b0 in range(0, G // 128, 4):
                        nb = min(4, G // 128 - b0)
                        ps = pm.next()
                        for bb in range(nb):
                            blk = b0 + bb
                            d_ = dg.next()
                            S.op("dve", lambda e: e.tensor_scalar(out=d_[:], in0=C["ident"][:],
                                                                  scalar1=CWt[:, blk, e_:e_ + 1], scalar2=None,
                                                                  op0=ALU.mult), reads=[C["ident"], CWt], writes=[d_])
                            S.op("pe", lambda e: e.matmul(ps[:, bb * 128:(bb + 1) * 128], lhsT=C["onesf"][:, :],
                                                          rhs=d_[:], start=True, stop=True),
                                 reads=[C["onesf"], d_], writes=[ps])
                        S.op("act", lambda e: e.activation(out=cw[:, b0 * 128:(b0 + nb) * 128], in_=ps[:, 0:nb * 128],
                                                           func=AF.Identity), reads=[ps], writes=[cw])
                for jj in range(NJ):
                    if e_ == 0 and jj == 6:
                        flush_ln()
                    prefetch(ji + 3)
                    w = loaded.pop(ji)
                    ji += 1
                    for t in range(G // TN):
                        p1, p3 = p13.next(), p13.next()
                        for (pp, wo) in [(p1, 0), (p3, 128)]:
                            for kk in range(8):
                                S.op("pe", lambda e, kk=kk: e.matmul(pp[:, 0:TN], lhsT=w[:, kk, wo:wo + 128],
                                                                     rhs=u2[:, kk, t * TN:(t + 1) * TN],
                                                                     start=(kk == 0), stop=(kk == 7)),
                                     reads=[w, u2], writes=[pp])
                        s_ = sil.next()
                        S.op("act", lambda e: e.activation(out=s_[:], in_=p1[:, 0:TN], func=AF.Silu), reads=[p1],
                             writes=[s_])
                        S.op("dve", lambda e: e.tensor_tensor(out=gT[:, jj, t * TN:(t + 1) * TN], in0=s_[:],
                                                              in1=p3[:, 0:TN], op=ALU.mult), reads=[s_, p3],
                             writes=[gT])
                for c in range(8):
                    prefetch(ji + 2)
                    w = loaded.pop(ji)
                    ji += 1
                    for t in range(G // TN):
                        pso = po.next()
                        for jj in range(NJ):
                            S.op("pe", lambda e, jj=jj: e.matmul(pso[:, 0:TN], lhsT=w[:, jj, :],
                                                                 rhs=gT[:, jj, t * TN:(t + 1) * TN], start=(jj == 0),
                                                                 stop=(jj == NJ - 1)), reads=[w, gT], writes=[pso])
                        for (lo, hi, j) in segs(g0 + t * TN, TN):
                            a, b = lo - g0 - t * TN, hi - g0 - t * TN
                            if moe:
                                tp = tmp.next()
                                S.op("dve", lambda e: e.tensor_tensor(out=tp[:, a:b], in0=pso[:, a:b],
                                                                      in1=cw[:, lo - g0:hi - g0], op=ALU.mult),
                                     reads=[pso, cw], writes=[tp])
                                S.op("dve", lambda e: e.scalar_tensor_tensor(
                                    out=hT[:, c, lo:hi], in0=tp[:, a:b], scalar=MODX[:, 5, c, j:j + 1],
                                    in1=hT[:, c, lo:hi], op0=ALU.mult, op1=ALU.add), reads=[tp, MODX, hT],
                                    writes=[hT])
                            else:
                                S.op("dve", lambda e: e.scalar_tensor_tensor(
                                    out=hT[:, c, lo:hi], in0=pso[:, a:b], scalar=MODX[:, 5, c, j:j + 1],
                                    in1=hT[:, c, lo:hi], op0=ALU.mult, op1=ALU.add), reads=[pso, MODX, hT],
                                    writes=[hT])
            pending.append(g0)
        flush_ln()


SMALL = dict(CW=[128, 2, 4], CB=[128, 2], GB=[128, 2, 2, 2], LAM=[128, 2, 2], GW=[128, 2, 2, 2, 128], SEL=[128, 2],
             SINK=[128, 8], DL=[128, 4, 32], DG=[128, 1], PM=[128, 4], LNG=[128, 2, 8], LNB=[128, 2, 8], RB=[128, 8])
VDR = NT // 2
A_OUT = dict(XL0=([128, NT], F32), XL1=([128, NT], F32), GT=([256, NT], F32), QS=([512, NT], BF16),
             KS=([256, NT], BF16), VS=([NT, 256], BF16), QD=([256, NT], BF16), KD=([256, NT], BF16),
             VD0=([VDR, 512], BF16), VD1=([VDR, 512], BF16))


def to_fm(x2d):
    return np.ascontiguousarray(x2d.T.reshape(8, 128, -1).transpose(1, 0, 2))


def from_fm(h):
    return np.ascontiguousarray(h.transpose(1, 0, 2).reshape(D_MODEL, -1).T)


def win_perm():
    lx, lg, sq, sk, sv, dq, dk, dv = 0, 256, 512, 1024, 1152, 1280, 1536, 1792
    cols = []
    cols += list(range(lx, lx + 256))
    cols += list(range(lg, lg + 256))
    cols += list(range(sq, sq + 512))
    sw64 = lambda base, h: [base + h * 64 + ((d + 32) % 64) for d in range(64)]
    for h in range(8):
        cols += sw64(sq, h)
    for hk in range(2):
        cols += list(range(sk + hk * 64, sk + hk * 64 + 64)) * 2
    for hk in range(2):
        cols += sw64(sk, hk) * 2
    sw32 = lambda base: [base + b * 32 + ((d + 16) % 32) for b in range(8) for d in range(32)]
    cols += list(range(dq, dq + 256))
    cols += sw32(dq)
    cols += list(range(dk, dk + 256))
    cols += sw32(dk)
    cols += list(range(sv, sv + 128))
    cols += list(range(dv, dv + 256))
    assert len(cols) == NW
    return np.array(cols)


def rope_tables(half):
    t = np.arange(HALF, dtype=np.float32) + np.float32(half * HALF)
    row = np.floor(t / 64).astype(np.float32)
    col = (t - row * 64).astype(np.float32)
    out = np.zeros((128, 4, HALF), np.float32)
    for (ti, hd) in [(0, 64), (2, 32)]:
        nf = hd // 4
        inv = (np.float32(10000.0) ** (-np.arange(nf, dtype=np.float32) / np.float32(nf))).astype(np.float32)
        ang = np.concatenate([row[:, None] * inv, col[:, None] * inv], -1).astype(np.float32)
        cs, sn = np.cos(ang).astype(np.float32), np.sin(ang).astype(np.float32)
        for p in range(128):
            d = p % hd
            jx = d % (hd // 2)
            out[p, ti] = cs[:, jx]
            out[p, ti + 1] = -sn[:, jx] if d < hd // 2 else sn[:, jx]
    return out


def swa_masks(half):
    m = np.zeros((128, 8, 512), np.float32)
    kk = np.arange(128)[:, None]
    q = np.arange(512)[None, :]
    for r in range(6):
        m[:, r] = (np.abs((r - 1) * 128 + kk - q) <= 128)
    m[:, 6] = m[:, 0] if half == 1 else 0.0
    m[:, 7] = m[:, 5] if half == 0 else 0.0
    return m.astype(NPBF)


def rep(v):
    return np.ascontiguousarray(np.broadcast_to(np.asarray(v, np.float32).reshape(1, -1), (128, np.size(v))))


def small_params(inp, layer, half):
    f = lambda a: np.ascontiguousarray(np.asarray(a, np.float32))
    p = {}
    p["CW"] = f(inp["lru_conv_w"][layer].reshape(4, 2, 128).transpose(2, 1, 0))
    p["CB"] = f(inp["lru_conv_b"][layer].reshape(2, 128).T)
    p["GB"] = f(inp["lru_gate_b"][layer].reshape(2, 2, 2, 128).transpose(3, 2, 0, 1))
    p["LAM"] = f(inp["lru_lam"][layer].reshape(2, 2, 128).transpose(2, 1, 0))
    gw = np.zeros((128, 2, 2, 2, 128), np.float32)
    w = inp["lru_gate_w"][layer]
    for c in range(2):
        for bb in range(2):
            blk = c * 2 + bb
            gw[bb * 64:(bb + 1) * 64, c, :, :, bb * 64:(bb + 1) * 64] = w[:, :, blk].transpose(2, 0, 1, 3)
    p["GW"] = gw
    sel = np.zeros((128, 2), np.float32)
    sel[:, half] = 1.0
    p["SEL"] = sel
    p["SINK"] = rep(inp["swa_sink"][layer])
    p["DL"] = rep(inp["diff_lam"][layer].reshape(-1)).reshape(128, 4, 32)
    p["DG"] = f(np.tile(inp["diff_norm_g"][layer], 2).reshape(128, 1))
    pm = np.zeros((128, 4), np.float32)
    for hp in range(2):
        for m in range(2):
            pm[:, hp * 2 + m] = ((np.arange(128) // 64) == hp) & (((np.arange(128) % 64) // 32) == m)
    p["PM"] = pm
    p["LNG"] = f(inp["ln_g"][layer].reshape(2, 8, 128).transpose(2, 0, 1))
    p["LNB"] = f(inp["ln_b"][layer].reshape(2, 8, 128).transpose(2, 0, 1))
    if layer % 2 == 1:
        p["RB"] = rep(inp["moe_router_b"][layer // 2])
    else:
        p["RB"] = np.zeros((128, 8), np.float32)
    return p


def ffn_layout(w1, w3, w2):
    E = w1.shape[0]
    a = w1.reshape(E, 8, 128, NJ, 128).transpose(0, 3, 2, 1, 4)
    b = w3.reshape(E, 8, 128, NJ, 128).transpose(0, 3, 2, 1, 4)
    w13 = np.ascontiguousarray(np.concatenate([a, b], axis=-1))
    w2r = np.ascontiguousarray(w2.reshape(E, NJ, 128, 8, 128).transpose(0, 3, 2, 1, 4))
    return w13, w2r


_PROGS = {}


def _prog(key, fn):
    if key not in _PROGS:
        _PROGS[key] = fn()
    return _PROGS[key]


PAIRS = [[0, 1], [2, 3], [4, 5], [6, 7]]
PUB = ["XL0", "XL1", "KS", "VS", "KD", "VD0", "VD1"]


def build_fused(depth=DEPTH):
    k = K()
    S = k.S
    nc = k.nc
    I = lambda n, s, dt: k.dram(n, s, dt, "ExternalInput")
    hT_d = I("hT", [128, 8, NT], F32)
    cc_d = I("cc", [128, 8, 2], F32)
    cst_d = I("ident", [128, 128], F32)
    rope_d = I("rope", [128, 4, HALF], F32)
    msk_d = I("MSK", [128, 8, 512], BF16)
    L = []
    for l in range(depth):
        moe = l % 2 == 1
        nexp = NEXP if moe else 1
        d = dict(adaw=I(f"adaw{l}", [D_MODEL, 6 * D_MODEL], F32), adab=I(f"adab{l}", [128, 48], F32),
                 win=I(f"win{l}", [D_MODEL, NW], F32), wout=I(f"wout{l}", [D_MODEL, D_MODEL], F32),
                 w13=I(f"w13_{l}", [nexp, NJ, 128, 8, 256], F32), w2=I(f"w2_{l}", [nexp, 8, 128, NJ, 128], F32),
                 rw=I(f"rw{l}", [D_MODEL, NEXP], F32) if moe else None,
                 sm={n: I(f"{n}{l}", s_, F32) for n, s_ in SMALL.items()})
        L.append(d)
    out_d = k.dram("hout", [128, 8, NT], F32, "ExternalOutput")
    scr = []
    for par in range(2):
        o = {n: Tile(nc.dram_tensor(f"{n}_{par}", list(sh), dt).ap()) for n, (sh, dt) in A_OUT.items()}
        gth = {n: Tile(nc.dram_tensor(f"{n}G_{par}", [2 * A_OUT[n][0][0], A_OUT[n][0][1]], A_OUT[n][1]).ap())
               for n in PUB}
        scr.append((o, gth))
    with k.es:
        hT = k.sb([128, 8, NT], F32, name="hT")
        MODX = k.sb([128, 8, 8, 2], F32, name="modx")
        S.dma("sp", hT[:], hT_d[:, :, :], reads=[hT_d], writes=[hT])
        C = emit_consts(k, cst_d)
        prm = {n: k.sb(s_, F32, name=n) for n, s_ in SMALL.items()}
        for l in range(depth):
            moe = l % 2 == 1
            lam_init = 0.8 - 0.6 * math.exp(-0.3 * l)
            o, gth = scr[l % 2]
            emit_mods(k, cc_d, L[l]["adaw"], L[l]["adab"], MODX)
            emit_phaseA(k, hT, MODX, L[l]["win"], rope_d, o)
            for n in PUB:
                S.coll("AllGather", PAIRS, o[n], gth[n])
            for n in SMALL:
                S.dma("sp", prm[n][:], L[l]["sm"][n].t, reads=[L[l]["sm"][n]], writes=[prm[n]])

            def G(n):
                t = Tile(gth[n].t.rearrange("(h r) n -> h r n", h=2))
                t.b = gth[n].b
                return t
            g = dict(XG=[G("XL0"), G("XL1")], GT=o["GT"], QS=o["QS"], KSO=o["KS"], KSG=G("KS"), VSO=o["VS"],
                     VSG=G("VS"), QD=o["QD"], KDO=o["KD"], KDG=G("KD"), VDO=o["VD0"], VDG=[G("VD0"), G("VD1")],
                     MSK=msk_d)
            with k.scope() as es:
                mixT = k.sb([128, 8, NT], BF16, es, "mixT")
                emit_lru(k, mixT, C, g, prm)
                emit_swa(k, mixT, C, g, prm)
                emit_diff(k, mixT, C, g, prm, lam_init)
                emit_phaseC(k, hT, mixT, MODX, C, L[l]["wout"], prm)
            emit_ffn(k, hT, MODX, C, prm, L[l]["w13"], L[l]["w2"], NEXP if moe else 1, L[l]["rw"])
        S.dma("sp", out_d[:, :, :], hT[:], reads=[hT], writes=[out_d])
        S.finish()
    return k.nc


def fused_inputs(inp, depth=DEPTH):
    x, c, ctx, c_ctx = inp["x"], inp["c"], inp["ctx"], inp["c_ctx"]
    cores = [(b, hf) for b in range(BATCH) for hf in range(2)]
    perm = win_perm()
    ropes = [rope_tables(hf) for hf in range(2)]
    masks = [swa_masks(hf) for hf in range(2)]
    shared = dict(ident=np.eye(128, dtype=np.float32))
    for l in range(depth):
        j = l // 2
        shared[f"adaw{l}"] = np.ascontiguousarray(inp["ada_w"][l])
        shared[f"adab{l}"] = np.ascontiguousarray(inp["ada_b"][l].reshape(48, 128).T)
        shared[f"win{l}"] = np.ascontiguousarray(inp["w_in"][l][:, perm])
        shared[f"wout{l}"] = np.ascontiguousarray(inp["w_out"][l])
        if l % 2 == 0:
            w13, w2r = ffn_layout(inp["ffn_w1"][j][None], inp["ffn_w3"][j][None], inp["ffn_w2"][j][None])
        else:
            w13, w2r = ffn_layout(inp["moe_w1"][j], inp["moe_w3"][j], inp["moe_w2"][j])
            shared[f"rw{l}"] = np.ascontiguousarray(inp["moe_router_w"][j])
        shared[f"w13_{l}"] = w13
        shared[f"w2_{l}"] = w2r
    in_maps = []
    for (b, hf) in cores:
        m = dict(shared)
        toks = np.concatenate([ctx[b], x[b, hf * HALF:(hf + 1) * HALF]], 0)
        m["hT"] = to_fm(toks)
        m["cc"] = np.ascontiguousarray(np.stack([c[b].reshape(8, 128).T, c_ctx.reshape(8, 128).T], -1))
        m["rope"] = ropes[hf]
        m["MSK"] = masks[hf]
        for l in range(depth):
            for n, v in small_params(inp, l, hf).items():
                m[f"{n}{l}"] = v
        in_maps.append(m)
    return cores, in_maps


def kernel(**inp):
    inp = {k_: np.asarray(v) for k_, v in inp.items()}
    cores, in_maps = fused_inputs(inp)
    res = run_bass_kernel_spmd(_prog("fused", build_fused), in_maps, core_ids=list(range(NCORES))).results
    out = np.zeros((BATCH, SEQ, D_MODEL), np.float32)
    for ci, (b, hf) in enumerate(cores):
        out[b, hf * HALF:(hf + 1) * HALF] = from_fm(np.asarray(res[ci]["hout"]))[CTX:]
    return out
```

```python
import math
from contextlib import ExitStack

import numpy as np
import ml_dtypes

import concourse.bass as bass
import concourse.mybir as mybir
from concourse.bass_utils import run_bass_kernel_spmd

F32 = mybir.dt.float32
BF16 = mybir.dt.bfloat16
AF = mybir.ActivationFunctionType
ALU = mybir.AluOpType
AX = mybir.AxisListType
NPBF = ml_dtypes.bfloat16

D_MODEL = 1024
BATCH = 4
SEQ = 4096
DEPTH = 4
CTX = 256
HALF = SEQ // 2
NT = CTX + HALF
D_FF = 2816
NJ = D_FF // 128
NEXP = 8
LN_EPS = 1e-5
ALPHA = (2.0 * DEPTH) ** 0.25
NW = 24 * 128 + 384
NCORES = 8


class Buf:
    __slots__ = ("w", "r", "excl")

    def __init__(self, excl=False):
        self.w = None
        self.r = {}
        self.excl = excl


class Tile:
    def __init__(self, t, excl=False):
        self.t = t
        self.b = Buf(excl)

    def __getitem__(self, idx):
        return self.t[idx]


class Sched:
    EPOCH = 16000
    NDMA = 28

    def __init__(self, nc, es):
        self.nc, self.es = nc, es
        self.eng = dict(pe=nc.tensor, act=nc.scalar, dve=nc.vector, pool=nc.gpsimd, sp=nc.sync)
        self.cnt = {}
        self.sem = {}
        self.own = {e: set() for e in self.eng}
        self.nsem = 0
        self.waited = {e: {} for e in self.eng}
        self.dsem = []
        self.dcnt = []
        self.dn = 0
        self.allsems = []
        self.csem = None
        self.ccnt = 0

    def _newsem(self, name):
        self.nsem += 1
        s = self.es.enter_context(self.nc.semaphore(f"{name}_{self.nsem}"))
        self.allsems.append(s)
        return s

    def _wait(self, e, deps):
        w = self.waited[e]
        for sem, val in deps:
            if w.get(sem, 0) < val:
                self.eng[e].wait_ge(sem, val)
                w[sem] = val

    def _deps(self, e, reads, writes):
        deps = []
        own = self.own[e]
        for b in reads:
            if b.w is not None:
                deps.append(b.w)
            if b.excl:
                deps.extend(b.r.items())
        for b in writes:
            if b.w is not None:
                deps.append(b.w)
            deps.extend(x for x in b.r.items() if x[0] not in own)
        if e == "pe":
            deps = [d for d in deps if d[0] not in own]
        return deps

    def _mark(self, tok, reads, writes):
        for b in reads:
            if b.excl:
                b.w = tok
                b.r = {}
            else:
                b.r[tok[0]] = tok[1]
        for b in writes:
            b.w = tok
            b.r = {}

    def op(self, e, fn, reads=(), writes=()):
        reads = [x.b if isinstance(x, Tile) else x for x in reads]
        writes = [x.b if isinstance(x, Tile) else x for x in writes]
        self._wait(e, self._deps(e, reads, writes))
        ins = fn(self.eng[e])
        if self.cnt.get(e, self.EPOCH) >= self.EPOCH:
            self.sem[e] = self._newsem(e)
            self.cnt[e] = 0
            self.own[e].add(self.sem[e])
        self.cnt[e] += 1
        ins.then_inc(self.sem[e], 1)
        tok = (self.sem[e], self.cnt[e])
        self._mark(tok, reads, writes)
        return tok

    def dma(self, q, out, in_, reads=(), writes=()):
        reads = [x.b if isinstance(x, Tile) else x for x in reads]
        writes = [x.b if isinstance(x, Tile) else x for x in writes]
        self._wait(q, self._deps(q, reads, writes))
        i = self.dn % self.NDMA
        self.dn += 1
        if i >= len(self.dsem):
            self.dsem.append(self._newsem("d"))
            self.dcnt.append(0)
        sem = self.dsem[i]
        if self.dcnt[i] > 0:
            self._wait(q, [(sem, self.dcnt[i])])
        self.dcnt[i] += 16
        self.eng[q].dma_start(out=out, in_=in_).then_inc(sem, 16)
        tok = (sem, self.dcnt[i])
        self._mark(tok, reads, writes)
        return tok

    def coll(self, kind, groups, src, dst):
        q = "pool"
        reads, writes = [src.b], [dst.b]
        self._wait(q, self._deps(q, reads, writes))
        if self.csem is None:
            self.csem = self._newsem("cc")
            self.ccnt = 0
        self.ccnt += 1
        self.nc.gpsimd.collective_compute(kind, ALU.bypass, replica_groups=groups, ins=[src.t.opt()],
                                          outs=[dst.t.opt()]).then_inc(self.csem, 1)
        tok = (self.csem, self.ccnt)
        self._mark(tok, reads, writes)
        return tok

    def barrier(self):
        deps = [(s, c) for s, c in zip(self.dsem, self.dcnt) if c > 0]
        deps += [(self.sem[e], self.cnt[e]) for e in self.sem]
        if self.csem is not None:
            deps.append((self.csem, self.ccnt))
        for e in self.eng:
            self._wait(e, deps)

    def finish(self):
        deps = [(s, c) for s, c in zip(self.dsem, self.dcnt) if c > 0]
        deps += [(self.sem[e], self.cnt[e]) for e in self.sem]
        if self.csem is not None:
            deps.append((self.csem, self.ccnt))
        self._wait("sp", deps)


class K:
    def __init__(self):
        self.nc = bass.Bass("TRN2", target_bir_lowering=False)
        self.es = ExitStack()
        self.S = Sched(self.nc, self.es)
        self.n = 0
        self.psum = None

    def dram(self, name, shape, dt, kind):
        t = self.nc.dram_tensor(name, list(shape), dt, kind=kind).ap()
        return Tile(t)

    def sb(self, shape, dt, es=None, name=None):
        self.n += 1
        t = (es or self.es).enter_context(self.nc.sbuf_tensor(f"{name or 't'}_{self.n}", list(shape), dt))
        return Tile(t)

    def scope(self):
        k = self

        class _Scope(ExitStack):
            def __exit__(self, *a):
                k.S.barrier()
                return super().__exit__(*a)
        return _Scope()

    def banks(self):
        if self.psum is None:
            self.psum, self.pairs = [], []
            for i in range(4):
                t = self.es.enter_context(self.nc.psum_tensor(f"pp{i}", [128, 1024], F32))
                self.pairs.append(Tile(t, excl=True))
                self.psum.append(Tile(t[:, 0:512], excl=True))
                self.psum.append(Tile(t[:, 512:1024], excl=True))
        return self.psum


class Rot:
    def __init__(self, items):
        self.items = items
        self.i = 0

    def next(self):
        x = self.items[self.i % len(self.items)]
        self.i += 1
        return x


def segs(c0, n):
    out = []
    if c0 < CTX:
        hi = min(CTX, c0 + n)
        out.append((c0, hi, 1))
        if c0 + n > CTX:
            out.append((CTX, c0 + n, 0))
    else:
        out.append((c0, c0 + n, 0))
    return out


def emit_consts(k, cst_d):
    S = k.S
    c = {}
    c["ident"] = k.sb([128, 128], F32, name="ident")
    S.dma("sp", c["ident"][:], cst_d[:, :], reads=[cst_d], writes=[c["ident"]])
    c["onesf"] = k.sb([128, 128], F32, name="onesf")
    S.op("dve", lambda e: e.memset(c["onesf"][:], 1.0), writes=[c["onesf"]])
    c["onesb"] = k.sb([128, 128], BF16, name="onesb")
    S.op("dve", lambda e: e.memset(c["onesb"][:], 1.0), writes=[c["onesb"]])
    c["eps"] = k.sb([128, 1], F32, name="eps")
    S.op("dve", lambda e: e.memset(c["eps"][:], LN_EPS), writes=[c["eps"]])
    return c


def emit_mods(k, cc_d, adaw_d, adab_d, MODX):
    S = k.S
    P = k.banks()
    with k.scope() as es:
        cc = k.sb([128, 8, 2], F32, es)
        sl = k.sb([128, 8, 2], BF16, es)
        adab = k.sb([128, 48], F32, es)
        wts = Rot([k.sb([128, 8, 512], BF16, es) for _ in range(3)])
        S.dma("sp", cc[:], cc_d[:, :, :], reads=[cc_d], writes=[cc])
        S.dma("sp", adab[:], adab_d[:, :], reads=[adab_d], writes=[adab])
        S.op("act", lambda e: e.activation(out=sl[:], in_=cc[:], func=AF.Silu), reads=[cc], writes=[sl])
        ps = P[0]
        wsrc = adaw_d.t.rearrange("(k p) n -> p k n", p=128)
        for piece in range(12):
            wt = wts.next()
            S.dma("pool", wt[:], wsrc[:, :, piece * 512:(piece + 1) * 512], reads=[adaw_d], writes=[wt])
            for m in range(4):
                ma = piece * 4 + m
                for kk in range(8):
                    S.op("pe", lambda e, wt=wt, m=m, kk=kk, ma=ma: e.matmul(
                        ps[:, ma * 2:ma * 2 + 2], lhsT=wt[:, kk, m * 128:(m + 1) * 128], rhs=sl[:, kk, :],
                        start=(kk == 0), stop=(kk == 7)), reads=[wt, sl], writes=[ps])
        ps3 = ps[:, 0:96].rearrange("p (m j) -> p m j", j=2)
        mx = MODX[:, 0:6, :, :].rearrange("p w c j -> p (w c) j")
        for j in range(2):
            S.op("dve", lambda e, j=j: e.tensor_tensor(out=mx[:, :, j], in0=ps3[:, :, j], in1=adab[:, :], op=ALU.add),
                 reads=[ps, adab], writes=[MODX])
        S.op("dve", lambda e: e.tensor_scalar_add(out=MODX[:, 6, :, :], in0=MODX[:, 1, :, :], scalar1=1.0),
             reads=[MODX], writes=[MODX])
        S.op("dve", lambda e: e.tensor_scalar_add(out=MODX[:, 7, :, :], in0=MODX[:, 4, :, :], scalar1=1.0),
             reads=[MODX], writes=[MODX])


def emit_phaseA(k, hT, MODX, win_d, rope_d, o):
    S = k.S
    P = k.banks()
    with k.scope() as es:
        WIN = k.sb([128, 8, NW], BF16, es, "win")
        wsrc = win_d.t.rearrange("(k p) n -> p k n", p=128)
        for pc in range(4):
            lo, hi = pc * (NW // 4), (pc + 1) * (NW // 4)
            S.dma("pool", WIN[:, :, lo:hi], wsrc[:, :, lo:hi], reads=[win_d], writes=[WIN])
        ub = Rot([k.sb([128, 8, 512], BF16, es, "u") for _ in range(2)])
        rt = Rot([k.sb([128, 4, 512], F32, es, "rope") for _ in range(2)])
        stf = Rot([k.sb([128, 512], F32, es, "stf") for _ in range(3)])
        stb = Rot([k.sb([128, 512], BF16, es, "stb") for _ in range(3)])
        tm1 = Rot([k.sb([128, 512], F32, es, "tm1") for _ in range(2)])
        tm2 = Rot([k.sb([128, 512], F32, es, "tm2") for _ in range(2)])
        vss = Rot([k.sb([128, 2, 128], BF16, es, "vss") for _ in range(2)])
        vds = Rot([k.sb([128, 4, 128], BF16, es, "vds") for _ in range(2)])
        for v in vss.items + vds.items:
            S.op("dve", lambda e, v=v: e.memset(v[:], 1.0), writes=[v])
        pb = Rot(P)

        tiles = [(0, 256, 1)] + [(CTX + i * 512, 512, 0) for i in range(4)]
        for (c0, N, j) in tiles:
            lat = j == 0
            u = ub.next()
            for kk in range(8):
                S.op("act", lambda e, kk=kk: e.activation(
                    out=u[:, kk, 0:N], in_=hT[:, kk, c0:c0 + N], func=AF.Identity,
                    scale=MODX[:, 6, kk, j:j + 1], bias=MODX[:, 0, kk, j:j + 1]), reads=[hT, MODX], writes=[u])
            if lat:
                R = rt.next()
                S.dma("sp", R[:], rope_d[:, :, c0 - CTX:c0 - CTX + 512], reads=[rope_d], writes=[R])

            def proj(ch, ps):
                for kk in range(8):
                    S.op("pe", lambda e, kk=kk: e.matmul(
                        ps[:, 0:N], lhsT=WIN[:, kk, ch * 128:(ch + 1) * 128], rhs=u[:, kk, 0:N],
                        start=(kk == 0), stop=(kk == 7)), reads=[WIN, u], writes=[ps])

            for c in range(2):
                ps = pb.next()
                proj(c, ps)
                st = stf.next()
                S.op("act", lambda e: e.activation(out=st[:, 0:N], in_=ps[:, 0:N], func=AF.Identity),
                     reads=[ps], writes=[st])
                S.dma("sp", o["XL%d" % c][:, c0:c0 + N], st[:, 0:N], reads=[st], writes=[o["XL%d" % c]])
            for c in range(2):
                ps = pb.next()
                proj(2 + c, ps)
                st = stf.next()
                S.op("act", lambda e: e.activation(out=st[:, 0:N], in_=ps[:, 0:N], func=AF.Gelu_apprx_tanh),
                     reads=[ps], writes=[st])
                S.dma("sp", o["GT"][c * 128:(c + 1) * 128, c0:c0 + N], st[:, 0:N], reads=[st], writes=[o["GT"]])
            for (nb, sb_, n, tc, dst) in [(4, 8, 4, 0, "QS"), (12, 14, 2, 0, "KS"), (16, 18, 2, 2, "QD"),
                                          (20, 22, 2, 2, "KD")]:
                for i in range(n):
                    psA = pb.next()
                    proj(nb + i, psA)
                    st = stb.next()
                    if lat:
                        psB = pb.next()
                        proj(sb_ + i, psB)
                        t1, t2 = tm1.next(), tm2.next()
                        S.op("dve", lambda e: e.tensor_tensor(out=t1[:], in0=psA[:, :], in1=R[:, tc, :], op=ALU.mult),
                             reads=[psA, R], writes=[t1])
                        S.op("dve", lambda e: e.tensor_tensor(out=t2[:], in0=psB[:, :], in1=R[:, tc + 1, :],
                                                              op=ALU.mult), reads=[psB, R], writes=[t2])
                        S.op("pool", lambda e: e.tensor_tensor(out=st[:], in0=t1[:], in1=t2[:], op=ALU.add),
                             reads=[t1, t2], writes=[st])
                    else:
                        S.op("act", lambda e: e.activation(out=st[:, 0:N], in_=psA[:, 0:N], func=AF.Identity),
                             reads=[psA], writes=[st])
                    S.dma("sp", o[dst][i * 128:(i + 1) * 128, c0:c0 + N], st[:, 0:N], reads=[st], writes=[o[dst]])
            for blk in range(N // 128):
                ps = pb.next()
                for kk in range(8):
                    S.op("pe", lambda e, kk=kk: e.matmul(
                        ps[:, 0:384], lhsT=u[:, kk, blk * 128:(blk + 1) * 128], rhs=WIN[:, kk, 3072:3456],
                        start=(kk == 0), stop=(kk == 7)), reads=[WIN, u], writes=[ps])
                vs, vd = vss.next(), vds.next()
                S.op("act", lambda e: e.activation(out=vs[:, :, 0:64],
                                                   in_=ps[:, 0:128].rearrange("p (h d) -> p h d", d=64),
                                                   func=AF.Identity), reads=[ps], writes=[vs])
                S.op("dve", lambda e: e.tensor_copy(out=vd[:, :, 0:64],
                                                    in_=ps[:, 128:384].rearrange("p (h d) -> p h d", d=64)),
                     reads=[ps], writes=[vd])
                r0 = c0 + blk * 128
                S.dma("sp", o["VS"][r0:r0 + 128, :], vs[:].rearrange("p h d -> p (h d)"), reads=[vs],
                      writes=[o["VS"]])
                vch, vr = r0 // VDR, r0 % VDR
                S.dma("sp", o["VD%d" % vch][vr:vr + 128, :], vd[:].rearrange("p h d -> p (h d)"), reads=[vd],
                      writes=[o["VD%d" % vch]])


def emit_lru(k, mixT, C, g, prm):
    S = k.S
    P = k.banks()
    with k.scope() as es:
        xs = k.sb([128, 1 + SEQ + 2], F32, es, "xs")
        xc = k.sb([128, 1 + CTX + 2], F32, es, "xc")
        U = k.sb([128, CTX + SEQ], F32, es, "U")
        OUT = k.sb([128, NT], F32, es, "lout")
        GTs = k.sb([128, NT], F32, es, "gts")
        tR = Rot([k.sb([128, 512], F32, es, "tR") for _ in range(4)])
        tI = Rot([k.sb([128, 512], F32, es, "tI") for _ in range(4)])
        tA = Rot([k.sb([128, 512], F32, es, "tA") for _ in range(4)])
        tH = Rot([k.sb([128, 512], F32, es, "tH") for _ in range(3)])
        sp = k.sb([128, 4], F32, es, "sp")
        nsp8 = k.sb([128, 2, 2], F32, es, "nsp8")
        nsp16 = k.sb([128, 2, 2], F32, es, "nsp16")
        lam = prm["LAM"]
        spv = sp[:, 0:4]
        lamv = lam[:].rearrange("p c d -> p (c d)")
        S.op("act", lambda e: e.activation(out=spv, in_=lamv, func=AF.Exp, scale=-1.0), reads=[lam], writes=[sp])
        S.op("dve", lambda e: e.tensor_scalar_add(out=spv, in0=spv, scalar1=1.0), reads=[sp], writes=[sp])
        S.op("act", lambda e: e.activation(out=spv, in_=spv, func=AF.Ln), reads=[sp], writes=[sp])
        S.op("dve", lambda e: e.tensor_scalar_mul(out=nsp8[:].rearrange("p c d -> p (c d)"), in0=spv, scalar1=-8.0),
             reads=[sp], writes=[nsp8])
        S.op("dve", lambda e: e.tensor_scalar_mul(out=nsp16[:].rearrange("p c d -> p (c d)"), in0=spv, scalar1=-16.0),
             reads=[sp], writes=[nsp16])
        pg = Rot(P[0:8])
        CW, CB, GB, GW, SEL = prm["CW"], prm["CB"], prm["GB"], prm["GW"], prm["SEL"]
        for c in range(2):
            rows = slice(c * 128, (c + 1) * 128)
            S.op("dve", lambda e: e.memset(xs[:, 0:1], 0.0), writes=[xs])
            S.op("dve", lambda e: e.memset(xs[:, 1 + SEQ:3 + SEQ], 0.0), writes=[xs])
            S.op("dve", lambda e: e.memset(xc[:, 0:1], 0.0), writes=[xc])
            S.op("dve", lambda e: e.memset(xc[:, 1 + CTX:3 + CTX], 0.0), writes=[xc])
            XGc = g["XG"][c]
            S.dma("sp", xs[:, 1:1 + HALF], XGc[0, :, CTX:NT], reads=[XGc], writes=[xs])
            S.dma("sp", xs[:, 1 + HALF:1 + SEQ], XGc[1, :, CTX:NT], reads=[XGc], writes=[xs])
            S.dma("sp", xc[:, 1:1 + CTX], XGc[0, :, 0:CTX], reads=[XGc], writes=[xc])
            S.dma("sp", GTs[:], g["GT"][rows, :], reads=[g["GT"]], writes=[GTs])
            for (src, L, d0) in [(xc, CTX, 0), (xs, SEQ, CTX)]:
                S.op("act", lambda e: e.activation(out=U[:, d0:d0 + L], in_=src[:, 1:1 + L], func=AF.Identity,
                                                   scale=CW[:, c, 1:2], bias=CB[:, c:c + 1]),
                     reads=[src, CW, CB], writes=[U])
                for (off, wi) in [(0, 0), (2, 2), (3, 3)]:
                    S.op("dve", lambda e, off=off, wi=wi: e.scalar_tensor_tensor(
                        out=U[:, d0:d0 + L], in0=src[:, off:off + L], scalar=CW[:, c, wi:wi + 1], in1=U[:, d0:d0 + L],
                        op0=ALU.mult, op1=ALU.add), reads=[src, CW, U], writes=[U])
            S.op("dve", lambda e: e.memset(OUT[:], 0.0), writes=[OUT])
            for d in range(2):
                lat = [(CTX + i * 512, 512, i) for i in range(8)]
                if d == 1:
                    lat = lat[::-1]
                state = None
                hprev = None
                seq = [(0, CTX, -1)] + lat
                for grp_ in [seq[0:1]] + [seq[i_:i_ + 2] for i_ in range(1, 9, 2)]:
                    items = []
                    for (u0, L, kind) in grp_:
                        pss = []
                        for gi in range(2):
                            ps = pg.next()
                            S.op("pe", lambda e: e.matmul(ps[:, 0:L], lhsT=GW[:, c, d, gi, :], rhs=U[:, u0:u0 + L],
                                                          start=True, stop=True), reads=[GW, U], writes=[ps])
                            pss.append(ps)
                        r, ii, a, h = tR.next(), tI.next(), tA.next(), tH.next()
                        S.op("act", lambda e: e.activation(out=r[:, 0:L], in_=pss[0][:, 0:L], func=AF.Sigmoid,
                                                           bias=GB[:, c, d, 0:1]), reads=[pss[0], GB], writes=[r])
                        S.op("act", lambda e: e.activation(out=ii[:, 0:L], in_=pss[1][:, 0:L], func=AF.Sigmoid,
                                                           bias=GB[:, c, d, 1:2]), reads=[pss[1], GB], writes=[ii])
                        items.append((u0, L, kind, r, ii, a, h))
                    for (u0, L, kind, r, ii, a, h) in items:
                        S.op("act", lambda e: e.activation(out=a[:, 0:L], in_=r[:, 0:L], func=AF.Exp,
                                                           scale=nsp8[:, c, d:d + 1]), reads=[r, nsp8], writes=[a])
                        S.op("pool", lambda e: e.tensor_tensor(out=r[:, 0:L], in0=a[:, 0:L], in1=a[:, 0:L],
                                                               op=ALU.mult), reads=[a], writes=[r])
                    for (u0, L, kind, r, ii, a, h) in items:
                        S.op("act", lambda e: e.activation(out=r[:, 0:L], in_=r[:, 0:L], func=AF.Sqrt, scale=-1.0,
                                                           bias=1.0), reads=[r], writes=[r])
                    for (u0, L, kind, r, ii, a, h) in items:
                        S.op("dve", lambda e: e.tensor_tensor(out=ii[:, 0:L], in0=ii[:, 0:L], in1=U[:, u0:u0 + L],
                                                              op=ALU.mult), reads=[ii, U], writes=[ii])
                        S.op("dve", lambda e: e.tensor_tensor(out=ii[:, 0:L], in0=ii[:, 0:L], in1=r[:, 0:L],
                                                              op=ALU.mult), reads=[ii, r], writes=[ii])
                        if d == 0:
                            vo, va, vb = h[:, 0:L], a[:, 0:L], ii[:, 0:L]
                        else:
                            vo, va, vb = h[:, L - 1::-1], a[:, L - 1::-1], ii[:, L - 1::-1]
                        init = 0.0 if state is None else state
                        rd = [a, ii] + ([hprev] if hprev is not None else [])
                        S.op("dve", lambda e: e.tensor_tensor_scan(out=vo, data0=va, data1=vb, initial=init,
                                                                   op0=ALU.mult, op1=ALU.add), reads=rd, writes=[h])
                        state = h[:, L - 1:L] if d == 0 else h[:, 0:1]
                        hprev = h
                        if kind < 0:
                            S.op("dve", lambda e: e.tensor_tensor(out=OUT[:, 0:CTX], in0=h[:, 0:L], in1=OUT[:, 0:CTX],
                                                                  op=ALU.add), reads=[h, OUT], writes=[OUT])
                        else:
                            hf = kind // 4
                            lo = CTX + (kind % 4) * 512
                            S.op("dve", lambda e: e.scalar_tensor_tensor(
                                out=OUT[:, lo:lo + L], in0=h[:, 0:L], scalar=SEL[:, hf:hf + 1], in1=OUT[:, lo:lo + L],
                                op0=ALU.mult, op1=ALU.add), reads=[h, OUT, SEL], writes=[OUT])
            S.op("pool", lambda e: e.tensor_tensor(out=mixT[:, c, :], in0=OUT[:], in1=GTs[:], op=ALU.mult),
                 reads=[OUT, GTs], writes=[mixT])


def pipeline(n, front, back, la, deferred):
    for i in range(n + la):
        if i < n:
            front(i)
        if i >= la:
            back(i - la)
        while deferred and deferred[0][0] <= i:
            deferred.pop(0)[1]()
    while deferred:
        deferred.pop(0)[1]()


def emit_swa(k, mixT, C, g, prm):
    S = k.S
    P = k.banks()
    LA = 5
    with k.scope() as es:
        KS = [[k.sb([128, CTX + 128 + HALF + 128], BF16, es, "ks") for _ in range(2)] for _ in range(2)]
        VSa = k.sb([128, 20, 256], BF16, es, "vsa")
        MSK = k.sb([128, 8, 512], BF16, es, "msk")
        ESK = k.sb([128, 8], F32, es, "esk")
        qb = Rot([k.sb([128, 4, 512], BF16, es, "qs") for _ in range(2)])
        Eb = Rot([k.sb([128, 512], BF16, es, "E") for _ in range(6)])
        den = Rot([k.sb([128, 512], F32, es, "den") for _ in range(2)])
        S.dma("sp", MSK[:], g["MSK"][:, :, :], reads=[g["MSK"]], writes=[MSK])
        S.op("act", lambda e: e.activation(out=ESK[:], in_=prm["SINK"][:], func=AF.Exp), reads=[prm["SINK"]],
             writes=[ESK])
        for hk in range(2):
            for hp in range(2):
                T_ = KS[hk][hp]
                ps_ = slice(hp * 64, hp * 64 + 64)
                zs_ = slice((1 - hp) * 64, (1 - hp) * 64 + 64)
                rows = slice(hk * 128 + hp * 64, hk * 128 + hp * 64 + 64)
                S.op("dve", lambda e: e.memset(T_[zs_, :], 0.0), writes=[T_])
                S.dma("sp", T_[ps_, 0:CTX], g["KSO"][rows, 0:CTX], reads=[g["KSO"]], writes=[T_])
                S.dma("sp", T_[ps_, CTX + 128:CTX + 128 + HALF], g["KSO"][rows, CTX:NT], reads=[g["KSO"]],
                      writes=[T_])
                S.dma("sp", T_[ps_, CTX:CTX + 128], g["KSG"][0, rows, NT - 128:NT], reads=[g["KSG"]], writes=[T_])
                S.dma("sp", T_[ps_, CTX + 128 + HALF:], g["KSG"][1, rows, CTX:CTX + 128], reads=[g["KSG"]],
                      writes=[T_])
        vo = g["VSO"].t.rearrange("(b p) n -> p b n", p=128)
        S.dma("sp", VSa[:, 0:2, :], vo[:, 0:2, :], reads=[g["VSO"]], writes=[VSa])
        S.dma("sp", VSa[:, 3:19, :], vo[:, 2:18, :], reads=[g["VSO"]], writes=[VSa])
        S.dma("sp", VSa[:, 2, :], g["VSG"][0, NT - 128:NT, :], reads=[g["VSG"]], writes=[VSa])
        S.dma("sp", VSa[:, 19, :], g["VSG"][1, CTX:CTX + 128, :], reads=[g["VSG"]], writes=[VSa])
        accs = Rot(P[0:2])
        scs = Rot(P[2:8])
        qsrc = g["QS"].t.rearrange("(c p) n -> p c n", p=128)
        tiles = [-1, 0, 1, 2, 3]
        Qt = {}

        def load_q(t):
            N = CTX if t < 0 else 512
            c0 = 0 if t < 0 else CTX + t * 512
            Q = qb.next()
            S.dma("sp", Q[:, :, 0:N], qsrc[:, :, c0:c0 + N], reads=[g["QS"]], writes=[Q])
            Qt[t] = Q

        units = []
        for ti, t in enumerate(tiles):
            N = CTX if t < 0 else 512
            c0 = 0 if t < 0 else CTX + t * 512
            blocks = [(0, 0, None), (1, 128, None)]
            if t >= 0:
                for r in range(6):
                    mk = r
                    if t == 0 and r == 0:
                        mk = 6
                    if t == 3 and r == 5:
                        mk = 7
                    blocks.append((2 + 4 * t + r, CTX + (4 * t + r) * 128, mk))
            for h in range(8):
                for bi, blk in enumerate(blocks):
                    units.append((ti, t, N, c0, h, bi, len(blocks), blk))
        Ef = {}
        cur = {}
        load_q(tiles[0])

        def front(i):
            ti, t, N, c0, h, bi, nb, (vb, kcol, mk) = units[i]
            if h == 0 and bi == 0 and ti + 1 < len(tiles):
                load_q(tiles[ti + 1])
            Q = Qt[t]
            qc, pb, hk = h // 2, (h % 2) * 64, h // 4
            sps, E = scs.next(), Eb.next()
            S.op("pe", lambda e: e.matmul(sps[:, 0:N], lhsT=KS[hk][h % 2][:, kcol:kcol + 128],
                                          rhs=Q[:, qc, 0:N], start=True, stop=True),
                 reads=[KS[hk][h % 2], Q], writes=[sps])
            S.op("act", lambda e: e.activation(out=E[:, 0:N], in_=sps[:, 0:N], func=AF.Exp, scale=0.125),
                 reads=[sps], writes=[E])
            if mk is not None:
                S.op("pool" if i % 2 else "dve", lambda e: e.tensor_tensor(out=E[:, 0:N], in0=E[:, 0:N],
                                                                          in1=MSK[:, mk, 0:N], op=ALU.mult),
                     reads=[E, MSK], writes=[E])
            Ef[i] = E

        def back(i):
            ti, t, N, c0, h, bi, nb, (vb, kcol, mk) = units[i]
            qc, pb, hk = h // 2, (h % 2) * 64, h // 4
            E = Ef.pop(i)
            if bi == 0:
                cur["acc"] = accs.next()
            acc = cur["acc"]
            S.op("pe", lambda e: e.matmul(acc[:, 0:N], lhsT=VSa[:, vb, hk * 128:(hk + 1) * 128],
                                          rhs=E[:, 0:N], start=(bi == 0), stop=(bi == nb - 1)),
                 reads=[VSa, E], writes=[acc])
            if bi == nb - 1:
                dn = den.next()
                S.op("dve", lambda e: e.tensor_scalar(out=dn[0:64, 0:N], in0=acc[64:128, 0:N],
                                                      scalar1=ESK[64:128, h:h + 1], scalar2=None, op0=ALU.add),
                     reads=[acc, ESK], writes=[dn])
                S.op("dve", lambda e: e.reciprocal(out=dn[0:64, 0:N], in_=dn[0:64, 0:N]), reads=[dn], writes=[dn])
                S.op("dve", lambda e: e.tensor_tensor(out=mixT[pb:pb + 64, 2 + qc, c0:c0 + N], in0=acc[0:64, 0:N],
                                                      in1=dn[0:64, 0:N], op=ALU.mult), reads=[acc, dn],
                     writes=[mixT])

        pipeline(len(units), front, back, LA, [])


def emit_diff(k, mixT, C, g, prm, lam_init):
    S = k.S
    P = k.banks()
    LA = 2
    with k.scope() as es:
        KD = k.sb([128, 2, CTX + SEQ], BF16, es, "kd")
        VDa = k.sb([128, 34, 512], BF16, es, "vda")
        qb = Rot([k.sb([128, 2, 512], BF16, es, "qd") for _ in range(2)])
        qm = [Rot([k.sb([128, 2, 512], BF16, es, "qm") for _ in range(2)]) for _ in range(4)]
        Eb = Rot([k.sb([128, 1024], BF16, es, "E") for _ in range(3)])
        tr = Rot([k.sb([128, 512], F32, es, "tr") for _ in range(2)])
        tt = Rot([k.sb([128, 512], F32, es, "tt") for _ in range(2)])
        tO = Rot([k.sb([128, 512], F32, es, "tO") for _ in range(1)])
        tq = Rot([k.sb([128, 512], F32, es, "tq") for _ in range(1)])
        lt = k.sb([128, 2, 32], F32, es, "lt")
        ls = k.sb([128, 2], F32, es, "ls")
        NLAM = k.sb([128, 1], F32, es, "nlam")
        GN = k.sb([128, 1], F32, es, "gn")
        DL = prm["DL"]
        for i in range(2):
            S.op("dve", lambda e, i=i: e.tensor_tensor(out=lt[:, i, :], in0=DL[:, 2 * i, :], in1=DL[:, 2 * i + 1, :],
                                                       op=ALU.mult), reads=[DL], writes=[lt])
        S.op("dve", lambda e: e.reduce_sum(out=ls[:], in_=lt[:], axis=AX.X), reads=[lt], writes=[ls])
        S.op("act", lambda e: e.activation(out=ls[:], in_=ls[:], func=AF.Exp), reads=[ls], writes=[ls])
        S.op("dve", lambda e: e.tensor_tensor(out=NLAM[:], in0=ls[:, 1:2], in1=ls[:, 0:1], op=ALU.subtract),
             reads=[ls], writes=[NLAM])
        S.op("dve", lambda e: e.tensor_scalar_add(out=NLAM[:], in0=NLAM[:], scalar1=-lam_init), reads=[NLAM],
             writes=[NLAM])
        S.op("dve", lambda e: e.tensor_scalar_mul(out=GN[:], in0=prm["DG"][:], scalar1=1.0 - lam_init),
             reads=[prm["DG"]], writes=[GN])
        ko = g["KDO"].t.rearrange("(c p) n -> p c n", p=128)
        S.dma("sp", KD[:, :, 0:CTX], ko[:, :, 0:CTX], reads=[g["KDO"]], writes=[KD])
        for hf in range(2):
            kg = g["KDG"][hf].rearrange("(c p) n -> p c n", p=128)
            S.dma("sp", KD[:, :, CTX + hf * HALF:CTX + (hf + 1) * HALF], kg[:, :, CTX:NT], reads=[g["KDG"]],
                  writes=[KD])
            vg0 = g["VDG"][0][hf].rearrange("(b p) n -> p b n", p=128)
            vg1 = g["VDG"][1][hf].rearrange("(b p) n -> p b n", p=128)
            S.dma("sp", VDa[:, 2 + hf * 16:2 + hf * 16 + 7, :], vg0[:, 2:9, :], reads=[g["VDG"][0]], writes=[VDa])
            S.dma("sp", VDa[:, 2 + hf * 16 + 7:2 + (hf + 1) * 16, :], vg1[:, 0:9, :], reads=[g["VDG"][1]],
                  writes=[VDa])
        vo = g["VDO"].t.rearrange("(b p) n -> p b n", p=128)
        S.dma("sp", VDa[:, 0:2, :], vo[:, 0:2, :], reads=[g["VDO"]], writes=[VDa])
        accs = Rot(P[0:2])
        pn = P[2]
        scs = Rot(k.pairs[2:4])
        qsrc = g["QD"].t.rearrange("(c p) n -> p c n", p=128)
        SC = 32 ** -0.5
        tiles = [-1, 0, 1, 2, 3]
        Qt = {}

        def load_q(t):
            N = CTX if t < 0 else 512
            c0 = 0 if t < 0 else CTX + t * 512
            Q = qb.next()
            S.dma("sp", Q[:, :, 0:N], qsrc[:, :, c0:c0 + N], reads=[g["QD"]], writes=[Q])
            Qm = [qm[v].next() for v in range(4)]
            for v in range(4):
                S.op("dve", lambda e: e.tensor_scalar(out=Qm[v][:, :, 0:N], in0=Q[:, :, 0:N],
                                                      scalar1=prm["PM"][:, v:v + 1], scalar2=None, op0=ALU.mult),
                     reads=[Q, prm["PM"]], writes=[Qm[v]])
            Qt[t] = Qm

        units = []
        for ti, t in enumerate(tiles):
            N = CTX if t < 0 else 512
            c0 = 0 if t < 0 else CTX + t * 512
            nkp = 1 if t < 0 else 17
            for h in range(4):
                for m in range(2):
                    for kp in range(nkp):
                        units.append((ti, t, N, c0, h, m, kp, nkp))
        Ef = {}
        cur = {}
        deferred = []
        load_q(tiles[0])

        def front(i):
            ti, t, N, c0, h, m, kp, nkp = units[i]
            if h == 0 and m == 0 and kp == 0 and ti + 1 < len(tiles):
                load_q(tiles[ti + 1])
            Qm = Qt[t]
            c = h // 2
            sps, E = scs.next(), Eb.next()
            qv = Qm[(h % 2) * 2 + m]
            for j in range(2):
                kb = 2 * kp + j
                S.op("pe", lambda e: e.matmul(sps[:, j * 512:j * 512 + N], lhsT=KD[:, c, kb * 128:(kb + 1) * 128],
                                              rhs=qv[:, c, 0:N], start=True, stop=True),
                     reads=[KD, qv], writes=[sps])
            S.op("act", lambda e: e.activation(out=E[:].rearrange("p (j n) -> p j n", j=2)[:, :, 0:N],
                                               in_=sps[:].rearrange("p (j n) -> p j n", j=2)[:, :, 0:N],
                                               func=AF.Exp, scale=SC), reads=[sps], writes=[E])
            Ef[i] = E

        def back(i):
            ti, t, N, c0, h, m, kp, nkp = units[i]
            c = h // 2
            E = Ef.pop(i)
            if m == 0 and kp == 0:
                cur["ac"] = [accs.next(), accs.next()]
            ac = cur["ac"]
            for j in range(2):
                kb = 2 * kp + j
                S.op("pe", lambda e: e.matmul(ac[m][:, 0:N], lhsT=VDa[:, kb, h * 128:(h + 1) * 128],
                                              rhs=E[:, j * 512:j * 512 + N], start=(kb == 0), stop=(kb == 2 * nkp - 1)),
                     reads=[VDa, E], writes=[ac[m]])
            if not (m == 1 and kp == nkp - 1):
                return
            ts = []
            for mm in range(2):
                r_, t_ = tr.next(), tt.next()
                S.op("dve", lambda e: e.reciprocal(out=r_[0:64, 0:N], in_=ac[mm][64:128, 0:N]), reads=[ac[mm]],
                     writes=[r_])
                S.op("dve", lambda e: e.tensor_tensor(out=t_[0:64, 0:N], in0=ac[mm][0:64, 0:N], in1=r_[0:64, 0:N],
                                                      op=ALU.mult), reads=[ac[mm], r_], writes=[t_])
                ts.append(t_)
            O, sq = tO.next(), tq.next()
            S.op("dve", lambda e: e.scalar_tensor_tensor(out=O[0:64, 0:N], in0=ts[1][0:64, 0:N],
                                                         scalar=NLAM[0:64, 0:1], in1=ts[0][0:64, 0:N],
                                                         op0=ALU.mult, op1=ALU.add), reads=ts + [NLAM],
                 writes=[O])
            S.op("act", lambda e: e.activation(out=sq[0:64, 0:N], in_=O[0:64, 0:N], func=AF.Square), reads=[O],
                 writes=[sq])

            def tail():
                S.op("pe", lambda e: e.matmul(pn[0:64, 0:N], lhsT=C["onesf"][0:64, 0:64], rhs=sq[0:64, 0:N],
                                              start=True, stop=True), reads=[C["onesf"], sq], writes=[pn])
                S.op("act", lambda e: e.activation(out=sq[0:64, 0:N], in_=pn[0:64, 0:N], func=AF.Sqrt,
                                                   scale=1.0 / 64.0, bias=C["eps"][0:64, :]),
                     reads=[pn, C["eps"]], writes=[sq])
                S.op("dve", lambda e: e.reciprocal(out=sq[0:64, 0:N], in_=sq[0:64, 0:N]), reads=[sq], writes=[sq])
                S.op("dve", lambda e: e.tensor_tensor(out=O[0:64, 0:N], in0=O[0:64, 0:N], in1=sq[0:64, 0:N],
                                                      op=ALU.mult), reads=[O, sq], writes=[O])
                ob = (h % 2) * 64
                S.op("dve", lambda e: e.tensor_scalar(out=mixT[ob:ob + 64, 6 + c, c0:c0 + N], in0=O[0:64, 0:N],
                                                      scalar1=GN[0:64, 0:1], scalar2=None, op0=ALU.mult),
                     reads=[O, GN], writes=[mixT])
            deferred.append((i + LA + min(3, 2 * nkp - 1), tail))

        pipeline(len(units), front, back, LA, deferred)


def emit_ln(k, hT, C, c0, N, LNG, LNB, which, es, pool):
    S = k.S
    ysq = pool["ysq"].next()
    S1, S2 = pool["ps"].next(), pool["ps"].next()
    for c in range(8):
        S.op("act", lambda e, c=c: e.activation(out=ysq[:, c, 0:N], in_=hT[:, c, c0:c0 + N], func=AF.Square),
             reads=[hT], writes=[ysq])
    for c in range(8):
        S.op("pe", lambda e, c=c: e.matmul(S1[:, 0:N], lhsT=C["onesf"][:, :], rhs=hT[:, c, c0:c0 + N], start=(c == 0),
                                           stop=(c == 7)), reads=[C["onesf"], hT], writes=[S1])
    for c in range(8):
        S.op("pe", lambda e, c=c: e.matmul(S2[:, 0:N], lhsT=C["onesb"][:, :], rhs=ysq[:, c, 0:N], start=(c == 0),
                                           stop=(c == 7)), reads=[C["onesb"], ysq], writes=[S2])
    mean, rstd, nmr = pool["f"].next(), pool["f"].next(), pool["f"].next()
    S.op("act", lambda e: e.activation(out=mean[:, 0:N], in_=S1[:, 0:N], func=AF.Identity, scale=1.0 / D_MODEL),
         reads=[S1], writes=[mean])
    S.op("dve", lambda e: e.tensor_tensor(out=rstd[:, 0:N], in0=mean[:, 0:N], in1=mean[:, 0:N], op=ALU.mult),
         reads=[mean], writes=[rstd])
    S.op("dve", lambda e: e.scalar_tensor_tensor(out=rstd[:, 0:N], in0=S2[:, 0:N], scalar=1.0 / D_MODEL,
                                                 in1=rstd[:, 0:N], op0=ALU.mult, op1=ALU.subtract),
         reads=[S2, rstd], writes=[rstd])
    S.op("act", lambda e: e.activation(out=rstd[:, 0:N], in_=rstd[:, 0:N], func=AF.Sqrt, bias=C["eps"][:, :]),
         reads=[rstd, C["eps"]], writes=[rstd])
    S.op("dve", lambda e: e.reciprocal(out=rstd[:, 0:N], in_=rstd[:, 0:N]), reads=[rstd], writes=[rstd])
    S.op("dve", lambda e: e.scalar_tensor_tensor(out=nmr[:, 0:N], in0=mean[:, 0:N], scalar=-1.0, in1=rstd[:, 0:N],
                                                 op0=ALU.mult, op1=ALU.mult), reads=[mean, rstd], writes=[nmr])
    for c in range(8):
        t1 = pool["t"].next()
        S.op("dve", lambda e, c=c: e.scalar_tensor_tensor(out=t1[:, 0:N], in0=hT[:, c, c0:c0 + N],
                                                          scalar=LNG[:, which, c:c + 1], in1=rstd[:, 0:N],
                                                          op0=ALU.mult, op1=ALU.mult), reads=[hT, LNG, rstd],
             writes=[t1])
        S.op("dve", lambda e, c=c: e.scalar_tensor_tensor(out=t1[:, 0:N], in0=nmr[:, 0:N],
                                                          scalar=LNG[:, which, c:c + 1], in1=t1[:, 0:N],
                                                          op0=ALU.mult, op1=ALU.add), reads=[nmr, LNG, t1],
             writes=[t1])
        S.op("act", lambda e, c=c: e.activation(out=hT[:, c, c0:c0 + N], in_=t1[:, 0:N], func=AF.Identity,
                                                bias=LNB[:, which, c:c + 1]), reads=[t1, LNB], writes=[hT])


def ln_pool(k, es, P, W=512, nf=6):
    return dict(ysq=Rot([k.sb([128, 8, W], BF16, es, "ysq") for _ in range(2)]),
                f=Rot([k.sb([128, W], F32, es, "lnf") for _ in range(nf)]),
                t=Rot([k.sb([128, W], F32, es, "lnt") for _ in range(3)]), ps=Rot(P))


def emit_phaseC(k, hT, mixT, MODX, C, wout_d, prm):
    S = k.S
    P = k.banks()
    with k.scope() as es:
        WO = k.sb([128, 8, D_MODEL], BF16, es, "wout")
        S.dma("pool", WO[:], wout_d.t.rearrange("(k p) n -> p k n", p=128), reads=[wout_d], writes=[WO])
        lp = ln_pool(k, es, P[6:8])
        tm = Rot([k.sb([128, 512], F32, es, "ctm") for _ in range(3)])
        pb = Rot(P[0:6])
        tiles = [(0, 256)] + [(CTX + i * 512, 512) for i in range(4)]
        prev = None
        for (c0, N) in tiles:
            j = 1 if c0 < CTX else 0
            for c in range(8):
                ps = pb.next()
                for kk in range(8):
                    S.op("pe", lambda e, kk=kk: e.matmul(ps[:, 0:N], lhsT=WO[:, kk, c * 128:(c + 1) * 128],
                                                         rhs=mixT[:, kk, c0:c0 + N], start=(kk == 0), stop=(kk == 7)),
                         reads=[WO, mixT], writes=[ps])
                t = tm.next()
                S.op("dve", lambda e: e.tensor_scalar(out=t[:, 0:N], in0=ps[:, 0:N], scalar1=MODX[:, 2, c, j:j + 1],
                                                      scalar2=None, op0=ALU.mult), reads=[ps, MODX], writes=[t])
                S.op("dve", lambda e: e.scalar_tensor_tensor(out=hT[:, c, c0:c0 + N], in0=hT[:, c, c0:c0 + N],
                                                             scalar=ALPHA, in1=t[:, 0:N], op0=ALU.mult, op1=ALU.add),
                     reads=[hT, t], writes=[hT])
            if prev is not None:
                emit_ln(k, hT, C, prev[0], prev[1], prm["LNG"], prm["LNB"], 0, es, lp)
            prev = (c0, N)
        emit_ln(k, hT, C, prev[0], prev[1], prm["LNG"], prm["LNB"], 0, es, lp)


def emit_ffn(k, hT, MODX, C, prm, w13_d, w2_d, nexp, rw_d=None, tok0=0, G=NT // 2, TN=384):
    S = k.S
    P = k.banks()
    moe = nexp > 1
    with k.scope() as es:
        u2 = k.sb([128, 8, G], BF16, es, "u2")
        gT = k.sb([128, NJ, G], BF16, es, "gT")
        W13 = Rot([k.sb([128, 8, 256], BF16, es, "w13") for _ in range(3)])
        W2 = Rot([k.sb([128, NJ, 128], BF16, es, "w2") for _ in range(2)])
        sil = Rot([k.sb([128, TN], F32, es, "sil") for _ in range(2)])
        tmp = Rot([k.sb([128, TN], F32, es, "ftmp") for _ in range(2)])
        lp = ln_pool(k, es, P[6:8], TN, nf=(6 if TN < 512 else 4))
        p13 = Rot(P[0:4])
        po = Rot(P[4:6])
        pm = Rot(P[6:8])
        if moe:
            RW = k.sb([128, 8, NEXP], BF16, es, "rw")
            S.dma("pool", RW[:], rw_d.t.rearrange("(k p) e -> p k e", p=128), reads=[rw_d], writes=[RW])
            CWt = k.sb([128, G // 128, NEXP], F32, es, "cw")
            cwT = Rot([k.sb([128, G], F32, es, "cwT") for _ in range(1)])
            rt = {n: k.sb([128, NEXP], F32, es, "r" + n) for n in ["lg", "eq", "l2", "sel", "ex"]}
            rs = {n: k.sb([128, 1], F32, es, "s" + n) for n in ["m1", "m2", "nm1", "sum"]}
            dg = Rot([k.sb([128, 128], F32, es, "dg") for _ in range(2)])

        jobs = []
        for grp in range(2):
            for e_ in range(nexp):
                for jj in range(NJ):
                    jobs.append(("w13", e_, jj))
                for c in range(8):
                    jobs.append(("w2", e_, c))
        loaded = {}
        state = {"next": 0}

        def prefetch(upto):
            while state["next"] < min(upto, len(jobs)):
                i = state["next"]
                kind, e_, x = jobs[i]
                if kind == "w13":
                    w = W13.next()
                    S.dma("pool", w[:], w13_d[e_, x], reads=[w13_d], writes=[w])
                else:
                    w = W2.next()
                    S.dma("pool", w[:], w2_d[e_, x], reads=[w2_d], writes=[w])
                loaded[i] = w
                state["next"] += 1

        pending = []

        def flush_ln():
            while pending:
                gg = pending.pop(0)
                for t in range(G // TN):
                    emit_ln(k, hT, C, gg + t * TN, TN, prm["LNG"], prm["LNB"], 1, es, lp)

        ji = 0
        for grp in range(2):
            g0 = tok0 + grp * G
            for (lo, hi, j) in segs(g0, G):
                for kk in range(8):
                    S.op("act", lambda e, kk=kk: e.activation(
                        out=u2[:, kk, lo - g0:hi - g0], in_=hT[:, kk, lo:hi], func=AF.Identity,
                        scale=MODX[:, 7, kk, j:j + 1], bias=MODX[:, 3, kk, j:j + 1]), reads=[hT, MODX], writes=[u2])
            S.op("dve", lambda e: e.tensor_scalar(out=hT[:, :, g0:g0 + G], in0=hT[:, :, g0:g0 + G], scalar1=ALPHA,
                                                  scalar2=None, op0=ALU.mult), reads=[hT], writes=[hT])
            if moe:
                for blk in range(G // 128):
                    ps = pm.next()
                    for kk in range(8):
                        S.op("pe", lambda e, kk=kk: e.matmul(ps[:, 0:NEXP], lhsT=u2[:, kk, blk * 128:(blk + 1) * 128],
                                                             rhs=RW[:, kk, :], start=(kk == 0), stop=(kk == 7)),
                             reads=[u2, RW], writes=[ps])
                    lg, eq, l2, sel, ex = rt["lg"], rt["eq"], rt["l2"], rt["sel"], rt["ex"]
                    m1, m2, nm1, sm = rs["m1"], rs["m2"], rs["nm1"], rs["sum"]
                    S.op("dve", lambda e: e.tensor_tensor(out=lg[:], in0=ps[:, 0:NEXP], in1=prm["RB"][:], op=ALU.add),
                         reads=[ps, prm["RB"]], writes=[lg])
                    S.op("dve", lambda e: e.reduce_max(out=m1[:], in_=lg[:], axis=AX.X), reads=[lg], writes=[m1])
                    S.op("dve", lambda e: e.tensor_scalar(out=eq[:], in0=lg[:], scalar1=m1[:, 0:1], scalar2=None,
                                                          op0=ALU.is_equal), reads=[lg, m1], writes=[eq])
                    S.op("dve", lambda e: e.scalar_tensor_tensor(out=l2[:], in0=eq[:], scalar=-1e30, in1=lg[:],
                                                                 op0=ALU.mult, op1=ALU.add), reads=[eq, lg],
                         writes=[l2])
                    S.op("dve", lambda e: e.reduce_max(out=m2[:], in_=l2[:], axis=AX.X), reads=[l2], writes=[m2])
                    S.op("dve", lambda e: e.tensor_scalar(out=sel[:], in0=lg[:], scalar1=m2[:, 0:1], scalar2=None,
                                                          op0=ALU.is_ge), reads=[lg, m2], writes=[sel])
                    S.op("dve", lambda e: e.tensor_scalar_mul(out=nm1[:], in0=m1[:], scalar1=-1.0), reads=[m1],
                         writes=[nm1])
                    S.op("act", lambda e: e.activation(out=ex[:], in_=lg[:], func=AF.Exp, bias=nm1[:, 0:1]),
                         reads=[lg, nm1], writes=[ex])
                    S.op("dve", lambda e: e.tensor_tensor(out=ex[:], in0=ex[:], in1=sel[:], op=ALU.mult),
                         reads=[ex, sel], writes=[ex])
                    S.op("dve", lambda e: e.reduce_sum(out=sm[:], in_=ex[:], axis=AX.X), reads=[ex], writes=[sm])
                    S.op("dve", lambda e: e.reciprocal(out=sm[:], in_=sm[:]), reads=[sm], writes=[sm])
                    S.op("dve", lambda e: e.tensor_scalar(out=CWt[:, blk, :], in0=ex[:], scalar1=sm[:, 0:1],
                                                          scalar2=None, op0=ALU.mult), reads=[ex, sm], writes=[CWt])
            for e_ in range(nexp):
                if moe:
                    cw = cwT.next()
                    for b0 in range(0, G // 128, 4):
                        nb = min(4, G // 128 - b0)
                        ps = pm.next()
                        for bb in range(nb):
                            blk = b0 + bb
                            d_ = dg.next()
                            S.op("dve", lambda e: e.tensor_scalar(out=d_[:], in0=C["ident"][:],
                                                                  scalar1=CWt[:, blk, e_:e_ + 1], scalar2=None,
                                                                  op0=ALU.mult), reads=[C["ident"], CWt], writes=[d_])
                            S.op("pe", lambda e: e.matmul(ps[:, bb * 128:(bb + 1) * 128], lhsT=C["onesf"][:, :],
                                                          rhs=d_[:], start=True, stop=True),
                                 reads=[C["onesf"], d_], writes=[ps])
                        S.op("act", lambda e: e.activation(out=cw[:, b0 * 128:(b0 + nb) * 128], in_=ps[:, 0:nb * 128],
                                                           func=AF.Identity), reads=[ps], writes=[cw])
                for jj in range(NJ):
                    if e_ == 0 and jj == 6:
                        flush_ln()
                    prefetch(ji + 3)
                    w = loaded.pop(ji)
                    ji += 1
                    for t in range(G // TN):
                        p1, p3 = p13.next(), p13.next()
                        for (pp, wo) in [(p1, 0), (p3, 128)]:
                            for kk in range(8):
                                S.op("pe", lambda e, kk=kk: e.matmul(pp[:, 0:TN], lhsT=w[:, kk, wo:wo + 128],
                                                                     rhs=u2[:, kk, t * TN:(t + 1) * TN],
                                                                     start=(kk == 0), stop=(kk == 7)),
                                     reads=[w, u2], writes=[pp])
                        s_ = sil.next()
                        S.op("act", lambda e: e.activation(out=s_[:], in_=p1[:, 0:TN], func=AF.Silu), reads=[p1],
                             writes=[s_])
                        S.op("dve", lambda e: e.tensor_tensor(out=gT[:, jj, t * TN:(t + 1) * TN], in0=s_[:],
                                                              in1=p3[:, 0:TN], op=ALU.mult), reads=[s_, p3],
                             writes=[gT])
                for c in range(8):
                    prefetch(ji + 2)
                    w = loaded.pop(ji)
                    ji += 1
                    for t in range(G // TN):
                        pso = po.next()
                        for jj in range(NJ):
                            S.op("pe", lambda e, jj=jj: e.matmul(pso[:, 0:TN], lhsT=w[:, jj, :],
                                                                 rhs=gT[:, jj, t * TN:(t + 1) * TN], start=(jj == 0),
                                                                 stop=(jj == NJ - 1)), reads=[w, gT], writes=[pso])
                        for (lo, hi, j) in segs(g0 + t * TN, TN):
                            a, b = lo - g0 - t * TN, hi - g0 - t * TN
                            if moe:
                                tp = tmp.next()
                                S.op("dve", lambda e: e.tensor_tensor(out=tp[:, a:b], in0=pso[:, a:b],
                                                                      in1=cw[:, lo - g0:hi - g0], op=ALU.mult),
                                     reads=[pso, cw], writes=[tp])
                                S.op("dve", lambda e: e.scalar_tensor_tensor(
                                    out=hT[:, c, lo:hi], in0=tp[:, a:b], scalar=MODX[:, 5, c, j:j + 1],
                                    in1=hT[:, c, lo:hi], op0=ALU.mult, op1=ALU.add), reads=[tp, MODX, hT],
                                    writes=[hT])
                            else:
                                S.op("dve", lambda e: e.scalar_tensor_tensor(
                                    out=hT[:, c, lo:hi], in0=pso[:, a:b], scalar=MODX[:, 5, c, j:j + 1],
                                    in1=hT[:, c, lo:hi], op0=ALU.mult, op1=ALU.add), reads=[pso, MODX, hT],
                                    writes=[hT])
            pending.append(g0)
        flush_ln()


SMALL = dict(CW=[128, 2, 4], CB=[128, 2], GB=[128, 2, 2, 2], LAM=[128, 2, 2], GW=[128, 2, 2, 2, 128], SEL=[128, 2],
             SINK=[128, 8], DL=[128, 4, 32], DG=[128, 1], PM=[128, 4], LNG=[128, 2, 8], LNB=[128, 2, 8], RB=[128, 8])
VDR = NT // 2
A_OUT = dict(XL0=([128, NT], F32), XL1=([128, NT], F32), GT=([256, NT], F32), QS=([512, NT], BF16),
             KS=([256, NT], BF16), VS=([NT, 256], BF16), QD=([256, NT], BF16), KD=([256, NT], BF16),
             VD0=([VDR, 512], BF16), VD1=([VDR, 512], BF16))


def to_fm(x2d):
    return np.ascontiguousarray(x2d.T.reshape(8, 128, -1).transpose(1, 0, 2))


def from_fm(h):
    return np.ascontiguousarray(h.transpose(1, 0, 2).reshape(D_MODEL, -1).T)


def win_perm():
    lx, lg, sq, sk, sv, dq, dk, dv = 0, 256, 512, 1024, 1152, 1280, 1536, 1792
    cols = []
    cols += list(range(lx, lx + 256))
    cols += list(range(lg, lg + 256))
    cols += list(range(sq, sq + 512))
    sw64 = lambda base, h: [base + h * 64 + ((d + 32) % 64) for d in range(64)]
    for h in range(8):
        cols += sw64(sq, h)
    for hk in range(2):
        cols += list(range(sk + hk * 64, sk + hk * 64 + 64)) * 2
    for hk in range(2):
        cols += sw64(sk, hk) * 2
    sw32 = lambda base: [base + b * 32 + ((d + 16) % 32) for b in range(8) for d in range(32)]
    cols += list(range(dq, dq + 256))
    cols += sw32(dq)
    cols += list(range(dk, dk + 256))
    cols += sw32(dk)
    cols += list(range(sv, sv + 128))
    cols += list(range(dv, dv + 256))
    assert len(cols) == NW
    return np.array(cols)


def rope_tables(half):
    t = np.arange(HALF, dtype=np.float32) + np.float32(half * HALF)
    row = np.floor(t / 64).astype(np.float32)
    col = (t - row * 64).astype(np.float32)
    out = np.zeros((128, 4, HALF), np.float32)
    for (ti, hd) in [(0, 64), (2, 32)]:
        nf = hd // 4
        inv = (np.float32(10000.0) ** (-np.arange(nf, dtype=np.float32) / np.float32(nf))).astype(np.float32)
        ang = np.concatenate([row[:, None] * inv, col[:, None] * inv], -1).astype(np.float32)
        cs, sn = np.cos(ang).astype(np.float32), np.sin(ang).astype(np.float32)
        for p in range(128):
            d = p % hd
            jx = d % (hd // 2)
            out[p, ti] = cs[:, jx]
            out[p, ti + 1] = -sn[:, jx] if d < hd // 2 else sn[:, jx]
    return out


def swa_masks(half):
    m = np.zeros((128, 8, 512), np.float32)
    kk = np.arange(128)[:, None]
    q = np.arange(512)[None, :]
    for r in range(6):
        m[:, r] = (np.abs((r - 1) * 128 + kk - q) <= 128)
    m[:, 6] = m[:, 0] if half == 1 else 0.0
    m[:, 7] = m[:, 5] if half == 0 else 0.0
    return m.astype(NPBF)


def rep(v):
    return np.ascontiguousarray(np.broadcast_to(np.asarray(v, np.float32).reshape(1, -1), (128, np.size(v))))


def small_params(inp, layer, half):
    f = lambda a: np.ascontiguousarray(np.asarray(a, np.float32))
    p = {}
    p["CW"] = f(inp["lru_conv_w"][layer].reshape(4, 2, 128).transpose(2, 1, 0))
    p["CB"] = f(inp["lru_conv_b"][layer].reshape(2, 128).T)
    p["GB"] = f(inp["lru_gate_b"][layer].reshape(2, 2, 2, 128).transpose(3, 2, 0, 1))
    p["LAM"] = f(inp["lru_lam"][layer].reshape(2, 2, 128).transpose(2, 1, 0))
    gw = np.zeros((128, 2, 2, 2, 128), np.float32)
    w = inp["lru_gate_w"][layer]
    for c in range(2):
        for bb in range(2):
            blk = c * 2 + bb
            gw[bb * 64:(bb + 1) * 64, c, :, :, bb * 64:(bb + 1) * 64] = w[:, :, blk].transpose(2, 0, 1, 3)
    p["GW"] = gw
    sel = np.zeros((128, 2), np.float32)
    sel[:, half] = 1.0
    p["SEL"] = sel
    p["SINK"] = rep(inp["swa_sink"][layer])
    p["DL"] = rep(inp["diff_lam"][layer].reshape(-1)).reshape(128, 4, 32)
    p["DG"] = f(np.tile(inp["diff_norm_g"][layer], 2).reshape(128, 1))
    pm = np.zeros((128, 4), np.float32)
    for hp in range(2):
        for m in range(2):
            pm[:, hp * 2 + m] = ((np.arange(128) // 64) == hp) & (((np.arange(128) % 64) // 32) == m)
    p["PM"] = pm
    p["LNG"] = f(inp["ln_g"][layer].reshape(2, 8, 128).transpose(2, 0, 1))
    p["LNB"] = f(inp["ln_b"][layer].reshape(2, 8, 128).transpose(2, 0, 1))
    if layer % 2 == 1:
        p["RB"] = rep(inp["moe_router_b"][layer // 2])
    else:
        p["RB"] = np.zeros((128, 8), np.float32)
    return p


def ffn_layout(w1, w3, w2):
    E = w1.shape[0]
    a = w1.reshape(E, 8, 128, NJ, 128).transpose(0, 3, 2, 1, 4)
    b = w3.reshape(E, 8, 128, NJ, 128).transpose(0, 3, 2, 1, 4)
    w13 = np.ascontiguousarray(np.concatenate([a, b], axis=-1))
    w2r = np.ascontiguousarray(w2.reshape(E, NJ, 128, 8, 128).transpose(0, 3, 2, 1, 4))
    return w13, w2r


_PROGS = {}


def _prog(key, fn):
    if key not in _PROGS:
        _PROGS[key] = fn()
    return _PROGS[key]


PAIRS = [[0, 1], [2, 3], [4, 5], [6, 7]]
PUB = ["XL0", "XL1", "KS", "VS", "KD", "VD0", "VD1"]


def build_fused(depth=DEPTH):
    k = K()
    S = k.S
    nc = k.nc
    I = lambda n, s, dt: k.dram(n, s, dt, "ExternalInput")
    hT_d = I("hT", [128, 8, NT], F32)
    cc_d = I("cc", [128, 8, 2], F32)
    cst_d = I("ident", [128, 128], F32)
    rope_d = I("rope", [128, 4, HALF], F32)
    msk_d = I("MSK", [128, 8, 512], BF16)
    L = []
    for l in range(depth):
        moe = l % 2 == 1
        nexp = NEXP if moe else 1
        d = dict(adaw=I(f"adaw{l}", [D_MODEL, 6 * D_MODEL], F32), adab=I(f"adab{l}", [128, 48], F32),
                 win=I(f"win{l}", [D_MODEL, NW], F32), wout=I(f"wout{l}", [D_MODEL, D_MODEL], F32),
                 w13=I(f"w13_{l}", [nexp, NJ, 128, 8, 256], F32), w2=I(f"w2_{l}", [nexp, 8, 128, NJ, 128], F32),
                 rw=I(f"rw{l}", [D_MODEL, NEXP], F32) if moe else None,
                 sm={n: I(f"{n}{l}", s_, F32) for n, s_ in SMALL.items()})
        L.append(d)
    out_d = k.dram("hout", [128, 8, NT], F32, "ExternalOutput")
    scr = []
    for par in range(2):
        o = {n: Tile(nc.dram_tensor(f"{n}_{par}", list(sh), dt).ap()) for n, (sh, dt) in A_OUT.items()}
        gth = {n: Tile(nc.dram_tensor(f"{n}G_{par}", [2 * A_OUT[n][0][0], A_OUT[n][0][1]], A_OUT[n][1]).ap())
               for n in PUB}
        scr.append((o, gth))
    with k.es:
        hT = k.sb([128, 8, NT], F32, name="hT")
        MODX = k.sb([128, 8, 8, 2], F32, name="modx")
        S.dma("sp", hT[:], hT_d[:, :, :], reads=[hT_d], writes=[hT])
        C = emit_consts(k, cst_d)
        prm = {n: k.sb(s_, F32, name=n) for n, s_ in SMALL.items()}
        for l in range(depth):
            moe = l % 2 == 1
            lam_init = 0.8 - 0.6 * math.exp(-0.3 * l)
            o, gth = scr[l % 2]
            emit_mods(k, cc_d, L[l]["adaw"], L[l]["adab"], MODX)
            emit_phaseA(k, hT, MODX, L[l]["win"], rope_d, o)
            for n in PUB:
                S.coll("AllGather", PAIRS, o[n], gth[n])
            for n in SMALL:
                S.dma("sp", prm[n][:], L[l]["sm"][n].t, reads=[L[l]["sm"][n]], writes=[prm[n]])

            def G(n):
                t = Tile(gth[n].t.rearrange("(h r) n -> h r n", h=2))
                t.b = gth[n].b
                return t
            g = dict(XG=[G("XL0"), G("XL1")], GT=o["GT"], QS=o["QS"], KSO=o["KS"], KSG=G("KS"), VSO=o["VS"],
                     VSG=G("VS"), QD=o["QD"], KDO=o["KD"], KDG=G("KD"), VDO=o["VD0"], VDG=[G("VD0"), G("VD1")],
                     MSK=msk_d)
            with k.scope() as es:
                mixT = k.sb([128, 8, NT], BF16, es, "mixT")
                emit_lru(k, mixT, C, g, prm)
                emit_swa(k, mixT, C, g, prm)
                emit_diff(k, mixT, C, g, prm, lam_init)
                emit_phaseC(k, hT, mixT, MODX, C, L[l]["wout"], prm)
            if l == depth - 1:
                emit_ffn(k, hT, MODX, C, prm, L[l]["w13"], L[l]["w2"], NEXP if moe else 1, L[l]["rw"],
                         tok0=CTX, G=HALF // 2, TN=512)
            else:
                emit_ffn(k, hT, MODX, C, prm, L[l]["w13"], L[l]["w2"], NEXP if moe else 1, L[l]["rw"])
        S.dma("sp", out_d[:, :, :], hT[:], reads=[hT], writes=[out_d])
        S.finish()
    return k.nc


def fused_inputs(inp, depth=DEPTH):
    x, c, ctx, c_ctx = inp["x"], inp["c"], inp["ctx"], inp["c_ctx"]
    cores = [(b, hf) for b in range(BATCH) for hf in range(2)]
    perm = win_perm()
    ropes = [rope_tables(hf) for hf in range(2)]
    masks = [swa_masks(hf) for hf in range(2)]
    shared = dict(ident=np.eye(128, dtype=np.float32))
    for l in range(depth):
        j = l // 2
        shared[f"adaw{l}"] = np.ascontiguousarray(inp["ada_w"][l])
        shared[f"adab{l}"] = np.ascontiguousarray(inp["ada_b"][l].reshape(48, 128).T)
        shared[f"win{l}"] = np.ascontiguousarray(inp["w_in"][l][:, perm])
        shared[f"wout{l}"] = np.ascontiguousarray(inp["w_out"][l])
        if l % 2 == 0:
            w13, w2r = ffn_layout(inp["ffn_w1"][j][None], inp["ffn_w3"][j][None], inp["ffn_w2"][j][None])
        else:
            w13, w2r = ffn_layout(inp["moe_w1"][j], inp["moe_w3"][j], inp["moe_w2"][j])
            shared[f"rw{l}"] = np.ascontiguousarray(inp["moe_router_w"][j])
        shared[f"w13_{l}"] = w13
        shared[f"w2_{l}"] = w2r
    in_maps = []
    for (b, hf) in cores:
        m = dict(shared)
        toks = np.concatenate([ctx[b], x[b, hf * HALF:(hf + 1) * HALF]], 0)
        m["hT"] = to_fm(toks)
        m["cc"] = np.ascontiguousarray(np.stack([c[b].reshape(8, 128).T, c_ctx.reshape(8, 128).T], -1))
        m["rope"] = ropes[hf]
        m["MSK"] = masks[hf]
        for l in range(depth):
            for n, v in small_params(inp, l, hf).items():
                m[f"{n}{l}"] = v
        in_maps.append(m)
    return cores, in_maps


def kernel(**inp):
    inp = {k_: np.asarray(v) for k_, v in inp.items()}
    cores, in_maps = fused_inputs(inp)
    res = run_bass_kernel_spmd(_prog("fused", build_fused), in_maps, core_ids=list(range(NCORES))).results
    out = np.zeros((BATCH, SEQ, D_MODEL), np.float32)
    for ci, (b, hf) in enumerate(cores):
        out[b, hf * HALF:(hf + 1) * HALF] = from_fm(np.asarray(res[ci]["hout"]))[CTX:]
    return out
```

```python
import math
from contextlib import ExitStack

import numpy as np
import ml_dtypes

import concourse.bass as bass
import concourse.mybir as mybir
from concourse.bass_utils import run_bass_kernel_spmd

F32 = mybir.dt.float32
BF16 = mybir.dt.bfloat16
AF = mybir.ActivationFunctionType
ALU = mybir.AluOpType
AX = mybir.AxisListType
NPBF = ml_dtypes.bfloat16

D_MODEL = 1024
BATCH = 4
SEQ = 4096
DEPTH = 4
CTX = 256
HALF = SEQ // 2
NT = CTX + HALF
D_FF = 2816
NJ = D_FF // 128
NEXP = 8
LN_EPS = 1e-5
ALPHA = (2.0 * DEPTH) ** 0.25
NW = 24 * 128 + 384
NCORES = 8


class Buf:
    __slots__ = ("w", "r", "excl")

    def __init__(self, excl=False):
        self.w = None
        self.r = {}
        self.excl = excl


class Tile:
    def __init__(self, t, excl=False):
        self.t = t
        self.b = Buf(excl)

    def __getitem__(self, idx):
        return self.t[idx]


class Sched:
    EPOCH = 16000
    NDMA = 28

    def __init__(self, nc, es):
        self.nc, self.es = nc, es
        self.eng = dict(pe=nc.tensor, act=nc.scalar, dve=nc.vector, pool=nc.gpsimd, sp=nc.sync)
        self.cnt = {}
        self.sem = {}
        self.own = {e: set() for e in self.eng}
        self.nsem = 0
        self.waited = {e: {} for e in self.eng}
        self.dsem = []
        self.dcnt = []
        self.dn = 0
        self.allsems = []
        self.csem = None
        self.ccnt = 0

    def _newsem(self, name):
        self.nsem += 1
        s = self.es.enter_context(self.nc.semaphore(f"{name}_{self.nsem}"))
        self.allsems.append(s)
        return s

    def _wait(self, e, deps):
        w = self.waited[e]
        for sem, val in deps:
            if w.get(sem, 0) < val:
                self.eng[e].wait_ge(sem, val)
                w[sem] = val

    def _deps(self, e, reads, writes):
        deps = []
        own = self.own[e]
        for b in reads:
            if b.w is not None:
                deps.append(b.w)
            if b.excl:
                deps.extend(b.r.items())
        for b in writes:
            if b.w is not None:
                deps.append(b.w)
            deps.extend(x for x in b.r.items() if x[0] not in own)
        if e == "pe":
            deps = [d for d in deps if d[0] not in own]
        return deps

    def _mark(self, tok, reads, writes):
        for b in reads:
            if b.excl:
                b.w = tok
                b.r = {}
            else:
                b.r[tok[0]] = tok[1]
        for b in writes:
            b.w = tok
            b.r = {}

    def op(self, e, fn, reads=(), writes=()):
        reads = [x.b if isinstance(x, Tile) else x for x in reads]
        writes = [x.b if isinstance(x, Tile) else x for x in writes]
        self._wait(e, self._deps(e, reads, writes))
        ins = fn(self.eng[e])
        if self.cnt.get(e, self.EPOCH) >= self.EPOCH:
            self.sem[e] = self._newsem(e)
            self.cnt[e] = 0
            self.own[e].add(self.sem[e])
        self.cnt[e] += 1
        ins.then_inc(self.sem[e], 1)
        tok = (self.sem[e], self.cnt[e])
        self._mark(tok, reads, writes)
        return tok

    def dma(self, q, out, in_, reads=(), writes=()):
        reads = [x.b if isinstance(x, Tile) else x for x in reads]
        writes = [x.b if isinstance(x, Tile) else x for x in writes]
        self._wait(q, self._deps(q, reads, writes))
        i = self.dn % self.NDMA
        self.dn += 1
        if i >= len(self.dsem):
            self.dsem.append(self._newsem("d"))
            self.dcnt.append(0)
        sem = self.dsem[i]
        if self.dcnt[i] > 0:
            self._wait(q, [(sem, self.dcnt[i])])
        self.dcnt[i] += 16
        self.eng[q].dma_start(out=out, in_=in_).then_inc(sem, 16)
        tok = (sem, self.dcnt[i])
        self._mark(tok, reads, writes)
        return tok

    def coll(self, kind, groups, src, dst):
        q = "pool"
        reads, writes = [src.b], [dst.b]
        self._wait(q, self._deps(q, reads, writes))
        if self.csem is None:
            self.csem = self._newsem("cc")
            self.ccnt = 0
        self.ccnt += 1
        self.nc.gpsimd.collective_compute(kind, ALU.bypass, replica_groups=groups, ins=[src.t.opt()],
                                          outs=[dst.t.opt()]).then_inc(self.csem, 1)
        tok = (self.csem, self.ccnt)
        self._mark(tok, reads, writes)
        return tok

    def barrier(self):
        deps = [(s, c) for s, c in zip(self.dsem, self.dcnt) if c > 0]
        deps += [(self.sem[e], self.cnt[e]) for e in self.sem]
        if self.csem is not None:
            deps.append((self.csem, self.ccnt))
        for e in self.eng:
            self._wait(e, deps)

    def finish(self):
        deps = [(s, c) for s, c in zip(self.dsem, self.dcnt) if c > 0]
        deps += [(self.sem[e], self.cnt[e]) for e in self.sem]
        if self.csem is not None:
            deps.append((self.csem, self.ccnt))
        self._wait("sp", deps)


class K:
    def __init__(self):
        self.nc = bass.Bass("TRN2", target_bir_lowering=False)
        self.es = ExitStack()
        self.S = Sched(self.nc, self.es)
        self.n = 0
        self.psum = None

    def dram(self, name, shape, dt, kind):
        t = self.nc.dram_tensor(name, list(shape), dt, kind=kind).ap()
        return Tile(t)

    def sb(self, shape, dt, es=None, name=None):
        self.n += 1
        t = (es or self.es).enter_context(self.nc.sbuf_tensor(f"{name or 't'}_{self.n}", list(shape), dt))
        return Tile(t)

    def scope(self):
        k = self

        class _Scope(ExitStack):
            def __exit__(self, *a):
                k.S.barrier()
                return super().__exit__(*a)
        return _Scope()

    def banks(self):
        if self.psum is None:
            self.psum, self.pairs = [], []
            for i in range(4):
                t = self.es.enter_context(self.nc.psum_tensor(f"pp{i}", [128, 1024], F32))
                self.pairs.append(Tile(t, excl=True))
                self.psum.append(Tile(t[:, 0:512], excl=True))
                self.psum.append(Tile(t[:, 512:1024], excl=True))
        return self.psum


class Rot:
    def __init__(self, items):
        self.items = items
        self.i = 0

    def next(self):
        x = self.items[self.i % len(self.items)]
        self.i += 1
        return x


def segs(c0, n):
    out = []
    if c0 < CTX:
        hi = min(CTX, c0 + n)
        out.append((c0, hi, 1))
        if c0 + n > CTX:
            out.append((CTX, c0 + n, 0))
    else:
        out.append((c0, c0 + n, 0))
    return out


def emit_consts(k, cst_d):
    S = k.S
    c = {}
    c["ident"] = k.sb([128, 128], F32, name="ident")
    S.dma("sp", c["ident"][:], cst_d[:, :], reads=[cst_d], writes=[c["ident"]])
    c["onesf"] = k.sb([128, 128], F32, name="onesf")
    S.op("dve", lambda e: e.memset(c["onesf"][:], 1.0), writes=[c["onesf"]])
    c["onesb"] = k.sb([128, 128], BF16, name="onesb")
    S.op("dve", lambda e: e.memset(c["onesb"][:], 1.0), writes=[c["onesb"]])
    c["eps"] = k.sb([128, 1], F32, name="eps")
    S.op("dve", lambda e: e.memset(c["eps"][:], LN_EPS), writes=[c["eps"]])
    return c


def emit_mods(k, cc_d, adaw_d, adab_d, MODX):
    S = k.S
    P = k.banks()
    with k.scope() as es:
        cc = k.sb([128, 8, 2], F32, es)
        sl = k.sb([128, 8, 2], BF16, es)
        adab = k.sb([128, 48], F32, es)
        wts = Rot([k.sb([128, 8, 512], BF16, es) for _ in range(3)])
        S.dma("sp", cc[:], cc_d[:, :, :], reads=[cc_d], writes=[cc])
        S.dma("sp", adab[:], adab_d[:, :], reads=[adab_d], writes=[adab])
        S.op("act", lambda e: e.activation(out=sl[:], in_=cc[:], func=AF.Silu), reads=[cc], writes=[sl])
        ps = P[0]
        wsrc = adaw_d.t.rearrange("(k p) n -> p k n", p=128)
        for piece in range(12):
            wt = wts.next()
            S.dma("pool", wt[:], wsrc[:, :, piece * 512:(piece + 1) * 512], reads=[adaw_d], writes=[wt])
            for m in range(4):
                ma = piece * 4 + m
                for kk in range(8):
                    S.op("pe", lambda e, wt=wt, m=m, kk=kk, ma=ma: e.matmul(
                        ps[:, ma * 2:ma * 2 + 2], lhsT=wt[:, kk, m * 128:(m + 1) * 128], rhs=sl[:, kk, :],
                        start=(kk == 0), stop=(kk == 7)), reads=[wt, sl], writes=[ps])
        ps3 = ps[:, 0:96].rearrange("p (m j) -> p m j", j=2)
        mx = MODX[:, 0:6, :, :].rearrange("p w c j -> p (w c) j")
        for j in range(2):
            S.op("dve", lambda e, j=j: e.tensor_tensor(out=mx[:, :, j], in0=ps3[:, :, j], in1=adab[:, :], op=ALU.add),
                 reads=[ps, adab], writes=[MODX])
        S.op("dve", lambda e: e.tensor_scalar_add(out=MODX[:, 6, :, :], in0=MODX[:, 1, :, :], scalar1=1.0),
             reads=[MODX], writes=[MODX])
        S.op("dve", lambda e: e.tensor_scalar_add(out=MODX[:, 7, :, :], in0=MODX[:, 4, :, :], scalar1=1.0),
             reads=[MODX], writes=[MODX])


def emit_phaseA(k, hT, MODX, win_d, rope_d, o):
    S = k.S
    P = k.banks()
    with k.scope() as es:
        WIN = k.sb([128, 8, NW], BF16, es, "win")
        wsrc = win_d.t.rearrange("(k p) n -> p k n", p=128)
        for pc in range(4):
            lo, hi = pc * (NW // 4), (pc + 1) * (NW // 4)
            S.dma("pool", WIN[:, :, lo:hi], wsrc[:, :, lo:hi], reads=[win_d], writes=[WIN])
        ub = Rot([k.sb([128, 8, 512], BF16, es, "u") for _ in range(2)])
        rt = Rot([k.sb([128, 4, 512], F32, es, "rope") for _ in range(2)])
        stf = Rot([k.sb([128, 512], F32, es, "stf") for _ in range(3)])
        stb = Rot([k.sb([128, 512], BF16, es, "stb") for _ in range(3)])
        tm1 = Rot([k.sb([128, 512], F32, es, "tm1") for _ in range(2)])
        tm2 = Rot([k.sb([128, 512], F32, es, "tm2") for _ in range(2)])
        vss = Rot([k.sb([128, 2, 128], BF16, es, "vss") for _ in range(2)])
        vds = Rot([k.sb([128, 4, 128], BF16, es, "vds") for _ in range(2)])
        for v in vss.items + vds.items:
            S.op("dve", lambda e, v=v: e.memset(v[:], 1.0), writes=[v])
        pb = Rot(P)

        tiles = [(0, 256, 1)] + [(CTX + i * 512, 512, 0) for i in range(4)]
        for (c0, N, j) in tiles:
            lat = j == 0
            u = ub.next()
            for kk in range(8):
                S.op("act", lambda e, kk=kk: e.activation(
                    out=u[:, kk, 0:N], in_=hT[:, kk, c0:c0 + N], func=AF.Identity,
                    scale=MODX[:, 6, kk, j:j + 1], bias=MODX[:, 0, kk, j:j + 1]), reads=[hT, MODX], writes=[u])
            if lat:
                R = rt.next()
                S.dma("sp", R[:], rope_d[:, :, c0 - CTX:c0 - CTX + 512], reads=[rope_d], writes=[R])

            def proj(ch, ps):
                for kk in range(8):
                    S.op("pe", lambda e, kk=kk: e.matmul(
                        ps[:, 0:N], lhsT=WIN[:, kk, ch * 128:(ch + 1) * 128], rhs=u[:, kk, 0:N],
                        start=(kk == 0), stop=(kk == 7)), reads=[WIN, u], writes=[ps])

            for c in range(2):
                ps = pb.next()
                proj(c, ps)
                st = stf.next()
                S.op("act", lambda e: e.activation(out=st[:, 0:N], in_=ps[:, 0:N], func=AF.Identity),
                     reads=[ps], writes=[st])
                S.dma("sp", o["XL%d" % c][:, c0:c0 + N], st[:, 0:N], reads=[st], writes=[o["XL%d" % c]])
            for c in range(2):
                ps = pb.next()
                proj(2 + c, ps)
                st = stf.next()
                S.op("act", lambda e: e.activation(out=st[:, 0:N], in_=ps[:, 0:N], func=AF.Gelu_apprx_tanh),
                     reads=[ps], writes=[st])
                S.dma("sp", o["GT"][c * 128:(c + 1) * 128, c0:c0 + N], st[:, 0:N], reads=[st], writes=[o["GT"]])
            for (nb, sb_, n, tc, dst) in [(4, 8, 4, 0, "QS"), (12, 14, 2, 0, "KS"), (16, 18, 2, 2, "QD"),
                                          (20, 22, 2, 2, "KD")]:
                for i in range(n):
                    psA = pb.next()
                    proj(nb + i, psA)
                    st = stb.next()
                    if lat:
                        psB = pb.next()
                        proj(sb_ + i, psB)
                        t1, t2 = tm1.next(), tm2.next()
                        S.op("dve", lambda e: e.tensor_tensor(out=t1[:], in0=psA[:, :], in1=R[:, tc, :], op=ALU.mult),
                             reads=[psA, R], writes=[t1])
                        S.op("dve", lambda e: e.tensor_tensor(out=t2[:], in0=psB[:, :], in1=R[:, tc + 1, :],
                                                              op=ALU.mult), reads=[psB, R], writes=[t2])
                        S.op("pool", lambda e: e.tensor_tensor(out=st[:], in0=t1[:], in1=t2[:], op=ALU.add),
                             reads=[t1, t2], writes=[st])
                    else:
                        S.op("act", lambda e: e.activation(out=st[:, 0:N], in_=psA[:, 0:N], func=AF.Identity),
                             reads=[psA], writes=[st])
                    S.dma("sp", o[dst][i * 128:(i + 1) * 128, c0:c0 + N], st[:, 0:N], reads=[st], writes=[o[dst]])
            for blk in range(N // 128):
                ps = pb.next()
                for kk in range(8):
                    S.op("pe", lambda e, kk=kk: e.matmul(
                        ps[:, 0:384], lhsT=u[:, kk, blk * 128:(blk + 1) * 128], rhs=WIN[:, kk, 3072:3456],
                        start=(kk == 0), stop=(kk == 7)), reads=[WIN, u], writes=[ps])
                vs, vd = vss.next(), vds.next()
                S.op("act", lambda e: e.activation(out=vs[:, :, 0:64],
                                                   in_=ps[:, 0:128].rearrange("p (h d) -> p h d", d=64),
                                                   func=AF.Identity), reads=[ps], writes=[vs])
                S.op("dve", lambda e: e.tensor_copy(out=vd[:, :, 0:64],
                                                    in_=ps[:, 128:384].rearrange("p (h d) -> p h d", d=64)),
                     reads=[ps], writes=[vd])
                r0 = c0 + blk * 128
                S.dma("sp", o["VS"][r0:r0 + 128, :], vs[:].rearrange("p h d -> p (h d)"), reads=[vs],
                      writes=[o["VS"]])
                vch, vr = r0 // VDR, r0 % VDR
                S.dma("sp", o["VD%d" % vch][vr:vr + 128, :], vd[:].rearrange("p h d -> p (h d)"), reads=[vd],
                      writes=[o["VD%d" % vch]])


def emit_lru(k, mixT, C, g, prm):
    S = k.S
    P = k.banks()
    with k.scope() as es:
        xs = k.sb([128, 1 + SEQ + 2], F32, es, "xs")
        xc = k.sb([128, 1 + CTX + 2], F32, es, "xc")
        U = k.sb([128, CTX + SEQ], F32, es, "U")
        OUT = k.sb([128, NT], F32, es, "lout")
        GTs = k.sb([128, NT], F32, es, "gts")
        tR = Rot([k.sb([128, 512], F32, es, "tR") for _ in range(4)])
        tI = Rot([k.sb([128, 512], F32, es, "tI") for _ in range(4)])
        tA = Rot([k.sb([128, 512], F32, es, "tA") for _ in range(4)])
        tH = Rot([k.sb([128, 512], F32, es, "tH") for _ in range(3)])
        sp = k.sb([128, 4], F32, es, "sp")
        nsp8 = k.sb([128, 2, 2], F32, es, "nsp8")
        nsp16 = k.sb([128, 2, 2], F32, es, "nsp16")
        lam = prm["LAM"]
        spv = sp[:, 0:4]
        lamv = lam[:].rearrange("p c d -> p (c d)")
        S.op("act", lambda e: e.activation(out=spv, in_=lamv, func=AF.Exp, scale=-1.0), reads=[lam], writes=[sp])
        S.op("dve", lambda e: e.tensor_scalar_add(out=spv, in0=spv, scalar1=1.0), reads=[sp], writes=[sp])
        S.op("act", lambda e: e.activation(out=spv, in_=spv, func=AF.Ln), reads=[sp], writes=[sp])
        S.op("dve", lambda e: e.tensor_scalar_mul(out=nsp8[:].rearrange("p c d -> p (c d)"), in0=spv, scalar1=-8.0),
             reads=[sp], writes=[nsp8])
        S.op("dve", lambda e: e.tensor_scalar_mul(out=nsp16[:].rearrange("p c d -> p (c d)"), in0=spv, scalar1=-16.0),
             reads=[sp], writes=[nsp16])
        pg = Rot(P[0:8])
        CW, CB, GB, GW, SEL = prm["CW"], prm["CB"], prm["GB"], prm["GW"], prm["SEL"]
        for c in range(2):
            rows = slice(c * 128, (c + 1) * 128)
            S.op("dve", lambda e: e.memset(xs[:, 0:1], 0.0), writes=[xs])
            S.op("dve", lambda e: e.memset(xs[:, 1 + SEQ:3 + SEQ], 0.0), writes=[xs])
            S.op("dve", lambda e: e.memset(xc[:, 0:1], 0.0), writes=[xc])
            S.op("dve", lambda e: e.memset(xc[:, 1 + CTX:3 + CTX], 0.0), writes=[xc])
            XGc = g["XG"][c]
            S.dma("sp", xs[:, 1:1 + HALF], XGc[0, :, CTX:NT], reads=[XGc], writes=[xs])
            S.dma("sp", xs[:, 1 + HALF:1 + SEQ], XGc[1, :, CTX:NT], reads=[XGc], writes=[xs])
            S.dma("sp", xc[:, 1:1 + CTX], XGc[0, :, 0:CTX], reads=[XGc], writes=[xc])
            S.dma("sp", GTs[:], g["GT"][rows, :], reads=[g["GT"]], writes=[GTs])
            for (src, L, d0) in [(xc, CTX, 0), (xs, SEQ, CTX)]:
                S.op("act", lambda e: e.activation(out=U[:, d0:d0 + L], in_=src[:, 1:1 + L], func=AF.Identity,
                                                   scale=CW[:, c, 1:2], bias=CB[:, c:c + 1]),
                     reads=[src, CW, CB], writes=[U])
                for (off, wi) in [(0, 0), (2, 2), (3, 3)]:
                    S.op("dve", lambda e, off=off, wi=wi: e.scalar_tensor_tensor(
                        out=U[:, d0:d0 + L], in0=src[:, off:off + L], scalar=CW[:, c, wi:wi + 1], in1=U[:, d0:d0 + L],
                        op0=ALU.mult, op1=ALU.add), reads=[src, CW, U], writes=[U])
            S.op("dve", lambda e: e.memset(OUT[:], 0.0), writes=[OUT])
            for d in range(2):
                lat = [(CTX + i * 512, 512, i) for i in range(8)]
                if d == 1:
                    lat = lat[::-1]
                state = None
                hprev = None
                seq = [(0, CTX, -1)] + lat
                for grp_ in [seq[0:1]] + [seq[i_:i_ + 2] for i_ in range(1, 9, 2)]:
                    items = []
                    for (u0, L, kind) in grp_:
                        pss = []
                        for gi in range(2):
                            ps = pg.next()
                            S.op("pe", lambda e: e.matmul(ps[:, 0:L], lhsT=GW[:, c, d, gi, :], rhs=U[:, u0:u0 + L],
                                                          start=True, stop=True), reads=[GW, U], writes=[ps])
                            pss.append(ps)
                        r, ii, a, h = tR.next(), tI.next(), tA.next(), tH.next()
                        S.op("act", lambda e: e.activation(out=r[:, 0:L], in_=pss[0][:, 0:L], func=AF.Sigmoid,
                                                           bias=GB[:, c, d, 0:1]), reads=[pss[0], GB], writes=[r])
                        S.op("act", lambda e: e.activation(out=ii[:, 0:L], in_=pss[1][:, 0:L], func=AF.Sigmoid,
                                                           bias=GB[:, c, d, 1:2]), reads=[pss[1], GB], writes=[ii])
                        items.append((u0, L, kind, r, ii, a, h))
                    for (u0, L, kind, r, ii, a, h) in items:
                        S.op("act", lambda e: e.activation(out=a[:, 0:L], in_=r[:, 0:L], func=AF.Exp,
                                                           scale=nsp8[:, c, d:d + 1]), reads=[r, nsp8], writes=[a])
                        S.op("pool", lambda e: e.tensor_tensor(out=r[:, 0:L], in0=a[:, 0:L], in1=a[:, 0:L],
                                                               op=ALU.mult), reads=[a], writes=[r])
                    for (u0, L, kind, r, ii, a, h) in items:
                        S.op("act", lambda e: e.activation(out=r[:, 0:L], in_=r[:, 0:L], func=AF.Sqrt, scale=-1.0,
                                                           bias=1.0), reads=[r], writes=[r])
                    for (u0, L, kind, r, ii, a, h) in items:
                        S.op("dve", lambda e: e.tensor_tensor(out=ii[:, 0:L], in0=ii[:, 0:L], in1=U[:, u0:u0 + L],
                                                              op=ALU.mult), reads=[ii, U], writes=[ii])
                        S.op("dve", lambda e: e.tensor_tensor(out=ii[:, 0:L], in0=ii[:, 0:L], in1=r[:, 0:L],
                                                              op=ALU.mult), reads=[ii, r], writes=[ii])
                        if d == 0:
                            vo, va, vb = h[:, 0:L], a[:, 0:L], ii[:, 0:L]
                        else:
                            vo, va, vb = h[:, L - 1::-1], a[:, L - 1::-1], ii[:, L - 1::-1]
                        init = 0.0 if state is None else state
                        rd = [a, ii] + ([hprev] if hprev is not None else [])
                        S.op("dve", lambda e: e.tensor_tensor_scan(out=vo, data0=va, data1=vb, initial=init,
                                                                   op0=ALU.mult, op1=ALU.add), reads=rd, writes=[h])
                        state = h[:, L - 1:L] if d == 0 else h[:, 0:1]
                        hprev = h
                        if kind < 0:
                            S.op("dve", lambda e: e.tensor_tensor(out=OUT[:, 0:CTX], in0=h[:, 0:L], in1=OUT[:, 0:CTX],
                                                                  op=ALU.add), reads=[h, OUT], writes=[OUT])
                        else:
                            hf = kind // 4
                            lo = CTX + (kind % 4) * 512
                            S.op("dve", lambda e: e.scalar_tensor_tensor(
                                out=OUT[:, lo:lo + L], in0=h[:, 0:L], scalar=SEL[:, hf:hf + 1], in1=OUT[:, lo:lo + L],
                                op0=ALU.mult, op1=ALU.add), reads=[h, OUT, SEL], writes=[OUT])
            S.op("pool", lambda e: e.tensor_tensor(out=mixT[:, c, :], in0=OUT[:], in1=GTs[:], op=ALU.mult),
                 reads=[OUT, GTs], writes=[mixT])


def pipeline(n, front, back, la, deferred):
    for i in range(n + la):
        if i < n:
            front(i)
        if i >= la:
            back(i - la)
        while deferred and deferred[0][0] <= i:
            deferred.pop(0)[1]()
    while deferred:
        deferred.pop(0)[1]()


def emit_swa(k, mixT, C, g, prm):
    S = k.S
    P = k.banks()
    LA = 5
    with k.scope() as es:
        KS = [[k.sb([128, CTX + 128 + HALF + 128], BF16, es, "ks") for _ in range(2)] for _ in range(2)]
        VSa = k.sb([128, 20, 256], BF16, es, "vsa")
        MSK = k.sb([128, 8, 512], BF16, es, "msk")
        ESK = k.sb([128, 8], F32, es, "esk")
        qb = Rot([k.sb([128, 4, 512], BF16, es, "qs") for _ in range(2)])
        Eb = Rot([k.sb([128, 512], BF16, es, "E") for _ in range(6)])
        den = Rot([k.sb([128, 512], F32, es, "den") for _ in range(2)])
        S.dma("sp", MSK[:], g["MSK"][:, :, :], reads=[g["MSK"]], writes=[MSK])
        S.op("act", lambda e: e.activation(out=ESK[:], in_=prm["SINK"][:], func=AF.Exp), reads=[prm["SINK"]],
             writes=[ESK])
        for hk in range(2):
            for hp in range(2):
                T_ = KS[hk][hp]
                ps_ = slice(hp * 64, hp * 64 + 64)
                zs_ = slice((1 - hp) * 64, (1 - hp) * 64 + 64)
                rows = slice(hk * 128 + hp * 64, hk * 128 + hp * 64 + 64)
                S.op("dve", lambda e: e.memset(T_[zs_, :], 0.0), writes=[T_])
                S.dma("act", T_[ps_, 0:CTX], g["KSO"][rows, 0:CTX], reads=[g["KSO"]], writes=[T_])
                S.dma("sp", T_[ps_, CTX + 128:CTX + 128 + HALF], g["KSO"][rows, CTX:NT], reads=[g["KSO"]],
                      writes=[T_])
                S.dma("act", T_[ps_, CTX:CTX + 128], g["KSG"][0, rows, NT - 128:NT], reads=[g["KSG"]], writes=[T_])
                S.dma("sp", T_[ps_, CTX + 128 + HALF:], g["KSG"][1, rows, CTX:CTX + 128], reads=[g["KSG"]],
                      writes=[T_])
        vo = g["VSO"].t.rearrange("(b p) n -> p b n", p=128)
        S.dma("act", VSa[:, 0:2, :], vo[:, 0:2, :], reads=[g["VSO"]], writes=[VSa])
        S.dma("sp", VSa[:, 3:19, :], vo[:, 2:18, :], reads=[g["VSO"]], writes=[VSa])
        S.dma("act", VSa[:, 2, :], g["VSG"][0, NT - 128:NT, :], reads=[g["VSG"]], writes=[VSa])
        S.dma("sp", VSa[:, 19, :], g["VSG"][1, CTX:CTX + 128, :], reads=[g["VSG"]], writes=[VSa])
        accs = Rot(P[0:2])
        scs = Rot(P[2:8])
        qsrc = g["QS"].t.rearrange("(c p) n -> p c n", p=128)
        tiles = [-1, 0, 1, 2, 3]
        Qt = {}

        def load_q(t):
            N = CTX if t < 0 else 512
            c0 = 0 if t < 0 else CTX + t * 512
            Q = qb.next()
            S.dma("sp", Q[:, :, 0:N], qsrc[:, :, c0:c0 + N], reads=[g["QS"]], writes=[Q])
            Qt[t] = Q

        units = []
        for ti, t in enumerate(tiles):
            N = CTX if t < 0 else 512
            c0 = 0 if t < 0 else CTX + t * 512
            blocks = [(0, 0, None), (1, 128, None)]
            if t >= 0:
                for r in range(6):
                    mk = r
                    if t == 0 and r == 0:
                        mk = 6
                    if t == 3 and r == 5:
                        mk = 7
                    blocks.append((2 + 4 * t + r, CTX + (4 * t + r) * 128, mk))
            for h in range(8):
                for bi, blk in enumerate(blocks):
                    units.append((ti, t, N, c0, h, bi, len(blocks), blk))
        Ef = {}
        cur = {}
        load_q(tiles[0])

        def front(i):
            ti, t, N, c0, h, bi, nb, (vb, kcol, mk) = units[i]
            if h == 0 and bi == 0 and ti + 1 < len(tiles):
                load_q(tiles[ti + 1])
            Q = Qt[t]
            qc, pb, hk = h // 2, (h % 2) * 64, h // 4
            sps, E = scs.next(), Eb.next()
            S.op("pe", lambda e: e.matmul(sps[:, 0:N], lhsT=KS[hk][h % 2][:, kcol:kcol + 128],
                                          rhs=Q[:, qc, 0:N], start=True, stop=True),
                 reads=[KS[hk][h % 2], Q], writes=[sps])
            S.op("act", lambda e: e.activation(out=E[:, 0:N], in_=sps[:, 0:N], func=AF.Exp, scale=0.125),
                 reads=[sps], writes=[E])
            if mk is not None:
                S.op("pool" if i % 2 else "dve", lambda e: e.tensor_tensor(out=E[:, 0:N], in0=E[:, 0:N],
                                                                          in1=MSK[:, mk, 0:N], op=ALU.mult),
                     reads=[E, MSK], writes=[E])
            Ef[i] = E

        def back(i):
            ti, t, N, c0, h, bi, nb, (vb, kcol, mk) = units[i]
            qc, pb, hk = h // 2, (h % 2) * 64, h // 4
            E = Ef.pop(i)
            if bi == 0:
                cur["acc"] = accs.next()
            acc = cur["acc"]
            S.op("pe", lambda e: e.matmul(acc[:, 0:N], lhsT=VSa[:, vb, hk * 128:(hk + 1) * 128],
                                          rhs=E[:, 0:N], start=(bi == 0), stop=(bi == nb - 1)),
                 reads=[VSa, E], writes=[acc])
            if bi == nb - 1:
                dn = den.next()
                S.op("dve", lambda e: e.tensor_scalar(out=dn[0:64, 0:N], in0=acc[64:128, 0:N],
                                                      scalar1=ESK[64:128, h:h + 1], scalar2=None, op0=ALU.add),
                     reads=[acc, ESK], writes=[dn])
                S.op("dve", lambda e: e.reciprocal(out=dn[0:64, 0:N], in_=dn[0:64, 0:N]), reads=[dn], writes=[dn])
                S.op("dve", lambda e: e.tensor_tensor(out=mixT[pb:pb + 64, 2 + qc, c0:c0 + N], in0=acc[0:64, 0:N],
                                                      in1=dn[0:64, 0:N], op=ALU.mult), reads=[acc, dn],
                     writes=[mixT])

        pipeline(len(units), front, back, LA, [])


def emit_diff(k, mixT, C, g, prm, lam_init):
    S = k.S
    P = k.banks()
    LA = 2
    with k.scope() as es:
        KD = k.sb([128, 2, CTX + SEQ], BF16, es, "kd")
        VDa = k.sb([128, 34, 512], BF16, es, "vda")
        qb = Rot([k.sb([128, 2, 512], BF16, es, "qd") for _ in range(2)])
        qm = [Rot([k.sb([128, 2, 512], BF16, es, "qm") for _ in range(2)]) for _ in range(4)]
        Eb = Rot([k.sb([128, 1024], BF16, es, "E") for _ in range(3)])
        tr = Rot([k.sb([128, 512], F32, es, "tr") for _ in range(2)])
        tt = Rot([k.sb([128, 512], F32, es, "tt") for _ in range(2)])
        tO = Rot([k.sb([128, 512], F32, es, "tO") for _ in range(1)])
        tq = Rot([k.sb([128, 512], F32, es, "tq") for _ in range(1)])
        lt = k.sb([128, 2, 32], F32, es, "lt")
        ls = k.sb([128, 2], F32, es, "ls")
        NLAM = k.sb([128, 1], F32, es, "nlam")
        GN = k.sb([128, 1], F32, es, "gn")
        DL = prm["DL"]
        for i in range(2):
            S.op("dve", lambda e, i=i: e.tensor_tensor(out=lt[:, i, :], in0=DL[:, 2 * i, :], in1=DL[:, 2 * i + 1, :],
                                                       op=ALU.mult), reads=[DL], writes=[lt])
        S.op("dve", lambda e: e.reduce_sum(out=ls[:], in_=lt[:], axis=AX.X), reads=[lt], writes=[ls])
        S.op("act", lambda e: e.activation(out=ls[:], in_=ls[:], func=AF.Exp), reads=[ls], writes=[ls])
        S.op("dve", lambda e: e.tensor_tensor(out=NLAM[:], in0=ls[:, 1:2], in1=ls[:, 0:1], op=ALU.subtract),
             reads=[ls], writes=[NLAM])
        S.op("dve", lambda e: e.tensor_scalar_add(out=NLAM[:], in0=NLAM[:], scalar1=-lam_init), reads=[NLAM],
             writes=[NLAM])
        S.op("dve", lambda e: e.tensor_scalar_mul(out=GN[:], in0=prm["DG"][:], scalar1=1.0 - lam_init),
             reads=[prm["DG"]], writes=[GN])
        ko = g["KDO"].t.rearrange("(c p) n -> p c n", p=128)
        S.dma("sp", KD[:, :, 0:CTX], ko[:, :, 0:CTX], reads=[g["KDO"]], writes=[KD])
        for hf in range(2):
            kg = g["KDG"][hf].rearrange("(c p) n -> p c n", p=128)
            S.dma("act", KD[:, :, CTX + hf * HALF:CTX + (hf + 1) * HALF], kg[:, :, CTX:NT], reads=[g["KDG"]],
                  writes=[KD])
            vg0 = g["VDG"][0][hf].rearrange("(b p) n -> p b n", p=128)
            vg1 = g["VDG"][1][hf].rearrange("(b p) n -> p b n", p=128)
            S.dma("sp", VDa[:, 2 + hf * 16:2 + hf * 16 + 7, :], vg0[:, 2:9, :], reads=[g["VDG"][0]], writes=[VDa])
            S.dma("act", VDa[:, 2 + hf * 16 + 7:2 + (hf + 1) * 16, :], vg1[:, 0:9, :], reads=[g["VDG"][1]],
                  writes=[VDa])
        vo = g["VDO"].t.rearrange("(b p) n -> p b n", p=128)
        S.dma("sp", VDa[:, 0:2, :], vo[:, 0:2, :], reads=[g["VDO"]], writes=[VDa])
        accs = Rot(P[0:2])
        pn = P[2]
        scs = Rot(k.pairs[2:4])
        qsrc = g["QD"].t.rearrange("(c p) n -> p c n", p=128)
        SC = 32 ** -0.5
        tiles = [-1, 0, 1, 2, 3]
        Qt = {}

        def load_q(t):
            N = CTX if t < 0 else 512
            c0 = 0 if t < 0 else CTX + t * 512
            Q = qb.next()
            S.dma("sp", Q[:, :, 0:N], qsrc[:, :, c0:c0 + N], reads=[g["QD"]], writes=[Q])
            Qm = [qm[v].next() for v in range(4)]
            for v in range(4):
                S.op("dve", lambda e: e.tensor_scalar(out=Qm[v][:, :, 0:N], in0=Q[:, :, 0:N],
                                                      scalar1=prm["PM"][:, v:v + 1], scalar2=None, op0=ALU.mult),
                     reads=[Q, prm["PM"]], writes=[Qm[v]])
            Qt[t] = Qm

        units = []
        for ti, t in enumerate(tiles):
            N = CTX if t < 0 else 512
            c0 = 0 if t < 0 else CTX + t * 512
            nkp = 1 if t < 0 else 17
            for h in range(4):
                for m in range(2):
                    for kp in range(nkp):
                        units.append((ti, t, N, c0, h, m, kp, nkp))
        Ef = {}
        cur = {}
        deferred = []
        load_q(tiles[0])

        def front(i):
            ti, t, N, c0, h, m, kp, nkp = units[i]
            if h == 0 and m == 0 and kp == 0 and ti + 1 < len(tiles):
                load_q(tiles[ti + 1])
            Qm = Qt[t]
            c = h // 2
            sps, E = scs.next(), Eb.next()
            qv = Qm[(h % 2) * 2 + m]
            for j in range(2):
                kb = 2 * kp + j
                S.op("pe", lambda e: e.matmul(sps[:, j * 512:j * 512 + N], lhsT=KD[:, c, kb * 128:(kb + 1) * 128],
                                              rhs=qv[:, c, 0:N], start=True, stop=True),
                     reads=[KD, qv], writes=[sps])
            S.op("act", lambda e: e.activation(out=E[:].rearrange("p (j n) -> p j n", j=2)[:, :, 0:N],
                                               in_=sps[:].rearrange("p (j n) -> p j n", j=2)[:, :, 0:N],
                                               func=AF.Exp, scale=SC), reads=[sps], writes=[E])
            Ef[i] = E

        def back(i):
            ti, t, N, c0, h, m, kp, nkp = units[i]
            c = h // 2
            E = Ef.pop(i)
            if m == 0 and kp == 0:
                cur["ac"] = [accs.next(), accs.next()]
            ac = cur["ac"]
            for j in range(2):
                kb = 2 * kp + j
                S.op("pe", lambda e: e.matmul(ac[m][:, 0:N], lhsT=VDa[:, kb, h * 128:(h + 1) * 128],
                                              rhs=E[:, j * 512:j * 512 + N], start=(kb == 0), stop=(kb == 2 * nkp - 1)),
                     reads=[VDa, E], writes=[ac[m]])
            if not (m == 1 and kp == nkp - 1):
                return
            ts = []
            for mm in range(2):
                r_, t_ = tr.next(), tt.next()
                S.op("dve", lambda e: e.reciprocal(out=r_[0:64, 0:N], in_=ac[mm][64:128, 0:N]), reads=[ac[mm]],
                     writes=[r_])
                S.op("dve", lambda e: e.tensor_tensor(out=t_[0:64, 0:N], in0=ac[mm][0:64, 0:N], in1=r_[0:64, 0:N],
                                                      op=ALU.mult), reads=[ac[mm], r_], writes=[t_])
                ts.append(t_)
            O, sq = tO.next(), tq.next()
            S.op("dve", lambda e: e.scalar_tensor_tensor(out=O[0:64, 0:N], in0=ts[1][0:64, 0:N],
                                                         scalar=NLAM[0:64, 0:1], in1=ts[0][0:64, 0:N],
                                                         op0=ALU.mult, op1=ALU.add), reads=ts + [NLAM],
                 writes=[O])
            S.op("act", lambda e: e.activation(out=sq[0:64, 0:N], in_=O[0:64, 0:N], func=AF.Square), reads=[O],
                 writes=[sq])

            def tail():
                S.op("pe", lambda e: e.matmul(pn[0:64, 0:N], lhsT=C["onesf"][0:64, 0:64], rhs=sq[0:64, 0:N],
                                              start=True, stop=True), reads=[C["onesf"], sq], writes=[pn])
                S.op("act", lambda e: e.activation(out=sq[0:64, 0:N], in_=pn[0:64, 0:N], func=AF.Sqrt,
                                                   scale=1.0 / 64.0, bias=C["eps"][0:64, :]),
                     reads=[pn, C["eps"]], writes=[sq])
                S.op("dve", lambda e: e.reciprocal(out=sq[0:64, 0:N], in_=sq[0:64, 0:N]), reads=[sq], writes=[sq])
                S.op("dve", lambda e: e.tensor_tensor(out=O[0:64, 0:N], in0=O[0:64, 0:N], in1=sq[0:64, 0:N],
                                                      op=ALU.mult), reads=[O, sq], writes=[O])
                ob = (h % 2) * 64
                S.op("dve", lambda e: e.tensor_scalar(out=mixT[ob:ob + 64, 6 + c, c0:c0 + N], in0=O[0:64, 0:N],
                                                      scalar1=GN[0:64, 0:1], scalar2=None, op0=ALU.mult),
                     reads=[O, GN], writes=[mixT])
            deferred.append((i + LA + min(3, 2 * nkp - 1), tail))

        pipeline(len(units), front, back, LA, deferred)


def emit_ln(k, hT, C, c0, N, LNG, LNB, which, es, pool):
    S = k.S
    ysq = pool["ysq"].next()
    S1, S2 = pool["ps"].next(), pool["ps"].next()
    for c in range(8):
        S.op("act", lambda e, c=c: e.activation(out=ysq[:, c, 0:N], in_=hT[:, c, c0:c0 + N], func=AF.Square),
             reads=[hT], writes=[ysq])
    for c in range(8):
        S.op("pe", lambda e, c=c: e.matmul(S1[:, 0:N], lhsT=C["onesf"][:, :], rhs=hT[:, c, c0:c0 + N], start=(c == 0),
                                           stop=(c == 7)), reads=[C["onesf"], hT], writes=[S1])
    for c in range(8):
        S.op("pe", lambda e, c=c: e.matmul(S2[:, 0:N], lhsT=C["onesb"][:, :], rhs=ysq[:, c, 0:N], start=(c == 0),
                                           stop=(c == 7)), reads=[C["onesb"], ysq], writes=[S2])
    mean, rstd, nmr = pool["f"].next(), pool["f"].next(), pool["f"].next()
    S.op("act", lambda e: e.activation(out=mean[:, 0:N], in_=S1[:, 0:N], func=AF.Identity, scale=1.0 / D_MODEL),
         reads=[S1], writes=[mean])
    S.op("dve", lambda e: e.tensor_tensor(out=rstd[:, 0:N], in0=mean[:, 0:N], in1=mean[:, 0:N], op=ALU.mult),
         reads=[mean], writes=[rstd])
    S.op("dve", lambda e: e.scalar_tensor_tensor(out=rstd[:, 0:N], in0=S2[:, 0:N], scalar=1.0 / D_MODEL,
                                                 in1=rstd[:, 0:N], op0=ALU.mult, op1=ALU.subtract),
         reads=[S2, rstd], writes=[rstd])
    S.op("act", lambda e: e.activation(out=rstd[:, 0:N], in_=rstd[:, 0:N], func=AF.Sqrt, bias=C["eps"][:, :]),
         reads=[rstd, C["eps"]], writes=[rstd])
    S.op("dve", lambda e: e.reciprocal(out=rstd[:, 0:N], in_=rstd[:, 0:N]), reads=[rstd], writes=[rstd])
    S.op("dve", lambda e: e.scalar_tensor_tensor(out=nmr[:, 0:N], in0=mean[:, 0:N], scalar=-1.0, in1=rstd[:, 0:N],
                                                 op0=ALU.mult, op1=ALU.mult), reads=[mean, rstd], writes=[nmr])
    for c in range(8):
        t1 = pool["t"].next()
        S.op("dve", lambda e, c=c: e.scalar_tensor_tensor(out=t1[:, 0:N], in0=hT[:, c, c0:c0 + N],
                                                          scalar=LNG[:, which, c:c + 1], in1=rstd[:, 0:N],
                                                          op0=ALU.mult, op1=ALU.mult), reads=[hT, LNG, rstd],
             writes=[t1])
        S.op("dve", lambda e, c=c: e.scalar_tensor_tensor(out=t1[:, 0:N], in0=nmr[:, 0:N],
                                                          scalar=LNG[:, which, c:c + 1], in1=t1[:, 0:N],
                                                          op0=ALU.mult, op1=ALU.add), reads=[nmr, LNG, t1],
             writes=[t1])
        S.op("act", lambda e, c=c: e.activation(out=hT[:, c, c0:c0 + N], in_=t1[:, 0:N], func=AF.Identity,
                                                bias=LNB[:, which, c:c + 1]), reads=[t1, LNB], writes=[hT])


def ln_pool(k, es, P, W=512, nf=6):
    return dict(ysq=Rot([k.sb([128, 8, W], BF16, es, "ysq") for _ in range(2)]),
                f=Rot([k.sb([128, W], F32, es, "lnf") for _ in range(nf)]),
                t=Rot([k.sb([128, W], F32, es, "lnt") for _ in range(3)]), ps=Rot(P))


def emit_phaseC(k, hT, mixT, MODX, C, wout_d, prm):
    S = k.S
    P = k.banks()
    with k.scope() as es:
        WO = k.sb([128, 8, D_MODEL], BF16, es, "wout")
        S.dma("pool", WO[:], wout_d.t.rearrange("(k p) n -> p k n", p=128), reads=[wout_d], writes=[WO])
        lp = ln_pool(k, es, P[6:8])
        tm = Rot([k.sb([128, 512], F32, es, "ctm") for _ in range(3)])
        pb = Rot(P[0:6])
        tiles = [(0, 256)] + [(CTX + i * 512, 512) for i in range(4)]
        prev = None
        for (c0, N) in tiles:
            j = 1 if c0 < CTX else 0
            for c in range(8):
                ps = pb.next()
                for kk in range(8):
                    S.op("pe", lambda e, kk=kk: e.matmul(ps[:, 0:N], lhsT=WO[:, kk, c * 128:(c + 1) * 128],
                                                         rhs=mixT[:, kk, c0:c0 + N], start=(kk == 0), stop=(kk == 7)),
                         reads=[WO, mixT], writes=[ps])
                t = tm.next()
                S.op("dve", lambda e: e.tensor_scalar(out=t[:, 0:N], in0=ps[:, 0:N], scalar1=MODX[:, 2, c, j:j + 1],
                                                      scalar2=None, op0=ALU.mult), reads=[ps, MODX], writes=[t])
                S.op("dve", lambda e: e.scalar_tensor_tensor(out=hT[:, c, c0:c0 + N], in0=hT[:, c, c0:c0 + N],
                                                             scalar=ALPHA, in1=t[:, 0:N], op0=ALU.mult, op1=ALU.add),
                     reads=[hT, t], writes=[hT])
            if prev is not None:
                emit_ln(k, hT, C, prev[0], prev[1], prm["LNG"], prm["LNB"], 0, es, lp)
            prev = (c0, N)
        emit_ln(k, hT, C, prev[0], prev[1], prm["LNG"], prm["LNB"], 0, es, lp)


def emit_ffn(k, hT, MODX, C, prm, w13_d, w2_d, nexp, rw_d=None, tok0=0, G=NT // 2, TN=384):
    S = k.S
    P = k.banks()
    moe = nexp > 1
    with k.scope() as es:
        u2 = k.sb([128, 8, G], BF16, es, "u2")
        gT = k.sb([128, NJ, G], BF16, es, "gT")
        W13 = Rot([k.sb([128, 8, 256], BF16, es, "w13") for _ in range(3)])
        W2 = Rot([k.sb([128, NJ, 128], BF16, es, "w2") for _ in range(2)])
        sil = Rot([k.sb([128, TN], F32, es, "sil") for _ in range(2)])
        tmp = Rot([k.sb([128, TN], F32, es, "ftmp") for _ in range(2)])
        lp = ln_pool(k, es, P[6:8], TN, nf=(6 if TN < 512 else 4))
        p13 = Rot(P[0:4])
        po = Rot(P[4:6])
        pm = Rot(P[6:8])
        if moe:
            RW = k.sb([128, 8, NEXP], BF16, es, "rw")
            S.dma("pool", RW[:], rw_d.t.rearrange("(k p) e -> p k e", p=128), reads=[rw_d], writes=[RW])
            CWt = k.sb([128, G // 128, NEXP], F32, es, "cw")
            cwT = Rot([k.sb([128, G], F32, es, "cwT") for _ in range(1)])
            rt = {n: k.sb([128, NEXP], F32, es, "r" + n) for n in ["lg", "eq", "l2", "sel", "ex"]}
            rs = {n: k.sb([128, 1], F32, es, "s" + n) for n in ["m1", "m2", "nm1", "sum"]}
            dg = Rot([k.sb([128, 128], F32, es, "dg") for _ in range(2)])

        jobs = []
        for grp in range(2):
            for e_ in range(nexp):
                for jj in range(NJ):
                    jobs.append(("w13", e_, jj))
                for c in range(8):
                    jobs.append(("w2", e_, c))
        loaded = {}
        state = {"next": 0}

        def prefetch(upto):
            while state["next"] < min(upto, len(jobs)):
                i = state["next"]
                kind, e_, x = jobs[i]
                if kind == "w13":
                    w = W13.next()
                    S.dma("pool", w[:], w13_d[e_, x], reads=[w13_d], writes=[w])
                else:
                    w = W2.next()
                    S.dma("pool", w[:], w2_d[e_, x], reads=[w2_d], writes=[w])
                loaded[i] = w
                state["next"] += 1

        pending = []

        def flush_ln():
            while pending:
                gg = pending.pop(0)
                for t in range(G // TN):
                    emit_ln(k, hT, C, gg + t * TN, TN, prm["LNG"], prm["LNB"], 1, es, lp)

        ji = 0
        for grp in range(2):
            g0 = tok0 + grp * G
            for (lo, hi, j) in segs(g0, G):
                for kk in range(8):
                    S.op("act", lambda e, kk=kk: e.activation(
                        out=u2[:, kk, lo - g0:hi - g0], in_=hT[:, kk, lo:hi], func=AF.Identity,
                        scale=MODX[:, 7, kk, j:j + 1], bias=MODX[:, 3, kk, j:j + 1]), reads=[hT, MODX], writes=[u2])
            S.op("dve", lambda e: e.tensor_scalar(out=hT[:, :, g0:g0 + G], in0=hT[:, :, g0:g0 + G], scalar1=ALPHA,
                                                  scalar2=None, op0=ALU.mult), reads=[hT], writes=[hT])
            if moe:
                for blk in range(G // 128):
                    ps = pm.next()
                    for kk in range(8):
                        S.op("pe", lambda e, kk=kk: e.matmul(ps[:, 0:NEXP], lhsT=u2[:, kk, blk * 128:(blk + 1) * 128],
                                                             rhs=RW[:, kk, :], start=(kk == 0), stop=(kk == 7)),
                             reads=[u2, RW], writes=[ps])
                    lg, eq, l2, sel, ex = rt["lg"], rt["eq"], rt["l2"], rt["sel"], rt["ex"]
                    m1, m2, nm1, sm = rs["m1"], rs["m2"], rs["nm1"], rs["sum"]
                    S.op("dve", lambda e: e.tensor_tensor(out=lg[:], in0=ps[:, 0:NEXP], in1=prm["RB"][:], op=ALU.add),
                         reads=[ps, prm["RB"]], writes=[lg])
                    S.op("dve", lambda e: e.reduce_max(out=m1[:], in_=lg[:], axis=AX.X), reads=[lg], writes=[m1])
                    S.op("dve", lambda e: e.tensor_scalar(out=eq[:], in0=lg[:], scalar1=m1[:, 0:1], scalar2=None,
                                                          op0=ALU.is_equal), reads=[lg, m1], writes=[eq])
                    S.op("dve", lambda e: e.scalar_tensor_tensor(out=l2[:], in0=eq[:], scalar=-1e30, in1=lg[:],
                                                                 op0=ALU.mult, op1=ALU.add), reads=[eq, lg],
                         writes=[l2])
                    S.op("dve", lambda e: e.reduce_max(out=m2[:], in_=l2[:], axis=AX.X), reads=[l2], writes=[m2])
                    S.op("dve", lambda e: e.tensor_scalar(out=sel[:], in0=lg[:], scalar1=m2[:, 0:1], scalar2=None,
                                                          op0=ALU.is_ge), reads=[lg, m2], writes=[sel])
                    S.op("dve", lambda e: e.tensor_scalar_mul(out=nm1[:], in0=m1[:], scalar1=-1.0), reads=[m1],
                         writes=[nm1])
                    S.op("act", lambda e: e.activation(out=ex[:], in_=lg[:], func=AF.Exp, bias=nm1[:, 0:1]),
                         reads=[lg, nm1], writes=[ex])
                    S.op("dve", lambda e: e.tensor_tensor(out=ex[:], in0=ex[:], in1=sel[:], op=ALU.mult),
                         reads=[ex, sel], writes=[ex])
                    S.op("dve", lambda e: e.reduce_sum(out=sm[:], in_=ex[:], axis=AX.X), reads=[ex], writes=[sm])
                    S.op("dve", lambda e: e.reciprocal(out=sm[:], in_=sm[:]), reads=[sm], writes=[sm])
                    S.op("dve", lambda e: e.tensor_scalar(out=CWt[:, blk, :], in0=ex[:], scalar1=sm[:, 0:1],
                                                          scalar2=None, op0=ALU.mult), reads=[ex, sm], writes=[CWt])
            for e_ in range(nexp):
                if moe:
                    cw = cwT.next()
                    for b0 in range(0, G // 128, 4):
                        nb = min(4, G // 128 - b0)
                        ps = pm.next()
                        for bb in range(nb):
                            blk = b0 + bb
                            d_ = dg.next()
                            S.op("dve", lambda e: e.tensor_scalar(out=d_[:], in0=C["ident"][:],
                                                                  scalar1=CWt[:, blk, e_:e_ + 1], scalar2=None,
                                                                  op0=ALU.mult), reads=[C["ident"], CWt], writes=[d_])
                            S.op("pe", lambda e: e.matmul(ps[:, bb * 128:(bb + 1) * 128], lhsT=C["onesf"][:, :],
                                                          rhs=d_[:], start=True, stop=True),
                                 reads=[C["onesf"], d_], writes=[ps])
                        S.op("act", lambda e: e.activation(out=cw[:, b0 * 128:(b0 + nb) * 128], in_=ps[:, 0:nb * 128],
                                                           func=AF.Identity), reads=[ps], writes=[cw])
                for jj in range(NJ):
                    if e_ == 0 and jj == 6:
                        flush_ln()
                    prefetch(ji + 3)
                    w = loaded.pop(ji)
                    ji += 1
                    for t in range(G // TN):
                        p1, p3 = p13.next(), p13.next()
                        for (pp, wo) in [(p1, 0), (p3, 128)]:
                            for kk in range(8):
                                S.op("pe", lambda e, kk=kk: e.matmul(pp[:, 0:TN], lhsT=w[:, kk, wo:wo + 128],
                                                                     rhs=u2[:, kk, t * TN:(t + 1) * TN],
                                                                     start=(kk == 0), stop=(kk == 7)),
                                     reads=[w, u2], writes=[pp])
                        s_ = sil.next()
                        S.op("act", lambda e: e.activation(out=s_[:], in_=p1[:, 0:TN], func=AF.Silu), reads=[p1],
                             writes=[s_])
                        S.op("dve", lambda e: e.tensor_tensor(out=gT[:, jj, t * TN:(t + 1) * TN], in0=s_[:],
                                                              in1=p3[:, 0:TN], op=ALU.mult), reads=[s_, p3],
                             writes=[gT])
                for c in range(8):
                    prefetch(ji + 2)
                    w = loaded.pop(ji)
                    ji += 1
                    for t in range(G // TN):
                        pso = po.next()
                        for jj in range(NJ):
                            S.op("pe", lambda e, jj=jj: e.matmul(pso[:, 0:TN], lhsT=w[:, jj, :],
                                                                 rhs=gT[:, jj, t * TN:(t + 1) * TN], start=(jj == 0),
                                                                 stop=(jj == NJ - 1)), reads=[w, gT], writes=[pso])
                        for (lo, hi, j) in segs(g0 + t * TN, TN):
                            a, b = lo - g0 - t * TN, hi - g0 - t * TN
                            if moe:
                                tp = tmp.next()
                                S.op("dve", lambda e: e.tensor_tensor(out=tp[:, a:b], in0=pso[:, a:b],
                                                                      in1=cw[:, lo - g0:hi - g0], op=ALU.mult),
                                     reads=[pso, cw], writes=[tp])
                                S.op("dve", lambda e: e.scalar_tensor_tensor(
                                    out=hT[:, c, lo:hi], in0=tp[:, a:b], scalar=MODX[:, 5, c, j:j + 1],
                                    in1=hT[:, c, lo:hi], op0=ALU.mult, op1=ALU.add), reads=[tp, MODX, hT],
                                    writes=[hT])
                            else:
                                S.op("dve", lambda e: e.scalar_tensor_tensor(
                                    out=hT[:, c, lo:hi], in0=pso[:, a:b], scalar=MODX[:, 5, c, j:j + 1],
                                    in1=hT[:, c, lo:hi], op0=ALU.mult, op1=ALU.add), reads=[pso, MODX, hT],
                                    writes=[hT])
            pending.append(g0)
        flush_ln()


SMALL = dict(CW=[128, 2, 4], CB=[128, 2], GB=[128, 2, 2, 2], LAM=[128, 2, 2], GW=[128, 2, 2, 2, 128], SEL=[128, 2],
             SINK=[128, 8], DL=[128, 4, 32], DG=[128, 1], PM=[128, 4], LNG=[128, 2, 8], LNB=[128, 2, 8], RB=[128, 8])
VDR = NT // 2
A_OUT = dict(XL0=([128, NT], F32), XL1=([128, NT], F32), GT=([256, NT], F32), QS=([512, NT], BF16),
             KS=([256, NT], BF16), VS=([NT, 256], BF16), QD=([256, NT], BF16), KD=([256, NT], BF16),
             VD0=([VDR, 512], BF16), VD1=([VDR, 512], BF16))


def to_fm(x2d):
    return np.ascontiguousarray(x2d.T.reshape(8, 128, -1).transpose(1, 0, 2))


def from_fm(h):
    return np.ascontiguousarray(h.transpose(1, 0, 2).reshape(D_MODEL, -1).T)


def win_perm():
    lx, lg, sq, sk, sv, dq, dk, dv = 0, 256, 512, 1024, 1152, 1280, 1536, 1792
    cols = []
    cols += list(range(lx, lx + 256))
    cols += list(range(lg, lg + 256))
    cols += list(range(sq, sq + 512))
    sw64 = lambda base, h: [base + h * 64 + ((d + 32) % 64) for d in range(64)]
    for h in range(8):
        cols += sw64(sq, h)
    for hk in range(2):
        cols += list(range(sk + hk * 64, sk + hk * 64 + 64)) * 2
    for hk in range(2):
        cols += sw64(sk, hk) * 2
    sw32 = lambda base: [base + b * 32 + ((d + 16) % 32) for b in range(8) for d in range(32)]
    cols += list(range(dq, dq + 256))
    cols += sw32(dq)
    cols += list(range(dk, dk + 256))
    cols += sw32(dk)
    cols += list(range(sv, sv + 128))
    cols += list(range(dv, dv + 256))
    assert len(cols) == NW
    return np.array(cols)


def rope_tables(half):
    t = np.arange(HALF, dtype=np.float32) + np.float32(half * HALF)
    row = np.floor(t / 64).astype(np.float32)
    col = (t - row * 64).astype(np.float32)
    out = np.zeros((128, 4, HALF), np.float32)
    for (ti, hd) in [(0, 64), (2, 32)]:
        nf = hd // 4
        inv = (np.float32(10000.0) ** (-np.arange(nf, dtype=np.float32) / np.float32(nf))).astype(np.float32)
        ang = np.concatenate([row[:, None] * inv, col[:, None] * inv], -1).astype(np.float32)
        cs, sn = np.cos(ang).astype(np.float32), np.sin(ang).astype(np.float32)
        for p in range(128):
            d = p % hd
            jx = d % (hd // 2)
            out[p, ti] = cs[:, jx]
            out[p, ti + 1] = -sn[:, jx] if d < hd // 2 else sn[:, jx]
    return out


def swa_masks(half):
    m = np.zeros((128, 8, 512), np.float32)
    kk = np.arange(128)[:, None]
    q = np.arange(512)[None, :]
    for r in range(6):
        m[:, r] = (np.abs((r - 1) * 128 + kk - q) <= 128)
    m[:, 6] = m[:, 0] if half == 1 else 0.0
    m[:, 7] = m[:, 5] if half == 0 else 0.0
    return m.astype(NPBF)


def rep(v):
    return np.ascontiguousarray(np.broadcast_to(np.asarray(v, np.float32).reshape(1, -1), (128, np.size(v))))


def small_params(inp, layer, half):
    f = lambda a: np.ascontiguousarray(np.asarray(a, np.float32))
    p = {}
    p["CW"] = f(inp["lru_conv_w"][layer].reshape(4, 2, 128).transpose(2, 1, 0))
    p["CB"] = f(inp["lru_conv_b"][layer].reshape(2, 128).T)
    p["GB"] = f(inp["lru_gate_b"][layer].reshape(2, 2, 2, 128).transpose(3, 2, 0, 1))
    p["LAM"] = f(inp["lru_lam"][layer].reshape(2, 2, 128).transpose(2, 1, 0))
    gw = np.zeros((128, 2, 2, 2, 128), np.float32)
    w = inp["lru_gate_w"][layer]
    for c in range(2):
        for bb in range(2):
            blk = c * 2 + bb
            gw[bb * 64:(bb + 1) * 64, c, :, :, bb * 64:(bb + 1) * 64] = w[:, :, blk].transpose(2, 0, 1, 3)
    p["GW"] = gw
    sel = np.zeros((128, 2), np.float32)
    sel[:, half] = 1.0
    p["SEL"] = sel
    p["SINK"] = rep(inp["swa_sink"][layer])
    p["DL"] = rep(inp["diff_lam"][layer].reshape(-1)).reshape(128, 4, 32)
    p["DG"] = f(np.tile(inp["diff_norm_g"][layer], 2).reshape(128, 1))
    pm = np.zeros((128, 4), np.float32)
    for hp in range(2):
        for m in range(2):
            pm[:, hp * 2 + m] = ((np.arange(128) // 64) == hp) & (((np.arange(128) % 64) // 32) == m)
    p["PM"] = pm
    p["LNG"] = f(inp["ln_g"][layer].reshape(2, 8, 128).transpose(2, 0, 1))
    p["LNB"] = f(inp["ln_b"][layer].reshape(2, 8, 128).transpose(2, 0, 1))
    if layer % 2 == 1:
        p["RB"] = rep(inp["moe_router_b"][layer // 2])
    else:
        p["RB"] = np.zeros((128, 8), np.float32)
    return p


def ffn_layout(w1, w3, w2):
    E = w1.shape[0]
    a = w1.reshape(E, 8, 128, NJ, 128).transpose(0, 3, 2, 1, 4)
    b = w3.reshape(E, 8, 128, NJ, 128).transpose(0, 3, 2, 1, 4)
    w13 = np.ascontiguousarray(np.concatenate([a, b], axis=-1))
    w2r = np.ascontiguousarray(w2.reshape(E, NJ, 128, 8, 128).transpose(0, 3, 2, 1, 4))
    return w13, w2r


_PROGS = {}


def _prog(key, fn):
    if key not in _PROGS:
        _PROGS[key] = fn()
    return _PROGS[key]


PAIRS = [[0, 1], [2, 3], [4, 5], [6, 7]]
PUB = ["XL0", "XL1", "KS", "VS", "KD", "VD0", "VD1"]


def build_fused(depth=DEPTH):
    k = K()
    S = k.S
    nc = k.nc
    I = lambda n, s, dt: k.dram(n, s, dt, "ExternalInput")
    hT_d = I("hT", [128, 8, NT], F32)
    cc_d = I("cc", [128, 8, 2], F32)
    cst_d = I("ident", [128, 128], F32)
    rope_d = I("rope", [128, 4, HALF], F32)
    msk_d = I("MSK", [128, 8, 512], BF16)
    L = []
    for l in range(depth):
        moe = l % 2 == 1
        nexp = NEXP if moe else 1
        d = dict(adaw=I(f"adaw{l}", [D_MODEL, 6 * D_MODEL], F32), adab=I(f"adab{l}", [128, 48], F32),
                 win=I(f"win{l}", [D_MODEL, NW], F32), wout=I(f"wout{l}", [D_MODEL, D_MODEL], F32),
                 w13=I(f"w13_{l}", [nexp, NJ, 128, 8, 256], F32), w2=I(f"w2_{l}", [nexp, 8, 128, NJ, 128], F32),
                 rw=I(f"rw{l}", [D_MODEL, NEXP], F32) if moe else None,
                 sm={n: I(f"{n}{l}", s_, F32) for n, s_ in SMALL.items()})
        L.append(d)
    out_d = k.dram("hout", [128, 8, NT], F32, "ExternalOutput")
    scr = []
    for par in range(2):
        o = {n: Tile(nc.dram_tensor(f"{n}_{par}", list(sh), dt).ap()) for n, (sh, dt) in A_OUT.items()}
        gth = {n: Tile(nc.dram_tensor(f"{n}G_{par}", [2 * A_OUT[n][0][0], A_OUT[n][0][1]], A_OUT[n][1]).ap())
               for n in PUB}
        scr.append((o, gth))
    with k.es:
        hT = k.sb([128, 8, NT], F32, name="hT")
        MODX = k.sb([128, 8, 8, 2], F32, name="modx")
        S.dma("sp", hT[:], hT_d[:, :, :], reads=[hT_d], writes=[hT])
        C = emit_consts(k, cst_d)
        prm = {n: k.sb(s_, F32, name=n) for n, s_ in SMALL.items()}
        for l in range(depth):
            moe = l % 2 == 1
            lam_init = 0.8 - 0.6 * math.exp(-0.3 * l)
            o, gth = scr[l % 2]
            emit_mods(k, cc_d, L[l]["adaw"], L[l]["adab"], MODX)
            emit_phaseA(k, hT, MODX, L[l]["win"], rope_d, o)
            for n in PUB:
                S.coll("AllGather", PAIRS, o[n], gth[n])
            for n in SMALL:
                S.dma("sp", prm[n][:], L[l]["sm"][n].t, reads=[L[l]["sm"][n]], writes=[prm[n]])

            def G(n):
                t = Tile(gth[n].t.rearrange("(h r) n -> h r n", h=2))
                t.b = gth[n].b
                return t
            g = dict(XG=[G("XL0"), G("XL1")], GT=o["GT"], QS=o["QS"], KSO=o["KS"], KSG=G("KS"), VSO=o["VS"],
                     VSG=G("VS"), QD=o["QD"], KDO=o["KD"], KDG=G("KD"), VDO=o["VD0"], VDG=[G("VD0"), G("VD1")],
                     MSK=msk_d)
            with k.scope() as es:
                mixT = k.sb([128, 8, NT], BF16, es, "mixT")
                emit_lru(k, mixT, C, g, prm)
                emit_swa(k, mixT, C, g, prm)
                emit_diff(k, mixT, C, g, prm, lam_init)
                emit_phaseC(k, hT, mixT, MODX, C, L[l]["wout"], prm)
            if l == depth - 1:
                emit_ffn(k, hT, MODX, C, prm, L[l]["w13"], L[l]["w2"], NEXP if moe else 1, L[l]["rw"],
                         tok0=CTX, G=HALF // 2, TN=512)
            else:
                emit_ffn(k, hT, MODX, C, prm, L[l]["w13"], L[l]["w2"], NEXP if moe else 1, L[l]["rw"])
        S.dma("sp", out_d[:, :, :], hT[:], reads=[hT], writes=[out_d])
        S.finish()
    return k.nc


def fused_inputs(inp, depth=DEPTH):
    x, c, ctx, c_ctx = inp["x"], inp["c"], inp["ctx"], inp["c_ctx"]
    cores = [(b, hf) for b in range(BATCH) for hf in range(2)]
    perm = win_perm()
    ropes = [rope_tables(hf) for hf in range(2)]
    masks = [swa_masks(hf) for hf in range(2)]
    shared = dict(ident=np.eye(128, dtype=np.float32))
    for l in range(depth):
        j = l // 2
        shared[f"adaw{l}"] = np.ascontiguousarray(inp["ada_w"][l])
        shared[f"adab{l}"] = np.ascontiguousarray(inp["ada_b"][l].reshape(48, 128).T)
        shared[f"win{l}"] = np.ascontiguousarray(inp["w_in"][l][:, perm])
        shared[f"wout{l}"] = np.ascontiguousarray(inp["w_out"][l])
        if l % 2 == 0:
            w13, w2r = ffn_layout(inp["ffn_w1"][j][None], inp["ffn_w3"][j][None], inp["ffn_w2"][j][None])
        else:
            w13, w2r = ffn_layout(inp["moe_w1"][j], inp["moe_w3"][j], inp["moe_w2"][j])
            shared[f"rw{l}"] = np.ascontiguousarray(inp["moe_router_w"][j])
        shared[f"w13_{l}"] = w13
        shared[f"w2_{l}"] = w2r
    in_maps = []
    for (b, hf) in cores:
        m = dict(shared)
        toks = np.concatenate([ctx[b], x[b, hf * HALF:(hf + 1) * HALF]], 0)
        m["hT"] = to_fm(toks)
        m["cc"] = np.ascontiguousarray(np.stack([c[b].reshape(8, 128).T, c_ctx.reshape(8, 128).T], -1))
        m["rope"] = ropes[hf]
        m["MSK"] = masks[hf]
        for l in range(depth):
            for n, v in small_params(inp, l, hf).items():
                m[f"{n}{l}"] = v
        in_maps.append(m)
    return cores, in_maps


def kernel(**inp):
    inp = {k_: np.asarray(v) for k_, v in inp.items()}
    cores, in_maps = fused_inputs(inp)
    res = run_bass_kernel_spmd(_prog("fused", build_fused), in_maps, core_ids=list(range(NCORES))).results
    out = np.zeros((BATCH, SEQ, D_MODEL), np.float32)
    for ci, (b, hf) in enumerate(cores):
        out[b, hf * HALF:(hf + 1) * HALF] = from_fm(np.asarray(res[ci]["hout"]))[CTX:]
    return out
```
